# Optimizing a Trainium2 kernel written in Bass

```python
import math
import jax, jax.numpy as jnp
from jax import lax
import numpy as np

D_MODEL = 1024
BATCH = 4
SEQ = 8192
DEPTH = 1
DEC_BATCH = 128
DEC_SEQ = 4
PAST_LEN = 8192
PAGE_SIZE = 128

HEAD_DIM = 64
HEADS_PER_GROUP = 4
DILATED_GROUPS = ((128, 1), (512, 4), (2048, 16))
N_ATTN_HEADS = len(DILATED_GROUPS) * HEADS_PER_GROUP
ATTN_WIDTH = N_ATTN_HEADS * HEAD_DIM
ATTN_OUT_WIDTH = HEADS_PER_GROUP * HEAD_DIM
ATTN_SCALE = HEAD_DIM ** -0.5
CONV_CH = D_MODEL // 2
CONV_WIDTH = 31
N_BUCKETS = 32
MAX_DISTANCE = 2048
N_EXPERT_GROUPS = 4
EXPERTS_PER_GROUP = 8
N_EXPERTS = N_EXPERT_GROUPS * EXPERTS_PER_GROUP
TOP_K_INNER = 2
D_FF_EXPERT = D_MODEL // 2
MOE_BLOCK = 128
IN_COLS = 3 * ATTN_WIDTH + 2 * CONV_CH + 2 * D_MODEL
SPLITS = (ATTN_WIDTH, 2 * ATTN_WIDTH, 3 * ATTN_WIDTH, 3 * ATTN_WIDTH + CONV_CH,
          3 * ATTN_WIDTH + 2 * CONV_CH, 3 * ATTN_WIDTH + 2 * CONV_CH + D_MODEL)
EPS = 1e-6
NEG_INF = -1e30

kernel_name = 'hybrid_conformer_dilated_attn_hmoe_step'


def _rmsnorm(x, g):
    xf = x.astype(jnp.float32)
    r = lax.rsqrt(jnp.mean(xf * xf, axis=-1, keepdims=True) + EPS)
    return (xf * r).astype(x.dtype) * g


def _modulation(c, w_mod, b_mod):
    m = jax.nn.silu(c) @ w_mod + b_mod
    return jnp.split(m[:, None, :], 6, axis=-1)


def _t5_bucket(dist):
    max_exact = N_BUCKETS // 2
    d_f = jnp.maximum(dist, 1).astype(jnp.float32)
    large = max_exact + (jnp.log(d_f / max_exact) / math.log(MAX_DISTANCE / max_exact)
                         * (N_BUCKETS - max_exact)).astype(jnp.int32)
    large = jnp.minimum(large, N_BUCKETS - 1)
    return jnp.where(dist < max_exact, dist, large)


def _dilated_prompt(q, k, v, bias_g, dil, n_keys):
    B, S, H, Dh = q.shape
    L = S // dil
    blk = n_keys
    nb = -(-L // blk)
    Lp = nb * blk
    N = B * dil

    def stride_split(t):
        t = t.reshape(B, L, dil, H, Dh).transpose(0, 2, 1, 3, 4).reshape(N, L, H, Dh)
        return jnp.pad(t, ((0, 0), (0, Lp - L), (0, 0), (0, 0)))

    def with_prev(t):
        tp = jnp.pad(t, ((0, 0), (blk, 0), (0, 0), (0, 0))).reshape(N, nb + 1, blk, H, Dh)
        return jnp.concatenate([tp[:, :-1], tp[:, 1:]], axis=2)

    qb = stride_split(q).reshape(N, nb, blk, H, Dh)
    kb = with_prev(stride_split(k))
    vb = with_prev(stride_split(v))
    i = jnp.arange(blk)[:, None]
    j = jnp.arange(2 * blk)[None, :]
    rel = i - j + blk
    blk_idx = jnp.arange(nb)[:, None, None]
    valid = (rel >= 0) & (rel <= n_keys) & (blk_idx * blk - blk + j >= 0)
    bias = bias_g[_t5_bucket(jnp.clip(rel, 0, n_keys) * dil)].transpose(2, 0, 1)
    logits = jnp.einsum('nbqhd,nbkhd->nbhqk', qb, kb,
                        preferred_element_type=jnp.float32) * ATTN_SCALE + bias[None, None]
    logits = jnp.where(valid[None, :, None], logits, NEG_INF)
    lse = jax.nn.logsumexp(logits, axis=-1)
    p = jnp.exp(logits - lse[..., None])
    o = jnp.einsum('nbhqk,nbkhd->nbqhd', p.astype(v.dtype), vb)
    o = o.reshape(N, Lp, H, Dh)[:, :L].reshape(B, dil, L, H, Dh).transpose(0, 2, 1, 3, 4).reshape(B, S, H, Dh)
    lse = lse.transpose(0, 1, 3, 2).reshape(N, Lp, H)[:, :L].reshape(B, dil, L, H).transpose(0, 2, 1, 3).reshape(B, S, H)
    return o, lse


def _dilated_sample(q, k_new, v_new, kv_buf, bias_g, win, dil, n_keys):
    Bd, T, H, Dh = q.shape
    Lc = kv_buf.shape[1]
    kv_all = jnp.concatenate([kv_buf, jnp.stack([k_new, v_new], axis=2)], axis=1)
    steps = jnp.arange(n_keys + 1)
    idx = Lc + jnp.arange(T)[:, None] - steps[None, :] * dil
    valid = idx >= 0
    g = jnp.take(kv_all, jnp.clip(idx, 0), axis=1)
    bias = bias_g[_t5_bucket(steps * dil)].T
    logits = jnp.einsum('bthd,btkhd->bhtk', q, g[:, :, :, 0],
                        preferred_element_type=jnp.float32) * ATTN_SCALE + bias[None, :, None, :]
    logits = jnp.where(valid[None, None], logits, NEG_INF)
    lse = jax.nn.logsumexp(logits, axis=-1)
    p = jnp.exp(logits - lse[..., None])
    o = jnp.einsum('bhtk,btkhd->bthd', p.astype(v_new.dtype), g[:, :, :, 1])
    new_buf = kv_all[:, max(0, Lc + T - win):]
    return o, lse.transpose(0, 2, 1), new_buf


def _depthwise_causal(u_ext, dw_w, dw_b):
    out = lax.conv_general_dilated(u_ext, dw_w[:, None, :].astype(u_ext.dtype), window_strides=(1,),
                                   padding='VALID', dimension_numbers=('NWC', 'WIO', 'NWC'),
                                   feature_group_count=CONV_CH)
    return out + dw_b


def _conv_tail(y, g, b):
    yf = y.astype(jnp.float32)
    mu = jnp.mean(yf, axis=-1, keepdims=True)
    var = jnp.mean(jnp.square(yf - mu), axis=-1, keepdims=True)
    yn = ((yf - mu) * lax.rsqrt(var + EPS)).astype(y.dtype) * g + b
    return jax.nn.silu(yn)


def _hier_moe(h, w_rg, b_rg, w_re, b_re, w_eg, w_eu, w_ed):
    T, D = h.shape
    g_logits = (h @ w_rg).astype(jnp.float32) + b_rg
    g_prob = jax.nn.softmax(g_logits, axis=-1)
    _, grp = lax.top_k(g_logits, 1)
    p_grp = jnp.take_along_axis(g_prob, grp, axis=-1)
    e_all = jnp.einsum('td,dge->tge', h, w_re).astype(jnp.float32) + b_re
    e_logits = jnp.take_along_axis(e_all, grp[:, :, None], axis=1)[:, 0]
    top_v, top_i = lax.top_k(e_logits, TOP_K_INNER)
    weights = p_grp * jax.nn.softmax(top_v, axis=-1)
    expert = grp * EXPERTS_PER_GROUP + top_i
    A = T * TOP_K_INNER
    e_flat = expert.reshape(A)
    tok_flat = jnp.repeat(jnp.arange(T, dtype=jnp.int32), TOP_K_INNER)
    order = jnp.argsort(e_flat)
    e_sorted = e_flat[order]
    counts = jnp.bincount(e_flat, length=N_EXPERTS)
    padded = (counts + MOE_BLOCK - 1) // MOE_BLOCK * MOE_BLOCK
    start = jnp.cumsum(counts) - counts
    pend = jnp.cumsum(padded)
    pstart = pend - padded
    dest_sorted = pstart[e_sorted] + jnp.arange(A) - start[e_sorted]
    n_blocks = -(-A // MOE_BLOCK) + N_EXPERTS
    cap = n_blocks * MOE_BLOCK
    slot_tok = jnp.full((cap,), T, jnp.int32).at[dest_sorted].set(tok_flat[order])
    block_expert = jnp.minimum(jnp.searchsorted(pend, jnp.arange(n_blocks) * MOE_BLOCK, side='right'),
                               N_EXPERTS - 1)
    h_pad = jnp.concatenate([h, jnp.zeros((1, D), h.dtype)], axis=0)
    xs = h_pad[slot_tok].reshape(n_blocks, MOE_BLOCK, D)

    def run_block(args):
        xb, e = args
        return (jax.nn.silu(xb @ w_eg[e]) * (xb @ w_eu[e])) @ w_ed[e]

    ys = lax.map(run_block, (xs, block_expert)).reshape(cap, D)
    dest = jnp.zeros((A,), jnp.int32).at[order].set(dest_sorted)
    y = ys[dest].reshape(T, TOP_K_INNER, D) * weights[..., None].astype(ys.dtype)
    return jnp.sum(y, axis=1)


def _layer(x, c, kv_bufs, conv_buf, rel_bias, lw):
    (norm_mix_g, norm_ffn_g, w_mod, b_mod, w_in, dw_w, dw_b, ln_conv_g, ln_conv_b,
     w_conv_out, w_attn_out, w_out, w_rg, b_rg, w_re, b_re, w_eg, w_eu, w_ed) = lw
    B, T, D = x.shape
    sh1, sc1, g1, sh2, sc2, g2 = _modulation(c, w_mod, b_mod)
    h = _rmsnorm(x, norm_mix_g) * (1 + sc1) + sh1
    z = h @ w_in
    q, k, v, u_lin, u_gate, gate_a, gate_b = jnp.split(z, SPLITS, axis=-1)
    q = q.reshape(B, T, N_ATTN_HEADS, HEAD_DIM)
    k = k.reshape(B, T, N_ATTN_HEADS, HEAD_DIM)
    v = v.reshape(B, T, N_ATTN_HEADS, HEAD_DIM)
    u = u_lin * jax.nn.sigmoid(u_gate)
    hist = jnp.zeros((B, CONV_WIDTH - 1, CONV_CH), u.dtype) if conv_buf is None else conv_buf
    u_ext = jnp.concatenate([hist, u], axis=1)
    new_conv = u_ext[:, -(CONV_WIDTH - 1):]
    a = _conv_tail(_depthwise_causal(u_ext, dw_w, dw_b), ln_conv_g, ln_conv_b) @ w_conv_out
    outs, lses, new_kv = [], [], []
    for gi, (win, dil) in enumerate(DILATED_GROUPS):
        sl = slice(gi * HEADS_PER_GROUP, (gi + 1) * HEADS_PER_GROUP)
        n_keys = win // dil
        qg, kg, vg = q[:, :, sl], k[:, :, sl], v[:, :, sl]
        bg = rel_bias[:, sl]
        if kv_bufs is None:
            o, lse = _dilated_prompt(qg, kg, vg, bg, dil, n_keys)
            kv = jnp.stack([kg, vg], axis=2)[:, -min(win, T):]
        else:
            o, lse, kv = _dilated_sample(qg, kg, vg, kv_bufs[gi], bg, win, dil, n_keys)
        outs.append(o)
        lses.append(lse)
        new_kv.append(kv)
    mix_w = jax.nn.softmax(jnp.stack(lses), axis=0)
    o = jnp.einsum('gbth,gbthd->bthd', mix_w.astype(outs[0].dtype), jnp.stack(outs))
    b = o.reshape(B, T, ATTN_OUT_WIDTH) @ w_attn_out
    mixed = jax.nn.sigmoid(gate_a) * a + jax.nn.sigmoid(gate_b) * b
    x = x + g1 * (mixed @ w_out)
    h2 = _rmsnorm(x, norm_ffn_g) * (1 + sc2) + sh2
    f = _hier_moe(h2.reshape(B * T, D), w_rg, b_rg, w_re, b_re, w_eg, w_eu, w_ed).reshape(B, T, D)
    x = x + g2 * f
    return x, new_kv, new_conv


def setup_inputs(seed: int = 0) -> dict:
    key = jax.random.key(seed)
    ks = iter(jax.random.split(key, 48))

    def nrm(shape, s):
        return jax.random.normal(next(ks), shape, jnp.float32) * s

    D, C, F = D_MODEL, CONV_CH, D_FF_EXPERT
    inp = {}
    inp['x_prompt'] = nrm((BATCH, SEQ, D), 1.0)
    inp['x_sample'] = nrm((DEC_BATCH, DEC_SEQ, D), 1.0)
    inp['c_prompt'] = nrm((BATCH, D), 1.0)
    inp['c_sample'] = nrm((DEC_BATCH, D), 1.0)
    inp['cache_kv_w128'] = nrm((DEPTH, DEC_BATCH, min(128, PAST_LEN), 2, HEADS_PER_GROUP, HEAD_DIM), 1.0)
    inp['cache_kv_w512'] = nrm((DEPTH, DEC_BATCH, min(512, PAST_LEN), 2, HEADS_PER_GROUP, HEAD_DIM), 1.0)
    inp['cache_kv_w2048'] = nrm((DEPTH, DEC_BATCH, min(2048, PAST_LEN), 2, HEADS_PER_GROUP, HEAD_DIM), 1.0)
    inp['state_conv'] = nrm((DEPTH, DEC_BATCH, CONV_WIDTH - 1, C), 0.5)
    inp['rel_bias'] = nrm((N_BUCKETS, N_ATTN_HEADS), 0.5)
    inp['norm_mix_g'] = 1.0 + nrm((DEPTH, D), 0.02)
    inp['norm_ffn_g'] = 1.0 + nrm((DEPTH, D), 0.02)
    inp['w_mod'] = nrm((DEPTH, D, 6 * D), 0.5 * D ** -0.5)
    inp['b_mod'] = nrm((DEPTH, 6 * D), 0.02)
    inp['w_in'] = nrm((DEPTH, D, IN_COLS), D ** -0.5)
    inp['dw_w'] = nrm((DEPTH, CONV_WIDTH, C), CONV_WIDTH ** -0.5)
    inp['dw_b'] = nrm((DEPTH, C), 0.02)
    inp['ln_conv_g'] = 1.0 + nrm((DEPTH, C), 0.02)
    inp['ln_conv_b'] = nrm((DEPTH, C), 0.02)
    inp['w_conv_out'] = nrm((DEPTH, C, D), C ** -0.5)
    inp['w_attn_out'] = nrm((DEPTH, ATTN_OUT_WIDTH, D), ATTN_OUT_WIDTH ** -0.5)
    inp['w_out'] = nrm((DEPTH, D, D), D ** -0.5)
    inp['w_router_group'] = nrm((DEPTH, D, N_EXPERT_GROUPS), D ** -0.5)
    inp['b_router_group'] = nrm((DEPTH, N_EXPERT_GROUPS), 0.01)
    inp['w_router_expert'] = nrm((DEPTH, D, N_EXPERT_GROUPS, EXPERTS_PER_GROUP), D ** -0.5)
    inp['b_router_expert'] = nrm((DEPTH, N_EXPERT_GROUPS, EXPERTS_PER_GROUP), 0.01)
    inp['w_exp_gate'] = nrm((DEPTH, N_EXPERTS, D, F), D ** -0.5)
    inp['w_exp_up'] = nrm((DEPTH, N_EXPERTS, D, F), D ** -0.5)
    inp['w_exp_down'] = nrm((DEPTH, N_EXPERTS, F, D), F ** -0.5)
    inp['norm_final_g'] = 1.0 + nrm((D,), 0.02)
    return inp


def reference(x_prompt, x_sample, c_prompt, c_sample, cache_kv_w128, cache_kv_w512, cache_kv_w2048,
              state_conv, rel_bias, norm_mix_g, norm_ffn_g, w_mod, b_mod, w_in, dw_w, dw_b,
              ln_conv_g, ln_conv_b, w_conv_out, w_attn_out, w_out, w_router_group, b_router_group,
              w_router_expert, b_router_expert, w_exp_gate, w_exp_up, w_exp_down, norm_final_g):
    yp, ys = x_prompt, x_sample
    p128, p512, p2048, pconv = [], [], [], []
    s128, s512, s2048, sconv = [], [], [], []
    for l in range(DEPTH):
        lw = (norm_mix_g[l], norm_ffn_g[l], w_mod[l], b_mod[l], w_in[l], dw_w[l], dw_b[l],
              ln_conv_g[l], ln_conv_b[l], w_conv_out[l], w_attn_out[l], w_out[l],
              w_router_group[l], b_router_group[l], w_router_expert[l], b_router_expert[l],
              w_exp_gate[l], w_exp_up[l], w_exp_down[l])
        yp, kv_p, conv_p = _layer(yp, c_prompt, None, None, rel_bias, lw)
        ys, kv_s, conv_s = _layer(ys, c_sample, (cache_kv_w128[l], cache_kv_w512[l], cache_kv_w2048[l]),
                                  state_conv[l], rel_bias, lw)
        p128.append(kv_p[0]); p512.append(kv_p[1]); p2048.append(kv_p[2]); pconv.append(conv_p)
        s128.append(kv_s[0]); s512.append(kv_s[1]); s2048.append(kv_s[2]); sconv.append(conv_s)
    yp = _rmsnorm(yp, norm_final_g)
    ys = _rmsnorm(ys, norm_final_g)
    return (yp, ys, jnp.stack(p128), jnp.stack(p512), jnp.stack(p2048), jnp.stack(pconv),
            jnp.stack(s128), jnp.stack(s512), jnp.stack(s2048), jnp.stack(sconv))
```

```python
import math
import numpy as np
from contextlib import ExitStack
import concourse.bass as bass
import concourse.mybir as mybir
from concourse.bass_utils import run_bass_kernel_spmd

F32 = mybir.dt.float32
BF16 = mybir.dt.bfloat16
I32 = mybir.dt.int32
U32 = mybir.dt.uint32
AF = mybir.ActivationFunctionType
ALU = mybir.AluOpType
AX = mybir.AxisListType

D = 1024
NCORE = 8
HALO = 2048
NOWN = 4096
NEXT = HALO + NOWN
NSQ = 16
NS = 64
NTOK = NEXT + NS
NOT = NOWN + NS
NTILE = 33
GROUPS = ((128, 1), (512, 4), (2048, 16))
EPS = 1e-6
NEXP = 32
BLK = 128
NBLK = (2 * NOT) // BLK + NEXP
CAP = NBLK * BLK
STOP_AFTER = None
DEBUG_SCR = False


class Ctx:
    KD = 8

    def __init__(self, nc, es, needed=None):
        self.nc = nc
        self.es = es
        self.needed = needed
        self.record = set()
        self.iidx = {e: 0 for e in ('pe', 'act', 'dve', 'pool')}
        self.eng = {'pe': nc.tensor, 'act': nc.scalar, 'dve': nc.vector, 'pool': nc.gpsimd, 'sp': nc.sync}
        self.csem = {e: es.enter_context(nc.semaphore('c_' + e)) for e in ('pe', 'act', 'dve', 'pool')}
        self.ccnt = {e: 0 for e in self.csem}
        self.dsem = {q: [es.enter_context(nc.semaphore('d_%s%d' % (q, i))) for i in range(self.KD)]
                     for q in ('sp', 'act', 'pool')}
        self.dcnt = {q: 0 for q in self.dsem}
        self.waited = {e: {} for e in self.eng}
        self.state = {}
        self.sbn = 0

    def sb(self, shape, dt, es=None):
        self.sbn += 1
        return (es or self.es).enter_context(self.nc.sbuf_tensor('sb%d' % self.sbn, list(shape), dt))

    def ps(self, shape, dt):
        self.sbn += 1
        return self.es.enter_context(self.nc.psum_tensor('ps%d' % self.sbn, list(shape), dt))

    def _wait(self, e, evs):
        best = {}
        for (sem, v, src) in evs:
            k = id(sem)
            if k not in best or best[k][1] < v:
                best[k] = (sem, v)
        for k, (sem, v) in best.items():
            if self.waited[e].get(k, 0) >= v:
                continue
            self.eng[e].wait_ge(sem, v)
            self.waited[e][k] = v
            if self.needed is None:
                for ce, cs in self.csem.items():
                    if cs is sem:
                        self.record.add((ce, v))

    def _deps(self, e, reads, writes):
        evs = []
        for k in reads:
            st = self.state.get(k)
            if st and st['w'] is not None:
                evs.append(st['w'])
        for k in writes:
            st = self.state.get(k)
            if st:
                if st['w'] is not None and (st['w'][2] != e or e != 'pe'):
                    evs.append(st['w'])
                for r in st['r']:
                    if r[2] != e or e != 'pe':
                        evs.append(r)
        return evs

    def _commit(self, ev, reads, writes):
        for k in reads:
            st = self.state.setdefault(k, {'w': None, 'r': []})
            st['r'] = [r for r in st['r'] if r[0] is not ev[0]] + [ev]
        for k in writes:
            self.state[k] = {'w': ev, 'r': []}

    @staticmethod
    def _psx(reads, writes):
        ps = [k for k in reads if k == 'pb7' or (isinstance(k, tuple) and k[0] == 'pb')]
        if not ps:
            return list(reads), list(writes)
        return [k for k in reads if k not in ps], list(writes) + [k for k in ps if k not in writes]

    def op(self, e, fn, reads=(), writes=()):
        reads, writes = self._psx(reads, writes)
        self._wait(e, self._deps(e, reads, writes))
        ins = fn()
        self.iidx[e] += 1
        if self.needed is None or (e, self.iidx[e]) in self.needed:
            self.ccnt[e] += 1
            ins.then_inc(self.csem[e], 1)
        ev = (self.csem[e], self.ccnt[e], e)
        self._commit(ev, reads, writes)
        return ev

    def dma(self, q, fn, reads=(), writes=()):
        j = self.dcnt[q]
        sem = self.dsem[q][j % self.KD]
        evs = self._deps(None, reads, writes)
        if j >= self.KD:
            evs.append((sem, 16 * (j // self.KD), 'dma_' + q))
        self._wait(q, evs)
        ins = fn()
        ins.then_inc(sem, 16)
        self.dcnt[q] += 1
        ev = (sem, 16 * (j // self.KD + 1), 'dma_' + q)
        self._commit(ev, reads, writes)
        return ev

    def wait_keys(self, e, keys):
        self._wait(e, self._deps(None, keys, ()))

    def barrier(self):
        evs = []
        for q in self.dsem:
            for i, sem in enumerate(self.dsem[q]):
                n = (self.dcnt[q] - i + self.KD - 1) // self.KD
                if n > 0:
                    evs.append((sem, 16 * n, 'x'))
        for e in self.csem:
            if self.ccnt[e]:
                evs.append((self.csem[e], self.ccnt[e], 'x'))
        for e in self.eng:
            self._wait(e, evs)

    def finish(self):
        evs = []
        for q in self.dsem:
            for i, sem in enumerate(self.dsem[q]):
                n = (self.dcnt[q] - i + self.KD - 1) // self.KD
                if n > 0:
                    evs.append((sem, 16 * n, 'x'))
        for e in self.csem:
            if self.ccnt[e]:
                evs.append((self.csem[e], self.ccnt[e], 'x'))
        self._wait('sp', evs)


def _t5_bucket_np(dist):
    dist = np.asarray(dist, np.int64)
    max_exact = 16
    d_f = np.maximum(dist, 1).astype(np.float32)
    large = max_exact + (np.log(d_f / np.float32(max_exact)) / np.float32(math.log(2048 / max_exact))
                         * np.float32(32 - max_exact)).astype(np.int32)
    large = np.minimum(large, 31)
    return np.where(dist < max_exact, dist, large)


def _structure_constants():
    ohw = np.zeros((32, 3 * 510), np.float32)
    vw = np.zeros((4, 3 * 510), np.float32)
    for g, (win, dil) in enumerate(GROUPS):
        for blk in range(2):
            for u in range(255):
                rel = u + 1 if blk == 0 else u - 127
                ok = (rel <= 128) if blk == 0 else (rel >= 0)
                if ok:
                    b = int(_t5_bucket_np(rel * dil))
                    ohw[b, g * 510 + blk * 255 + u] = 1.0
                    vw[:, g * 510 + blk * 255 + u] = 1.0
    sel = np.zeros((17, 192), np.float32)
    sel[0, 0:128] = 1.0
    for t in range(64):
        sel[1 + t // 4, 128 + t] = 1.0
    bd = np.zeros((64, 128), np.float32)
    for k in range(64):
        for q in range(64):
            if k // 4 == q // 4:
                bd[k, q] = 1.0
        bd[k, 64 + k] = 1.0
    return ohw, vw, sel, bd


def _os_env(k):
    import os
    return os.environ.get(k)


def build_nc(needed=None):
    nc = bass.Bass("TRN2", target_bir_lowering=False)

    def din(name, shape, dt=F32):
        return nc.dram_tensor(name, list(shape), dt, kind="ExternalInput")

    def dout(name, shape, dt=F32):
        return nc.dram_tensor(name, list(shape), dt, kind="ExternalOutput")

    def dscr(name, shape, dt=F32):
        return nc.dram_tensor(name, list(shape), dt, kind="ExternalOutput" if DEBUG_SCR else "Internal")

    xp_t = din("xp", [NEXT, D]); xs_t = din("xs", [NS, D]); cmod_t = din("cmod", [17, D]); hv_t = din("hv", [128, 1])
    ck_t = [din("ck%d" % w, [NSQ, w, 512]) for (w, _) in GROUPS]
    sconv_t = din("sconv", [NSQ, 30, 512])
    relb_t = din("rel_bias", [32, 12])
    gmix_t = din("norm_mix_g", [1, D]); gffn_t = din("norm_ffn_g", [1, D]); gfin_t = din("norm_final_g", [1, D])
    wmod_t = din("w_mod", [D, 6 * D]); bmod_t = din("b_mod", [1, 6 * D])
    win_t = din("w_in", [D, 5376])
    dww_t = din("dw_w", [31, 512]); dwb_t = din("dw_b", [1, 512]); lng_t = din("ln_g", [1, 512]); lnb_t = din("ln_b", [1, 512])
    wco_t = din("w_conv_out", [512, D]); wao_t = din("w_attn_out", [256, D]); wo_t = din("w_out", [D, D])
    wrt_t = din("w_rt", [D, 36]); brt_t = din("b_rt", [1, 36])
    if STOP_AFTER is None:
        weg_t = din("w_eg", [NEXP, D, 512]); weu_t = din("w_eu", [NEXP, D, 512]); wed_t = din("w_ed", [NEXP, 512, D])
    ohw_t = din("ohw", [32, 1530]); vw_t = din("vw", [4, 1530]); sel_t = din("sel", [17, 192]); bd_t = din("bd", [64, 128])

    yp_t = dout("yp", [NOWN, D]); ys_t = dout("ys", [NS, D])
    kvp_t = [dout("kvp%d" % w, [w, 512]) for (w, _) in GROUPS]
    convp_t = dout("convp", [30, 512])
    kvs_t = [dout("kvs%d" % w, [NSQ, w, 512]) for (w, _) in GROUPS]
    convs_t = dout("convs", [NSQ, 30, 512])

    modrows_t = dscr("modrows", [4, 192, D])
    wd_t = dscr("wdscr", [3, 4, 510])
    ebd_t = dscr("ebd", [3, 128, 1024])
    sg_t = dscr("sgscr", [16, 128, NOT], BF16)
    yts_t = dscr("ytscr", [4, 128, NOT], BF16)
    acc_t = dscr("accscr", [3, NOWN, 260])
    accs_t = dscr("accsscr", [NS, 260])
    x1_t = dscr("x1scr", [NOT, D])
    xsd_t = dscr("xsdisp", [CAP, D], BF16)
    ysd_t = dscr("ysdisp", [CAP, D])

    xp = xp_t.ap(); xs = xs_t.ap(); win = win_t.ap()

    with ExitStack() as es:
        c = Ctx(nc, es, needed)
        nc._mk_ctx = c
        PB = [c.ps([128, 512], F32) for _ in range(8)]

        def pbf(i):
            return PB[i][:].bitcast(BF16)

        identf = c.sb([128, 128], F32); ident = c.sb([128, 128], BF16)
        onesb = c.sb([128, 128], BF16); onesf = c.sb([128, 128], F32)
        suf = c.sb([128, 128], F32); jf = c.sb([128, 128], F32)
        epsb = c.sb([128, 1], F32); hv = c.sb([128, 1], F32); one1 = c.sb([128, 1], F32)
        c.op('pool', lambda: nc.gpsimd.memset(onesf[:], 1.0), writes=['onesf'])
        c.op('pool', lambda: nc.gpsimd.memset(onesb[:], 1.0), writes=['onesb'])
        c.op('pool', lambda: nc.gpsimd.memset(epsb[:], EPS), writes=['epsb'])
        c.op('pool', lambda: nc.gpsimd.memset(one1[:], 1.0), writes=['one1'])
        c.op('pool', lambda: nc.gpsimd.affine_select(out=identf[:], in_=onesf[:], pattern=[[-1, 128]], compare_op=ALU.is_equal,
                                                       fill=0.0, base=0, channel_multiplier=1), reads=['onesf'], writes=['identf'])
        c.op('pool', lambda: nc.gpsimd.affine_select(out=jf[:], in_=onesf[:], pattern=[[1, 128]], compare_op=ALU.is_equal,
                                                       fill=0.0, base=-127, channel_multiplier=1), reads=['onesf'], writes=['jf'])
        c.op('pool', lambda: nc.gpsimd.affine_select(out=suf[:], in_=onesf[:], pattern=[[1, 128]], compare_op=ALU.is_gt,
                                                       fill=0.0, base=0, channel_multiplier=-1), reads=['onesf'], writes=['suf'])
        c.op('dve', lambda: nc.vector.tensor_copy(out=ident[:], in_=identf[:]), reads=['identf'], writes=['ident'])
        c.dma('sp', lambda: nc.sync.dma_start(out=hv[:], in_=hv_t.ap()), writes=['hv'])


        es_hT = ExitStack()
        es.enter_context(es_hT)
        hT = c.sb([128, 8, NTOK], BF16, es_hT)
        for g, (wing, d) in enumerate(GROUPS):
            nsp = 4 if g == 2 else 1
            for q4 in range(nsp):
                bs = slice(q4 * (NSQ // nsp), (q4 + 1) * (NSQ // nsp))
                A_ = (1, 4, 28)[g]
                c.dma('act', lambda g=g, wing=wing, bs=bs, A_=A_: nc.scalar.dma_start(out=kvs_t[g].ap()[bs, 0:wing - 4, :].rearrange("b (a r) c -> b a (r c)", a=A_),
                      in_=ck_t[g].ap()[bs, 4:wing, :].rearrange("b (a r) c -> b a (r c)", a=A_)), writes=[('kvs_shift', g, q4)])
        es_mod = ExitStack()
        a1p = c.sb([128, D], F32, es_mod); b1p = c.sb([128, D], F32, es_mod); a1s = c.sb([64, D], F32, es_mod); b1s = c.sb([64, D], F32, es_mod)
        es0 = ExitStack()
        with es0:
            cm = c.sb([17, D], F32, es0); scm = c.sb([17, D], F32, es0); scT = c.sb([128, 8, 17], F32, es0)
            mtok = c.sb([17, 6 * D], F32, es0); selm = c.sb([17, 192], F32, es0)
            gmb = c.sb([128, D], F32, es0); gfb = c.sb([128, D], F32, es0)
            c.dma('sp', lambda: nc.sync.dma_start(out=cm[:], in_=cmod_t.ap()), writes=['cm'])
            c.dma('sp', lambda: nc.sync.dma_start(out=selm[:], in_=sel_t.ap()), writes=['selm'])
            c.dma('sp', lambda: nc.sync.dma_start(out=gmb[:], in_=gmix_t.ap().partition_broadcast(128)), writes=['gmb'])
            c.dma('sp', lambda: nc.sync.dma_start(out=gfb[:], in_=gffn_t.ap().partition_broadcast(128)), writes=['gfb'])
            c.op('act', lambda: nc.scalar.activation(out=scm[:], in_=cm[:], func=AF.Silu), reads=['cm'], writes=['scm'])
            for k in range(8):
                c.op('pe', lambda k=k: nc.tensor.transpose(out=PB[0][:, k * 17:(k + 1) * 17], in_=scm[0:17, k * 128:(k + 1) * 128],
                                                           identity=identf[0:17, 0:17]), reads=['scm', 'identf'], writes=['pb0'])
            c.op('dve', lambda: nc.vector.tensor_copy(out=scT[:].rearrange("p k s -> p (k s)"), in_=PB[0][:, 0:136]), reads=['pb0'], writes=['scT'])
            wmod = wmod_t.ap().rearrange("(k p) n -> p k n", p=128)
            wbs = [c.sb([128, 8, 256], F32, es0) for _ in range(2)]
            bbs = [c.sb([17, 256], F32, es0) for _ in range(2)]
            for nb in range(24):
                wb = wbs[nb % 2]
                bb = bbs[nb % 2]
                c.dma('sp', lambda wb=wb, nb=nb: nc.sync.dma_start(out=wb[:], in_=wmod[:, :, nb * 256:(nb + 1) * 256]), writes=[('wb', nb % 2)])
                c.dma('sp', lambda bb=bb, nb=nb: nc.sync.dma_start(out=bb[:], in_=bmod_t.ap()[:, nb * 256:(nb + 1) * 256].partition_broadcast(17)),
                      writes=[('bb', nb % 2)])
                pbk = 1 + nb % 2
                for k in range(8):
                    c.op('pe', lambda wb=wb, k=k, pbk=pbk: nc.tensor.matmul(PB[pbk][0:17, 0:256], lhsT=scT[:, k, :], rhs=wb[:, k, :], start=(k == 0), stop=(k == 7)),
                         reads=['scT', ('wb', nb % 2)], writes=[('pb', pbk)])
                c.op('dve', lambda bb=bb, nb=nb, pbk=pbk: nc.vector.tensor_tensor(out=mtok[:, nb * 256:(nb + 1) * 256], in0=PB[pbk][0:17, 0:256], in1=bb[:], op=ALU.add),
                     reads=[('pb', pbk), ('bb', nb % 2)], writes=['mtok'])
            rowst = [c.sb([128, D], F32, es0) for _ in range(2)]
            for kind in range(6):
                for part, (c0, n) in enumerate(((0, 128), (128, 64))):
                    rt = rowst[(kind * 2 + part) % 2]
                    rk = ('rowst', (kind * 2 + part) % 2)
                    for hf in range(2):
                        c.op('pe', lambda hf=hf, c0=c0, n=n, kind=kind: nc.tensor.matmul(PB[3 + hf][0:n, :], lhsT=selm[0:17, c0:c0 + n],
                             rhs=mtok[0:17, kind * D + hf * 512: kind * D + hf * 512 + 512], start=True, stop=True),
                             reads=['selm', 'mtok'], writes=[('pb', 3 + hf)])
                    dst = None; dk = 'nokey'
                    if kind == 0:
                        dst = (b1p, b1s)[part]; dk = ('b1', part)
                    elif kind == 1:
                        dst = (a1p, a1s)[part]; dk = ('a1', part)
                    for hf in range(2):
                        sl = slice(hf * 512, hf * 512 + 512)
                        if kind in (1, 4):
                            gb = gmb if kind == 1 else gfb
                            tgt = dst if dst is not None else rt
                            c.op('dve', lambda hf=hf, n=n, gb=gb, tgt=tgt, sl=sl: nc.vector.scalar_tensor_tensor(out=tgt[0:n, sl], in0=PB[3 + hf][0:n, :], scalar=1.0,
                                 in1=gb[0:n, sl], op0=ALU.add, op1=ALU.mult), reads=[('pb', 3 + hf), 'gmb', 'gfb'], writes=[rk, dk])
                        else:
                            tgt = dst if dst is not None else rt
                            c.op('act', lambda hf=hf, n=n, tgt=tgt, sl=sl: nc.scalar.copy(out=tgt[0:n, sl], in_=PB[3 + hf][0:n, :]),
                                 reads=[('pb', 3 + hf)], writes=[rk, dk])
                    if kind >= 2:
                        c.dma('sp', lambda rt=rt, c0=c0, n=n, kind=kind: nc.sync.dma_start(out=modrows_t.ap()[kind - 2, c0:c0 + n, :], in_=rt[0:n, :]),
                              reads=[rk], writes=[('modrows', kind - 2, part)])
        c.barrier()
        es0 = ExitStack()
        with es0:
            rb = c.sb([32, 12], F32, es0); ohw = c.sb([32, 1530], F32, es0); vw = c.sb([4, 1530], F32, es0)
            wsb = c.sb([4, 1530], F32, es0); hall = c.sb([128, 24, 128], F32, es0); ebst = c.sb([128, 3, 1024], F32, es0)
            c.dma('sp', lambda: nc.sync.dma_start(out=rb[:], in_=relb_t.ap()), writes=['rb'])
            c.dma('sp', lambda: nc.sync.dma_start(out=ohw[:], in_=ohw_t.ap()), writes=['ohw'])
            c.dma('sp', lambda: nc.sync.dma_start(out=vw[:], in_=vw_t.ap()), writes=['vw'])
            for g in range(3):
                c.op('pe', lambda g=g: nc.tensor.matmul(PB[5][0:4, 0:510], lhsT=rb[:, 4 * g:4 * g + 4], rhs=ohw[:, g * 510:(g + 1) * 510], start=True, stop=True),
                     reads=['rb', 'ohw'], writes=[('pb', 5)])
                c.op('act', lambda g=g: nc.scalar.activation(out=wsb[:, g * 510:(g + 1) * 510], in_=PB[5][0:4, 0:510], func=AF.Exp), reads=[('pb', 5)], writes=['wsb'])
            c.op('dve', lambda: nc.vector.tensor_tensor(out=wsb[:], in0=wsb[:], in1=vw[:], op=ALU.mult), reads=['wsb', 'vw'], writes=['wsb'])
            c.dma('sp', lambda: nc.sync.dma_start(out=wd_t.ap().rearrange("g h u -> h g u"), in_=wsb[:].rearrange("h (g u) -> h g u", g=3)), reads=['wsb'], writes=['wd'])
            for g in range(3):
                for h in range(4):
                    for blk in range(2):
                        idx = (g * 4 + h) * 2 + blk
                        src = bass.AP(wd_t, (g * 4 + h) * 510 + blk * 255, [[1, 128], [1, 128]])
                        c.dma('sp', lambda idx=idx, src=src: nc.sync.dma_start(out=hall[:, idx, :], in_=src), reads=['wd'], writes=[('hall', idx)])
            for g in range(3):
                for hf in range(2):
                    c.op('pe', lambda g=g, hf=hf: nc.tensor.matmul(PB[6 + hf][:, :], lhsT=jf[:], rhs=hall[:, g * 8 + hf * 4: g * 8 + hf * 4 + 4, :].rearrange("p a q -> p (a q)"),
                         start=True, stop=True), reads=['jf'] + [('hall', g * 8 + hf * 4 + i) for i in range(4)], writes=[('pb', 6 + hf)])
                    c.op('act', lambda g=g, hf=hf: nc.scalar.copy(out=ebst[:, g, hf * 512:(hf + 1) * 512], in_=PB[6 + hf][:, :]), reads=[('pb', 6 + hf)], writes=[('ebst', g)])
                c.dma('sp', lambda g=g: nc.sync.dma_start(out=ebd_t.ap()[g], in_=ebst[:, g, :]), reads=[('ebst', g)], writes=[('ebd', g)])

        c.barrier()
        def norm_to_T(tidx, src_ap, n, arow, brow, akey, bkey, bufs):
            xt, t1, hb, stt, junk = bufs
            s = tidx % 2
            c.dma('sp', lambda: nc.sync.dma_start(out=xt[s][0:n, :], in_=src_ap), writes=[('xt', s)])
            c.op('act', lambda: nc.scalar.activation(out=junk[0:n, :], in_=xt[s][0:n, :], func=AF.Square, accum_out=stt[s][0:n, 0:1]),
                 reads=[('xt', s)], writes=[('stt', s), 'junk'])
            c.op('act', lambda: nc.scalar.activation(out=stt[s][0:n, 1:2], in_=stt[s][0:n, 0:1], func=AF.Sqrt, scale=1.0 / D, bias=epsb[0:n, :]),
                 reads=[('stt', s), 'epsb'], writes=[('stt', s)])
            c.op('dve', lambda: nc.vector.reciprocal(out=stt[s][0:n, 2:3], in_=stt[s][0:n, 1:2]), reads=[('stt', s)], writes=[('stt', s)])
            c.op('dve', lambda: nc.vector.scalar_tensor_tensor(out=t1[s][0:n, :], in0=xt[s][0:n, :], scalar=stt[s][0:n, 2:3], in1=arow[0:n, :],
                                                                op0=ALU.mult, op1=ALU.mult), reads=[('xt', s), ('stt', s), akey], writes=[('t1', s)])
            c.op('pool', lambda: nc.gpsimd.tensor_tensor(out=hb[s][0:n, :], in0=t1[s][0:n, :], in1=brow[0:n, :], op=ALU.add),
                 reads=[('t1', s), bkey], writes=[('hb', s)])
            pv = pbf(s).rearrange("p (k t) -> p k t", k=8)
            for k in range(8):
                c.op('pe', lambda k=k: nc.tensor.transpose(out=pv[:, k, 0:n], in_=hb[s][0:n, k * 128:(k + 1) * 128], identity=ident[0:n, 0:n]),
                     reads=[('hb', s), 'ident'], writes=[('pb', s)])
            return s, pv

        esA = ExitStack()
        with esA:
            xt = [c.sb([128, D], F32, esA) for _ in range(2)]; t1 = [c.sb([128, D], F32, esA) for _ in range(2)]
            hb = [c.sb([128, D], BF16, esA) for _ in range(2)]; stt = [c.sb([128, 4], F32, esA) for _ in range(2)]
            junk = c.sb([128, D], BF16, esA)
            bufsA = (xt, t1, hb, stt, junk)
            for t in range(49):
                if t < 48:
                    n = 128; src = xp[t * 128:(t + 1) * 128, :]; ar, br = a1p, b1p; col = t * 128; pk = 0
                else:
                    n = 64; src = xs; ar, br = a1s, b1s; col = NEXT; pk = 1
                s, pv = norm_to_T(t, src, n, ar, br, ('a1', pk), ('b1', pk), bufsA)
                c.op('act', lambda pv=pv, col=col, n=n: nc.scalar.copy(out=hT[:, :, col:col + n], in_=pv[:, :, 0:n]), reads=[('pb', s)], writes=['hT'])
        c.barrier()
        es_mod.close()

        winr = win.rearrange("(k p) n -> p k n", p=128)

        def load_w(dst, c0, ncol, key):
            c.dma('pool', lambda: nc.gpsimd.dma_start(out=dst, in_=winr[:, :, c0:c0 + ncol]), writes=[key])

        def proj_T(ps_ap, pkey, wt, wkey, hcols):
            for k in range(8):
                c.op('pe', lambda k=k: nc.tensor.matmul(ps_ap, lhsT=wt[:, k, :], rhs=hT[:, k, hcols], start=(k == 0), stop=(k == 7)),
                     reads=['hT', wkey], writes=[pkey])

        def psv(i, sl=slice(None), n=512):
            return PB[i][sl, 0:n]

        OT = [(HALO + 512 * m, 512 * m, 512) for m in range(8)] + [(NEXT, NOWN, NS)]

        esG = ExitStack()
        with esG:
            wj = [c.sb([128, 8, 128], BF16, esG) for _ in range(2)]
            sgrow = [c.sb([128, NOT], BF16, esG) for _ in range(2)]
            for j in range(16):
                s = j % 2
                load_w(wj[s][:], 3328 + j * 128, 128, ('wj', s))
                for m, (hc, oi, n) in enumerate(OT):
                    pi = m % 2
                    pa = psv(pi, n=n)
                    proj_T(pa, ('pb', pi), wj[s], ('wj', s), slice(hc, hc + n))
                    c.op('act', lambda pa=pa, oi=oi, n=n, s=s: nc.scalar.activation(out=sgrow[s][:, oi:oi + n], in_=pa, func=AF.Sigmoid),
                         reads=[('pb', pi)], writes=[('sgrow', s)])
                c.dma('sp', lambda j=j, s=s: nc.sync.dma_start(out=sg_t.ap()[j], in_=sgrow[s][:]), reads=[('sgrow', s)], writes=[('sg', j)])

        c.barrier()
        if STOP_AFTER == 'A':
            c.finish()
            return nc

        esC = ExitStack()
        with esC:
            yT = c.sb([128, 4, NOT], BF16, esC)
            dwT = c.sb([128, 4, 31], F32, esC); dwb = c.sb([128, 4], F32, esC); lng = c.sb([128, 4], F32, esC); lnb = c.sb([128, 4], F32, esC)
            with nc.allow_non_contiguous_dma(reason="tiny per-channel parameter loads"):
                for cc in range(4):
                    c.dma('sp', lambda cc=cc: nc.sync.dma_start(out=dwT[:, cc, :], in_=dww_t.ap()[:, cc * 128:(cc + 1) * 128].rearrange("j p -> p j")), writes=[('dwT', cc)])
                c.dma('sp', lambda: nc.sync.dma_start(out=dwb[:], in_=dwb_t.ap().rearrange("o (c p) -> p (o c)", p=128)), writes=['dwb'])
                c.dma('sp', lambda: nc.sync.dma_start(out=lng[:], in_=lng_t.ap().rearrange("o (c p) -> p (o c)", p=128)), writes=['lng'])
                c.dma('sp', lambda: nc.sync.dma_start(out=lnb[:], in_=lnb_t.ap().rearrange("o (c p) -> p (o c)", p=128)), writes=['lnb'])
            uxT = c.sb([128, 4, NSQ, 34], BF16, esC)
            uTf = c.sb([128, 4, 94], F32, esC)
            sct = [c.sb([120, 512], F32, esC)] * 2
            for i4 in range(4):
                s = i4 % 2
                c.dma('sp', lambda i4=i4, s=s: nc.sync.dma_start(out=sct[s][:], in_=sconv_t.ap()[4 * i4:4 * i4 + 4].rearrange("b t c -> (b t) c")), writes=[('sct', 0)])
                for cc in range(4):
                    c.op('pe', lambda cc=cc, s=s: nc.tensor.transpose(out=PB[2][:, 0:120], in_=sct[s][:, cc * 128:(cc + 1) * 128], identity=identf[0:120, 0:120]),
                         reads=[('sct', 0), 'identf'], writes=[('pb', 2)])
                    c.op('act', lambda cc=cc, i4=i4: nc.scalar.copy(out=uxT[:, cc, 4 * i4:4 * i4 + 4, 0:30], in_=PB[2][:, 0:120].rearrange("p (b t) -> p b t", b=4)),
                         reads=[('pb', 2)], writes=['uxT'])
            c.dma('sp', lambda: nc.sync.dma_start(out=convs_t.ap()[:, 0:26, :], in_=sconv_t.ap()[:, 4:30, :]), writes=['convs_a'])
            esC1 = ExitStack()
            wul = [c.sb([128, 8, 128], BF16, esC1) for _ in range(2)]; wug = [c.sb([128, 8, 128], BF16, esC1) for _ in range(2)]
            diag = [c.sb([128, 31, 128], BF16, esC1)] * 2
            ucT = [c.sb([128, 30 + NOWN], BF16, esC1)] * 2
            sgt = [c.sb([128, 512], F32, esC1) for _ in range(2)]
            UT = [(HALO - 30, -30, 30)] + OT
            for cc in range(4):
                s = cc % 2
                load_w(wul[s][:], 2304 + cc * 128, 128, ('wul', s))
                load_w(wug[s][:], 2816 + cc * 128, 128, ('wug', s))
                for j in range(31):
                    eng = 'dve' if j % 2 == 0 else 'pool'
                    e_ = nc.vector if eng == 'dve' else nc.gpsimd
                    c.op(eng, lambda j=j, e_=e_: e_.tensor_scalar(out=diag[s][:, j, :], in0=identf[:], scalar1=dwT[:, cc, j:j + 1], scalar2=None, op0=ALU.mult),
                         reads=['identf', ('dwT', cc)], writes=[('diag', 0, j)])
                for m, (hc, oi, n) in enumerate(UT):
                    pl = psv(0, n=n); pg = psv(1, n=n)
                    proj_T(pl, ('pb', 0), wul[s], ('wul', s), slice(hc, hc + n))
                    proj_T(pg, ('pb', 1), wug[s], ('wug', s), slice(hc, hc + n))
                    b_ = m % 2
                    c.op('act', lambda pg=pg, n=n, b_=b_: nc.scalar.activation(out=sgt[b_][:, 0:n], in_=pg, func=AF.Sigmoid), reads=[('pb', 1)], writes=[('sgt', b_)])
                    if oi < 0:
                        c.op('dve', lambda pl=pl, n=n, b_=b_: nc.vector.tensor_tensor(out=sgt[b_][:, 0:n], in0=pl, in1=sgt[b_][:, 0:n], op=ALU.mult),
                             reads=[('pb', 0), ('sgt', b_)], writes=[('sgt', b_)])
                        c.op('dve', lambda n=n, b_=b_: nc.vector.tensor_scalar(out=ucT[s][:, 0:30], in0=sgt[b_][:, 0:n], scalar1=hv[:, 0:1], scalar2=None, op0=ALU.mult),
                             reads=[('sgt', b_), 'hv'], writes=[('ucT', 0, 0)])
                    elif oi < NOWN:
                        c.op('dve', lambda pl=pl, n=n, b_=b_, oi=oi: nc.vector.tensor_tensor(out=ucT[s][:, 30 + oi:30 + oi + n], in0=pl, in1=sgt[b_][:, 0:n], op=ALU.mult),
                             reads=[('pb', 0), ('sgt', b_)], writes=[('ucT', 0, 1 + oi // 512)])
                        if oi == NOWN - 512:
                            c.op('dve', lambda pl=pl, b_=b_: nc.vector.tensor_tensor(out=uTf[:, cc, 0:30], in0=pl[:, 482:512], in1=sgt[b_][:, 482:512], op=ALU.mult),
                                 reads=[('pb', 0), ('sgt', b_)], writes=[('uTf', cc)])
                    else:
                        c.op('dve', lambda pl=pl, b_=b_: nc.vector.tensor_tensor(out=uTf[:, cc, 30:94], in0=pl, in1=sgt[b_][:, 0:64], op=ALU.mult),
                             reads=[('pb', 0), ('sgt', b_)], writes=[('uTf', cc)])
                        c.op('dve', lambda: nc.vector.tensor_copy(out=uxT[:, cc, :, 30:34], in_=uTf[:, cc, 30:94].rearrange("p (b t) -> p b t", t=4)),
                             reads=[('uTf', cc)], writes=['uxT'])
                dkeys = [('diag', 0, j) for j in range(31)]
                for m in range(8):
                    py = psv(2 + m % 2)
                    for j in range(31):
                        c.op('pe', lambda j=j, m=m, py=py: nc.tensor.matmul(py, lhsT=diag[s][:, j, :], rhs=ucT[s][:, 512 * m + j: 512 * m + j + 512], start=(j == 0), stop=(j == 30)),
                             reads=[dkeys[j], ('ucT', 0, 0), ('ucT', 0, 1 + m), ('ucT', 0, m)], writes=[('pb', 2 + m % 2)])
                    c.op('act', lambda m=m, py=py: nc.scalar.activation(out=yT[:, cc, 512 * m:512 * m + 512], in_=py, func=AF.Identity, bias=dwb[:, cc:cc + 1], scale=1.0),
                         reads=[('pb', 2 + m % 2), 'dwb'], writes=[('yT', m)])
                pys = PB[2][:, 0:64]
                for j in range(31):
                    c.op('pe', lambda j=j: nc.tensor.matmul(pys.rearrange("p (b t) -> p b t", t=4), lhsT=diag[s][:, j, :], rhs=uxT[:, cc, :, j:j + 4], start=(j == 0), stop=(j == 30)),
                         reads=[dkeys[j], 'uxT'], writes=[('pb', 2)])
                c.op('act', lambda: nc.scalar.activation(out=yT[:, cc, NOWN:NOT], in_=pys, func=AF.Identity, bias=dwb[:, cc:cc + 1], scale=1.0),
                     reads=[('pb', 2), 'dwb'], writes=[('yT', 8)])
            c.barrier()
            esC1.close()
            cpo = c.sb([94, 512], F32, esC)
            for cc in range(4):
                c.op('pe', lambda cc=cc: nc.tensor.transpose(out=PB[0][0:94, 0:128], in_=uTf[:, cc, :], identity=identf[:]), reads=[('uTf', cc), 'identf'], writes=[('pb', 0)])
                c.op('act', lambda cc=cc: nc.scalar.copy(out=cpo[:, cc * 128:(cc + 1) * 128], in_=PB[0][0:94, 0:128]), reads=[('pb', 0)], writes=['cpo'])
            c.dma('sp', lambda: nc.sync.dma_start(out=convp_t.ap(), in_=cpo[0:30, :]), reads=['cpo'], writes=['convp'])
            for b in range(NSQ):
                c.dma('sp', lambda b=b: nc.sync.dma_start(out=convs_t.ap()[b, 26:30, :], in_=cpo[30 + 4 * b:34 + 4 * b, :]), reads=['cpo'], writes=[('convs_b', b)])
            sq = c.sb([128, 4, 512], BF16, esC); mean = c.sb([128, 512], F32, esC); msq = c.sb([128, 512], F32, esC)
            var = c.sb([128, 512], F32, esC); rstd = c.sb([128, 512], F32, esC); tt = c.sb([128, 4, 512], F32, esC)
            for m, (hc, oi, n) in enumerate(OT):
                yk = ('yT', m)
                c.op('act', lambda oi=oi, n=n: nc.scalar.activation(out=sq[:, :, 0:n], in_=yT[:, :, oi:oi + n], func=AF.Square), reads=[yk], writes=['sq'])
                p1 = psv(4, n=n); p2 = psv(5, n=n)
                for cc in range(4):
                    c.op('pe', lambda cc=cc, p1=p1, oi=oi, n=n: nc.tensor.matmul(p1, lhsT=onesb[:], rhs=yT[:, cc, oi:oi + n], start=(cc == 0), stop=(cc == 3)),
                         reads=[yk, 'onesb'], writes=[('pb', 4)])
                for cc in range(4):
                    c.op('pe', lambda cc=cc, p2=p2, n=n: nc.tensor.matmul(p2, lhsT=onesb[:], rhs=sq[:, cc, 0:n], start=(cc == 0), stop=(cc == 3)),
                         reads=['sq', 'onesb'], writes=[('pb', 5)])
                c.op('dve', lambda p1=p1, n=n: nc.vector.tensor_scalar(out=mean[:, 0:n], in0=p1, scalar1=1.0 / 512, scalar2=None, op0=ALU.mult), reads=[('pb', 4)], writes=['mean'])
                c.op('pool', lambda n=n: nc.gpsimd.tensor_tensor(out=msq[:, 0:n], in0=mean[:, 0:n], in1=mean[:, 0:n], op=ALU.mult), reads=['mean'], writes=['msq'])
                c.op('dve', lambda p2=p2, n=n: nc.vector.scalar_tensor_tensor(out=var[:, 0:n], in0=p2, scalar=1.0 / 512, in1=msq[:, 0:n], op0=ALU.mult, op1=ALU.subtract),
                     reads=[('pb', 5), 'msq'], writes=['var'])
                c.op('act', lambda n=n: nc.scalar.activation(out=var[:, 0:n], in_=var[:, 0:n], func=AF.Sqrt, scale=1.0, bias=epsb[:, :]), reads=['var', 'epsb'], writes=['var'])
                c.op('dve', lambda n=n: nc.vector.reciprocal(out=rstd[:, 0:n], in_=var[:, 0:n]), reads=['var'], writes=['rstd'])
                c.op('dve', lambda oi=oi, n=n: nc.vector.tensor_tensor(out=tt[:, :, 0:n], in0=yT[:, :, oi:oi + n], in1=mean[:, 0:n].unsqueeze(1).to_broadcast([128, 4, n]), op=ALU.subtract),
                     reads=[yk, 'mean'], writes=['tt'])
                c.op('pool', lambda n=n: nc.gpsimd.tensor_tensor(out=tt[:, :, 0:n], in0=tt[:, :, 0:n], in1=rstd[:, 0:n].unsqueeze(1).to_broadcast([128, 4, n]), op=ALU.mult),
                     reads=['tt', 'rstd'], writes=['tt'])
                for cc in range(4):
                    c.op('act', lambda cc=cc, oi=oi, n=n: nc.scalar.activation(out=yT[:, cc, oi:oi + n], in_=tt[:, cc, 0:n], func=AF.Silu, bias=lnb[:, cc:cc + 1], scale=lng[:, cc:cc + 1]),
                         reads=['tt', 'lng', 'lnb'], writes=[yk])
            for cc in range(4):
                c.dma('sp', lambda cc=cc: nc.sync.dma_start(out=yts_t.ap()[cc], in_=yT[:, cc, :]), reads=[('yT', m) for m in range(9)], writes=[('yts', cc)])

        c.barrier()
        if STOP_AFTER == 'C':
            c.finish()
            return nc

        esD = ExitStack()
        with esD:
            ebf = c.sb([128, 1024], F32, esD)
            smk = c.sb([128, 9, 4, 4], F32, esD)
            nmk = c.sb([64, 3, 4, 64], F32, esD)
            bdm = c.sb([64, 128], F32, esD)
            pz = [c.sb([128, 4, 64], BF16, esD) for _ in range(NSQ)]
            v1n = c.sb([64, 3, 4, 65], BF16, esD)
            wkv = c.sb([128, 8, 512], BF16, esD)
            wq = c.sb([128, 8, 128], BF16, esD); wk = c.sb([128, 8, 128], BF16, esD)
            qT = c.sb([128, NOT], BF16, esD); kT = c.sb([128, NTOK], BF16, esD)
            qs = c.sb([128, 2, 64], BF16, esD); ks = c.sb([128, 2, 64], BF16, esD)
            v1 = c.sb([128, 48, 4, 65], BF16, esD)
            kvst = [c.sb([128, 512], F32, esD) for _ in range(2)]
            ef = [c.sb([128, 512], F32, esD) for _ in range(2)]
            pt = [c.sb([128, 512], BF16, esD) for _ in range(2)]
            oev = [c.sb([128, 130], F32, esD) for _ in range(2)]
            ctile = [c.sb([128, 512], F32, esD) for _ in range(2)]
            kTs = [c.sb([128, 2, 128], BF16, esD) for _ in range(2)]
            v1s = [c.sb([128, 4, 65], BF16, esD) for _ in range(2)]
            ess = c.sb([128, 16], F32, esD)
            en = c.sb([64, 256], F32, esD); pn = c.sb([64, 4, 64], BF16, esD)
            osv = c.sb([64, 260], F32, esD)
            c.dma('sp', lambda: nc.sync.dma_start(out=bdm[:], in_=bd_t.ap()), writes=['bdm'])
            c.op('pool', lambda: nc.gpsimd.memset(smk[:], 0.0), writes=['smk'])
            for b in range(NSQ):
                c.op('pool', lambda b=b: nc.gpsimd.memset(pz[b][:], 0.0), writes=[('pz', b)])
            for s in range(2):
                c.op('pool', lambda s=s: nc.gpsimd.memset(v1s[s][:], 1.0), writes=[('v1s', s)])
            c.op('pool', lambda: nc.gpsimd.memset(v1n[:], 1.0), writes=['v1n'])
            n_os = 1
            zl = c.sb([128, 64], BF16, esD); zr = c.sb([128, 260], BF16, esD)
            c.op('pool', lambda: nc.gpsimd.memset(zl[:], 0.0), writes=['zl'])
            c.op('pool', lambda: nc.gpsimd.memset(zr[:], 0.0), writes=['zr'])
            c.op('pe', lambda: nc.tensor.matmul(PB[7][0:64, 0:260], lhsT=zl[:], rhs=zr[:], start=True, stop=False), reads=['zl', 'zr'], writes=['pb7'])
            qb_i = 0
            for g, (wing, d) in enumerate(GROUPS):
                e0 = HALO - wing
                nbl = (NEXT - e0) // (128 * d)
                ebv = ebf[:].rearrange("p (h b q) -> p h b q", h=4, b=2)
                c.dma('sp', lambda g=g: nc.sync.dma_start(out=ebf[:], in_=ebd_t.ap()[g]), reads=[('ebd', g)], writes=['ebf'])
                if g == 0:
                    c.op('dve', lambda: nc.vector.tensor_copy(out=smk[:, 0, :, :], in_=ebv[:, :, 0, 0:4]), reads=['ebf'], writes=['smk'])
                else:
                    for r in range(4):
                        c.op('dve', lambda r=r, g=g: nc.vector.tensor_copy(out=smk[:, 1 + 4 * (g - 1) + r, :, r:r + 1], in_=ebv[:, :, 0, 0:1]), reads=['ebf'], writes=['smk'])
                bsel = bdm[:, 0:64] if g == 0 else bdm[:, 64:128]
                c.op('dve', lambda g=g, bsel=bsel: nc.vector.tensor_tensor(out=nmk[:, g, :, :], in0=ebv[0:64, :, 1, 0:64], in1=bsel.unsqueeze(1).to_broadcast([64, 4, 64]), op=ALU.mult),
                     reads=['ebf', 'bdm'], writes=['nmk'])
                c.dma('pool', lambda g=g: nc.gpsimd.dma_start(out=wkv[:, :, 0:256], in_=winr[:, :, 768 + 256 * g: 1024 + 256 * g]), writes=['wkv_k'])
                c.dma('pool', lambda g=g: nc.gpsimd.dma_start(out=wkv[:, :, 256:512], in_=winr[:, :, 1536 + 256 * g: 1792 + 256 * g]), writes=['wkv_v'])
                nkv = 0
                for r in range(d):
                    for i in range(nbl):
                        bi = r * nbl + i
                        cs0 = e0 + r + 128 * d * i
                        hsl = slice(cs0, cs0 + 127 * d + 1, d)
                        pi = 4 + nkv % 3
                        for k in range(8):
                            c.op('pe', lambda k=k, hsl=hsl, pi=pi: nc.tensor.matmul(PB[pi][:, :], lhsT=hT[:, k, hsl], rhs=wkv[:, k, :], start=(k == 0), stop=(k == 7)),
                                 reads=['hT', 'wkv_k', 'wkv_v'], writes=[('pb', pi)])
                        vc = hv if i == 0 else one1
                        c.op('dve', lambda bi=bi, pi=pi, vc=vc: nc.vector.tensor_scalar(out=v1[:, bi, :, 0:64], in0=PB[pi][:, 256:512].rearrange("p (h e) -> p h e", h=4),
                             scalar1=vc[:, 0:1], scalar2=None, op0=ALU.mult), reads=[('pb', pi), 'hv', 'one1'], writes=[('v1', bi)])
                        c.op('pool', lambda bi=bi, vc=vc: nc.gpsimd.tensor_copy(out=v1[:, bi, :, 64:65], in_=vc[:, 0:1].unsqueeze(1).to_broadcast([128, 4, 1])),
                             reads=['hv', 'one1'], writes=[('v1o', bi)])
                        if i == nbl - 1:
                            s = nkv % 2
                            c.op('act', lambda s=s, pi=pi: nc.scalar.copy(out=kvst[s][:], in_=PB[pi][:, :]), reads=[('pb', pi)], writes=[('kvst', s)])
                            c.dma('sp', lambda s=s, r=r, g=g, d=d, wing=wing: nc.sync.dma_start(out=kvp_t[g].ap()[r:wing:d, :], in_=kvst[s][:]), reads=[('kvst', s)], writes=[('kvp', g, r)])
                        nkv += 1
                pi = 4 + nkv % 3
                for k in range(8):
                    c.op('pe', lambda k=k, pi=pi: nc.tensor.matmul(PB[pi][0:64, :], lhsT=hT[:, k, NEXT:NTOK], rhs=wkv[:, k, :], start=(k == 0), stop=(k == 7)),
                         reads=['hT', 'wkv_k', 'wkv_v'], writes=[('pb', pi)])
                s = nkv % 2
                c.op('act', lambda s=s, pi=pi: nc.scalar.copy(out=kvst[s][0:64, :], in_=PB[pi][0:64, :]), reads=[('pb', pi)], writes=[('kvst', s)])
                c.op('dve', lambda g=g, pi=pi: nc.vector.tensor_copy(out=v1n[:, g, :, 0:64], in_=PB[pi][0:64, 256:512].rearrange("p (h e) -> p h e", h=4)), reads=[('pb', pi)], writes=['v1n'])
                for b in range(NSQ):
                    c.dma('sp', lambda b=b, s=s, g=g, wing=wing: nc.sync.dma_start(out=kvs_t[g].ap()[b, wing - 4:wing, :], in_=kvst[s][4 * b:4 * b + 4, :]), reads=[('kvst', s)], writes=[('kvsn', g, b)])
                for pair in range(2):
                    if _os_env('MK_SKIP_ATT'):
                        continue
                    c.dma('pool', lambda g=g, pair=pair: nc.gpsimd.dma_start(out=wq[:], in_=winr[:, :, 256 * g + 128 * pair: 256 * g + 128 * pair + 128]), writes=['wq'])
                    c.dma('pool', lambda g=g, pair=pair: nc.gpsimd.dma_start(out=wk[:], in_=winr[:, :, 768 + 256 * g + 128 * pair: 768 + 256 * g + 128 * pair + 128]), writes=['wk'])
                    npj = 0
                    for (hc, oi, n) in OT:
                        pi = 4 + npj % 3
                        proj_T(PB[pi][:, 0:n], ('pb', pi), wq, 'wq', slice(hc, hc + n))
                        c.op('act', lambda pi=pi, oi=oi, n=n: nc.scalar.copy(out=qT[:, oi:oi + n], in_=PB[pi][:, 0:n]), reads=[('pb', pi)], writes=['qT'])
                        npj += 1
                    cs_ = e0
                    while cs_ < NTOK:
                        n = min(512, NTOK - cs_)
                        pi = 4 + npj % 3
                        proj_T(PB[pi][:, 0:n], ('pb', pi), wk, 'wk', slice(cs_, cs_ + n))
                        if npj % 2 == 0:
                            c.op('dve', lambda pi=pi, cs_=cs_, n=n: nc.vector.tensor_copy(out=kT[:, cs_:cs_ + n], in_=PB[pi][:, 0:n]), reads=[('pb', pi)], writes=['kT'])
                        else:
                            c.op('act', lambda pi=pi, cs_=cs_, n=n: nc.scalar.copy(out=kT[:, cs_:cs_ + n], in_=PB[pi][:, 0:n]), reads=[('pb', pi)], writes=['kT_a'])
                        npj += 1
                        cs_ += n
                    c.op('dve', lambda pair=pair: nc.vector.tensor_copy(out=qs[:, pair, :], in_=qT[:, NOWN:NOT]), reads=['qT'], writes=['qs'])
                    c.op('dve', lambda pair=pair: nc.vector.tensor_copy(out=ks[:, pair, :], in_=kT[:, NEXT:NTOK]), reads=['kT', 'kT_a'], writes=['ks'])
                    for r in range(d):
                        for i in range(1, nbl):
                            s = qb_i % 2
                            qb_i += 1
                            q0 = r + 128 * d * (i - 1)
                            qsl = slice(q0, q0 + 127 * d + 1, d)
                            SBK = ((0, 1), (4, 5))[s]
                            for hh in range(2):
                                po = hh * 64
                                for blk in range(2):
                                    k0 = e0 + r + 128 * d * (i - 1 + blk)
                                    ksl = slice(k0, k0 + 127 * d + 1, d)
                                    c.op('pe', lambda bk=SBK[hh], po=po, ksl=ksl, qsl=qsl, blk=blk: nc.tensor.matmul(PB[bk][:, blk * 128:blk * 128 + 128], lhsT=kT[po:po + 64, ksl], rhs=qT[po:po + 64, qsl], start=True, stop=True),
                                         reads=['kT', 'kT_a', 'qT'], writes=[('pb', SBK[hh])])
                            for hh in range(2):
                                c.op('act', lambda s=s, hh=hh, bk=SBK[hh]: nc.scalar.activation(out=ef[s][:, hh * 256:(hh + 1) * 256], in_=PB[bk][:, 0:256], func=AF.Exp, scale=0.125),
                                     reads=[('pb', SBK[hh])], writes=[('ef', s, hh)])
                            c.op('dve', lambda s=s, pair=pair: nc.vector.tensor_tensor(out=pt[s][:], in0=ef[s][:], in1=ebf[:, pair * 512:(pair + 1) * 512], op=ALU.mult),
                                 reads=[('ef', s, 0), ('ef', s, 1), 'ebf'], writes=[('pt', s)])
                            for hh in range(2):
                                for blk in range(2):
                                    bi = r * nbl + i - 1 + blk
                                    col = (hh * 2 + blk) * 128
                                    c.op('pe', lambda s=s, hh=hh, blk=blk, bi=bi, col=col, pair=pair: nc.tensor.matmul(PB[2 + s][:, hh * 65:hh * 65 + 65], lhsT=pt[s][:, col:col + 128],
                                         rhs=v1[:, bi, 2 * pair + hh, :], start=(blk == 0), stop=(blk == 1)), reads=[('pt', s), ('v1', bi), ('v1o', bi)], writes=[('pb', 2 + s)])
                            c.op('dve', lambda s=s: nc.vector.tensor_copy(out=oev[s][:], in_=PB[2 + s][:, 0:130]), reads=[('pb', 2 + s)], writes=[('oev', s)])
                            c.dma('sp', lambda s=s, g=g, q0=q0, d=d, pair=pair: nc.sync.dma_start(out=acc_t.ap()[g, q0:q0 + 127 * d + 1:d, pair * 130:(pair + 1) * 130], in_=oev[s][:]),
                                  reads=[('oev', s)], writes=[('acc', g, q0, pair)])
                import os as _os
                ntile = 1 if g == 0 else 4
                if _os.environ.get('MK_SKIP_SAMPLE'):
                    continue
                for b in range(NSQ):
                    for r in range(ntile):
                        s = (b * ntile + r) % 2
                        tix = 0 if g == 0 else 1 + 4 * (g - 1) + r
                        c.dma('sp', lambda s=s, b=b, r=r, g=g, d=d, wing=wing: nc.sync.dma_start(out=ctile[s][:], in_=ck_t[g].ap()[b, r:wing:d, :]), writes=[('ctile', s)])
                        for pr in range(2):
                            c.op('pe', lambda s=s, pr=pr: nc.tensor.transpose(out=PB[6][:, pr * 128:(pr + 1) * 128], in_=ctile[s][:, pr * 128:(pr + 1) * 128], identity=identf[:]),
                                 reads=[('ctile', s), 'identf'], writes=[('pb', 6)])
                        c.op('act', lambda s=s: nc.scalar.copy(out=kTs[s][:].rearrange("p a k -> p (a k)"), in_=PB[6][:, 0:256]), reads=[('pb', 6)], writes=[('kTs', s)])
                        c.op('pool', lambda s=s: nc.gpsimd.tensor_copy(out=v1s[s][:, :, 0:64], in_=ctile[s][:, 256:512].rearrange("p (h e) -> p h e", h=4)), reads=[('ctile', s)], writes=[('v1s', s)])
                        for h in range(4):
                            po = (h % 2) * 64
                            bk = 6 if h % 2 == 0 else 5
                            c.op('pe', lambda s=s, h=h, po=po, b=b, bk=bk: nc.tensor.matmul(PB[bk][:, 256 + 4 * h:260 + 4 * h], lhsT=kTs[s][po:po + 64, h // 2, :], rhs=qs[po:po + 64, h // 2, 4 * b:4 * b + 4], start=True, stop=True),
                                 reads=[('kTs', s), 'qs'], writes=[('pb', bk)])
                        essv = ess[:].rearrange("p (h q) -> p h q", h=4)
                        for par, bk in ((0, 6), (1, 5)):
                            c.op('act', lambda par=par, bk=bk, essv=essv: nc.scalar.activation(out=essv[:, par:4:2, :], in_=PB[bk][:, 256:272].rearrange("p (h q) -> p h q", h=4)[:, par:4:2, :], func=AF.Exp, scale=0.125),
                                 reads=[('pb', bk)], writes=[('ess', par)])
                        c.op('dve', lambda b=b, tix=tix: nc.vector.tensor_tensor(out=pz[b][:, :, 4 * b:4 * b + 4], in0=ess[:].rearrange("p (h q) -> p h q", h=4), in1=smk[:, tix, :, :], op=ALU.mult),
                             reads=[('ess', 0), ('ess', 1), 'smk'], writes=[('pz', b)])
                        for h in range(4):
                            c.op('pe', lambda s=s, h=h, b=b, st=(n_os == 0): nc.tensor.matmul(PB[7][0:64, 65 * h:65 * h + 65], lhsT=pz[b][:, h, :], rhs=v1s[s][:, h, :], start=st, stop=False),
                                 reads=[('pz', b), ('v1s', s)], writes=['pb7'])
                        n_os += 1
                for h in range(4):
                    po = (h % 2) * 64
                    bk = 6 if h % 2 == 0 else 5
                    c.op('pe', lambda h=h, po=po, bk=bk: nc.tensor.matmul(PB[bk][0:64, 64 * h:64 * h + 64], lhsT=ks[po:po + 64, h // 2, :], rhs=qs[po:po + 64, h // 2, :], start=True, stop=True),
                         reads=['ks', 'qs'], writes=[('pb', bk)])
                for h in range(4):
                    bk = 6 if h % 2 == 0 else 5
                    c.op('act', lambda h=h, bk=bk: nc.scalar.activation(out=en[:, 64 * h:64 * h + 64], in_=PB[bk][0:64, 64 * h:64 * h + 64], func=AF.Exp, scale=0.125), reads=[('pb', bk)], writes=[('en', h)])
                c.op('dve', lambda g=g: nc.vector.tensor_tensor(out=pn[:], in0=en[:].rearrange("p (h q) -> p h q", h=4), in1=nmk[:, g, :, :], op=ALU.mult), reads=[('en', 0), ('en', 1), ('en', 2), ('en', 3), 'nmk'], writes=['pn'])
                for h in range(4):
                    c.op('pe', lambda h=h, g=g: nc.tensor.matmul(PB[7][0:64, 65 * h:65 * h + 65], lhsT=pn[:, h, :], rhs=v1n[:, g, h, :], start=False, stop=(g == 2 and h == 3)),
                         reads=['pn', 'v1n'], writes=['pb7'])
            c.op('act', lambda: nc.scalar.copy(out=osv[:], in_=PB[7][0:64, 0:260]), reads=['pb7'], writes=['osv'])
            c.dma('sp', lambda: nc.sync.dma_start(out=accs_t.ap(), in_=osv[:]), reads=['osv'], writes=['accs'])
        c.barrier()
        es_hT.close()
        if STOP_AFTER == 'D':
            c.finish()
            return nc
        wcor = wco_t.ap().rearrange("(c p) n -> p c n", p=128)
        waor = wao_t.ap().rearrange("(c p) n -> p c n", p=128)
        wor = wo_t.ap().rearrange("(c p) n -> p c n", p=128)
        wrtr = wrt_t.ap().rearrange("(c p) n -> p c n", p=128)
        h2d_t = dscr("h2scr", [NOT, D], BF16)
        esE = ExitStack()
        es.enter_context(esE)
        lgall = c.sb([128, NTILE, 36], F32, esE)
        esE1 = ExitStack()
        with esE1:
            wco = c.sb([128, 4, D], BF16, esE1); wao = c.sb([128, 2, D], BF16, esE1); wo = c.sb([128, 8, D], BF16, esE1)
            wrt = c.sb([128, 8, 36], BF16, esE1); brtb = c.sb([128, 36], F32, esE1)
            c.dma('pool', lambda: nc.gpsimd.dma_start(out=wco[:], in_=wcor), writes=['wco'])
            c.dma('pool', lambda: nc.gpsimd.dma_start(out=wao[:], in_=waor), writes=['wao'])
            c.dma('pool', lambda: nc.gpsimd.dma_start(out=wo[:], in_=wor), writes=['wo'])
            c.dma('pool', lambda: nc.gpsimd.dma_start(out=wrt[:], in_=wrtr), writes=['wrt'])
            c.dma('sp', lambda: nc.sync.dma_start(out=brtb[:], in_=brt_t.ap().partition_broadcast(128)), writes=['brtb'])
            mrow = {}
            for kind, nm in ((0, 'g1'), (1, 'b2'), (2, 'a2')):
                for part, (c0, n) in enumerate(((0, 128), (128, 64))):
                    t_ = c.sb([n, D], F32, esE1)
                    c.dma('sp', lambda t_=t_, kind=kind, c0=c0, n=n: nc.sync.dma_start(out=t_[:], in_=modrows_t.ap()[kind, c0:c0 + n, :]),
                          reads=[('modrows', kind, part)], writes=[('mrow', nm, part)])
                    mrow[(nm, part)] = t_
            c.op('pool', lambda: nc.gpsimd.memset(lgall[:], 0.0), writes=['lgall'])
            ytile = [c.sb([128, 4, 512], BF16, esE1) for _ in range(2)]
            oT = c.sb([128, 2, 512], BF16, esE1)
            acc3 = [c.sb([128, 3, 260], F32, esE1) for _ in range(2)]
            osum = c.sb([128, 260], F32, esE1); rden = c.sb([128, 4], F32, esE1); ob = c.sb([128, 256], BF16, esE1)
            sga = [c.sb([128, 512], BF16, esE1) for _ in range(2)]; sgb = [c.sb([128, 512], BF16, esE1) for _ in range(2)]
            m1 = [c.sb([128, 512], F32, esE1) for _ in range(2)]; m2 = [c.sb([128, 512], F32, esE1) for _ in range(2)]
            mixT = c.sb([128, 8, 512], BF16, esE1)
            xt2 = [c.sb([128, D], F32, esE1) for _ in range(2)]; tmp = [c.sb([128, D], F32, esE1) for _ in range(2)]
            x1t = [c.sb([128, D], F32, esE1) for _ in range(2)]; t2 = [c.sb([128, D], F32, esE1) for _ in range(2)]
            h2b = [c.sb([128, D], BF16, esE1) for _ in range(2)]
            h2T = c.sb([128, 8, 128], BF16, esE1); st2 = [c.sb([128, 4], F32, esE1) for _ in range(2)]
            junk2 = c.sb([128, D], BF16, esE1)
            sub_i = 0
            for M, (hc, oi, n) in enumerate(OT):
                part = 0 if M < 8 else 1
                rr = 128 if M < 8 else 64
                nsub = n // rr
                ys_ = M % 2
                c.dma('sp', lambda ys_=ys_, oi=oi, n=n: nc.sync.dma_start(out=ytile[ys_][:, :, 0:n], in_=yts_t.ap()[:, :, oi:oi + n].rearrange("c p t -> p c t")),
                      reads=[('yts', cc) for cc in range(4)], writes=[('ytile', ys_)])
                for t in range(nsub):
                    a_ = (M * 4 + t) % 2
                    r0 = oi + rr * t
                    if M < 8:
                        c.dma('sp', lambda a_=a_, r0=r0: nc.sync.dma_start(out=acc3[a_][:], in_=acc_t.ap()[:, r0:r0 + 128, :].rearrange("g t c -> t g c")),
                              reads=[k for k in c.state if isinstance(k, tuple) and k[0] == 'acc'], writes=[('acc3', a_)])
                        c.op('dve', lambda a_=a_: nc.vector.tensor_tensor(out=osum[:], in0=acc3[a_][:, 0, :], in1=acc3[a_][:, 1, :], op=ALU.add), reads=[('acc3', a_)], writes=['osum'])
                        c.op('dve', lambda a_=a_: nc.vector.tensor_tensor(out=osum[:], in0=osum[:], in1=acc3[a_][:, 2, :], op=ALU.add), reads=[('acc3', a_), 'osum'], writes=['osum'])
                    else:
                        c.dma('sp', lambda a_=a_: nc.sync.dma_start(out=acc3[a_][0:64, 0, :], in_=accs_t.ap()), reads=['accs'], writes=[('acc3', a_)])
                        c.op('dve', lambda a_=a_: nc.vector.tensor_copy(out=osum[0:64, :], in_=acc3[a_][0:64, 0, :]), reads=[('acc3', a_)], writes=['osum'])
                    ov = osum[0:rr, :].rearrange("p (h e) -> p h e", h=4)
                    c.op('dve', lambda ov=ov, rr=rr: nc.vector.reciprocal(out=rden[0:rr, :].unsqueeze(2), in_=ov[:, :, 64:65]), reads=['osum'], writes=['rden'])
                    c.op('dve', lambda ov=ov, rr=rr: nc.vector.tensor_tensor(out=ob[0:rr, :].rearrange("p (h e) -> p h e", h=4), in0=ov[:, :, 0:64],
                         in1=rden[0:rr, :].unsqueeze(2).to_broadcast([rr, 4, 64]), op=ALU.mult), reads=['osum', 'rden'], writes=['ob'])
                    pv4 = pbf(4).rearrange("p (k t) -> p k t", k=8)
                    for k in range(2):
                        c.op('pe', lambda k=k, rr=rr, pv4=pv4: nc.tensor.transpose(out=pv4[:, k, 0:rr], in_=ob[0:rr, k * 128:(k + 1) * 128], identity=ident[0:rr, 0:rr]),
                             reads=['ob', 'ident'], writes=[('pb', 4)])
                    c.op('act', lambda t=t, rr=rr, pv4=pv4: nc.scalar.copy(out=oT[:, :, rr * t:rr * t + rr], in_=pv4[:, 0:2, 0:rr]), reads=[('pb', 4)], writes=['oT'])
                for j in range(8):
                    gs = j % 2
                    c.dma('sp', lambda gs=gs, j=j, oi=oi, n=n: nc.sync.dma_start(out=sga[gs][:, 0:n], in_=sg_t.ap()[j, :, oi:oi + n]), reads=[('sg', j)], writes=[('sga', gs)])
                    c.dma('sp', lambda gs=gs, j=j, oi=oi, n=n: nc.sync.dma_start(out=sgb[gs][:, 0:n], in_=sg_t.ap()[8 + j, :, oi:oi + n]), reads=[('sg', 8 + j)], writes=[('sgb', gs)])
                    pa_i, pb_i = (0, 1) if gs == 0 else (6, 7)
                    for cc in range(4):
                        c.op('pe', lambda cc=cc, j=j, pa_i=pa_i, ys_=ys_, n=n: nc.tensor.matmul(PB[pa_i][:, 0:n], lhsT=wco[:, cc, j * 128:(j + 1) * 128], rhs=ytile[ys_][:, cc, 0:n], start=(cc == 0), stop=(cc == 3)),
                             reads=['wco', ('ytile', ys_)], writes=[('pb', pa_i)])
                    for cc in range(2):
                        c.op('pe', lambda cc=cc, j=j, pb_i=pb_i, n=n: nc.tensor.matmul(PB[pb_i][:, 0:n], lhsT=wao[:, cc, j * 128:(j + 1) * 128], rhs=oT[:, cc, 0:n], start=(cc == 0), stop=(cc == 1)),
                             reads=['wao', 'oT'], writes=[('pb', pb_i)])
                    c.op('dve', lambda gs=gs, pa_i=pa_i, n=n: nc.vector.tensor_tensor(out=m1[gs][:, 0:n], in0=PB[pa_i][:, 0:n], in1=sga[gs][:, 0:n], op=ALU.mult), reads=[('pb', pa_i), ('sga', gs)], writes=[('m1', gs)])
                    c.op('dve', lambda gs=gs, pb_i=pb_i, n=n: nc.vector.tensor_tensor(out=m2[gs][:, 0:n], in0=PB[pb_i][:, 0:n], in1=sgb[gs][:, 0:n], op=ALU.mult), reads=[('pb', pb_i), ('sgb', gs)], writes=[('m2', gs)])
                    c.op('pool', lambda gs=gs, j=j, n=n: nc.gpsimd.tensor_tensor(out=mixT[:, j, 0:n], in0=m1[gs][:, 0:n], in1=m2[gs][:, 0:n], op=ALU.add), reads=[('m1', gs), ('m2', gs)], writes=[('mixT', j)])
                for t in range(nsub):
                    xs_ = sub_i % 2
                    sub_i += 1
                    r0 = oi + rr * t
                    tile_i = r0 // 128 if M < 8 else 32
                    src = xp[HALO + r0:HALO + r0 + 128, :] if M < 8 else xs
                    c.dma('sp', lambda xs_=xs_, src=src, rr=rr: nc.sync.dma_start(out=xt2[xs_][0:rr, :], in_=src), writes=[('xt2', xs_)])
                    for hf in range(2):
                        for j in range(8):
                            c.op('pe', lambda hf=hf, j=j, t=t, rr=rr: nc.tensor.matmul(PB[2 + hf][0:rr, :], lhsT=mixT[:, j, rr * t:rr * t + rr], rhs=wo[:, j, hf * 512:(hf + 1) * 512], start=(j == 0), stop=(j == 7)),
                                 reads=[('mixT', j), 'wo'], writes=[('pb', 2 + hf)])
                        c.op('dve', lambda hf=hf, xs_=xs_, rr=rr, part=part: nc.vector.tensor_tensor(out=tmp[xs_][0:rr, hf * 512:(hf + 1) * 512], in0=PB[2 + hf][0:rr, :],
                             in1=mrow[('g1', part)][0:rr, hf * 512:(hf + 1) * 512], op=ALU.mult), reads=[('pb', 2 + hf), ('mrow', 'g1', part)], writes=[('tmp', xs_)])
                    c.op('pool', lambda xs_=xs_, rr=rr: nc.gpsimd.tensor_tensor(out=x1t[xs_][0:rr, :], in0=tmp[xs_][0:rr, :], in1=xt2[xs_][0:rr, :], op=ALU.add),
                         reads=[('tmp', xs_), ('xt2', xs_)], writes=[('x1t', xs_)])
                    c.dma('sp', lambda xs_=xs_, r0=r0, rr=rr: nc.sync.dma_start(out=x1_t.ap()[r0:r0 + rr, :], in_=x1t[xs_][0:rr, :]), reads=[('x1t', xs_)], writes=[('x1', tile_i)])
                    c.op('act', lambda xs_=xs_, rr=rr: nc.scalar.activation(out=junk2[0:rr, :], in_=x1t[xs_][0:rr, :], func=AF.Square, accum_out=st2[xs_][0:rr, 0:1]),
                         reads=[('x1t', xs_)], writes=[('st2', xs_), 'junk2'])
                    c.op('act', lambda xs_=xs_, rr=rr: nc.scalar.activation(out=st2[xs_][0:rr, 1:2], in_=st2[xs_][0:rr, 0:1], func=AF.Sqrt, scale=1.0 / D, bias=epsb[0:rr, :]),
                         reads=[('st2', xs_), 'epsb'], writes=[('st2', xs_)])
                    c.op('dve', lambda xs_=xs_, rr=rr: nc.vector.reciprocal(out=st2[xs_][0:rr, 2:3], in_=st2[xs_][0:rr, 1:2]), reads=[('st2', xs_)], writes=[('st2', xs_)])
                    c.op('dve', lambda xs_=xs_, rr=rr, part=part: nc.vector.scalar_tensor_tensor(out=t2[xs_][0:rr, :], in0=x1t[xs_][0:rr, :], scalar=st2[xs_][0:rr, 2:3],
                         in1=mrow[('a2', part)][0:rr, :], op0=ALU.mult, op1=ALU.mult), reads=[('x1t', xs_), ('st2', xs_), ('mrow', 'a2', part)], writes=[('t2', xs_)])
                    c.op('pool', lambda xs_=xs_, rr=rr, part=part: nc.gpsimd.tensor_tensor(out=h2b[xs_][0:rr, :], in0=t2[xs_][0:rr, :], in1=mrow[('b2', part)][0:rr, :], op=ALU.add),
                         reads=[('t2', xs_), ('mrow', 'b2', part)], writes=[('h2b', xs_)])
                    c.dma('sp', lambda xs_=xs_, r0=r0, rr=rr: nc.sync.dma_start(out=h2d_t.ap()[r0:r0 + rr, :], in_=h2b[xs_][0:rr, :]), reads=[('h2b', xs_)], writes=[('h2d', tile_i)])
                    pv5 = pbf(5).rearrange("p (k t) -> p k t", k=8)
                    for k in range(8):
                        c.op('pe', lambda k=k, xs_=xs_, rr=rr, pv5=pv5: nc.tensor.transpose(out=pv5[:, k, 0:rr], in_=h2b[xs_][0:rr, k * 128:(k + 1) * 128], identity=ident[0:rr, 0:rr]),
                             reads=[('h2b', xs_), 'ident'], writes=[('pb', 5)])
                    c.op('act', lambda rr=rr, pv5=pv5: nc.scalar.copy(out=h2T[:, :, 0:rr], in_=pv5[:, :, 0:rr]), reads=[('pb', 5)], writes=['h2T'])
                    for k in range(8):
                        c.op('pe', lambda k=k, rr=rr: nc.tensor.matmul(PB[4][0:rr, 0:36], lhsT=h2T[:, k, 0:rr], rhs=wrt[:, k, :], start=(k == 0), stop=(k == 7)),
                             reads=['h2T', 'wrt'], writes=[('pb', 4)])
                    c.op('dve', lambda rr=rr, tile_i=tile_i: nc.vector.tensor_tensor(out=lgall[0:rr, tile_i, :], in0=PB[4][0:rr, 0:36], in1=brtb[0:rr, :], op=ALU.add),
                         reads=[('pb', 4), 'brtb', 'lgall'], writes=['lgall'])
        c.barrier()
        if STOP_AFTER == 'E':
            c.finish()
            return nc
        NT = NTILE
        esF = ExitStack()
        with esF:
            def ft(shape, dt=F32):
                return c.sb(shape, dt, esF)
            gmx = ft([128, NT]); ohg = ft([128, NT, 4]); gsh = ft([128, NT, 4]); gex = ft([128, NT, 4]); gsum = ft([128, NT]); pgr = ft([128, NT])
            pen = ft([128, NT, 4]); em = ft([128, NT, 32]); m8 = ft([128, NT, 8]); i8 = ft([128, NT, 8], U32)
            e0f = ft([128, NT]); e1f = ft([128, NT]); dv = ft([128, NT]); w0 = ft([128, NT]); w1 = ft([128, NT])
            oh0 = ft([128, NT, 32]); oh1 = ft([128, NT, 32]); mm_ = ft([128, NT, 32]); cs = ft([128, NT + 1, 32])
            base = ft([128, NT, 32]); prod = ft([128, NT, 32]); d0f = ft([128, NT]); d1f = ft([128, NT])
            d0i = ft([128, NT], I32); d1i = ft([128, NT], I32)
            io32i = ft([128, 32], I32); io32 = ft([128, 32]); thri = ft([128, NBLK], I32); thr = ft([128, NBLK])
            cnt = ft([128, 32]); cni = ft([128, 32], I32); pad = ft([128, 32]); pa_ = ft([128, 32]); pb_ = ft([128, 32]); pst = ft([128, 32])
            cmpb = ft([128, NBLK, 32]); bef = ft([128, NBLK])
            c.op('pool', lambda: nc.gpsimd.iota(io32i[:], pattern=[[1, 32]], base=0, channel_multiplier=0), writes=['io32i'])
            c.op('pool', lambda: nc.gpsimd.iota(thri[:], pattern=[[128, NBLK]], base=0, channel_multiplier=0), writes=['thri'])
            c.op('dve', lambda: nc.vector.tensor_copy(out=io32[:], in_=io32i[:]), reads=['io32i'], writes=['io32'])
            c.op('dve', lambda: nc.vector.tensor_copy(out=thr[:], in_=thri[:]), reads=['thri'], writes=['thr'])
            R = ['lgall']
            gl = lgall[:, :, 0:4]
            V = nc.vector
            c.op('dve', lambda: V.tensor_reduce(out=gmx[:], in_=gl, axis=AX.X, op=ALU.max), reads=R, writes=['gmx'])
            c.op('dve', lambda: V.tensor_tensor(out=ohg[:], in0=gl, in1=gmx[:].unsqueeze(2).to_broadcast([128, NT, 4]), op=ALU.is_equal), reads=R + ['gmx'], writes=['ohg'])
            c.op('dve', lambda: V.tensor_tensor(out=gsh[:], in0=gl, in1=gmx[:].unsqueeze(2).to_broadcast([128, NT, 4]), op=ALU.subtract), reads=R + ['gmx'], writes=['gsh'])
            c.op('act', lambda: nc.scalar.activation(out=gex[:], in_=gsh[:], func=AF.Exp), reads=['gsh'], writes=['gex'])
            c.op('dve', lambda: V.tensor_reduce(out=gsum[:], in_=gex[:], axis=AX.X, op=ALU.add), reads=['gex'], writes=['gsum'])
            c.op('dve', lambda: V.reciprocal(out=pgr[:], in_=gsum[:]), reads=['gsum'], writes=['pgr'])
            c.op('dve', lambda: V.tensor_scalar(out=pen[:], in0=ohg[:], scalar1=-1.0, scalar2=1e30, op0=ALU.add, op1=ALU.mult), reads=['ohg'], writes=['pen'])
            c.op('dve', lambda: V.tensor_tensor(out=em[:].rearrange("p t (g e) -> p t g e", g=4), in0=lgall[:, :, 4:36].rearrange("p t (g e) -> p t g e", g=4),
                 in1=pen[:].unsqueeze(3).to_broadcast([128, NT, 4, 8]), op=ALU.add), reads=R + ['pen'], writes=['em'])
            for i in range(NT):
                c.op('dve', lambda i=i: V.max(out=m8[:, i, :], in_=em[:, i, :]), reads=['em'], writes=['m8'])
                c.op('dve', lambda i=i: V.max_index(out=i8[:, i, :], in_max=m8[:, i, :], in_values=em[:, i, :]), reads=['em', 'm8'], writes=['i8'])
            c.op('dve', lambda: V.tensor_copy(out=e0f[:], in_=i8[:, :, 0]), reads=['i8'], writes=['e0f'])
            c.op('dve', lambda: V.tensor_copy(out=e1f[:], in_=i8[:, :, 1]), reads=['i8'], writes=['e1f'])
            c.op('dve', lambda: V.tensor_tensor(out=dv[:], in0=m8[:, :, 1], in1=m8[:, :, 0], op=ALU.subtract), reads=['m8'], writes=['dv'])
            c.op('act', lambda: nc.scalar.activation(out=dv[:], in_=dv[:], func=AF.Exp), reads=['dv'], writes=['dv'])
            c.op('dve', lambda: V.tensor_scalar(out=dv[:], in0=dv[:], scalar1=1.0, scalar2=None, op0=ALU.add), reads=['dv'], writes=['dv'])
            c.op('dve', lambda: V.reciprocal(out=w0[:], in_=dv[:]), reads=['dv'], writes=['w0'])
            c.op('dve', lambda: V.tensor_tensor(out=w0[:], in0=w0[:], in1=pgr[:], op=ALU.mult), reads=['w0', 'pgr'], writes=['w0'])
            c.op('dve', lambda: V.tensor_tensor(out=w1[:], in0=pgr[:], in1=w0[:], op=ALU.subtract), reads=['w0', 'pgr'], writes=['w1'])
            iob = io32[:].unsqueeze(1).to_broadcast([128, NT, 32])
            c.op('dve', lambda: V.tensor_tensor(out=oh0[:], in0=iob, in1=e0f[:].unsqueeze(2).to_broadcast([128, NT, 32]), op=ALU.is_equal), reads=['io32', 'e0f'], writes=['oh0'])
            c.op('dve', lambda: V.tensor_tensor(out=oh1[:], in0=iob, in1=e1f[:].unsqueeze(2).to_broadcast([128, NT, 32]), op=ALU.is_equal), reads=['io32', 'e1f'], writes=['oh1'])
            c.op('dve', lambda: V.memset(oh0[64:128, NT - 1, :], 0.0), reads=['oh0'], writes=['oh0'])
            c.op('dve', lambda: V.memset(oh1[64:128, NT - 1, :], 0.0), reads=['oh1'], writes=['oh1'])
            c.op('dve', lambda: V.tensor_tensor(out=mm_[:], in0=oh0[:], in1=oh1[:], op=ALU.add), reads=['oh0', 'oh1'], writes=['mm'])
            c.op('dve', lambda: V.memset(cs[:, 0, :], 0.0), writes=['cs'])
            for i in range(NT):
                c.op('dve', lambda i=i: V.tensor_tensor(out=cs[:, i + 1, :], in0=cs[:, i, :], in1=mm_[:, i, :], op=ALU.add), reads=['cs', 'mm'], writes=['cs'])
            for i in range(NT):
                bk = i // 16
                co = (i % 16) * 32
                c.op('pe', lambda i=i, bk=bk, co=co: nc.tensor.matmul(PB[bk][:, co:co + 32], lhsT=suf[:], rhs=mm_[:, i, :], start=True, stop=False), reads=['suf', 'mm'], writes=[('pb', bk)])
                c.op('pe', lambda i=i, bk=bk, co=co: nc.tensor.matmul(PB[bk][:, co:co + 32], lhsT=onesf[:], rhs=cs[:, i, :], start=False, stop=True), reads=['onesf', 'cs'], writes=[('pb', bk)])
            c.op('pe', lambda: nc.tensor.matmul(PB[3][:, 0:32], lhsT=onesf[:], rhs=cs[:, NT, :], start=True, stop=True), reads=['onesf', 'cs'], writes=[('pb', 3)])
            c.op('dve', lambda: V.tensor_scalar(out=cni[:], in0=PB[3][:, 0:32], scalar1=127.0, scalar2=None, op0=ALU.add), reads=[('pb', 3)], writes=['cni'])
            c.op('dve', lambda: V.tensor_scalar(out=cni[:], in0=cni[:], scalar1=7, scalar2=7, op0=ALU.arith_shift_right, op1=ALU.logical_shift_left), reads=['cni'], writes=['cni'])
            c.op('dve', lambda: V.tensor_copy(out=pad[:], in_=cni[:]), reads=['cni'], writes=['pad'])
            src_, dst_ = pad, pa_
            for sft in (1, 2, 4, 8, 16):
                c.op('dve', lambda src_=src_, dst_=dst_, sft=sft: V.tensor_copy(out=dst_[:, 0:sft], in_=src_[:, 0:sft]), reads=['pfx', 'pad'], writes=['pfx'])
                c.op('dve', lambda src_=src_, dst_=dst_, sft=sft: V.tensor_tensor(out=dst_[:, sft:32], in0=src_[:, sft:32], in1=src_[:, 0:32 - sft], op=ALU.add), reads=['pfx', 'pad'], writes=['pfx'])
                src_, dst_ = dst_, (pb_ if dst_ is pa_ else pa_)
            pend = src_
            c.op('dve', lambda: V.tensor_tensor(out=pst[:], in0=pend[:], in1=pad[:], op=ALU.subtract), reads=['pfx', 'pad'], writes=['pst'])
            for bk in range(3):
                t0 = bk * 16
                nt_ = min(16, NT - t0)
                c.op('dve', lambda bk=bk, t0=t0, nt_=nt_: V.tensor_tensor(out=base[:, t0:t0 + nt_, :], in0=PB[bk][:, 0:nt_ * 32].rearrange("p (t e) -> p t e", e=32),
                     in1=pst[:].unsqueeze(1).to_broadcast([128, nt_, 32]), op=ALU.add), reads=[('pb', bk), 'pst'], writes=['base'])
            for oh_, df_, di_, nm in ((oh0, d0f, d0i, 'd0'), (oh1, d1f, d1i, 'd1')):
                c.op('dve', lambda oh_=oh_: V.tensor_tensor(out=prod[:], in0=oh_[:], in1=base[:], op=ALU.mult), reads=['oh0', 'oh1', 'base'], writes=['prod'])
                c.op('dve', lambda df_=df_: V.tensor_reduce(out=df_[:], in_=prod[:], axis=AX.X, op=ALU.add), reads=['prod'], writes=[nm + 'f'])
                c.op('dve', lambda df_=df_, di_=di_: V.tensor_copy(out=di_[:], in_=df_[:]), reads=[nm + 'f'], writes=[nm])
            c.op('dve', lambda: V.tensor_tensor(out=cmpb[:], in0=pend[:].unsqueeze(1).to_broadcast([128, NBLK, 32]), in1=thr[:].unsqueeze(2).to_broadcast([128, NBLK, 32]), op=ALU.is_le),
                 reads=['pfx', 'thr'], writes=['cmpb'])
            c.op('dve', lambda: V.tensor_reduce(out=bef[:], in_=cmpb[:], axis=AX.X, op=ALU.add), reads=['cmpb'], writes=['bef'])
            c.op('dve', lambda: V.tensor_scalar(out=bef[:], in0=bef[:], scalar1=31.0, scalar2=None, op0=ALU.min), reads=['bef'], writes=['bef'])
            pio = ft([128, 1], I32); piof = ft([128, 1]); widxf = ft([128, NBLK]); widx = ft([128, NBLK], I32)
            c.op('pool', lambda: nc.gpsimd.iota(pio[:], pattern=[[0, 1]], base=0, channel_multiplier=1), writes=['pio'])
            c.op('dve', lambda: V.tensor_copy(out=piof[:], in_=pio[:]), reads=['pio'], writes=['piof'])
            c.op('dve', lambda: V.tensor_scalar(out=widxf[:], in0=bef[:], scalar1=128.0, scalar2=piof[:, 0:1], op0=ALU.mult, op1=ALU.add), reads=['bef', 'piof'], writes=['widxf'])
            c.op('dve', lambda: V.tensor_copy(out=widx[:], in_=widxf[:]), reads=['widxf'], writes=['widx'])
            zt = ft([128, D], BF16)
            c.op('pool', lambda: nc.gpsimd.memset(zt[:], 0.0), writes=['zt'])
            xsv = xsd_t.ap().rearrange("(b p) d -> p b d", p=128)
            zkeys = []
            for q4 in range(4):
                b0 = q4 * 25
                nb_ = min(25, NBLK - b0)
                c.dma('sp', lambda b0=b0, nb_=nb_: nc.sync.dma_start(out=xsv[:, b0:b0 + nb_, :], in_=zt[:].unsqueeze(1).to_broadcast([128, nb_, D])), reads=['zt'], writes=[('xsz', q4)])
                zkeys.append(('xsz', q4))
            h2t = [ft([128, D], BF16) for _ in range(2)]
            skeys = []
            for i in range(NT):
                s = i % 2
                rr = 128 if i < NT - 1 else 64
                c.dma('sp', lambda s=s, i=i, rr=rr: nc.sync.dma_start(out=h2t[s][0:rr, :], in_=h2d_t.ap()[i * 128:i * 128 + rr, :]), reads=[('h2d', i)], writes=[('h2t', s)])
                for di_, nm in ((d0i, 'd0'), (d1i, 'd1')):
                    c.dma('pool', lambda s=s, i=i, rr=rr, di_=di_: nc.gpsimd.indirect_dma_start(out=xsd_t.ap(), out_offset=bass.IndirectOffsetOnAxis(ap=di_[0:rr, i:i + 1], axis=0),
                          in_=h2t[s][0:rr, :], in_offset=None), reads=[('h2t', s), nm] + zkeys, writes=[('xss', i, nm)])
                    skeys.append(('xss', i, nm))
            xsb = [ft([128, D], BF16) for _ in range(2)]; xsT = [ft([128, 8, 128], BF16) for _ in range(2)]
            wg = [ft([128, 8, 512], BF16) for _ in range(2)]; wu = [ft([128, 8, 512], BF16) for _ in range(2)]; wd = [ft([128, 4, D], BF16) for _ in range(2)]
            actt = [ft([128, 512]) for _ in range(2)]; ab = [ft([128, 512], BF16) for _ in range(2)]; aT = [ft([128, 4, 128], BF16) for _ in range(2)]
            yev = [ft([128, D]) for _ in range(2)]
            wegv = weg_t.ap().rearrange("e (p k) f -> (e p) (k f)", p=128)
            weuv = weu_t.ap().rearrange("e (p k) f -> (e p) (k f)", p=128)
            wedv = wed_t.ap().rearrange("e (p k) f -> (e p) (k f)", p=128)
            for b in range(NBLK):
                s = b % 2
                for wt_, wv_, nm in ((wg, wegv, 'wg'), (wu, weuv, 'wu'), (wd, wedv, 'wd')):
                    c.dma('pool', lambda s=s, b=b, wt_=wt_, wv_=wv_: nc.gpsimd.indirect_dma_start(out=wt_[s][:].rearrange("p k f -> p (k f)"), out_offset=None, in_=wv_,
                          in_offset=bass.IndirectOffsetOnAxis(ap=widx[:, b:b + 1], axis=0)), reads=['widx'], writes=[(nm, s)])
                c.dma('sp', lambda s=s, b=b: nc.sync.dma_start(out=xsb[s][:], in_=xsd_t.ap()[b * 128:(b + 1) * 128, :]), reads=skeys + zkeys, writes=[('xsb', s)])
                pv0 = pbf(0).rearrange("p (k t) -> p k t", k=8)
                for k in range(8):
                    c.op('pe', lambda k=k, s=s, pv0=pv0: nc.tensor.transpose(out=pv0[:, k, :], in_=xsb[s][:, k:k + 8 * 127 + 1:8], identity=ident[:]), reads=[('xsb', s), 'ident'], writes=[('pb', 0)])
                c.op('act', lambda s=s, pv0=pv0: nc.scalar.copy(out=xsT[s][:], in_=pv0), reads=[('pb', 0)], writes=[('xsT', s)])
                gi, ui = (1, 2) if s == 0 else (6, 7)
                for k in range(8):
                    c.op('pe', lambda k=k, s=s, gi=gi: nc.tensor.matmul(PB[gi][:, :], lhsT=xsT[s][:, k, :], rhs=wg[s][:, k, :], start=(k == 0), stop=(k == 7)), reads=[('xsT', s), ('wg', s)], writes=[('pb', gi)])
                for k in range(8):
                    c.op('pe', lambda k=k, s=s, ui=ui: nc.tensor.matmul(PB[ui][:, :], lhsT=xsT[s][:, k, :], rhs=wu[s][:, k, :], start=(k == 0), stop=(k == 7)), reads=[('xsT', s), ('wu', s)], writes=[('pb', ui)])
                c.op('act', lambda s=s, gi=gi: nc.scalar.activation(out=actt[s][:], in_=PB[gi][:, :], func=AF.Silu), reads=[('pb', gi)], writes=[('actt', s)])
                c.op('dve', lambda s=s, ui=ui: V.tensor_tensor(out=ab[s][:], in0=PB[ui][:, :], in1=actt[s][:], op=ALU.mult), reads=[('pb', ui), ('actt', s)], writes=[('ab', s)])
                pv3 = pbf(3).rearrange("p (k t) -> p k t", k=8)
                for k in range(4):
                    c.op('pe', lambda k=k, s=s, pv3=pv3: nc.tensor.transpose(out=pv3[:, k, :], in_=ab[s][:, k:k + 4 * 127 + 1:4], identity=ident[:]), reads=[('ab', s), 'ident'], writes=[('pb', 3)])
                c.op('dve', lambda s=s, pv3=pv3: V.tensor_copy(out=aT[s][:], in_=pv3[:, 0:4, :]), reads=[('pb', 3)], writes=[('aT', s)])
                for hf in range(2):
                    for k in range(4):
                        c.op('pe', lambda k=k, s=s, hf=hf: nc.tensor.matmul(PB[4 + hf][:, :], lhsT=aT[s][:, k, :], rhs=wd[s][:, k, hf * 512:(hf + 1) * 512], start=(k == 0), stop=(k == 3)),
                             reads=[('aT', s), ('wd', s)], writes=[('pb', 4 + hf)])
                c.op('act', lambda s=s: nc.scalar.copy(out=yev[s][:, 0:512], in_=PB[4][:, :]), reads=[('pb', 4)], writes=[('yev', s, 0)])
                c.op('dve', lambda s=s: V.tensor_copy(out=yev[s][:, 512:1024], in_=PB[5][:, :]), reads=[('pb', 5)], writes=[('yev', s, 1)])
                c.dma('sp', lambda s=s, b=b: nc.sync.dma_start(out=ysd_t.ap()[b * 128:(b + 1) * 128, :], in_=yev[s][:]), reads=[('yev', s, 0), ('yev', s, 1)], writes=[('ysd', b)])
            ykeys = [('ysd', b) for b in range(NBLK)]
            g2r = {}
            for part, (c0, n) in enumerate(((0, 128), (128, 64))):
                t_ = ft([n, D])
                c.dma('sp', lambda t_=t_, c0=c0, n=n: nc.sync.dma_start(out=t_[:], in_=modrows_t.ap()[3, c0:c0 + n, :]), reads=[('modrows', 3, part)], writes=[('g2r', part)])
                g2r[part] = t_
            gfin = ft([128, D])
            c.dma('sp', lambda: nc.sync.dma_start(out=gfin[:], in_=gfin_t.ap().partition_broadcast(128)), writes=['gfin'])
            y0 = [ft([128, D]) for _ in range(2)]; y1 = [ft([128, D]) for _ in range(2)]; x1r = [ft([128, D]) for _ in range(2)]
            fa = [ft([128, D]) for _ in range(2)]; st3 = [ft([128, 4]) for _ in range(2)]
            junk3 = ft([128, D], BF16)
            for i in range(NT):
                s = i % 2
                rr = 128 if i < NT - 1 else 64
                part = 0 if i < NT - 1 else 1
                c.dma('pool', lambda s=s, i=i, rr=rr: nc.gpsimd.indirect_dma_start(out=y0[s][0:rr, :], out_offset=None, in_=ysd_t.ap(),
                      in_offset=bass.IndirectOffsetOnAxis(ap=d0i[0:rr, i:i + 1], axis=0)), reads=ykeys + ['d0'], writes=[('y0', s)])
                c.dma('pool', lambda s=s, i=i, rr=rr: nc.gpsimd.indirect_dma_start(out=y1[s][0:rr, :], out_offset=None, in_=ysd_t.ap(),
                      in_offset=bass.IndirectOffsetOnAxis(ap=d1i[0:rr, i:i + 1], axis=0)), reads=ykeys + ['d1'], writes=[('y1', s)])
                c.dma('sp', lambda s=s, i=i, rr=rr: nc.sync.dma_start(out=x1r[s][0:rr, :], in_=x1_t.ap()[i * 128:i * 128 + rr, :]), reads=[('x1', i)], writes=[('x1r', s)])
                c.op('dve', lambda s=s, i=i, rr=rr: V.tensor_scalar(out=fa[s][0:rr, :], in0=y0[s][0:rr, :], scalar1=w0[0:rr, i:i + 1], scalar2=None, op0=ALU.mult), reads=[('y0', s), 'w0'], writes=[('fa', s)])
                c.op('dve', lambda s=s, i=i, rr=rr: V.scalar_tensor_tensor(out=fa[s][0:rr, :], in0=y1[s][0:rr, :], scalar=w1[0:rr, i:i + 1], in1=fa[s][0:rr, :], op0=ALU.mult, op1=ALU.add),
                     reads=[('y1', s), 'w1', ('fa', s)], writes=[('fa', s)])
                c.op('pool', lambda s=s, rr=rr, part=part: nc.gpsimd.tensor_tensor(out=fa[s][0:rr, :], in0=fa[s][0:rr, :], in1=g2r[part][0:rr, :], op=ALU.mult), reads=[('fa', s), ('g2r', part)], writes=[('fa', s)])
                c.op('pool', lambda s=s, rr=rr: nc.gpsimd.tensor_tensor(out=x1r[s][0:rr, :], in0=fa[s][0:rr, :], in1=x1r[s][0:rr, :], op=ALU.add), reads=[('fa', s), ('x1r', s)], writes=[('x1r', s)])
                c.op('act', lambda s=s, rr=rr: nc.scalar.activation(out=junk3[0:rr, :], in_=x1r[s][0:rr, :], func=AF.Square, accum_out=st3[s][0:rr, 0:1]), reads=[('x1r', s)], writes=[('st3', s), 'junk3'])
                c.op('act', lambda s=s, rr=rr: nc.scalar.activation(out=st3[s][0:rr, 1:2], in_=st3[s][0:rr, 0:1], func=AF.Sqrt, scale=1.0 / D, bias=epsb[0:rr, :]), reads=[('st3', s), 'epsb'], writes=[('st3', s)])
                c.op('dve', lambda s=s, rr=rr: V.reciprocal(out=st3[s][0:rr, 2:3], in_=st3[s][0:rr, 1:2]), reads=[('st3', s)], writes=[('st3', s)])
                c.op('dve', lambda s=s, rr=rr: V.scalar_tensor_tensor(out=y0[s][0:rr, :], in0=x1r[s][0:rr, :], scalar=st3[s][0:rr, 2:3], in1=gfin[0:rr, :], op0=ALU.mult, op1=ALU.mult),
                     reads=[('x1r', s), ('st3', s), 'gfin'], writes=[('y0', s)])
                dst = yp_t.ap()[i * 128:(i + 1) * 128, :] if i < NT - 1 else ys_t.ap()
                c.dma('sp', lambda s=s, rr=rr, dst=dst: nc.sync.dma_start(out=dst, in_=y0[s][0:rr, :]), reads=[('y0', s)], writes=[('yout', i)])
        c.finish()
    return nc


def build_two_pass():
    nc1 = build_nc(None)
    needed = set(nc1._mk_ctx.record)
    return build_nc(needed)


def _prep_inputs(inp):
    f = lambda a: np.ascontiguousarray(a, dtype=np.float32)
    ohw, vw, sel, bd = _structure_constants()
    shared = {
        "rel_bias": f(inp["rel_bias"]), "norm_mix_g": f(inp["norm_mix_g"][0][None]), "norm_ffn_g": f(inp["norm_ffn_g"][0][None]),
        "norm_final_g": f(inp["norm_final_g"][None]), "w_mod": f(inp["w_mod"][0]), "b_mod": f(inp["b_mod"][0][None]),
        "w_in": f(inp["w_in"][0]), "dw_w": f(inp["dw_w"][0]), "dw_b": f(inp["dw_b"][0][None]), "ln_g": f(inp["ln_conv_g"][0][None]),
        "ln_b": f(inp["ln_conv_b"][0][None]), "w_conv_out": f(inp["w_conv_out"][0]), "w_attn_out": f(inp["w_attn_out"][0]),
        "w_out": f(inp["w_out"][0]),
        "w_rt": f(np.concatenate([inp["w_router_group"][0], inp["w_router_expert"][0].reshape(D, 32)], axis=1)),
        "b_rt": f(np.concatenate([inp["b_router_group"][0], inp["b_router_expert"][0].reshape(32)])[None]),
        "w_eg": f(inp["w_exp_gate"][0]), "w_eu": f(inp["w_exp_up"][0]), "w_ed": f(inp["w_exp_down"][0]),
        "ohw": ohw, "vw": vw, "sel": sel, "bd": bd,
    }
    maps = []
    for cid in range(NCORE):
        b, half = cid // 2, cid % 2
        xp = np.zeros((NEXT, D), np.float32)
        xp[HALO:] = inp["x_prompt"][b, half * NOWN:(half + 1) * NOWN]
        if half == 1:
            xp[:HALO] = inp["x_prompt"][b, NOWN - HALO:NOWN]
        sl = slice(cid * NSQ, (cid + 1) * NSQ)
        m = dict(shared)
        m["xp"] = xp
        m["xs"] = f(inp["x_sample"][sl].reshape(NS, D))
        m["cmod"] = f(np.concatenate([inp["c_prompt"][b][None], inp["c_sample"][sl]], axis=0))
        m["hv"] = np.full((128, 1), float(half), np.float32)
        m["ck128"] = f(inp["cache_kv_w128"][0, sl].reshape(NSQ, 128, 512))
        m["ck512"] = f(inp["cache_kv_w512"][0, sl].reshape(NSQ, 512, 512))
        m["ck2048"] = f(inp["cache_kv_w2048"][0, sl].reshape(NSQ, 2048, 512))
        m["sconv"] = f(inp["state_conv"][0, sl])
        maps.append(m)
    return maps


_NC_CACHE = {}


def kernel(**inp):
    import time as _t
    t0 = _t.time()
    maps = _prep_inputs(inp)
    t1 = _t.time()
    if "nc" not in _NC_CACHE:
        _NC_CACHE["nc"] = build_two_pass()
    nc = _NC_CACHE["nc"]
    t2 = _t.time()
    if STOP_AFTER is not None:
        for m in maps:
            for k in ("w_eg", "w_eu", "w_ed"):
                m.pop(k, None)
    res = run_bass_kernel_spmd(nc, maps, core_ids=list(range(NCORE)))
    print("[kernel] prep %.1fs build %.1fs run %.1fs" % (t1 - t0, t2 - t1, _t.time() - t2), flush=True)
    R = res.results
    _NC_CACHE['last'] = R
    B = 4
    yp = np.zeros((B, 8192, D), np.float32); ys = np.zeros((128, 4, D), np.float32)
    kvp = [np.zeros((1, B, w, 2, 4, 64), np.float32) for (w, _) in GROUPS]
    convp = np.zeros((1, B, 30, 512), np.float32)
    kvs = [np.zeros((1, 128, w, 2, 4, 64), np.float32) for (w, _) in GROUPS]
    convs = np.zeros((1, 128, 30, 512), np.float32)
    for cid in range(NCORE):
        b, half = cid // 2, cid % 2
        r = R[cid]
        sl = slice(cid * NSQ, (cid + 1) * NSQ)
        if "yp" in r:
            yp[b, half * NOWN:(half + 1) * NOWN] = r["yp"]
            ys[sl] = r["ys"].reshape(NSQ, 4, D)
        for gi, (w, _) in enumerate(GROUPS):
            if half == 1:
                kvp[gi][0, b] = r["kvp%d" % w].reshape(w, 2, 4, 64)
            kvs[gi][0, sl] = r["kvs%d" % w].reshape(NSQ, w, 2, 4, 64)
        if half == 1:
            convp[0, b] = r["convp"]
        convs[0, sl] = r["convs"]
    return (yp, ys, kvp[0], kvp[1], kvp[2], convp, kvs[0], kvs[1], kvs[2], convs)
```

```python
import math
import numpy as np
from contextlib import ExitStack
import concourse.bass as bass
import concourse.mybir as mybir
from concourse.bass_utils import run_bass_kernel_spmd

F32 = mybir.dt.float32
BF16 = mybir.dt.bfloat16
I32 = mybir.dt.int32
U32 = mybir.dt.uint32
AF = mybir.ActivationFunctionType
ALU = mybir.AluOpType
AX = mybir.AxisListType

D = 1024
NCORE = 8
HALO = 2048
NOWN = 4096
NEXT = HALO + NOWN
NSQ = 16
NS = 64
NTOK = NEXT + NS
NOT = NOWN + NS
NTILE = 33
GROUPS = ((128, 1), (512, 4), (2048, 16))
EPS = 1e-6
NEXP = 32
BLK = 512
SUBB = BLK // 128
NBLK = (2 * NOT) // BLK + NEXP
CAP = NBLK * BLK
STOP_AFTER = None
DEBUG_SCR = False


class Ctx:
    KD = 8

    def __init__(self, nc, es, needed=None):
        self.nc = nc
        self.es = es
        self.needed = needed
        self.record = set()
        self.iidx = {e: 0 for e in ('pe', 'act', 'dve', 'pool')}
        self.eng = {'pe': nc.tensor, 'act': nc.scalar, 'dve': nc.vector, 'pool': nc.gpsimd, 'sp': nc.sync}
        self.csem = {e: es.enter_context(nc.semaphore('c_' + e)) for e in ('pe', 'act', 'dve', 'pool')}
        self.ccnt = {e: 0 for e in self.csem}
        self.dsem = {q: [es.enter_context(nc.semaphore('d_%s%d' % (q, i))) for i in range(self.KD)]
                     for q in ('sp', 'act', 'pool')}
        self.dcnt = {q: 0 for q in self.dsem}
        self.waited = {e: {} for e in self.eng}
        self.state = {}
        self.sbn = 0

    def sb(self, shape, dt, es=None):
        self.sbn += 1
        return (es or self.es).enter_context(self.nc.sbuf_tensor('sb%d' % self.sbn, list(shape), dt))

    def ps(self, shape, dt):
        self.sbn += 1
        return self.es.enter_context(self.nc.psum_tensor('ps%d' % self.sbn, list(shape), dt))

    def _wait(self, e, evs):
        best = {}
        for (sem, v, src) in evs:
            k = id(sem)
            if k not in best or best[k][1] < v:
                best[k] = (sem, v)
        for k, (sem, v) in best.items():
            if self.waited[e].get(k, 0) >= v:
                continue
            self.eng[e].wait_ge(sem, v)
            self.waited[e][k] = v
            if self.needed is None:
                for ce, cs in self.csem.items():
                    if cs is sem:
                        self.record.add((ce, v))

    def _deps(self, e, reads, writes):
        evs = []
        for k in reads:
            st = self.state.get(k)
            if st and st['w'] is not None:
                evs.append(st['w'])
        for k in writes:
            st = self.state.get(k)
            if st:
                if st['w'] is not None and (st['w'][2] != e or e != 'pe'):
                    evs.append(st['w'])
                for r in st['r']:
                    if r[2] != e or e != 'pe':
                        evs.append(r)
        return evs

    def _commit(self, ev, reads, writes):
        for k in reads:
            st = self.state.setdefault(k, {'w': None, 'r': []})
            st['r'] = [r for r in st['r'] if r[0] is not ev[0]] + [ev]
        for k in writes:
            self.state[k] = {'w': ev, 'r': []}

    @staticmethod
    def _psx(reads, writes):
        ps = [k for k in reads if k == 'pb7' or (isinstance(k, tuple) and k[0] == 'pb')]
        if not ps:
            return list(reads), list(writes)
        return [k for k in reads if k not in ps], list(writes) + [k for k in ps if k not in writes]

    def op(self, e, fn, reads=(), writes=()):
        reads, writes = self._psx(reads, writes)
        self._wait(e, self._deps(e, reads, writes))
        ins = fn()
        self.iidx[e] += 1
        if self.needed is None or (e, self.iidx[e]) in self.needed:
            self.ccnt[e] += 1
            ins.then_inc(self.csem[e], 1)
        ev = (self.csem[e], self.ccnt[e], e)
        self._commit(ev, reads, writes)
        return ev

    def dma(self, q, fn, reads=(), writes=()):
        j = self.dcnt[q]
        sem = self.dsem[q][j % self.KD]
        evs = self._deps(None, reads, writes)
        if j >= self.KD:
            evs.append((sem, 16 * (j // self.KD), 'dma_' + q))
        self._wait(q, evs)
        ins = fn()
        ins.then_inc(sem, 16)
        self.dcnt[q] += 1
        ev = (sem, 16 * (j // self.KD + 1), 'dma_' + q)
        self._commit(ev, reads, writes)
        return ev

    def wait_keys(self, e, keys):
        self._wait(e, self._deps(None, keys, ()))

    def barrier(self):
        evs = []
        for q in self.dsem:
            for i, sem in enumerate(self.dsem[q]):
                n = (self.dcnt[q] - i + self.KD - 1) // self.KD
                if n > 0:
                    evs.append((sem, 16 * n, 'x'))
        for e in self.csem:
            if self.ccnt[e]:
                evs.append((self.csem[e], self.ccnt[e], 'x'))
        for e in self.eng:
            self._wait(e, evs)

    def finish(self):
        evs = []
        for q in self.dsem:
            for i, sem in enumerate(self.dsem[q]):
                n = (self.dcnt[q] - i + self.KD - 1) // self.KD
                if n > 0:
                    evs.append((sem, 16 * n, 'x'))
        for e in self.csem:
            if self.ccnt[e]:
                evs.append((self.csem[e], self.ccnt[e], 'x'))
        self._wait('sp', evs)


def _t5_bucket_np(dist):
    dist = np.asarray(dist, np.int64)
    max_exact = 16
    d_f = np.maximum(dist, 1).astype(np.float32)
    large = max_exact + (np.log(d_f / np.float32(max_exact)) / np.float32(math.log(2048 / max_exact))
                         * np.float32(32 - max_exact)).astype(np.int32)
    large = np.minimum(large, 31)
    return np.where(dist < max_exact, dist, large)


def _structure_constants():
    ohw = np.zeros((32, 3 * 510), np.float32)
    vw = np.zeros((4, 3 * 510), np.float32)
    for g, (win, dil) in enumerate(GROUPS):
        for blk in range(2):
            for u in range(255):
                rel = u + 1 if blk == 0 else u - 127
                ok = (rel <= 128) if blk == 0 else (rel >= 0)
                if ok:
                    b = int(_t5_bucket_np(rel * dil))
                    ohw[b, g * 510 + blk * 255 + u] = 1.0
                    vw[:, g * 510 + blk * 255 + u] = 1.0
    sel = np.zeros((17, 192), np.float32)
    sel[0, 0:128] = 1.0
    for t in range(64):
        sel[1 + t // 4, 128 + t] = 1.0
    bd = np.zeros((64, 128), np.float32)
    for k in range(64):
        for q in range(64):
            if k // 4 == q // 4:
                bd[k, q] = 1.0
        bd[k, 64 + k] = 1.0
    return ohw, vw, sel, bd


def _os_env(k):
    import os
    return os.environ.get(k)


def build_nc(needed=None):
    nc = bass.Bass("TRN2", target_bir_lowering=False)

    def din(name, shape, dt=F32):
        return nc.dram_tensor(name, list(shape), dt, kind="ExternalInput")

    def dout(name, shape, dt=F32):
        return nc.dram_tensor(name, list(shape), dt, kind="ExternalOutput")

    def dscr(name, shape, dt=F32):
        return nc.dram_tensor(name, list(shape), dt, kind="ExternalOutput" if DEBUG_SCR else "Internal")

    xp_t = din("xp", [NEXT, D]); xs_t = din("xs", [NS, D]); cmod_t = din("cmod", [17, D]); hv_t = din("hv", [128, 1])
    ck_t = [din("ck%d" % w, [NSQ, w, 512]) for (w, _) in GROUPS]
    sconv_t = din("sconv", [NSQ, 30, 512])
    relb_t = din("rel_bias", [32, 12])
    gmix_t = din("norm_mix_g", [1, D]); gffn_t = din("norm_ffn_g", [1, D]); gfin_t = din("norm_final_g", [1, D])
    wmod_t = din("w_mod", [D, 6 * D]); bmod_t = din("b_mod", [1, 6 * D])
    win_t = din("w_in", [D, 5376])
    dww_t = din("dw_w", [31, 512]); dwb_t = din("dw_b", [1, 512]); lng_t = din("ln_g", [1, 512]); lnb_t = din("ln_b", [1, 512])
    wco_t = din("w_conv_out", [512, D]); wao_t = din("w_attn_out", [256, D]); wo_t = din("w_out", [D, D])
    wrt_t = din("w_rt", [D, 36]); brt_t = din("b_rt", [1, 36])
    if STOP_AFTER is None:
        weg_t = din("w_eg", [NEXP, D, 512]); weu_t = din("w_eu", [NEXP, D, 512]); wed_t = din("w_ed", [NEXP, 512, D])
    ohw_t = din("ohw", [32, 1530]); vw_t = din("vw", [4, 1530]); sel_t = din("sel", [17, 192]); bd_t = din("bd", [64, 128])

    yp_t = dout("yp", [NOWN, D]); ys_t = dout("ys", [NS, D])
    kvp_t = [dout("kvp%d" % w, [w, 512]) for (w, _) in GROUPS]
    convp_t = dout("convp", [30, 512])
    kvs_t = [dout("kvs%d" % w, [NSQ, w, 512]) for (w, _) in GROUPS]
    convs_t = dout("convs", [NSQ, 30, 512])

    modrows_t = dscr("modrows", [4, 192, D])
    wd_t = dscr("wdscr", [3, 4, 510])
    ebd_t = dscr("ebd", [3, 128, 1024])
    sg_t = dscr("sgscr", [16, 128, NOT], BF16)
    yts_t = dscr("ytscr", [4, 128, NOT], BF16)
    acc_t = dscr("accscr", [3, NOWN, 260])
    accs_t = dscr("accsscr", [NS, 260])
    x1_t = dscr("x1scr", [NOT, D])
    xsd_t = dscr("xsdisp", [CAP, D], BF16)
    ysd_t = dscr("ysdisp", [CAP, D])

    xp = xp_t.ap(); xs = xs_t.ap(); win = win_t.ap()

    with ExitStack() as es:
        c = Ctx(nc, es, needed)
        nc._mk_ctx = c
        PB = [c.ps([128, 512], F32) for _ in range(8)]

        def pbf(i):
            return PB[i][:].bitcast(BF16)

        identf = c.sb([128, 128], F32); ident = c.sb([128, 128], BF16)
        onesb = c.sb([128, 128], BF16); onesf = c.sb([128, 128], F32)
        suf = c.sb([128, 128], F32); jf = c.sb([128, 128], F32)
        epsb = c.sb([128, 1], F32); hv = c.sb([128, 1], F32); one1 = c.sb([128, 1], F32)
        c.op('pool', lambda: nc.gpsimd.memset(onesf[:], 1.0), writes=['onesf'])
        c.op('pool', lambda: nc.gpsimd.memset(onesb[:], 1.0), writes=['onesb'])
        c.op('pool', lambda: nc.gpsimd.memset(epsb[:], EPS), writes=['epsb'])
        c.op('pool', lambda: nc.gpsimd.memset(one1[:], 1.0), writes=['one1'])
        c.op('pool', lambda: nc.gpsimd.affine_select(out=identf[:], in_=onesf[:], pattern=[[-1, 128]], compare_op=ALU.is_equal,
                                                       fill=0.0, base=0, channel_multiplier=1), reads=['onesf'], writes=['identf'])
        c.op('pool', lambda: nc.gpsimd.affine_select(out=jf[:], in_=onesf[:], pattern=[[1, 128]], compare_op=ALU.is_equal,
                                                       fill=0.0, base=-127, channel_multiplier=1), reads=['onesf'], writes=['jf'])
        c.op('pool', lambda: nc.gpsimd.affine_select(out=suf[:], in_=onesf[:], pattern=[[1, 128]], compare_op=ALU.is_gt,
                                                       fill=0.0, base=0, channel_multiplier=-1), reads=['onesf'], writes=['suf'])
        c.op('dve', lambda: nc.vector.tensor_copy(out=ident[:], in_=identf[:]), reads=['identf'], writes=['ident'])
        c.dma('sp', lambda: nc.sync.dma_start(out=hv[:], in_=hv_t.ap()), writes=['hv'])


        es_hT = ExitStack()
        es.enter_context(es_hT)
        hT = c.sb([128, 8, NTOK], BF16, es_hT)
        for g, (wing, d) in enumerate(GROUPS):
            nsp = 4 if g == 2 else 1
            for q4 in range(nsp):
                bs = slice(q4 * (NSQ // nsp), (q4 + 1) * (NSQ // nsp))
                A_ = (1, 4, 28)[g]
                c.dma('act', lambda g=g, wing=wing, bs=bs, A_=A_: nc.scalar.dma_start(out=kvs_t[g].ap()[bs, 0:wing - 4, :].rearrange("b (a r) c -> b a (r c)", a=A_),
                      in_=ck_t[g].ap()[bs, 4:wing, :].rearrange("b (a r) c -> b a (r c)", a=A_)), writes=[('kvs_shift', g, q4)])
        es_mod = ExitStack()
        a1p = c.sb([128, D], F32, es_mod); b1p = c.sb([128, D], F32, es_mod); a1s = c.sb([64, D], F32, es_mod); b1s = c.sb([64, D], F32, es_mod)
        es0 = ExitStack()
        with es0:
            cm = c.sb([17, D], F32, es0); scm = c.sb([17, D], F32, es0); scT = c.sb([128, 8, 17], F32, es0)
            mtok = c.sb([17, 6 * D], F32, es0); selm = c.sb([17, 192], F32, es0)
            gmb = c.sb([128, D], F32, es0); gfb = c.sb([128, D], F32, es0)
            c.dma('sp', lambda: nc.sync.dma_start(out=cm[:], in_=cmod_t.ap()), writes=['cm'])
            c.dma('sp', lambda: nc.sync.dma_start(out=selm[:], in_=sel_t.ap()), writes=['selm'])
            c.dma('sp', lambda: nc.sync.dma_start(out=gmb[:], in_=gmix_t.ap().partition_broadcast(128)), writes=['gmb'])
            c.dma('sp', lambda: nc.sync.dma_start(out=gfb[:], in_=gffn_t.ap().partition_broadcast(128)), writes=['gfb'])
            c.op('act', lambda: nc.scalar.activation(out=scm[:], in_=cm[:], func=AF.Silu), reads=['cm'], writes=['scm'])
            for k in range(8):
                c.op('pe', lambda k=k: nc.tensor.transpose(out=PB[0][:, k * 17:(k + 1) * 17], in_=scm[0:17, k * 128:(k + 1) * 128],
                                                           identity=identf[0:17, 0:17]), reads=['scm', 'identf'], writes=['pb0'])
            c.op('dve', lambda: nc.vector.tensor_copy(out=scT[:].rearrange("p k s -> p (k s)"), in_=PB[0][:, 0:136]), reads=['pb0'], writes=['scT'])
            wmod = wmod_t.ap().rearrange("(k p) n -> p k n", p=128)
            wbs = [c.sb([128, 8, 256], F32, es0) for _ in range(2)]
            bbs = [c.sb([17, 256], F32, es0) for _ in range(2)]
            for nb in range(24):
                wb = wbs[nb % 2]
                bb = bbs[nb % 2]
                c.dma('sp', lambda wb=wb, nb=nb: nc.sync.dma_start(out=wb[:], in_=wmod[:, :, nb * 256:(nb + 1) * 256]), writes=[('wb', nb % 2)])
                c.dma('sp', lambda bb=bb, nb=nb: nc.sync.dma_start(out=bb[:], in_=bmod_t.ap()[:, nb * 256:(nb + 1) * 256].partition_broadcast(17)),
                      writes=[('bb', nb % 2)])
                pbk = 1 + nb % 2
                for k in range(8):
                    c.op('pe', lambda wb=wb, k=k, pbk=pbk: nc.tensor.matmul(PB[pbk][0:17, 0:256], lhsT=scT[:, k, :], rhs=wb[:, k, :], start=(k == 0), stop=(k == 7)),
                         reads=['scT', ('wb', nb % 2)], writes=[('pb', pbk)])
                c.op('dve', lambda bb=bb, nb=nb, pbk=pbk: nc.vector.tensor_tensor(out=mtok[:, nb * 256:(nb + 1) * 256], in0=PB[pbk][0:17, 0:256], in1=bb[:], op=ALU.add),
                     reads=[('pb', pbk), ('bb', nb % 2)], writes=['mtok'])
            rowst = [c.sb([128, D], F32, es0) for _ in range(2)]
            for kind in range(6):
                for part, (c0, n) in enumerate(((0, 128), (128, 64))):
                    rt = rowst[(kind * 2 + part) % 2]
                    rk = ('rowst', (kind * 2 + part) % 2)
                    for hf in range(2):
                        c.op('pe', lambda hf=hf, c0=c0, n=n, kind=kind: nc.tensor.matmul(PB[3 + hf][0:n, :], lhsT=selm[0:17, c0:c0 + n],
                             rhs=mtok[0:17, kind * D + hf * 512: kind * D + hf * 512 + 512], start=True, stop=True),
                             reads=['selm', 'mtok'], writes=[('pb', 3 + hf)])
                    dst = None; dk = 'nokey'
                    if kind == 0:
                        dst = (b1p, b1s)[part]; dk = ('b1', part)
                    elif kind == 1:
                        dst = (a1p, a1s)[part]; dk = ('a1', part)
                    for hf in range(2):
                        sl = slice(hf * 512, hf * 512 + 512)
                        if kind in (1, 4):
                            gb = gmb if kind == 1 else gfb
                            tgt = dst if dst is not None else rt
                            c.op('dve', lambda hf=hf, n=n, gb=gb, tgt=tgt, sl=sl: nc.vector.scalar_tensor_tensor(out=tgt[0:n, sl], in0=PB[3 + hf][0:n, :], scalar=1.0,
                                 in1=gb[0:n, sl], op0=ALU.add, op1=ALU.mult), reads=[('pb', 3 + hf), 'gmb', 'gfb'], writes=[rk, dk])
                        else:
                            tgt = dst if dst is not None else rt
                            c.op('act', lambda hf=hf, n=n, tgt=tgt, sl=sl: nc.scalar.copy(out=tgt[0:n, sl], in_=PB[3 + hf][0:n, :]),
                                 reads=[('pb', 3 + hf)], writes=[rk, dk])
                    if kind >= 2:
                        c.dma('sp', lambda rt=rt, c0=c0, n=n, kind=kind: nc.sync.dma_start(out=modrows_t.ap()[kind - 2, c0:c0 + n, :], in_=rt[0:n, :]),
                              reads=[rk], writes=[('modrows', kind - 2, part)])
        c.barrier()
        es0 = ExitStack()
        with es0:
            rb = c.sb([32, 12], F32, es0); ohw = c.sb([32, 1530], F32, es0); vw = c.sb([4, 1530], F32, es0)
            wsb = c.sb([4, 1530], F32, es0); hall = c.sb([128, 24, 128], F32, es0); ebst = c.sb([128, 3, 1024], F32, es0)
            c.dma('sp', lambda: nc.sync.dma_start(out=rb[:], in_=relb_t.ap()), writes=['rb'])
            c.dma('sp', lambda: nc.sync.dma_start(out=ohw[:], in_=ohw_t.ap()), writes=['ohw'])
            c.dma('sp', lambda: nc.sync.dma_start(out=vw[:], in_=vw_t.ap()), writes=['vw'])
            for g in range(3):
                c.op('pe', lambda g=g: nc.tensor.matmul(PB[5][0:4, 0:510], lhsT=rb[:, 4 * g:4 * g + 4], rhs=ohw[:, g * 510:(g + 1) * 510], start=True, stop=True),
                     reads=['rb', 'ohw'], writes=[('pb', 5)])
                c.op('act', lambda g=g: nc.scalar.activation(out=wsb[:, g * 510:(g + 1) * 510], in_=PB[5][0:4, 0:510], func=AF.Exp), reads=[('pb', 5)], writes=['wsb'])
            c.op('dve', lambda: nc.vector.tensor_tensor(out=wsb[:], in0=wsb[:], in1=vw[:], op=ALU.mult), reads=['wsb', 'vw'], writes=['wsb'])
            c.dma('sp', lambda: nc.sync.dma_start(out=wd_t.ap().rearrange("g h u -> h g u"), in_=wsb[:].rearrange("h (g u) -> h g u", g=3)), reads=['wsb'], writes=['wd'])
            for g in range(3):
                for h in range(4):
                    for blk in range(2):
                        idx = (g * 4 + h) * 2 + blk
                        src = bass.AP(wd_t, (g * 4 + h) * 510 + blk * 255, [[1, 128], [1, 128]])
                        c.dma('sp', lambda idx=idx, src=src: nc.sync.dma_start(out=hall[:, idx, :], in_=src), reads=['wd'], writes=[('hall', idx)])
            for g in range(3):
                for hf in range(2):
                    c.op('pe', lambda g=g, hf=hf: nc.tensor.matmul(PB[6 + hf][:, :], lhsT=jf[:], rhs=hall[:, g * 8 + hf * 4: g * 8 + hf * 4 + 4, :].rearrange("p a q -> p (a q)"),
                         start=True, stop=True), reads=['jf'] + [('hall', g * 8 + hf * 4 + i) for i in range(4)], writes=[('pb', 6 + hf)])
                    c.op('act', lambda g=g, hf=hf: nc.scalar.copy(out=ebst[:, g, hf * 512:(hf + 1) * 512], in_=PB[6 + hf][:, :]), reads=[('pb', 6 + hf)], writes=[('ebst', g)])
                c.dma('sp', lambda g=g: nc.sync.dma_start(out=ebd_t.ap()[g], in_=ebst[:, g, :]), reads=[('ebst', g)], writes=[('ebd', g)])

        c.barrier()
        def norm_to_T(tidx, src_ap, n, arow, brow, akey, bkey, bufs):
            xt, t1, hb, stt, junk = bufs
            s = tidx % 2
            c.dma('sp', lambda: nc.sync.dma_start(out=xt[s][0:n, :], in_=src_ap), writes=[('xt', s)])
            c.op('act', lambda: nc.scalar.activation(out=junk[0:n, :], in_=xt[s][0:n, :], func=AF.Square, accum_out=stt[s][0:n, 0:1]),
                 reads=[('xt', s)], writes=[('stt', s), 'junk'])
            c.op('act', lambda: nc.scalar.activation(out=stt[s][0:n, 1:2], in_=stt[s][0:n, 0:1], func=AF.Sqrt, scale=1.0 / D, bias=epsb[0:n, :]),
                 reads=[('stt', s), 'epsb'], writes=[('stt', s)])
            c.op('dve', lambda: nc.vector.reciprocal(out=stt[s][0:n, 2:3], in_=stt[s][0:n, 1:2]), reads=[('stt', s)], writes=[('stt', s)])
            c.op('dve', lambda: nc.vector.scalar_tensor_tensor(out=t1[s][0:n, :], in0=xt[s][0:n, :], scalar=stt[s][0:n, 2:3], in1=arow[0:n, :],
                                                                op0=ALU.mult, op1=ALU.mult), reads=[('xt', s), ('stt', s), akey], writes=[('t1', s)])
            c.op('pool', lambda: nc.gpsimd.tensor_tensor(out=hb[s][0:n, :], in0=t1[s][0:n, :], in1=brow[0:n, :], op=ALU.add),
                 reads=[('t1', s), bkey], writes=[('hb', s)])
            pv = pbf(s).rearrange("p (k t) -> p k t", k=8)
            for k in range(8):
                c.op('pe', lambda k=k: nc.tensor.transpose(out=pv[:, k, 0:n], in_=hb[s][0:n, k * 128:(k + 1) * 128], identity=ident[0:n, 0:n]),
                     reads=[('hb', s), 'ident'], writes=[('pb', s)])
            return s, pv

        esA = ExitStack()
        with esA:
            xt = [c.sb([128, D], F32, esA) for _ in range(2)]; t1 = [c.sb([128, D], F32, esA) for _ in range(2)]
            hb = [c.sb([128, D], BF16, esA) for _ in range(2)]; stt = [c.sb([128, 4], F32, esA) for _ in range(2)]
            junk = c.sb([128, D], BF16, esA)
            bufsA = (xt, t1, hb, stt, junk)
            for t in range(49):
                if t < 48:
                    n = 128; src = xp[t * 128:(t + 1) * 128, :]; ar, br = a1p, b1p; col = t * 128; pk = 0
                else:
                    n = 64; src = xs; ar, br = a1s, b1s; col = NEXT; pk = 1
                s, pv = norm_to_T(t, src, n, ar, br, ('a1', pk), ('b1', pk), bufsA)
                c.op('act', lambda pv=pv, col=col, n=n: nc.scalar.copy(out=hT[:, :, col:col + n], in_=pv[:, :, 0:n]), reads=[('pb', s)], writes=['hT'])
        c.barrier()
        es_mod.close()

        winr = win.rearrange("(k p) n -> p k n", p=128)

        def load_w(dst, c0, ncol, key):
            c.dma('pool', lambda: nc.gpsimd.dma_start(out=dst, in_=winr[:, :, c0:c0 + ncol]), writes=[key])

        def proj_T(ps_ap, pkey, wt, wkey, hcols):
            for k in range(8):
                c.op('pe', lambda k=k: nc.tensor.matmul(ps_ap, lhsT=wt[:, k, :], rhs=hT[:, k, hcols], start=(k == 0), stop=(k == 7)),
                     reads=['hT', wkey], writes=[pkey])

        def psv(i, sl=slice(None), n=512):
            return PB[i][sl, 0:n]

        OT = [(HALO + 512 * m, 512 * m, 512) for m in range(8)] + [(NEXT, NOWN, NS)]

        esG = ExitStack()
        with esG:
            wj = [c.sb([128, 8, 128], BF16, esG) for _ in range(2)]
            sgrow = [c.sb([128, NOT], BF16, esG) for _ in range(2)]
            for j in range(16):
                s = j % 2
                load_w(wj[s][:], 3328 + j * 128, 128, ('wj', s))
                for m, (hc, oi, n) in enumerate(OT):
                    pi = m % 2
                    pa = psv(pi, n=n)
                    proj_T(pa, ('pb', pi), wj[s], ('wj', s), slice(hc, hc + n))
                    c.op('act', lambda pa=pa, oi=oi, n=n, s=s: nc.scalar.activation(out=sgrow[s][:, oi:oi + n], in_=pa, func=AF.Sigmoid),
                         reads=[('pb', pi)], writes=[('sgrow', s)])
                c.dma('sp', lambda j=j, s=s: nc.sync.dma_start(out=sg_t.ap()[j], in_=sgrow[s][:]), reads=[('sgrow', s)], writes=[('sg', j)])

        c.barrier()
        if STOP_AFTER == 'A':
            c.finish()
            return nc

        esC = ExitStack()
        with esC:
            yT = c.sb([128, 4, NOT], BF16, esC)
            dwT = c.sb([128, 4, 31], F32, esC); dwb = c.sb([128, 4], F32, esC); lng = c.sb([128, 4], F32, esC); lnb = c.sb([128, 4], F32, esC)
            with nc.allow_non_contiguous_dma(reason="tiny per-channel parameter loads"):
                for cc in range(4):
                    c.dma('sp', lambda cc=cc: nc.sync.dma_start(out=dwT[:, cc, :], in_=dww_t.ap()[:, cc * 128:(cc + 1) * 128].rearrange("j p -> p j")), writes=[('dwT', cc)])
                c.dma('sp', lambda: nc.sync.dma_start(out=dwb[:], in_=dwb_t.ap().rearrange("o (c p) -> p (o c)", p=128)), writes=['dwb'])
                c.dma('sp', lambda: nc.sync.dma_start(out=lng[:], in_=lng_t.ap().rearrange("o (c p) -> p (o c)", p=128)), writes=['lng'])
                c.dma('sp', lambda: nc.sync.dma_start(out=lnb[:], in_=lnb_t.ap().rearrange("o (c p) -> p (o c)", p=128)), writes=['lnb'])
            uxT = c.sb([128, 4, NSQ, 34], BF16, esC)
            uTf = c.sb([128, 4, 94], F32, esC)
            sct = [c.sb([120, 512], F32, esC)] * 2
            for i4 in range(4):
                s = i4 % 2
                c.dma('sp', lambda i4=i4, s=s: nc.sync.dma_start(out=sct[s][:], in_=sconv_t.ap()[4 * i4:4 * i4 + 4].rearrange("b t c -> (b t) c")), writes=[('sct', 0)])
                for cc in range(4):
                    c.op('pe', lambda cc=cc, s=s: nc.tensor.transpose(out=PB[2][:, 0:120], in_=sct[s][:, cc * 128:(cc + 1) * 128], identity=identf[0:120, 0:120]),
                         reads=[('sct', 0), 'identf'], writes=[('pb', 2)])
                    c.op('act', lambda cc=cc, i4=i4: nc.scalar.copy(out=uxT[:, cc, 4 * i4:4 * i4 + 4, 0:30], in_=PB[2][:, 0:120].rearrange("p (b t) -> p b t", b=4)),
                         reads=[('pb', 2)], writes=['uxT'])
            c.dma('sp', lambda: nc.sync.dma_start(out=convs_t.ap()[:, 0:26, :], in_=sconv_t.ap()[:, 4:30, :]), writes=['convs_a'])
            esC1 = ExitStack()
            wul = [c.sb([128, 8, 128], BF16, esC1) for _ in range(2)]; wug = [c.sb([128, 8, 128], BF16, esC1) for _ in range(2)]
            diag = [c.sb([128, 31, 128], BF16, esC1)] * 2
            ucT = [c.sb([128, 30 + NOWN], BF16, esC1)] * 2
            sgt = [c.sb([128, 512], F32, esC1) for _ in range(2)]
            UT = [(HALO - 30, -30, 30)] + OT
            for cc in range(4):
                s = cc % 2
                load_w(wul[s][:], 2304 + cc * 128, 128, ('wul', s))
                load_w(wug[s][:], 2816 + cc * 128, 128, ('wug', s))
                for j in range(31):
                    eng = 'dve' if j % 2 == 0 else 'pool'
                    e_ = nc.vector if eng == 'dve' else nc.gpsimd
                    c.op(eng, lambda j=j, e_=e_: e_.tensor_scalar(out=diag[s][:, j, :], in0=identf[:], scalar1=dwT[:, cc, j:j + 1], scalar2=None, op0=ALU.mult),
                         reads=['identf', ('dwT', cc)], writes=[('diag', 0, j)])
                for m, (hc, oi, n) in enumerate(UT):
                    pl = psv(0, n=n); pg = psv(1, n=n)
                    proj_T(pl, ('pb', 0), wul[s], ('wul', s), slice(hc, hc + n))
                    proj_T(pg, ('pb', 1), wug[s], ('wug', s), slice(hc, hc + n))
                    b_ = m % 2
                    c.op('act', lambda pg=pg, n=n, b_=b_: nc.scalar.activation(out=sgt[b_][:, 0:n], in_=pg, func=AF.Sigmoid), reads=[('pb', 1)], writes=[('sgt', b_)])
                    if oi < 0:
                        c.op('dve', lambda pl=pl, n=n, b_=b_: nc.vector.tensor_tensor(out=sgt[b_][:, 0:n], in0=pl, in1=sgt[b_][:, 0:n], op=ALU.mult),
                             reads=[('pb', 0), ('sgt', b_)], writes=[('sgt', b_)])
                        c.op('dve', lambda n=n, b_=b_: nc.vector.tensor_scalar(out=ucT[s][:, 0:30], in0=sgt[b_][:, 0:n], scalar1=hv[:, 0:1], scalar2=None, op0=ALU.mult),
                             reads=[('sgt', b_), 'hv'], writes=[('ucT', 0, 0)])
                    elif oi < NOWN:
                        c.op('dve', lambda pl=pl, n=n, b_=b_, oi=oi: nc.vector.tensor_tensor(out=ucT[s][:, 30 + oi:30 + oi + n], in0=pl, in1=sgt[b_][:, 0:n], op=ALU.mult),
                             reads=[('pb', 0), ('sgt', b_)], writes=[('ucT', 0, 1 + oi // 512)])
                        if oi == NOWN - 512:
                            c.op('dve', lambda pl=pl, b_=b_: nc.vector.tensor_tensor(out=uTf[:, cc, 0:30], in0=pl[:, 482:512], in1=sgt[b_][:, 482:512], op=ALU.mult),
                                 reads=[('pb', 0), ('sgt', b_)], writes=[('uTf', cc)])
                    else:
                        c.op('dve', lambda pl=pl, b_=b_: nc.vector.tensor_tensor(out=uTf[:, cc, 30:94], in0=pl, in1=sgt[b_][:, 0:64], op=ALU.mult),
                             reads=[('pb', 0), ('sgt', b_)], writes=[('uTf', cc)])
                        c.op('dve', lambda: nc.vector.tensor_copy(out=uxT[:, cc, :, 30:34], in_=uTf[:, cc, 30:94].rearrange("p (b t) -> p b t", t=4)),
                             reads=[('uTf', cc)], writes=['uxT'])
                dkeys = [('diag', 0, j) for j in range(31)]
                for m in range(8):
                    py = psv(2 + m % 2)
                    for j in range(31):
                        c.op('pe', lambda j=j, m=m, py=py: nc.tensor.matmul(py, lhsT=diag[s][:, j, :], rhs=ucT[s][:, 512 * m + j: 512 * m + j + 512], start=(j == 0), stop=(j == 30)),
                             reads=[dkeys[j], ('ucT', 0, 0), ('ucT', 0, 1 + m), ('ucT', 0, m)], writes=[('pb', 2 + m % 2)])
                    c.op('act', lambda m=m, py=py: nc.scalar.activation(out=yT[:, cc, 512 * m:512 * m + 512], in_=py, func=AF.Identity, bias=dwb[:, cc:cc + 1], scale=1.0),
                         reads=[('pb', 2 + m % 2), 'dwb'], writes=[('yT', m)])
                pys = PB[2][:, 0:64]
                for j in range(31):
                    c.op('pe', lambda j=j: nc.tensor.matmul(pys.rearrange("p (b t) -> p b t", t=4), lhsT=diag[s][:, j, :], rhs=uxT[:, cc, :, j:j + 4], start=(j == 0), stop=(j == 30)),
                         reads=[dkeys[j], 'uxT'], writes=[('pb', 2)])
                c.op('act', lambda: nc.scalar.activation(out=yT[:, cc, NOWN:NOT], in_=pys, func=AF.Identity, bias=dwb[:, cc:cc + 1], scale=1.0),
                     reads=[('pb', 2), 'dwb'], writes=[('yT', 8)])
            c.barrier()
            esC1.close()
            cpo = c.sb([94, 512], F32, esC)
            for cc in range(4):
                c.op('pe', lambda cc=cc: nc.tensor.transpose(out=PB[0][0:94, 0:128], in_=uTf[:, cc, :], identity=identf[:]), reads=[('uTf', cc), 'identf'], writes=[('pb', 0)])
                c.op('act', lambda cc=cc: nc.scalar.copy(out=cpo[:, cc * 128:(cc + 1) * 128], in_=PB[0][0:94, 0:128]), reads=[('pb', 0)], writes=['cpo'])
            c.dma('sp', lambda: nc.sync.dma_start(out=convp_t.ap(), in_=cpo[0:30, :]), reads=['cpo'], writes=['convp'])
            for b in range(NSQ):
                c.dma('sp', lambda b=b: nc.sync.dma_start(out=convs_t.ap()[b, 26:30, :], in_=cpo[30 + 4 * b:34 + 4 * b, :]), reads=['cpo'], writes=[('convs_b', b)])
            sq = c.sb([128, 4, 512], BF16, esC); mean = c.sb([128, 512], F32, esC); msq = c.sb([128, 512], F32, esC)
            var = c.sb([128, 512], F32, esC); rstd = c.sb([128, 512], F32, esC); tt = c.sb([128, 4, 512], F32, esC)
            for m, (hc, oi, n) in enumerate(OT):
                yk = ('yT', m)
                c.op('act', lambda oi=oi, n=n: nc.scalar.activation(out=sq[:, :, 0:n], in_=yT[:, :, oi:oi + n], func=AF.Square), reads=[yk], writes=['sq'])
                p1 = psv(4, n=n); p2 = psv(5, n=n)
                for cc in range(4):
                    c.op('pe', lambda cc=cc, p1=p1, oi=oi, n=n: nc.tensor.matmul(p1, lhsT=onesb[:], rhs=yT[:, cc, oi:oi + n], start=(cc == 0), stop=(cc == 3)),
                         reads=[yk, 'onesb'], writes=[('pb', 4)])
                for cc in range(4):
                    c.op('pe', lambda cc=cc, p2=p2, n=n: nc.tensor.matmul(p2, lhsT=onesb[:], rhs=sq[:, cc, 0:n], start=(cc == 0), stop=(cc == 3)),
                         reads=['sq', 'onesb'], writes=[('pb', 5)])
                c.op('dve', lambda p1=p1, n=n: nc.vector.tensor_scalar(out=mean[:, 0:n], in0=p1, scalar1=1.0 / 512, scalar2=None, op0=ALU.mult), reads=[('pb', 4)], writes=['mean'])
                c.op('pool', lambda n=n: nc.gpsimd.tensor_tensor(out=msq[:, 0:n], in0=mean[:, 0:n], in1=mean[:, 0:n], op=ALU.mult), reads=['mean'], writes=['msq'])
                c.op('dve', lambda p2=p2, n=n: nc.vector.scalar_tensor_tensor(out=var[:, 0:n], in0=p2, scalar=1.0 / 512, in1=msq[:, 0:n], op0=ALU.mult, op1=ALU.subtract),
                     reads=[('pb', 5), 'msq'], writes=['var'])
                c.op('act', lambda n=n: nc.scalar.activation(out=var[:, 0:n], in_=var[:, 0:n], func=AF.Sqrt, scale=1.0, bias=epsb[:, :]), reads=['var', 'epsb'], writes=['var'])
                c.op('dve', lambda n=n: nc.vector.reciprocal(out=rstd[:, 0:n], in_=var[:, 0:n]), reads=['var'], writes=['rstd'])
                c.op('dve', lambda oi=oi, n=n: nc.vector.tensor_tensor(out=tt[:, :, 0:n], in0=yT[:, :, oi:oi + n], in1=mean[:, 0:n].unsqueeze(1).to_broadcast([128, 4, n]), op=ALU.subtract),
                     reads=[yk, 'mean'], writes=['tt'])
                c.op('pool', lambda n=n: nc.gpsimd.tensor_tensor(out=tt[:, :, 0:n], in0=tt[:, :, 0:n], in1=rstd[:, 0:n].unsqueeze(1).to_broadcast([128, 4, n]), op=ALU.mult),
                     reads=['tt', 'rstd'], writes=['tt'])
                for cc in range(4):
                    c.op('act', lambda cc=cc, oi=oi, n=n: nc.scalar.activation(out=yT[:, cc, oi:oi + n], in_=tt[:, cc, 0:n], func=AF.Silu, bias=lnb[:, cc:cc + 1], scale=lng[:, cc:cc + 1]),
                         reads=['tt', 'lng', 'lnb'], writes=[yk])
            for cc in range(4):
                c.dma('sp', lambda cc=cc: nc.sync.dma_start(out=yts_t.ap()[cc], in_=yT[:, cc, :]), reads=[('yT', m) for m in range(9)], writes=[('yts', cc)])

        c.barrier()
        if STOP_AFTER == 'C':
            c.finish()
            return nc

        esD = ExitStack()
        with esD:
            ebf = c.sb([128, 1024], F32, esD)
            smk = c.sb([128, 9, 4, 4], F32, esD)
            nmk = c.sb([64, 3, 4, 64], F32, esD)
            bdm = c.sb([64, 128], F32, esD)
            pz = [c.sb([128, 4, 64], BF16, esD) for _ in range(NSQ)]
            v1n = c.sb([64, 3, 4, 65], BF16, esD)
            wkv = c.sb([128, 8, 512], BF16, esD)
            wq = c.sb([128, 8, 128], BF16, esD); wk = c.sb([128, 8, 128], BF16, esD)
            qT = c.sb([128, NOT], BF16, esD); kT = c.sb([128, NTOK], BF16, esD)
            qs = c.sb([128, 2, 64], BF16, esD); ks = c.sb([128, 2, 64], BF16, esD)
            v1 = c.sb([128, 48, 4, 65], BF16, esD)
            kvst = [c.sb([128, 512], F32, esD) for _ in range(2)]
            ef = [c.sb([128, 512], F32, esD) for _ in range(2)]
            pt = [c.sb([128, 512], BF16, esD) for _ in range(2)]
            oev = [c.sb([128, 130], F32, esD) for _ in range(2)]
            ctile = [c.sb([128, 512], F32, esD) for _ in range(2)]
            kTs = [c.sb([128, 2, 128], BF16, esD) for _ in range(2)]
            v1s = [c.sb([128, 4, 65], BF16, esD) for _ in range(2)]
            ess2 = [c.sb([128, 16], F32, esD) for _ in range(2)]
            en = c.sb([64, 256], F32, esD); pn = c.sb([64, 4, 64], BF16, esD)
            osv = c.sb([64, 260], F32, esD)
            c.dma('sp', lambda: nc.sync.dma_start(out=bdm[:], in_=bd_t.ap()), writes=['bdm'])
            c.op('pool', lambda: nc.gpsimd.memset(smk[:], 0.0), writes=['smk'])
            for b in range(NSQ):
                c.op('pool', lambda b=b: nc.gpsimd.memset(pz[b][:], 0.0), writes=[('pz', b)])
            for s in range(2):
                c.op('pool', lambda s=s: nc.gpsimd.memset(v1s[s][:], 1.0), writes=[('v1s', s)])
            c.op('pool', lambda: nc.gpsimd.memset(v1n[:], 1.0), writes=['v1n'])
            n_os = 1
            zl = c.sb([128, 64], BF16, esD); zr = c.sb([128, 260], BF16, esD)
            c.op('pool', lambda: nc.gpsimd.memset(zl[:], 0.0), writes=['zl'])
            c.op('pool', lambda: nc.gpsimd.memset(zr[:], 0.0), writes=['zr'])
            c.op('pe', lambda: nc.tensor.matmul(PB[7][0:64, 0:260], lhsT=zl[:], rhs=zr[:], start=True, stop=False), reads=['zl', 'zr'], writes=['pb7'])
            qb_i = 0
            for g, (wing, d) in enumerate(GROUPS):
                e0 = HALO - wing
                nbl = (NEXT - e0) // (128 * d)
                ebv = ebf[:].rearrange("p (h b q) -> p h b q", h=4, b=2)
                c.dma('sp', lambda g=g: nc.sync.dma_start(out=ebf[:], in_=ebd_t.ap()[g]), reads=[('ebd', g)], writes=['ebf'])
                if g == 0:
                    c.op('dve', lambda: nc.vector.tensor_copy(out=smk[:, 0, :, :], in_=ebv[:, :, 0, 0:4]), reads=['ebf'], writes=['smk'])
                else:
                    for r in range(4):
                        c.op('dve', lambda r=r, g=g: nc.vector.tensor_copy(out=smk[:, 1 + 4 * (g - 1) + r, :, r:r + 1], in_=ebv[:, :, 0, 0:1]), reads=['ebf'], writes=['smk'])
                bsel = bdm[:, 0:64] if g == 0 else bdm[:, 64:128]
                c.op('dve', lambda g=g, bsel=bsel: nc.vector.tensor_tensor(out=nmk[:, g, :, :], in0=ebv[0:64, :, 1, 0:64], in1=bsel.unsqueeze(1).to_broadcast([64, 4, 64]), op=ALU.mult),
                     reads=['ebf', 'bdm'], writes=['nmk'])
                c.dma('pool', lambda g=g: nc.gpsimd.dma_start(out=wkv[:, :, 0:256], in_=winr[:, :, 768 + 256 * g: 1024 + 256 * g]), writes=['wkv_k'])
                c.dma('pool', lambda g=g: nc.gpsimd.dma_start(out=wkv[:, :, 256:512], in_=winr[:, :, 1536 + 256 * g: 1792 + 256 * g]), writes=['wkv_v'])
                nkv = 0
                for r in range(d):
                    for i in range(nbl):
                        bi = r * nbl + i
                        cs0 = e0 + r + 128 * d * i
                        hsl = slice(cs0, cs0 + 127 * d + 1, d)
                        pi = 4 + nkv % 3
                        for k in range(8):
                            c.op('pe', lambda k=k, hsl=hsl, pi=pi: nc.tensor.matmul(PB[pi][:, :], lhsT=hT[:, k, hsl], rhs=wkv[:, k, :], start=(k == 0), stop=(k == 7)),
                                 reads=['hT', 'wkv_k', 'wkv_v'], writes=[('pb', pi)])
                        vc = hv if i == 0 else one1
                        c.op('dve', lambda bi=bi, pi=pi, vc=vc: nc.vector.tensor_scalar(out=v1[:, bi, :, 0:64], in0=PB[pi][:, 256:512].rearrange("p (h e) -> p h e", h=4),
                             scalar1=vc[:, 0:1], scalar2=None, op0=ALU.mult), reads=[('pb', pi), 'hv', 'one1'], writes=[('v1', bi)])
                        c.op('pool', lambda bi=bi, vc=vc: nc.gpsimd.tensor_copy(out=v1[:, bi, :, 64:65], in_=vc[:, 0:1].unsqueeze(1).to_broadcast([128, 4, 1])),
                             reads=['hv', 'one1'], writes=[('v1o', bi)])
                        if i == nbl - 1:
                            s = nkv % 2
                            c.op('act', lambda s=s, pi=pi: nc.scalar.copy(out=kvst[s][:], in_=PB[pi][:, :]), reads=[('pb', pi)], writes=[('kvst', s)])
                            c.dma('sp', lambda s=s, r=r, g=g, d=d, wing=wing: nc.sync.dma_start(out=kvp_t[g].ap()[r:wing:d, :], in_=kvst[s][:]), reads=[('kvst', s)], writes=[('kvp', g, r)])
                        nkv += 1
                pi = 4 + nkv % 3
                for k in range(8):
                    c.op('pe', lambda k=k, pi=pi: nc.tensor.matmul(PB[pi][0:64, :], lhsT=hT[:, k, NEXT:NTOK], rhs=wkv[:, k, :], start=(k == 0), stop=(k == 7)),
                         reads=['hT', 'wkv_k', 'wkv_v'], writes=[('pb', pi)])
                s = nkv % 2
                c.op('act', lambda s=s, pi=pi: nc.scalar.copy(out=kvst[s][0:64, :], in_=PB[pi][0:64, :]), reads=[('pb', pi)], writes=[('kvst', s)])
                c.op('dve', lambda g=g, pi=pi: nc.vector.tensor_copy(out=v1n[:, g, :, 0:64], in_=PB[pi][0:64, 256:512].rearrange("p (h e) -> p h e", h=4)), reads=[('pb', pi)], writes=['v1n'])
                for b in range(NSQ):
                    c.dma('sp', lambda b=b, s=s, g=g, wing=wing: nc.sync.dma_start(out=kvs_t[g].ap()[b, wing - 4:wing, :], in_=kvst[s][4 * b:4 * b + 4, :]), reads=[('kvst', s)], writes=[('kvsn', g, b)])
                for pair in range(2):
                    if _os_env('MK_SKIP_ATT'):
                        continue
                    c.dma('pool', lambda g=g, pair=pair: nc.gpsimd.dma_start(out=wq[:], in_=winr[:, :, 256 * g + 128 * pair: 256 * g + 128 * pair + 128]), writes=['wq'])
                    c.dma('pool', lambda g=g, pair=pair: nc.gpsimd.dma_start(out=wk[:], in_=winr[:, :, 768 + 256 * g + 128 * pair: 768 + 256 * g + 128 * pair + 128]), writes=['wk'])
                    npj = 0
                    for (hc, oi, n) in OT:
                        pi = 4 + npj % 3
                        proj_T(PB[pi][:, 0:n], ('pb', pi), wq, 'wq', slice(hc, hc + n))
                        c.op('act', lambda pi=pi, oi=oi, n=n: nc.scalar.copy(out=qT[:, oi:oi + n], in_=PB[pi][:, 0:n]), reads=[('pb', pi)], writes=['qT'])
                        npj += 1
                    cs_ = e0
                    while cs_ < NTOK:
                        n = min(512, NTOK - cs_)
                        pi = 4 + npj % 3
                        proj_T(PB[pi][:, 0:n], ('pb', pi), wk, 'wk', slice(cs_, cs_ + n))
                        if npj % 2 == 0:
                            c.op('dve', lambda pi=pi, cs_=cs_, n=n: nc.vector.tensor_copy(out=kT[:, cs_:cs_ + n], in_=PB[pi][:, 0:n]), reads=[('pb', pi)], writes=['kT'])
                        else:
                            c.op('act', lambda pi=pi, cs_=cs_, n=n: nc.scalar.copy(out=kT[:, cs_:cs_ + n], in_=PB[pi][:, 0:n]), reads=[('pb', pi)], writes=['kT_a'])
                        npj += 1
                        cs_ += n
                    c.op('dve', lambda pair=pair: nc.vector.tensor_copy(out=qs[:, pair, :], in_=qT[:, NOWN:NOT]), reads=['qT'], writes=['qs'])
                    c.op('dve', lambda pair=pair: nc.vector.tensor_copy(out=ks[:, pair, :], in_=kT[:, NEXT:NTOK]), reads=['kT', 'kT_a'], writes=['ks'])
                    for r in range(d):
                        for i in range(1, nbl):
                            s = qb_i % 2
                            qb_i += 1
                            q0 = r + 128 * d * (i - 1)
                            qsl = slice(q0, q0 + 127 * d + 1, d)
                            SBK = ((0, 1), (4, 5))[s]
                            for hh in range(2):
                                po = hh * 64
                                for blk in range(2):
                                    k0 = e0 + r + 128 * d * (i - 1 + blk)
                                    ksl = slice(k0, k0 + 127 * d + 1, d)
                                    c.op('pe', lambda bk=SBK[hh], po=po, ksl=ksl, qsl=qsl, blk=blk: nc.tensor.matmul(PB[bk][:, blk * 128:blk * 128 + 128], lhsT=kT[po:po + 64, ksl], rhs=qT[po:po + 64, qsl], start=True, stop=True),
                                         reads=['kT', 'kT_a', 'qT'], writes=[('pb', SBK[hh])])
                            for hh in range(2):
                                c.op('act', lambda s=s, hh=hh, bk=SBK[hh]: nc.scalar.activation(out=ef[s][:, hh * 256:(hh + 1) * 256], in_=PB[bk][:, 0:256], func=AF.Exp, scale=0.125),
                                     reads=[('pb', SBK[hh])], writes=[('ef', s, hh)])
                            c.op('dve', lambda s=s, pair=pair: nc.vector.tensor_tensor(out=pt[s][:], in0=ef[s][:], in1=ebf[:, pair * 512:(pair + 1) * 512], op=ALU.mult),
                                 reads=[('ef', s, 0), ('ef', s, 1), 'ebf'], writes=[('pt', s)])
                            for hh in range(2):
                                for blk in range(2):
                                    bi = r * nbl + i - 1 + blk
                                    col = (hh * 2 + blk) * 128
                                    c.op('pe', lambda s=s, hh=hh, blk=blk, bi=bi, col=col, pair=pair: nc.tensor.matmul(PB[2 + s][:, hh * 65:hh * 65 + 65], lhsT=pt[s][:, col:col + 128],
                                         rhs=v1[:, bi, 2 * pair + hh, :], start=(blk == 0), stop=(blk == 1)), reads=[('pt', s), ('v1', bi), ('v1o', bi)], writes=[('pb', 2 + s)])
                            c.op('dve', lambda s=s: nc.vector.tensor_copy(out=oev[s][:], in_=PB[2 + s][:, 0:130]), reads=[('pb', 2 + s)], writes=[('oev', s)])
                            c.dma('sp', lambda s=s, g=g, q0=q0, d=d, pair=pair: nc.sync.dma_start(out=acc_t.ap()[g, q0:q0 + 127 * d + 1:d, pair * 130:(pair + 1) * 130], in_=oev[s][:]),
                                  reads=[('oev', s)], writes=[('acc', g, q0, pair)])
                import os as _os
                ntile = 1 if g == 0 else 4
                if _os.environ.get('MK_SKIP_SAMPLE'):
                    continue
                for b in range(NSQ):
                    for r in range(ntile):
                        s = (b * ntile + r) % 2
                        tix = 0 if g == 0 else 1 + 4 * (g - 1) + r
                        c.dma('sp', lambda s=s, b=b, r=r, g=g, d=d, wing=wing: nc.sync.dma_start(out=ctile[s][:], in_=ck_t[g].ap()[b, r:wing:d, :]), writes=[('ctile', s)])
                        TB = (0, 1)[s]
                        for pr in range(2):
                            c.op('pe', lambda s=s, pr=pr, TB=TB: nc.tensor.transpose(out=PB[TB][:, pr * 128:(pr + 1) * 128], in_=ctile[s][:, pr * 128:(pr + 1) * 128], identity=identf[:]),
                                 reads=[('ctile', s), 'identf'], writes=[('pb', TB)])
                        c.op('act', lambda s=s, TB=TB: nc.scalar.copy(out=kTs[s][:].rearrange("p a k -> p (a k)"), in_=PB[TB][:, 0:256]), reads=[('pb', TB)], writes=[('kTs', s)])
                        c.op('pool', lambda s=s: nc.gpsimd.tensor_copy(out=v1s[s][:, :, 0:64], in_=ctile[s][:, 256:512].rearrange("p (h e) -> p h e", h=4)), reads=[('ctile', s)], writes=[('v1s', s)])
                        for h in range(4):
                            po = (h % 2) * 64
                            bk = (2, 3)[s] if h % 2 == 0 else (4, 5)[s]
                            c.op('pe', lambda s=s, h=h, po=po, b=b, bk=bk: nc.tensor.matmul(PB[bk][:, 256 + 4 * h:260 + 4 * h], lhsT=kTs[s][po:po + 64, h // 2, :], rhs=qs[po:po + 64, h // 2, 4 * b:4 * b + 4], start=True, stop=True),
                                 reads=[('kTs', s), 'qs'], writes=[('pb', bk)])
                        ess = ess2[s]
                        essv = ess[:].rearrange("p (h q) -> p h q", h=4)
                        for par, bk in ((0, (2, 3)[s]), (1, (4, 5)[s])):
                            c.op('act', lambda par=par, bk=bk, essv=essv: nc.scalar.activation(out=essv[:, par:4:2, :], in_=PB[bk][:, 256:272].rearrange("p (h q) -> p h q", h=4)[:, par:4:2, :], func=AF.Exp, scale=0.125),
                                 reads=[('pb', bk)], writes=[('ess', s, par)])
                        c.op('dve', lambda b=b, tix=tix, ess=ess: nc.vector.tensor_tensor(out=pz[b][:, :, 4 * b:4 * b + 4], in0=ess[:].rearrange("p (h q) -> p h q", h=4), in1=smk[:, tix, :, :], op=ALU.mult),
                             reads=[('ess', s, 0), ('ess', s, 1), 'smk'], writes=[('pz', b)])
                        for h in range(4):
                            c.op('pe', lambda s=s, h=h, b=b, st=(n_os == 0): nc.tensor.matmul(PB[7][0:64, 65 * h:65 * h + 65], lhsT=pz[b][:, h, :], rhs=v1s[s][:, h, :], start=st, stop=False),
                                 reads=[('pz', b), ('v1s', s)], writes=['pb7'])
                        n_os += 1
                for h in range(4):
                    po = (h % 2) * 64
                    bk = 6 if h % 2 == 0 else 5
                    c.op('pe', lambda h=h, po=po, bk=bk: nc.tensor.matmul(PB[bk][0:64, 64 * h:64 * h + 64], lhsT=ks[po:po + 64, h // 2, :], rhs=qs[po:po + 64, h // 2, :], start=True, stop=True),
                         reads=['ks', 'qs'], writes=[('pb', bk)])
                for h in range(4):
                    bk = 6 if h % 2 == 0 else 5
                    c.op('act', lambda h=h, bk=bk: nc.scalar.activation(out=en[:, 64 * h:64 * h + 64], in_=PB[bk][0:64, 64 * h:64 * h + 64], func=AF.Exp, scale=0.125), reads=[('pb', bk)], writes=[('en', h)])
                c.op('dve', lambda g=g: nc.vector.tensor_tensor(out=pn[:], in0=en[:].rearrange("p (h q) -> p h q", h=4), in1=nmk[:, g, :, :], op=ALU.mult), reads=[('en', 0), ('en', 1), ('en', 2), ('en', 3), 'nmk'], writes=['pn'])
                for h in range(4):
                    c.op('pe', lambda h=h, g=g: nc.tensor.matmul(PB[7][0:64, 65 * h:65 * h + 65], lhsT=pn[:, h, :], rhs=v1n[:, g, h, :], start=False, stop=(g == 2 and h == 3)),
                         reads=['pn', 'v1n'], writes=['pb7'])
            c.op('act', lambda: nc.scalar.copy(out=osv[:], in_=PB[7][0:64, 0:260]), reads=['pb7'], writes=['osv'])
            c.dma('sp', lambda: nc.sync.dma_start(out=accs_t.ap(), in_=osv[:]), reads=['osv'], writes=['accs'])
        c.barrier()
        es_hT.close()
        if STOP_AFTER == 'D':
            c.finish()
            return nc
        wcor = wco_t.ap().rearrange("(c p) n -> p c n", p=128)
        waor = wao_t.ap().rearrange("(c p) n -> p c n", p=128)
        wor = wo_t.ap().rearrange("(c p) n -> p c n", p=128)
        wrtr = wrt_t.ap().rearrange("(c p) n -> p c n", p=128)
        h2d_t = dscr("h2scr", [NOT, D], BF16)
        esE = ExitStack()
        es.enter_context(esE)
        lgall = c.sb([128, NTILE, 36], F32, esE)
        esE1 = ExitStack()
        with esE1:
            wco = c.sb([128, 4, D], BF16, esE1); wao = c.sb([128, 2, D], BF16, esE1); wo = c.sb([128, 8, D], BF16, esE1)
            wrt = c.sb([128, 8, 36], BF16, esE1); brtb = c.sb([128, 36], F32, esE1)
            c.dma('pool', lambda: nc.gpsimd.dma_start(out=wco[:], in_=wcor), writes=['wco'])
            c.dma('pool', lambda: nc.gpsimd.dma_start(out=wao[:], in_=waor), writes=['wao'])
            c.dma('pool', lambda: nc.gpsimd.dma_start(out=wo[:], in_=wor), writes=['wo'])
            c.dma('pool', lambda: nc.gpsimd.dma_start(out=wrt[:], in_=wrtr), writes=['wrt'])
            c.dma('sp', lambda: nc.sync.dma_start(out=brtb[:], in_=brt_t.ap().partition_broadcast(128)), writes=['brtb'])
            mrow = {}
            for kind, nm in ((0, 'g1'), (1, 'b2'), (2, 'a2')):
                for part, (c0, n) in enumerate(((0, 128), (128, 64))):
                    t_ = c.sb([n, D], F32, esE1)
                    c.dma('sp', lambda t_=t_, kind=kind, c0=c0, n=n: nc.sync.dma_start(out=t_[:], in_=modrows_t.ap()[kind, c0:c0 + n, :]),
                          reads=[('modrows', kind, part)], writes=[('mrow', nm, part)])
                    mrow[(nm, part)] = t_
            c.op('pool', lambda: nc.gpsimd.memset(lgall[:], 0.0), writes=['lgall'])
            ytile = [c.sb([128, 4, 512], BF16, esE1) for _ in range(2)]
            oT = c.sb([128, 2, 512], BF16, esE1)
            acc3 = [c.sb([128, 3, 260], F32, esE1) for _ in range(2)]
            osum = c.sb([128, 260], F32, esE1); rden = c.sb([128, 4], F32, esE1); ob = c.sb([128, 256], BF16, esE1)
            sga = [c.sb([128, 512], BF16, esE1) for _ in range(2)]; sgb = [c.sb([128, 512], BF16, esE1) for _ in range(2)]
            m1 = [c.sb([128, 512], F32, esE1) for _ in range(2)]; m2 = [c.sb([128, 512], F32, esE1) for _ in range(2)]
            mixT = c.sb([128, 8, 512], BF16, esE1)
            xt2 = [c.sb([128, D], F32, esE1) for _ in range(2)]; tmp = [c.sb([128, D], F32, esE1) for _ in range(2)]
            x1t = [c.sb([128, D], F32, esE1) for _ in range(2)]; t2 = [c.sb([128, D], F32, esE1) for _ in range(2)]
            h2b = [c.sb([128, D], BF16, esE1) for _ in range(2)]
            h2T = c.sb([128, 8, 128], BF16, esE1); st2 = [c.sb([128, 4], F32, esE1) for _ in range(2)]
            junk2 = c.sb([128, D], BF16, esE1)
            sub_i = 0
            for M, (hc, oi, n) in enumerate(OT):
                part = 0 if M < 8 else 1
                rr = 128 if M < 8 else 64
                nsub = n // rr
                ys_ = M % 2
                c.dma('sp', lambda ys_=ys_, oi=oi, n=n: nc.sync.dma_start(out=ytile[ys_][:, :, 0:n], in_=yts_t.ap()[:, :, oi:oi + n].rearrange("c p t -> p c t")),
                      reads=[('yts', cc) for cc in range(4)], writes=[('ytile', ys_)])
                for t in range(nsub):
                    a_ = (M * 4 + t) % 2
                    r0 = oi + rr * t
                    if M < 8:
                        c.dma('sp', lambda a_=a_, r0=r0: nc.sync.dma_start(out=acc3[a_][:], in_=acc_t.ap()[:, r0:r0 + 128, :].rearrange("g t c -> t g c")),
                              reads=[k for k in c.state if isinstance(k, tuple) and k[0] == 'acc'], writes=[('acc3', a_)])
                        c.op('dve', lambda a_=a_: nc.vector.tensor_tensor(out=osum[:], in0=acc3[a_][:, 0, :], in1=acc3[a_][:, 1, :], op=ALU.add), reads=[('acc3', a_)], writes=['osum'])
                        c.op('dve', lambda a_=a_: nc.vector.tensor_tensor(out=osum[:], in0=osum[:], in1=acc3[a_][:, 2, :], op=ALU.add), reads=[('acc3', a_), 'osum'], writes=['osum'])
                    else:
                        c.dma('sp', lambda a_=a_: nc.sync.dma_start(out=acc3[a_][0:64, 0, :], in_=accs_t.ap()), reads=['accs'], writes=[('acc3', a_)])
                        c.op('dve', lambda a_=a_: nc.vector.tensor_copy(out=osum[0:64, :], in_=acc3[a_][0:64, 0, :]), reads=[('acc3', a_)], writes=['osum'])
                    ov = osum[0:rr, :].rearrange("p (h e) -> p h e", h=4)
                    c.op('dve', lambda ov=ov, rr=rr: nc.vector.reciprocal(out=rden[0:rr, :].unsqueeze(2), in_=ov[:, :, 64:65]), reads=['osum'], writes=['rden'])
                    c.op('dve', lambda ov=ov, rr=rr: nc.vector.tensor_tensor(out=ob[0:rr, :].rearrange("p (h e) -> p h e", h=4), in0=ov[:, :, 0:64],
                         in1=rden[0:rr, :].unsqueeze(2).to_broadcast([rr, 4, 64]), op=ALU.mult), reads=['osum', 'rden'], writes=['ob'])
                    pv4 = pbf(4).rearrange("p (k t) -> p k t", k=8)
                    for k in range(2):
                        c.op('pe', lambda k=k, rr=rr, pv4=pv4: nc.tensor.transpose(out=pv4[:, k, 0:rr], in_=ob[0:rr, k * 128:(k + 1) * 128], identity=ident[0:rr, 0:rr]),
                             reads=['ob', 'ident'], writes=[('pb', 4)])
                    c.op('act', lambda t=t, rr=rr, pv4=pv4: nc.scalar.copy(out=oT[:, :, rr * t:rr * t + rr], in_=pv4[:, 0:2, 0:rr]), reads=[('pb', 4)], writes=['oT'])
                for j in range(8):
                    gs = j % 2
                    c.dma('sp', lambda gs=gs, j=j, oi=oi, n=n: nc.sync.dma_start(out=sga[gs][:, 0:n], in_=sg_t.ap()[j, :, oi:oi + n]), reads=[('sg', j)], writes=[('sga', gs)])
                    c.dma('sp', lambda gs=gs, j=j, oi=oi, n=n: nc.sync.dma_start(out=sgb[gs][:, 0:n], in_=sg_t.ap()[8 + j, :, oi:oi + n]), reads=[('sg', 8 + j)], writes=[('sgb', gs)])
                    pa_i, pb_i = (0, 1) if gs == 0 else (6, 7)
                    for cc in range(4):
                        c.op('pe', lambda cc=cc, j=j, pa_i=pa_i, ys_=ys_, n=n: nc.tensor.matmul(PB[pa_i][:, 0:n], lhsT=wco[:, cc, j * 128:(j + 1) * 128], rhs=ytile[ys_][:, cc, 0:n], start=(cc == 0), stop=(cc == 3)),
                             reads=['wco', ('ytile', ys_)], writes=[('pb', pa_i)])
                    for cc in range(2):
                        c.op('pe', lambda cc=cc, j=j, pb_i=pb_i, n=n: nc.tensor.matmul(PB[pb_i][:, 0:n], lhsT=wao[:, cc, j * 128:(j + 1) * 128], rhs=oT[:, cc, 0:n], start=(cc == 0), stop=(cc == 1)),
                             reads=['wao', 'oT'], writes=[('pb', pb_i)])
                    c.op('dve', lambda gs=gs, pa_i=pa_i, n=n: nc.vector.tensor_tensor(out=m1[gs][:, 0:n], in0=PB[pa_i][:, 0:n], in1=sga[gs][:, 0:n], op=ALU.mult), reads=[('pb', pa_i), ('sga', gs)], writes=[('m1', gs)])
                    c.op('dve', lambda gs=gs, pb_i=pb_i, n=n: nc.vector.tensor_tensor(out=m2[gs][:, 0:n], in0=PB[pb_i][:, 0:n], in1=sgb[gs][:, 0:n], op=ALU.mult), reads=[('pb', pb_i), ('sgb', gs)], writes=[('m2', gs)])
                    c.op('pool', lambda gs=gs, j=j, n=n: nc.gpsimd.tensor_tensor(out=mixT[:, j, 0:n], in0=m1[gs][:, 0:n], in1=m2[gs][:, 0:n], op=ALU.add), reads=[('m1', gs), ('m2', gs)], writes=[('mixT', j)])
                for t in range(nsub):
                    xs_ = sub_i % 2
                    sub_i += 1
                    r0 = oi + rr * t
                    tile_i = r0 // 128 if M < 8 else 32
                    src = xp[HALO + r0:HALO + r0 + 128, :] if M < 8 else xs
                    c.dma('sp', lambda xs_=xs_, src=src, rr=rr: nc.sync.dma_start(out=xt2[xs_][0:rr, :], in_=src), writes=[('xt2', xs_)])
                    for hf in range(2):
                        for j in range(8):
                            c.op('pe', lambda hf=hf, j=j, t=t, rr=rr: nc.tensor.matmul(PB[2 + hf][0:rr, :], lhsT=mixT[:, j, rr * t:rr * t + rr], rhs=wo[:, j, hf * 512:(hf + 1) * 512], start=(j == 0), stop=(j == 7)),
                                 reads=[('mixT', j), 'wo'], writes=[('pb', 2 + hf)])
                        c.op('dve', lambda hf=hf, xs_=xs_, rr=rr, part=part: nc.vector.tensor_tensor(out=tmp[xs_][0:rr, hf * 512:(hf + 1) * 512], in0=PB[2 + hf][0:rr, :],
                             in1=mrow[('g1', part)][0:rr, hf * 512:(hf + 1) * 512], op=ALU.mult), reads=[('pb', 2 + hf), ('mrow', 'g1', part)], writes=[('tmp', xs_)])
                    c.op('pool', lambda xs_=xs_, rr=rr: nc.gpsimd.tensor_tensor(out=x1t[xs_][0:rr, :], in0=tmp[xs_][0:rr, :], in1=xt2[xs_][0:rr, :], op=ALU.add),
                         reads=[('tmp', xs_), ('xt2', xs_)], writes=[('x1t', xs_)])
                    c.dma('act', lambda xs_=xs_, r0=r0, rr=rr: nc.scalar.dma_start(out=x1_t.ap()[r0:r0 + rr, :], in_=x1t[xs_][0:rr, :]), reads=[('x1t', xs_)], writes=[('x1', tile_i)])
                    c.op('act', lambda xs_=xs_, rr=rr: nc.scalar.activation(out=junk2[0:rr, :], in_=x1t[xs_][0:rr, :], func=AF.Square, accum_out=st2[xs_][0:rr, 0:1]),
                         reads=[('x1t', xs_)], writes=[('st2', xs_), 'junk2'])
                    c.op('act', lambda xs_=xs_, rr=rr: nc.scalar.activation(out=st2[xs_][0:rr, 1:2], in_=st2[xs_][0:rr, 0:1], func=AF.Sqrt, scale=1.0 / D, bias=epsb[0:rr, :]),
                         reads=[('st2', xs_), 'epsb'], writes=[('st2', xs_)])
                    c.op('dve', lambda xs_=xs_, rr=rr: nc.vector.reciprocal(out=st2[xs_][0:rr, 2:3], in_=st2[xs_][0:rr, 1:2]), reads=[('st2', xs_)], writes=[('st2', xs_)])
                    c.op('dve', lambda xs_=xs_, rr=rr, part=part: nc.vector.scalar_tensor_tensor(out=t2[xs_][0:rr, :], in0=x1t[xs_][0:rr, :], scalar=st2[xs_][0:rr, 2:3],
                         in1=mrow[('a2', part)][0:rr, :], op0=ALU.mult, op1=ALU.mult), reads=[('x1t', xs_), ('st2', xs_), ('mrow', 'a2', part)], writes=[('t2', xs_)])
                    c.op('pool', lambda xs_=xs_, rr=rr, part=part: nc.gpsimd.tensor_tensor(out=h2b[xs_][0:rr, :], in0=t2[xs_][0:rr, :], in1=mrow[('b2', part)][0:rr, :], op=ALU.add),
                         reads=[('t2', xs_), ('mrow', 'b2', part)], writes=[('h2b', xs_)])
                    c.dma('act', lambda xs_=xs_, r0=r0, rr=rr: nc.scalar.dma_start(out=h2d_t.ap()[r0:r0 + rr, :], in_=h2b[xs_][0:rr, :]), reads=[('h2b', xs_)], writes=[('h2d', tile_i)])
                    pv5 = pbf(5).rearrange("p (k t) -> p k t", k=8)
                    for k in range(8):
                        c.op('pe', lambda k=k, xs_=xs_, rr=rr, pv5=pv5: nc.tensor.transpose(out=pv5[:, k, 0:rr], in_=h2b[xs_][0:rr, k * 128:(k + 1) * 128], identity=ident[0:rr, 0:rr]),
                             reads=[('h2b', xs_), 'ident'], writes=[('pb', 5)])
                    c.op('act', lambda rr=rr, pv5=pv5: nc.scalar.copy(out=h2T[:, :, 0:rr], in_=pv5[:, :, 0:rr]), reads=[('pb', 5)], writes=['h2T'])
                    for k in range(8):
                        c.op('pe', lambda k=k, rr=rr: nc.tensor.matmul(PB[4][0:rr, 0:36], lhsT=h2T[:, k, 0:rr], rhs=wrt[:, k, :], start=(k == 0), stop=(k == 7)),
                             reads=['h2T', 'wrt'], writes=[('pb', 4)])
                    c.op('dve', lambda rr=rr, tile_i=tile_i: nc.vector.tensor_tensor(out=lgall[0:rr, tile_i, :], in0=PB[4][0:rr, 0:36], in1=brtb[0:rr, :], op=ALU.add),
                         reads=[('pb', 4), 'brtb', 'lgall'], writes=['lgall'])
        c.barrier()
        if STOP_AFTER == 'E':
            c.finish()
            return nc
        NT = NTILE
        esF = ExitStack()
        with esF:
            def ft(shape, dt=F32):
                return c.sb(shape, dt, esF)
            gmx = ft([128, NT]); ohg = ft([128, NT, 4]); gsh = ft([128, NT, 4]); gex = ft([128, NT, 4]); gsum = ft([128, NT]); pgr = ft([128, NT])
            pen = ft([128, NT, 4]); em = ft([128, NT, 32]); m8 = ft([128, NT, 8]); i8 = ft([128, NT, 8], U32)
            e0f = ft([128, NT]); e1f = ft([128, NT]); dv = ft([128, NT]); w0 = ft([128, NT]); w1 = ft([128, NT])
            oh0 = ft([128, NT, 32]); oh1 = ft([128, NT, 32]); mm_ = ft([128, NT, 32]); cs = ft([128, NT + 1, 32])
            base = ft([128, NT, 32]); prod = ft([128, NT, 32]); d0f = ft([128, NT]); d1f = ft([128, NT])
            d0i = ft([128, NT], I32); d1i = ft([128, NT], I32)
            io32i = ft([128, 32], I32); io32 = ft([128, 32]); thri = ft([128, NBLK], I32); thr = ft([128, NBLK])
            cnt = ft([128, 32]); cni = ft([128, 32], I32); pad = ft([128, 32]); pa_ = ft([128, 32]); pb_ = ft([128, 32]); pst = ft([128, 32])
            cmpb = ft([128, NBLK, 32]); bef = ft([128, NBLK])
            c.op('pool', lambda: nc.gpsimd.iota(io32i[:], pattern=[[1, 32]], base=0, channel_multiplier=0), writes=['io32i'])
            c.op('pool', lambda: nc.gpsimd.iota(thri[:], pattern=[[BLK, NBLK]], base=0, channel_multiplier=0), writes=['thri'])
            c.op('dve', lambda: nc.vector.tensor_copy(out=io32[:], in_=io32i[:]), reads=['io32i'], writes=['io32'])
            c.op('dve', lambda: nc.vector.tensor_copy(out=thr[:], in_=thri[:]), reads=['thri'], writes=['thr'])
            R = ['lgall']
            gl = lgall[:, :, 0:4]
            V = nc.vector
            c.op('dve', lambda: V.tensor_reduce(out=gmx[:], in_=gl, axis=AX.X, op=ALU.max), reads=R, writes=['gmx'])
            c.op('dve', lambda: V.tensor_tensor(out=ohg[:], in0=gl, in1=gmx[:].unsqueeze(2).to_broadcast([128, NT, 4]), op=ALU.is_equal), reads=R + ['gmx'], writes=['ohg'])
            c.op('dve', lambda: V.tensor_tensor(out=gsh[:], in0=gl, in1=gmx[:].unsqueeze(2).to_broadcast([128, NT, 4]), op=ALU.subtract), reads=R + ['gmx'], writes=['gsh'])
            c.op('act', lambda: nc.scalar.activation(out=gex[:], in_=gsh[:], func=AF.Exp), reads=['gsh'], writes=['gex'])
            c.op('dve', lambda: V.tensor_reduce(out=gsum[:], in_=gex[:], axis=AX.X, op=ALU.add), reads=['gex'], writes=['gsum'])
            c.op('dve', lambda: V.reciprocal(out=pgr[:], in_=gsum[:]), reads=['gsum'], writes=['pgr'])
            c.op('dve', lambda: V.tensor_scalar(out=pen[:], in0=ohg[:], scalar1=-1.0, scalar2=1e30, op0=ALU.add, op1=ALU.mult), reads=['ohg'], writes=['pen'])
            c.op('dve', lambda: V.tensor_tensor(out=em[:].rearrange("p t (g e) -> p t g e", g=4), in0=lgall[:, :, 4:36].rearrange("p t (g e) -> p t g e", g=4),
                 in1=pen[:].unsqueeze(3).to_broadcast([128, NT, 4, 8]), op=ALU.add), reads=R + ['pen'], writes=['em'])
            for i in range(NT):
                c.op('dve', lambda i=i: V.max(out=m8[:, i, :], in_=em[:, i, :]), reads=['em'], writes=['m8'])
                c.op('dve', lambda i=i: V.max_index(out=i8[:, i, :], in_max=m8[:, i, :], in_values=em[:, i, :]), reads=['em', 'm8'], writes=['i8'])
            c.op('dve', lambda: V.tensor_copy(out=e0f[:], in_=i8[:, :, 0]), reads=['i8'], writes=['e0f'])
            c.op('dve', lambda: V.tensor_copy(out=e1f[:], in_=i8[:, :, 1]), reads=['i8'], writes=['e1f'])
            c.op('dve', lambda: V.tensor_tensor(out=dv[:], in0=m8[:, :, 1], in1=m8[:, :, 0], op=ALU.subtract), reads=['m8'], writes=['dv'])
            c.op('act', lambda: nc.scalar.activation(out=dv[:], in_=dv[:], func=AF.Exp), reads=['dv'], writes=['dv'])
            c.op('dve', lambda: V.tensor_scalar(out=dv[:], in0=dv[:], scalar1=1.0, scalar2=None, op0=ALU.add), reads=['dv'], writes=['dv'])
            c.op('dve', lambda: V.reciprocal(out=w0[:], in_=dv[:]), reads=['dv'], writes=['w0'])
            c.op('dve', lambda: V.tensor_tensor(out=w0[:], in0=w0[:], in1=pgr[:], op=ALU.mult), reads=['w0', 'pgr'], writes=['w0'])
            c.op('dve', lambda: V.tensor_tensor(out=w1[:], in0=pgr[:], in1=w0[:], op=ALU.subtract), reads=['w0', 'pgr'], writes=['w1'])
            iob = io32[:].unsqueeze(1).to_broadcast([128, NT, 32])
            c.op('dve', lambda: V.tensor_tensor(out=oh0[:], in0=iob, in1=e0f[:].unsqueeze(2).to_broadcast([128, NT, 32]), op=ALU.is_equal), reads=['io32', 'e0f'], writes=['oh0'])
            c.op('dve', lambda: V.tensor_tensor(out=oh1[:], in0=iob, in1=e1f[:].unsqueeze(2).to_broadcast([128, NT, 32]), op=ALU.is_equal), reads=['io32', 'e1f'], writes=['oh1'])
            c.op('dve', lambda: V.memset(oh0[64:128, NT - 1, :], 0.0), reads=['oh0'], writes=['oh0'])
            c.op('dve', lambda: V.memset(oh1[64:128, NT - 1, :], 0.0), reads=['oh1'], writes=['oh1'])
            c.op('dve', lambda: V.tensor_tensor(out=mm_[:], in0=oh0[:], in1=oh1[:], op=ALU.add), reads=['oh0', 'oh1'], writes=['mm'])
            c.op('dve', lambda: V.memset(cs[:, 0, :], 0.0), writes=['cs'])
            for i in range(NT):
                c.op('dve', lambda i=i: V.tensor_tensor(out=cs[:, i + 1, :], in0=cs[:, i, :], in1=mm_[:, i, :], op=ALU.add), reads=['cs', 'mm'], writes=['cs'])
            for i in range(NT):
                bk = i // 16
                co = (i % 16) * 32
                c.op('pe', lambda i=i, bk=bk, co=co: nc.tensor.matmul(PB[bk][:, co:co + 32], lhsT=suf[:], rhs=mm_[:, i, :], start=True, stop=False), reads=['suf', 'mm'], writes=[('pb', bk)])
                c.op('pe', lambda i=i, bk=bk, co=co: nc.tensor.matmul(PB[bk][:, co:co + 32], lhsT=onesf[:], rhs=cs[:, i, :], start=False, stop=True), reads=['onesf', 'cs'], writes=[('pb', bk)])
            c.op('pe', lambda: nc.tensor.matmul(PB[3][:, 0:32], lhsT=onesf[:], rhs=cs[:, NT, :], start=True, stop=True), reads=['onesf', 'cs'], writes=[('pb', 3)])
            c.op('dve', lambda: V.tensor_scalar(out=cni[:], in0=PB[3][:, 0:32], scalar1=float(BLK - 1), scalar2=None, op0=ALU.add), reads=[('pb', 3)], writes=['cni'])
            c.op('dve', lambda: V.tensor_scalar(out=cni[:], in0=cni[:], scalar1=int(math.log2(BLK)), scalar2=int(math.log2(BLK)), op0=ALU.arith_shift_right, op1=ALU.logical_shift_left), reads=['cni'], writes=['cni'])
            c.op('dve', lambda: V.tensor_copy(out=pad[:], in_=cni[:]), reads=['cni'], writes=['pad'])
            src_, dst_ = pad, pa_
            for sft in (1, 2, 4, 8, 16):
                c.op('dve', lambda src_=src_, dst_=dst_, sft=sft: V.tensor_copy(out=dst_[:, 0:sft], in_=src_[:, 0:sft]), reads=['pfx', 'pad'], writes=['pfx'])
                c.op('dve', lambda src_=src_, dst_=dst_, sft=sft: V.tensor_tensor(out=dst_[:, sft:32], in0=src_[:, sft:32], in1=src_[:, 0:32 - sft], op=ALU.add), reads=['pfx', 'pad'], writes=['pfx'])
                src_, dst_ = dst_, (pb_ if dst_ is pa_ else pa_)
            pend = src_
            c.op('dve', lambda: V.tensor_tensor(out=pst[:], in0=pend[:], in1=pad[:], op=ALU.subtract), reads=['pfx', 'pad'], writes=['pst'])
            for bk in range(3):
                t0 = bk * 16
                nt_ = min(16, NT - t0)
                c.op('dve', lambda bk=bk, t0=t0, nt_=nt_: V.tensor_tensor(out=base[:, t0:t0 + nt_, :], in0=PB[bk][:, 0:nt_ * 32].rearrange("p (t e) -> p t e", e=32),
                     in1=pst[:].unsqueeze(1).to_broadcast([128, nt_, 32]), op=ALU.add), reads=[('pb', bk), 'pst'], writes=['base'])
            for oh_, df_, di_, nm in ((oh0, d0f, d0i, 'd0'), (oh1, d1f, d1i, 'd1')):
                c.op('dve', lambda oh_=oh_: V.tensor_tensor(out=prod[:], in0=oh_[:], in1=base[:], op=ALU.mult), reads=['oh0', 'oh1', 'base'], writes=['prod'])
                c.op('dve', lambda df_=df_: V.tensor_reduce(out=df_[:], in_=prod[:], axis=AX.X, op=ALU.add), reads=['prod'], writes=[nm + 'f'])
                c.op('dve', lambda df_=df_, di_=di_: V.tensor_copy(out=di_[:], in_=df_[:]), reads=[nm + 'f'], writes=[nm])
            c.op('dve', lambda: V.tensor_tensor(out=cmpb[:], in0=pend[:].unsqueeze(1).to_broadcast([128, NBLK, 32]), in1=thr[:].unsqueeze(2).to_broadcast([128, NBLK, 32]), op=ALU.is_le),
                 reads=['pfx', 'thr'], writes=['cmpb'])
            c.op('dve', lambda: V.tensor_reduce(out=bef[:], in_=cmpb[:], axis=AX.X, op=ALU.add), reads=['cmpb'], writes=['bef'])
            c.op('dve', lambda: V.tensor_scalar(out=bef[:], in0=bef[:], scalar1=31.0, scalar2=None, op0=ALU.min), reads=['bef'], writes=['bef'])
            pio = ft([128, 1], I32); piof = ft([128, 1]); widxf = ft([128, NBLK]); widx = ft([128, NBLK], I32)
            c.op('pool', lambda: nc.gpsimd.iota(pio[:], pattern=[[0, 1]], base=0, channel_multiplier=1), writes=['pio'])
            c.op('dve', lambda: V.tensor_copy(out=piof[:], in_=pio[:]), reads=['pio'], writes=['piof'])
            c.op('dve', lambda: V.tensor_scalar(out=widxf[:], in0=bef[:], scalar1=128.0, scalar2=piof[:, 0:1], op0=ALU.mult, op1=ALU.add), reads=['bef', 'piof'], writes=['widxf'])
            c.op('dve', lambda: V.tensor_copy(out=widx[:], in_=widxf[:]), reads=['widxf'], writes=['widx'])
            zt = ft([128, D], BF16)
            c.op('pool', lambda: nc.gpsimd.memset(zt[:], 0.0), writes=['zt'])
            xsv = xsd_t.ap().rearrange("(b p) d -> p b d", p=128)
            zkeys = []
            NRB = CAP // 128
            for q4 in range(8):
                b0 = q4 * (NRB // 8)
                nb_ = NRB // 8
                c.dma('act', lambda b0=b0, nb_=nb_: nc.scalar.dma_start(out=xsv[:, b0:b0 + nb_, :], in_=zt[:].unsqueeze(1).to_broadcast([128, nb_, D])), reads=['zt'], writes=[('xsz', q4)])
                zkeys.append(('xsz', q4))
            h2t = [ft([128, D], BF16) for _ in range(2)]
            skeys = []
            for i in range(NT):
                s = i % 2
                rr = 128 if i < NT - 1 else 64
                c.dma('sp', lambda s=s, i=i, rr=rr: nc.sync.dma_start(out=h2t[s][0:rr, :], in_=h2d_t.ap()[i * 128:i * 128 + rr, :]), reads=[('h2d', i)], writes=[('h2t', s)])
                for di_, nm in ((d0i, 'd0'), (d1i, 'd1')):
                    c.dma('pool', lambda s=s, i=i, rr=rr, di_=di_: nc.gpsimd.indirect_dma_start(out=xsd_t.ap(), out_offset=bass.IndirectOffsetOnAxis(ap=di_[0:rr, i:i + 1], axis=0),
                          in_=h2t[s][0:rr, :], in_offset=None), reads=[('h2t', s), nm] + zkeys, writes=[('xss', i, nm)])
                    skeys.append(('xss', i, nm))
            xsb = [ft([128, D], BF16) for _ in range(2)]; xsT = [ft([128, 8, 128], BF16) for _ in range(2)]
            wg = [ft([128, 8, 512], BF16) for _ in range(2)]; wu = [ft([128, 8, 512], BF16) for _ in range(2)]; wd = [ft([128, 4, D], BF16) for _ in range(2)]
            actt = [ft([128, 512]) for _ in range(2)]; ab = [ft([128, 512], BF16) for _ in range(2)]; aT = [ft([128, 4, 128], BF16) for _ in range(2)]
            yev = [ft([128, D]) for _ in range(2)]
            wegv = weg_t.ap().rearrange("e (p k) f -> (e p) (k f)", p=128)
            weuv = weu_t.ap().rearrange("e (p k) f -> (e p) (k f)", p=128)
            wedv = wed_t.ap().rearrange("e (p k) f -> (e p) (k f)", p=128)
            items = [(b, sub) for b in range(NBLK) for sub in range(SUBB)]

            def load_xsb(n):
                b, sub = items[n]
                xs_ = n % 2
                r0 = b * BLK + sub * 128
                c.dma('sp', lambda xs_=xs_, r0=r0: nc.sync.dma_start(out=xsb[xs_][:], in_=xsd_t.ap()[r0:r0 + 128, :]), reads=skeys + zkeys, writes=[('xsb', xs_)])

            load_xsb(0)
            for n, (b, sub) in enumerate(items):
                s = b % 2
                xs_ = n % 2
                if sub == 0:
                    for wt_, wv_, nm in ((wg, wegv, 'wg'), (wu, weuv, 'wu'), (wd, wedv, 'wd')):
                        c.dma('pool', lambda s=s, b=b, wt_=wt_, wv_=wv_: nc.gpsimd.indirect_dma_start(out=wt_[s][:].rearrange("p k f -> p (k f)"), out_offset=None, in_=wv_,
                              in_offset=bass.IndirectOffsetOnAxis(ap=widx[:, b:b + 1], axis=0)), reads=['widx'], writes=[(nm, s)])
                if n + 1 < len(items):
                    load_xsb(n + 1)
                pv0 = pbf(0).rearrange("p (k t) -> p k t", k=8)
                for k in range(8):
                    c.op('pe', lambda k=k, xs_=xs_, pv0=pv0: nc.tensor.transpose(out=pv0[:, k, :], in_=xsb[xs_][:, k:k + 8 * 127 + 1:8], identity=ident[:]), reads=[('xsb', xs_), 'ident'], writes=[('pb', 0)])
                c.op('act', lambda xs_=xs_, pv0=pv0: nc.scalar.copy(out=xsT[xs_][:], in_=pv0), reads=[('pb', 0)], writes=[('xsT', xs_)])
                gi, ui = (1, 2) if xs_ == 0 else (6, 7)
                for k in range(8):
                    c.op('pe', lambda k=k, s=s, xs_=xs_, gi=gi: nc.tensor.matmul(PB[gi][:, :], lhsT=xsT[xs_][:, k, :], rhs=wg[s][:, k, :], start=(k == 0), stop=(k == 7)), reads=[('xsT', xs_), ('wg', s)], writes=[('pb', gi)])
                for k in range(8):
                    c.op('pe', lambda k=k, s=s, xs_=xs_, ui=ui: nc.tensor.matmul(PB[ui][:, :], lhsT=xsT[xs_][:, k, :], rhs=wu[s][:, k, :], start=(k == 0), stop=(k == 7)), reads=[('xsT', xs_), ('wu', s)], writes=[('pb', ui)])
                c.op('act', lambda xs_=xs_, gi=gi: nc.scalar.activation(out=actt[xs_][:], in_=PB[gi][:, :], func=AF.Silu), reads=[('pb', gi)], writes=[('actt', xs_)])
                c.op('dve', lambda xs_=xs_, ui=ui: V.tensor_tensor(out=ab[xs_][:], in0=PB[ui][:, :], in1=actt[xs_][:], op=ALU.mult), reads=[('pb', ui), ('actt', xs_)], writes=[('ab', xs_)])
                pv3 = pbf(3).rearrange("p (k t) -> p k t", k=8)
                for k in range(4):
                    c.op('pe', lambda k=k, xs_=xs_, pv3=pv3: nc.tensor.transpose(out=pv3[:, k, :], in_=ab[xs_][:, k:k + 4 * 127 + 1:4], identity=ident[:]), reads=[('ab', xs_), 'ident'], writes=[('pb', 3)])
                c.op('dve', lambda xs_=xs_, pv3=pv3: V.tensor_copy(out=aT[xs_][:], in_=pv3[:, 0:4, :]), reads=[('pb', 3)], writes=[('aT', xs_)])
                for hf in range(2):
                    for k in range(4):
                        c.op('pe', lambda k=k, s=s, xs_=xs_, hf=hf: nc.tensor.matmul(PB[4 + hf][:, :], lhsT=aT[xs_][:, k, :], rhs=wd[s][:, k, hf * 512:(hf + 1) * 512], start=(k == 0), stop=(k == 3)),
                             reads=[('aT', xs_), ('wd', s)], writes=[('pb', 4 + hf)])
                c.op('act', lambda xs_=xs_: nc.scalar.copy(out=yev[xs_][:, 0:512], in_=PB[4][:, :]), reads=[('pb', 4)], writes=[('yev', xs_, 0)])
                c.op('dve', lambda xs_=xs_: V.tensor_copy(out=yev[xs_][:, 512:1024], in_=PB[5][:, :]), reads=[('pb', 5)], writes=[('yev', xs_, 1)])
                r0 = b * BLK + sub * 128
                c.dma('act', lambda xs_=xs_, r0=r0: nc.scalar.dma_start(out=ysd_t.ap()[r0:r0 + 128, :], in_=yev[xs_][:]), reads=[('yev', xs_, 0), ('yev', xs_, 1)], writes=[('ysd', n)])
            ykeys = [('ysd', n) for n in range(len(items))]
            g2r = {}
            for part, (c0, n) in enumerate(((0, 128), (128, 64))):
                t_ = ft([n, D])
                c.dma('sp', lambda t_=t_, c0=c0, n=n: nc.sync.dma_start(out=t_[:], in_=modrows_t.ap()[3, c0:c0 + n, :]), reads=[('modrows', 3, part)], writes=[('g2r', part)])
                g2r[part] = t_
            gfin = ft([128, D])
            c.dma('sp', lambda: nc.sync.dma_start(out=gfin[:], in_=gfin_t.ap().partition_broadcast(128)), writes=['gfin'])
            y0 = [ft([128, D]) for _ in range(2)]; y1 = [ft([128, D]) for _ in range(2)]; x1r = [ft([128, D]) for _ in range(2)]
            fa = [ft([128, D]) for _ in range(2)]; st3 = [ft([128, 4]) for _ in range(2)]
            junk3 = ft([128, D], BF16)
            for i in range(NT):
                s = i % 2
                rr = 128 if i < NT - 1 else 64
                part = 0 if i < NT - 1 else 1
                c.dma('pool', lambda s=s, i=i, rr=rr: nc.gpsimd.indirect_dma_start(out=y0[s][0:rr, :], out_offset=None, in_=ysd_t.ap(),
                      in_offset=bass.IndirectOffsetOnAxis(ap=d0i[0:rr, i:i + 1], axis=0)), reads=ykeys + ['d0'], writes=[('y0', s)])
                c.dma('pool', lambda s=s, i=i, rr=rr: nc.gpsimd.indirect_dma_start(out=y1[s][0:rr, :], out_offset=None, in_=ysd_t.ap(),
                      in_offset=bass.IndirectOffsetOnAxis(ap=d1i[0:rr, i:i + 1], axis=0)), reads=ykeys + ['d1'], writes=[('y1', s)])
                c.dma('sp', lambda s=s, i=i, rr=rr: nc.sync.dma_start(out=x1r[s][0:rr, :], in_=x1_t.ap()[i * 128:i * 128 + rr, :]), reads=[('x1', i)], writes=[('x1r', s)])
                c.op('dve', lambda s=s, i=i, rr=rr: V.tensor_scalar(out=fa[s][0:rr, :], in0=y0[s][0:rr, :], scalar1=w0[0:rr, i:i + 1], scalar2=None, op0=ALU.mult), reads=[('y0', s), 'w0'], writes=[('fa', s)])
                c.op('dve', lambda s=s, i=i, rr=rr: V.scalar_tensor_tensor(out=fa[s][0:rr, :], in0=y1[s][0:rr, :], scalar=w1[0:rr, i:i + 1], in1=fa[s][0:rr, :], op0=ALU.mult, op1=ALU.add),
                     reads=[('y1', s), 'w1', ('fa', s)], writes=[('fa', s)])
                c.op('pool', lambda s=s, rr=rr, part=part: nc.gpsimd.tensor_tensor(out=fa[s][0:rr, :], in0=fa[s][0:rr, :], in1=g2r[part][0:rr, :], op=ALU.mult), reads=[('fa', s), ('g2r', part)], writes=[('fa', s)])
                c.op('pool', lambda s=s, rr=rr: nc.gpsimd.tensor_tensor(out=x1r[s][0:rr, :], in0=fa[s][0:rr, :], in1=x1r[s][0:rr, :], op=ALU.add), reads=[('fa', s), ('x1r', s)], writes=[('x1r', s)])
                c.op('act', lambda s=s, rr=rr: nc.scalar.activation(out=junk3[0:rr, :], in_=x1r[s][0:rr, :], func=AF.Square, accum_out=st3[s][0:rr, 0:1]), reads=[('x1r', s)], writes=[('st3', s), 'junk3'])
                c.op('act', lambda s=s, rr=rr: nc.scalar.activation(out=st3[s][0:rr, 1:2], in_=st3[s][0:rr, 0:1], func=AF.Sqrt, scale=1.0 / D, bias=epsb[0:rr, :]), reads=[('st3', s), 'epsb'], writes=[('st3', s)])
                c.op('dve', lambda s=s, rr=rr: V.reciprocal(out=st3[s][0:rr, 2:3], in_=st3[s][0:rr, 1:2]), reads=[('st3', s)], writes=[('st3', s)])
                c.op('dve', lambda s=s, rr=rr: V.scalar_tensor_tensor(out=y0[s][0:rr, :], in0=x1r[s][0:rr, :], scalar=st3[s][0:rr, 2:3], in1=gfin[0:rr, :], op0=ALU.mult, op1=ALU.mult),
                     reads=[('x1r', s), ('st3', s), 'gfin'], writes=[('y0', s)])
                dst = yp_t.ap()[i * 128:(i + 1) * 128, :] if i < NT - 1 else ys_t.ap()
                c.dma('act', lambda s=s, rr=rr, dst=dst: nc.scalar.dma_start(out=dst, in_=y0[s][0:rr, :]), reads=[('y0', s)], writes=[('yout', i)])
        c.finish()
    return nc


def build_two_pass():
    nc1 = build_nc(None)
    needed = set(nc1._mk_ctx.record)
    return build_nc(needed)


def _prep_inputs(inp):
    f = lambda a: np.ascontiguousarray(a, dtype=np.float32)
    ohw, vw, sel, bd = _structure_constants()
    shared = {
        "rel_bias": f(inp["rel_bias"]), "norm_mix_g": f(inp["norm_mix_g"][0][None]), "norm_ffn_g": f(inp["norm_ffn_g"][0][None]),
        "norm_final_g": f(inp["norm_final_g"][None]), "w_mod": f(inp["w_mod"][0]), "b_mod": f(inp["b_mod"][0][None]),
        "w_in": f(inp["w_in"][0]), "dw_w": f(inp["dw_w"][0]), "dw_b": f(inp["dw_b"][0][None]), "ln_g": f(inp["ln_conv_g"][0][None]),
        "ln_b": f(inp["ln_conv_b"][0][None]), "w_conv_out": f(inp["w_conv_out"][0]), "w_attn_out": f(inp["w_attn_out"][0]),
        "w_out": f(inp["w_out"][0]),
        "w_rt": f(np.concatenate([inp["w_router_group"][0], inp["w_router_expert"][0].reshape(D, 32)], axis=1)),
        "b_rt": f(np.concatenate([inp["b_router_group"][0], inp["b_router_expert"][0].reshape(32)])[None]),
        "w_eg": f(inp["w_exp_gate"][0]), "w_eu": f(inp["w_exp_up"][0]), "w_ed": f(inp["w_exp_down"][0]),
        "ohw": ohw, "vw": vw, "sel": sel, "bd": bd,
    }
    maps = []
    for cid in range(NCORE):
        b, half = cid // 2, cid % 2
        xp = np.zeros((NEXT, D), np.float32)
        xp[HALO:] = inp["x_prompt"][b, half * NOWN:(half + 1) * NOWN]
        if half == 1:
            xp[:HALO] = inp["x_prompt"][b, NOWN - HALO:NOWN]
        sl = slice(cid * NSQ, (cid + 1) * NSQ)
        m = dict(shared)
        m["xp"] = xp
        m["xs"] = f(inp["x_sample"][sl].reshape(NS, D))
        m["cmod"] = f(np.concatenate([inp["c_prompt"][b][None], inp["c_sample"][sl]], axis=0))
        m["hv"] = np.full((128, 1), float(half), np.float32)
        m["ck128"] = f(inp["cache_kv_w128"][0, sl].reshape(NSQ, 128, 512))
        m["ck512"] = f(inp["cache_kv_w512"][0, sl].reshape(NSQ, 512, 512))
        m["ck2048"] = f(inp["cache_kv_w2048"][0, sl].reshape(NSQ, 2048, 512))
        m["sconv"] = f(inp["state_conv"][0, sl])
        maps.append(m)
    return maps


_NC_CACHE = {}


def kernel(**inp):
    import time as _t
    t0 = _t.time()
    maps = _prep_inputs(inp)
    t1 = _t.time()
    if "nc" not in _NC_CACHE:
        _NC_CACHE["nc"] = build_two_pass()
    nc = _NC_CACHE["nc"]
    t2 = _t.time()
    if STOP_AFTER is not None:
        for m in maps:
            for k in ("w_eg", "w_eu", "w_ed"):
                m.pop(k, None)
    res = run_bass_kernel_spmd(nc, maps, core_ids=list(range(NCORE)))
    print("[kernel] prep %.1fs build %.1fs run %.1fs" % (t1 - t0, t2 - t1, _t.time() - t2), flush=True)
    R = res.results
    _NC_CACHE['last'] = R
    B = 4
    yp = np.zeros((B, 8192, D), np.float32); ys = np.zeros((128, 4, D), np.float32)
    kvp = [np.zeros((1, B, w, 2, 4, 64), np.float32) for (w, _) in GROUPS]
    convp = np.zeros((1, B, 30, 512), np.float32)
    kvs = [np.zeros((1, 128, w, 2, 4, 64), np.float32) for (w, _) in GROUPS]
    convs = np.zeros((1, 128, 30, 512), np.float32)
    for cid in range(NCORE):
        b, half = cid // 2, cid % 2
        r = R[cid]
        sl = slice(cid * NSQ, (cid + 1) * NSQ)
        if "yp" in r:
            yp[b, half * NOWN:(half + 1) * NOWN] = r["yp"]
            ys[sl] = r["ys"].reshape(NSQ, 4, D)
        for gi, (w, _) in enumerate(GROUPS):
            if half == 1:
                kvp[gi][0, b] = r["kvp%d" % w].reshape(w, 2, 4, 64)
            kvs[gi][0, sl] = r["kvs%d" % w].reshape(NSQ, w, 2, 4, 64)
        if half == 1:
            convp[0, b] = r["convp"]
        convs[0, sl] = r["convs"]
    return (yp, ys, kvp[0], kvp[1], kvp[2], convp, kvs[0], kvs[1], kvs[2], convs)
```

```python
import math
import numpy as np
from contextlib import ExitStack
import concourse.bass as bass
import concourse.mybir as mybir
from concourse.bass_utils import run_bass_kernel_spmd

F32 = mybir.dt.float32
BF16 = mybir.dt.bfloat16
I32 = mybir.dt.int32
U32 = mybir.dt.uint32
AF = mybir.ActivationFunctionType
ALU = mybir.AluOpType
AX = mybir.AxisListType

D = 1024
NCORE = 8
HALO = 2048
NOWN = 4096
NEXT = HALO + NOWN
NSQ = 16
NS = 64
NTOK = NEXT + NS
NOT = NOWN + NS
NTILE = 33
GROUPS = ((128, 1), (512, 4), (2048, 16))
EPS = 1e-6
NEXP = 32
BLK = 512
SUBB = BLK // 128
NBLK = (2 * NOT) // BLK + NEXP
CAP = NBLK * BLK
STOP_AFTER = None
DEBUG_SCR = False


class Ctx:
    KD = 8

    def __init__(self, nc, es, needed=None):
        self.nc = nc
        self.es = es
        self.needed = needed
        self.record = set()
        self.iidx = {e: 0 for e in ('pe', 'act', 'dve', 'pool')}
        self.eng = {'pe': nc.tensor, 'act': nc.scalar, 'dve': nc.vector, 'pool': nc.gpsimd, 'sp': nc.sync}
        self.csem = {e: es.enter_context(nc.semaphore('c_' + e)) for e in ('pe', 'act', 'dve', 'pool')}
        self.ccnt = {e: 0 for e in self.csem}
        self.dsem = {q: [es.enter_context(nc.semaphore('d_%s%d' % (q, i))) for i in range(self.KD)]
                     for q in ('sp', 'act', 'pool')}
        self.dcnt = {q: 0 for q in self.dsem}
        self.waited = {e: {} for e in self.eng}
        self.state = {}
        self.sbn = 0

    def sb(self, shape, dt, es=None):
        self.sbn += 1
        return (es or self.es).enter_context(self.nc.sbuf_tensor('sb%d' % self.sbn, list(shape), dt))

    def ps(self, shape, dt):
        self.sbn += 1
        return self.es.enter_context(self.nc.psum_tensor('ps%d' % self.sbn, list(shape), dt))

    def _wait(self, e, evs):
        best = {}
        for (sem, v, src) in evs:
            k = id(sem)
            if k not in best or best[k][1] < v:
                best[k] = (sem, v)
        for k, (sem, v) in best.items():
            if self.waited[e].get(k, 0) >= v:
                continue
            self.eng[e].wait_ge(sem, v)
            self.waited[e][k] = v
            if self.needed is None:
                for ce, cs in self.csem.items():
                    if cs is sem:
                        self.record.add((ce, v))

    def _deps(self, e, reads, writes):
        evs = []
        for k in reads:
            st = self.state.get(k)
            if st and st['w'] is not None:
                evs.append(st['w'])
        for k in writes:
            st = self.state.get(k)
            if st:
                if st['w'] is not None and (st['w'][2] != e or e != 'pe'):
                    evs.append(st['w'])
                for r in st['r']:
                    if r[2] != e or e != 'pe':
                        evs.append(r)
        return evs

    def _commit(self, ev, reads, writes):
        for k in reads:
            st = self.state.setdefault(k, {'w': None, 'r': []})
            st['r'] = [r for r in st['r'] if r[0] is not ev[0]] + [ev]
        for k in writes:
            self.state[k] = {'w': ev, 'r': []}

    @staticmethod
    def _psx(reads, writes):
        ps = [k for k in reads if k == 'pb7' or (isinstance(k, tuple) and k[0] == 'pb')]
        if not ps:
            return list(reads), list(writes)
        return [k for k in reads if k not in ps], list(writes) + [k for k in ps if k not in writes]

    def op(self, e, fn, reads=(), writes=()):
        reads, writes = self._psx(reads, writes)
        self._wait(e, self._deps(e, reads, writes))
        ins = fn()
        self.iidx[e] += 1
        if self.needed is None or (e, self.iidx[e]) in self.needed:
            self.ccnt[e] += 1
            ins.then_inc(self.csem[e], 1)
        ev = (self.csem[e], self.ccnt[e], e)
        self._commit(ev, reads, writes)
        return ev

    def dma(self, q, fn, reads=(), writes=()):
        j = self.dcnt[q]
        sem = self.dsem[q][j % self.KD]
        evs = self._deps(None, reads, writes)
        if j >= self.KD:
            evs.append((sem, 16 * (j // self.KD), 'dma_' + q))
        self._wait(q, evs)
        ins = fn()
        ins.then_inc(sem, 16)
        self.dcnt[q] += 1
        ev = (sem, 16 * (j // self.KD + 1), 'dma_' + q)
        self._commit(ev, reads, writes)
        return ev

    def wait_keys(self, e, keys):
        self._wait(e, self._deps(None, keys, ()))

    def barrier(self):
        evs = []
        for q in self.dsem:
            for i, sem in enumerate(self.dsem[q]):
                n = (self.dcnt[q] - i + self.KD - 1) // self.KD
                if n > 0:
                    evs.append((sem, 16 * n, 'x'))
        for e in self.csem:
            if self.ccnt[e]:
                evs.append((self.csem[e], self.ccnt[e], 'x'))
        for e in self.eng:
            self._wait(e, evs)

    def finish(self):
        evs = []
        for q in self.dsem:
            for i, sem in enumerate(self.dsem[q]):
                n = (self.dcnt[q] - i + self.KD - 1) // self.KD
                if n > 0:
                    evs.append((sem, 16 * n, 'x'))
        for e in self.csem:
            if self.ccnt[e]:
                evs.append((self.csem[e], self.ccnt[e], 'x'))
        self._wait('sp', evs)


def _t5_bucket_np(dist):
    dist = np.asarray(dist, np.int64)
    max_exact = 16
    d_f = np.maximum(dist, 1).astype(np.float32)
    large = max_exact + (np.log(d_f / np.float32(max_exact)) / np.float32(math.log(2048 / max_exact))
                         * np.float32(32 - max_exact)).astype(np.int32)
    large = np.minimum(large, 31)
    return np.where(dist < max_exact, dist, large)


def _structure_constants():
    ohw = np.zeros((32, 3 * 510), np.float32)
    vw = np.zeros((4, 3 * 510), np.float32)
    for g, (win, dil) in enumerate(GROUPS):
        for blk in range(2):
            for u in range(255):
                rel = u + 1 if blk == 0 else u - 127
                ok = (rel <= 128) if blk == 0 else (rel >= 0)
                if ok:
                    b = int(_t5_bucket_np(rel * dil))
                    ohw[b, g * 510 + blk * 255 + u] = 1.0
                    vw[:, g * 510 + blk * 255 + u] = 1.0
    sel = np.zeros((17, 192), np.float32)
    sel[0, 0:128] = 1.0
    for t in range(64):
        sel[1 + t // 4, 128 + t] = 1.0
    bd = np.zeros((64, 128), np.float32)
    for k in range(64):
        for q in range(64):
            if k // 4 == q // 4:
                bd[k, q] = 1.0
        bd[k, 64 + k] = 1.0
    return ohw, vw, sel, bd


def _os_env(k):
    import os
    return os.environ.get(k)


def build_nc(needed=None):
    nc = bass.Bass("TRN2", target_bir_lowering=False)

    def din(name, shape, dt=F32):
        return nc.dram_tensor(name, list(shape), dt, kind="ExternalInput")

    def dout(name, shape, dt=F32):
        return nc.dram_tensor(name, list(shape), dt, kind="ExternalOutput")

    def dscr(name, shape, dt=F32):
        return nc.dram_tensor(name, list(shape), dt, kind="ExternalOutput" if DEBUG_SCR else "Internal")

    xp_t = din("xp", [NEXT, D]); xs_t = din("xs", [NS, D]); cmod_t = din("cmod", [17, D]); hv_t = din("hv", [128, 1])
    ck_t = [din("ck%d" % w, [NSQ, w, 512]) for (w, _) in GROUPS]
    sconv_t = din("sconv", [NSQ, 30, 512])
    relb_t = din("rel_bias", [32, 12])
    gmix_t = din("norm_mix_g", [1, D]); gffn_t = din("norm_ffn_g", [1, D]); gfin_t = din("norm_final_g", [1, D])
    wmod_t = din("w_mod", [D, 6 * D]); bmod_t = din("b_mod", [1, 6 * D])
    win_t = din("w_in", [D, 5376])
    dww_t = din("dw_w", [31, 512]); dwb_t = din("dw_b", [1, 512]); lng_t = din("ln_g", [1, 512]); lnb_t = din("ln_b", [1, 512])
    wco_t = din("w_conv_out", [512, D]); wao_t = din("w_attn_out", [256, D]); wo_t = din("w_out", [D, D])
    wrt_t = din("w_rt", [D, 36]); brt_t = din("b_rt", [1, 36])
    if STOP_AFTER is None:
        weg_t = din("w_eg", [NEXP, D, 512]); weu_t = din("w_eu", [NEXP, D, 512]); wed_t = din("w_ed", [NEXP, 512, D])
    ohw_t = din("ohw", [32, 1530]); vw_t = din("vw", [4, 1530]); sel_t = din("sel", [17, 192]); bd_t = din("bd", [64, 128])

    yp_t = dout("yp", [NOWN, D]); ys_t = dout("ys", [NS, D])
    kvp_t = [dout("kvp%d" % w, [w, 512]) for (w, _) in GROUPS]
    convp_t = dout("convp", [30, 512])
    kvs_t = [dout("kvs%d" % w, [NSQ, w, 512]) for (w, _) in GROUPS]
    convs_t = dout("convs", [NSQ, 30, 512])

    modrows_t = dscr("modrows", [4, 192, D])
    wd_t = dscr("wdscr", [3, 4, 510])
    ebd_t = dscr("ebd", [3, 128, 1024])
    sg_t = dscr("sgscr", [16, 128, NOT], BF16)
    yts_t = dscr("ytscr", [4, 128, NOT], BF16)
    acc_t = dscr("accscr", [3, NOWN, 260])
    accs_t = dscr("accsscr", [NS, 260])
    x1_t = dscr("x1scr", [NOT, D])
    xsd_t = dscr("xsdisp", [CAP, D], BF16)
    ysd_t = dscr("ysdisp", [CAP, D])

    xp = xp_t.ap(); xs = xs_t.ap(); win = win_t.ap()

    with ExitStack() as es:
        c = Ctx(nc, es, needed)
        nc._mk_ctx = c
        PB = [c.ps([128, 512], F32) for _ in range(8)]

        def pbf(i):
            return PB[i][:].bitcast(BF16)

        identf = c.sb([128, 128], F32); ident = c.sb([128, 128], BF16)
        onesb = c.sb([128, 128], BF16); onesf = c.sb([128, 128], F32)
        suf = c.sb([128, 128], F32); jf = c.sb([128, 128], F32)
        epsb = c.sb([128, 1], F32); hv = c.sb([128, 1], F32); one1 = c.sb([128, 1], F32)
        c.op('pool', lambda: nc.gpsimd.memset(onesf[:], 1.0), writes=['onesf'])
        c.op('pool', lambda: nc.gpsimd.memset(onesb[:], 1.0), writes=['onesb'])
        c.op('pool', lambda: nc.gpsimd.memset(epsb[:], EPS), writes=['epsb'])
        c.op('pool', lambda: nc.gpsimd.memset(one1[:], 1.0), writes=['one1'])
        c.op('pool', lambda: nc.gpsimd.affine_select(out=identf[:], in_=onesf[:], pattern=[[-1, 128]], compare_op=ALU.is_equal,
                                                       fill=0.0, base=0, channel_multiplier=1), reads=['onesf'], writes=['identf'])
        c.op('pool', lambda: nc.gpsimd.affine_select(out=jf[:], in_=onesf[:], pattern=[[1, 128]], compare_op=ALU.is_equal,
                                                       fill=0.0, base=-127, channel_multiplier=1), reads=['onesf'], writes=['jf'])
        c.op('pool', lambda: nc.gpsimd.affine_select(out=suf[:], in_=onesf[:], pattern=[[1, 128]], compare_op=ALU.is_gt,
                                                       fill=0.0, base=0, channel_multiplier=-1), reads=['onesf'], writes=['suf'])
        c.op('dve', lambda: nc.vector.tensor_copy(out=ident[:], in_=identf[:]), reads=['identf'], writes=['ident'])
        c.dma('sp', lambda: nc.sync.dma_start(out=hv[:], in_=hv_t.ap()), writes=['hv'])


        es_hT = ExitStack()
        es.enter_context(es_hT)
        hT = c.sb([128, 8, NTOK], BF16, es_hT)
        es_mod = ExitStack()
        a1p = c.sb([128, D], F32, es_mod); b1p = c.sb([128, D], F32, es_mod); a1s = c.sb([64, D], F32, es_mod); b1s = c.sb([64, D], F32, es_mod)
        es0 = ExitStack()
        with es0:
            cm = c.sb([17, D], F32, es0); scm = c.sb([17, D], F32, es0); scT = c.sb([128, 8, 17], F32, es0)
            mtok = c.sb([17, 6 * D], F32, es0); selm = c.sb([17, 192], F32, es0)
            gmb = c.sb([128, D], F32, es0); gfb = c.sb([128, D], F32, es0)
            c.dma('sp', lambda: nc.sync.dma_start(out=cm[:], in_=cmod_t.ap()), writes=['cm'])
            c.dma('sp', lambda: nc.sync.dma_start(out=selm[:], in_=sel_t.ap()), writes=['selm'])
            c.dma('sp', lambda: nc.sync.dma_start(out=gmb[:], in_=gmix_t.ap().partition_broadcast(128)), writes=['gmb'])
            c.dma('sp', lambda: nc.sync.dma_start(out=gfb[:], in_=gffn_t.ap().partition_broadcast(128)), writes=['gfb'])
            c.op('act', lambda: nc.scalar.activation(out=scm[:], in_=cm[:], func=AF.Silu), reads=['cm'], writes=['scm'])
            for k in range(8):
                c.op('pe', lambda k=k: nc.tensor.transpose(out=PB[0][:, k * 17:(k + 1) * 17], in_=scm[0:17, k * 128:(k + 1) * 128],
                                                           identity=identf[0:17, 0:17]), reads=['scm', 'identf'], writes=['pb0'])
            c.op('dve', lambda: nc.vector.tensor_copy(out=scT[:].rearrange("p k s -> p (k s)"), in_=PB[0][:, 0:136]), reads=['pb0'], writes=['scT'])
            wmod = wmod_t.ap().rearrange("(k p) n -> p k n", p=128)
            wbs = [c.sb([128, 8, 512], F32, es0) for _ in range(2)]
            bbs = [c.sb([17, 512], F32, es0) for _ in range(2)]
            for nb in range(12):
                wb = wbs[nb % 2]
                bb = bbs[nb % 2]
                c.dma('sp', lambda wb=wb, nb=nb: nc.sync.dma_start(out=wb[:], in_=wmod[:, :, nb * 512:(nb + 1) * 512]), writes=[('wb', nb % 2)])
                c.dma('sp', lambda bb=bb, nb=nb: nc.sync.dma_start(out=bb[:], in_=bmod_t.ap()[:, nb * 512:(nb + 1) * 512].partition_broadcast(17)),
                      writes=[('bb', nb % 2)])
                pbk = 1 + nb % 2
                for k in range(8):
                    c.op('pe', lambda wb=wb, k=k, pbk=pbk: nc.tensor.matmul(PB[pbk][0:17, 0:512], lhsT=scT[:, k, :], rhs=wb[:, k, :], start=(k == 0), stop=(k == 7)),
                         reads=['scT', ('wb', nb % 2)], writes=[('pb', pbk)])
                c.op('dve', lambda bb=bb, nb=nb, pbk=pbk: nc.vector.tensor_tensor(out=mtok[:, nb * 512:(nb + 1) * 512], in0=PB[pbk][0:17, 0:512], in1=bb[:], op=ALU.add),
                     reads=[('pb', pbk), ('bb', nb % 2)], writes=['mtok'])
            rowst = [c.sb([128, D], F32, es0) for _ in range(2)]
            for kind in range(6):
                for part, (c0, n) in enumerate(((0, 128), (128, 64))):
                    rt = rowst[(kind * 2 + part) % 2]
                    rk = ('rowst', (kind * 2 + part) % 2)
                    for hf in range(2):
                        c.op('pe', lambda hf=hf, c0=c0, n=n, kind=kind: nc.tensor.matmul(PB[3 + hf][0:n, :], lhsT=selm[0:17, c0:c0 + n],
                             rhs=mtok[0:17, kind * D + hf * 512: kind * D + hf * 512 + 512], start=True, stop=True),
                             reads=['selm', 'mtok'], writes=[('pb', 3 + hf)])
                    dst = None; dk = 'nokey'
                    if kind == 0:
                        dst = (b1p, b1s)[part]; dk = ('b1', part)
                    elif kind == 1:
                        dst = (a1p, a1s)[part]; dk = ('a1', part)
                    for hf in range(2):
                        sl = slice(hf * 512, hf * 512 + 512)
                        if kind in (1, 4):
                            gb = gmb if kind == 1 else gfb
                            tgt = dst if dst is not None else rt
                            c.op('dve', lambda hf=hf, n=n, gb=gb, tgt=tgt, sl=sl: nc.vector.scalar_tensor_tensor(out=tgt[0:n, sl], in0=PB[3 + hf][0:n, :], scalar=1.0,
                                 in1=gb[0:n, sl], op0=ALU.add, op1=ALU.mult), reads=[('pb', 3 + hf), 'gmb', 'gfb'], writes=[rk, dk])
                        else:
                            tgt = dst if dst is not None else rt
                            c.op('act', lambda hf=hf, n=n, tgt=tgt, sl=sl: nc.scalar.copy(out=tgt[0:n, sl], in_=PB[3 + hf][0:n, :]),
                                 reads=[('pb', 3 + hf)], writes=[rk, dk])
                    if kind >= 2:
                        c.dma('sp', lambda rt=rt, c0=c0, n=n, kind=kind: nc.sync.dma_start(out=modrows_t.ap()[kind - 2, c0:c0 + n, :], in_=rt[0:n, :]),
                              reads=[rk], writes=[('modrows', kind - 2, part)])
        c.barrier()
        es0 = ExitStack()
        with es0:
            rb = c.sb([32, 12], F32, es0); ohw = c.sb([32, 1530], F32, es0); vw = c.sb([4, 1530], F32, es0)
            wsb = c.sb([4, 1530], F32, es0); hall = c.sb([128, 24, 128], F32, es0); ebst = c.sb([128, 3, 1024], F32, es0)
            c.dma('sp', lambda: nc.sync.dma_start(out=rb[:], in_=relb_t.ap()), writes=['rb'])
            c.dma('sp', lambda: nc.sync.dma_start(out=ohw[:], in_=ohw_t.ap()), writes=['ohw'])
            c.dma('sp', lambda: nc.sync.dma_start(out=vw[:], in_=vw_t.ap()), writes=['vw'])
            for g in range(3):
                c.op('pe', lambda g=g: nc.tensor.matmul(PB[5][0:4, 0:510], lhsT=rb[:, 4 * g:4 * g + 4], rhs=ohw[:, g * 510:(g + 1) * 510], start=True, stop=True),
                     reads=['rb', 'ohw'], writes=[('pb', 5)])
                c.op('act', lambda g=g: nc.scalar.activation(out=wsb[:, g * 510:(g + 1) * 510], in_=PB[5][0:4, 0:510], func=AF.Exp), reads=[('pb', 5)], writes=['wsb'])
            c.op('dve', lambda: nc.vector.tensor_tensor(out=wsb[:], in0=wsb[:], in1=vw[:], op=ALU.mult), reads=['wsb', 'vw'], writes=['wsb'])
            c.dma('sp', lambda: nc.sync.dma_start(out=wd_t.ap().rearrange("g h u -> h g u"), in_=wsb[:].rearrange("h (g u) -> h g u", g=3)), reads=['wsb'], writes=['wd'])
            for g in range(3):
                for h in range(4):
                    for blk in range(2):
                        idx = (g * 4 + h) * 2 + blk
                        src = bass.AP(wd_t, (g * 4 + h) * 510 + blk * 255, [[1, 128], [1, 128]])
                        c.dma('sp', lambda idx=idx, src=src: nc.sync.dma_start(out=hall[:, idx, :], in_=src), reads=['wd'], writes=[('hall', idx)])
            for g in range(3):
                for hf in range(2):
                    c.op('pe', lambda g=g, hf=hf: nc.tensor.matmul(PB[6 + hf][:, :], lhsT=jf[:], rhs=hall[:, g * 8 + hf * 4: g * 8 + hf * 4 + 4, :].rearrange("p a q -> p (a q)"),
                         start=True, stop=True), reads=['jf'] + [('hall', g * 8 + hf * 4 + i) for i in range(4)], writes=[('pb', 6 + hf)])
                    c.op('act', lambda g=g, hf=hf: nc.scalar.copy(out=ebst[:, g, hf * 512:(hf + 1) * 512], in_=PB[6 + hf][:, :]), reads=[('pb', 6 + hf)], writes=[('ebst', g)])
                c.dma('sp', lambda g=g: nc.sync.dma_start(out=ebd_t.ap()[g], in_=ebst[:, g, :]), reads=[('ebst', g)], writes=[('ebd', g)])

        c.barrier()
        def norm_to_T(tidx, src_ap, n, arow, brow, akey, bkey, bufs):
            xt, t1, hb, stt, junk = bufs
            s = tidx % 2
            c.dma('sp', lambda: nc.sync.dma_start(out=xt[s][0:n, :], in_=src_ap), writes=[('xt', s)])
            c.op('act', lambda: nc.scalar.activation(out=junk[0:n, :], in_=xt[s][0:n, :], func=AF.Square, accum_out=stt[s][0:n, 0:1]),
                 reads=[('xt', s)], writes=[('stt', s), 'junk'])
            c.op('act', lambda: nc.scalar.activation(out=stt[s][0:n, 1:2], in_=stt[s][0:n, 0:1], func=AF.Sqrt, scale=1.0 / D, bias=epsb[0:n, :]),
                 reads=[('stt', s), 'epsb'], writes=[('stt', s)])
            c.op('dve', lambda: nc.vector.reciprocal(out=stt[s][0:n, 2:3], in_=stt[s][0:n, 1:2]), reads=[('stt', s)], writes=[('stt', s)])
            c.op('dve', lambda: nc.vector.scalar_tensor_tensor(out=t1[s][0:n, :], in0=xt[s][0:n, :], scalar=stt[s][0:n, 2:3], in1=arow[0:n, :],
                                                                op0=ALU.mult, op1=ALU.mult), reads=[('xt', s), ('stt', s), akey], writes=[('t1', s)])
            c.op('pool', lambda: nc.gpsimd.tensor_tensor(out=hb[s][0:n, :], in0=t1[s][0:n, :], in1=brow[0:n, :], op=ALU.add),
                 reads=[('t1', s), bkey], writes=[('hb', s)])
            pv = pbf(s).rearrange("p (k t) -> p k t", k=8)
            for k in range(8):
                c.op('pe', lambda k=k: nc.tensor.transpose(out=pv[:, k, 0:n], in_=hb[s][0:n, k * 128:(k + 1) * 128], identity=ident[0:n, 0:n]),
                     reads=[('hb', s), 'ident'], writes=[('pb', s)])
            return s, pv

        esA = ExitStack()
        with esA:
            xt = [c.sb([128, D], F32, esA) for _ in range(2)]; t1 = [c.sb([128, D], F32, esA) for _ in range(2)]
            hb = [c.sb([128, D], BF16, esA) for _ in range(2)]; stt = [c.sb([128, 4], F32, esA) for _ in range(2)]
            junk = c.sb([128, D], BF16, esA)
            bufsA = (xt, t1, hb, stt, junk)
            for t in range(49):
                if t < 48:
                    n = 128; src = xp[t * 128:(t + 1) * 128, :]; ar, br = a1p, b1p; col = t * 128; pk = 0
                else:
                    n = 64; src = xs; ar, br = a1s, b1s; col = NEXT; pk = 1
                s, pv = norm_to_T(t, src, n, ar, br, ('a1', pk), ('b1', pk), bufsA)
                c.op('act', lambda pv=pv, col=col, n=n: nc.scalar.copy(out=hT[:, :, col:col + n], in_=pv[:, :, 0:n]), reads=[('pb', s)], writes=['hT'])
        c.barrier()
        es_mod.close()

        winr = win.rearrange("(k p) n -> p k n", p=128)

        def load_w(dst, c0, ncol, key):
            c.dma('pool', lambda: nc.gpsimd.dma_start(out=dst, in_=winr[:, :, c0:c0 + ncol]), writes=[key])

        def proj_T(ps_ap, pkey, wt, wkey, hcols):
            for k in range(8):
                c.op('pe', lambda k=k: nc.tensor.matmul(ps_ap, lhsT=wt[:, k, :], rhs=hT[:, k, hcols], start=(k == 0), stop=(k == 7)),
                     reads=['hT', wkey], writes=[pkey])

        def psv(i, sl=slice(None), n=512):
            return PB[i][sl, 0:n]

        OT = [(HALO + 512 * m, 512 * m, 512) for m in range(8)] + [(NEXT, NOWN, NS)]

        for g, (wing, d) in enumerate(GROUPS):
            nsp = 4 if g == 2 else 1
            for q4 in range(nsp):
                bs = slice(q4 * (NSQ // nsp), (q4 + 1) * (NSQ // nsp))
                A_ = (1, 4, 28)[g]
                c.dma('act', lambda g=g, wing=wing, bs=bs, A_=A_: nc.scalar.dma_start(out=kvs_t[g].ap()[bs, 0:wing - 4, :].rearrange("b (a r) c -> b a (r c)", a=A_),
                      in_=ck_t[g].ap()[bs, 4:wing, :].rearrange("b (a r) c -> b a (r c)", a=A_)), writes=[('kvs_shift', g, q4)])
        esG = ExitStack()
        with esG:
            wj = [c.sb([128, 8, 128], BF16, esG) for _ in range(2)]
            sgrow = [c.sb([128, NOT], BF16, esG) for _ in range(2)]
            for j in range(16):
                s = j % 2
                load_w(wj[s][:], 3328 + j * 128, 128, ('wj', s))
                for m, (hc, oi, n) in enumerate(OT):
                    pi = m % 2
                    pa = psv(pi, n=n)
                    proj_T(pa, ('pb', pi), wj[s], ('wj', s), slice(hc, hc + n))
                    c.op('act', lambda pa=pa, oi=oi, n=n, s=s: nc.scalar.activation(out=sgrow[s][:, oi:oi + n], in_=pa, func=AF.Sigmoid),
                         reads=[('pb', pi)], writes=[('sgrow', s)])
                c.dma('sp', lambda j=j, s=s: nc.sync.dma_start(out=sg_t.ap()[j], in_=sgrow[s][:]), reads=[('sgrow', s)], writes=[('sg', j)])

        c.barrier()
        if STOP_AFTER == 'A':
            c.finish()
            return nc

        esC = ExitStack()
        with esC:
            yT = c.sb([128, 4, NOT], BF16, esC)
            dwT = c.sb([128, 4, 31], F32, esC); dwb = c.sb([128, 4], F32, esC); lng = c.sb([128, 4], F32, esC); lnb = c.sb([128, 4], F32, esC)
            with nc.allow_non_contiguous_dma(reason="tiny per-channel parameter loads"):
                for cc in range(4):
                    c.dma('sp', lambda cc=cc: nc.sync.dma_start(out=dwT[:, cc, :], in_=dww_t.ap()[:, cc * 128:(cc + 1) * 128].rearrange("j p -> p j")), writes=[('dwT', cc)])
                c.dma('sp', lambda: nc.sync.dma_start(out=dwb[:], in_=dwb_t.ap().rearrange("o (c p) -> p (o c)", p=128)), writes=['dwb'])
                c.dma('sp', lambda: nc.sync.dma_start(out=lng[:], in_=lng_t.ap().rearrange("o (c p) -> p (o c)", p=128)), writes=['lng'])
                c.dma('sp', lambda: nc.sync.dma_start(out=lnb[:], in_=lnb_t.ap().rearrange("o (c p) -> p (o c)", p=128)), writes=['lnb'])
            uxT = c.sb([128, 4, NSQ, 34], BF16, esC)
            uTf = c.sb([128, 4, 94], F32, esC)
            sct = [c.sb([120, 512], F32, esC)] * 2
            for i4 in range(4):
                s = i4 % 2
                c.dma('sp', lambda i4=i4, s=s: nc.sync.dma_start(out=sct[s][:], in_=sconv_t.ap()[4 * i4:4 * i4 + 4].rearrange("b t c -> (b t) c")), writes=[('sct', 0)])
                for cc in range(4):
                    c.op('pe', lambda cc=cc, s=s: nc.tensor.transpose(out=PB[2][:, 0:120], in_=sct[s][:, cc * 128:(cc + 1) * 128], identity=identf[0:120, 0:120]),
                         reads=[('sct', 0), 'identf'], writes=[('pb', 2)])
                    c.op('act', lambda cc=cc, i4=i4: nc.scalar.copy(out=uxT[:, cc, 4 * i4:4 * i4 + 4, 0:30], in_=PB[2][:, 0:120].rearrange("p (b t) -> p b t", b=4)),
                         reads=[('pb', 2)], writes=['uxT'])
            c.dma('sp', lambda: nc.sync.dma_start(out=convs_t.ap()[:, 0:26, :], in_=sconv_t.ap()[:, 4:30, :]), writes=['convs_a'])
            esC1 = ExitStack()
            wul = [c.sb([128, 8, 128], BF16, esC1) for _ in range(2)]; wug = [c.sb([128, 8, 128], BF16, esC1) for _ in range(2)]
            diag = [c.sb([128, 31, 128], BF16, esC1)] * 2
            ucT = [c.sb([128, 30 + NOWN], BF16, esC1)] * 2
            sgt = [c.sb([128, 512], F32, esC1) for _ in range(2)]
            UT = [(HALO - 30, -30, 30)] + OT
            for cc in range(4):
                s = cc % 2
                load_w(wul[s][:], 2304 + cc * 128, 128, ('wul', s))
                load_w(wug[s][:], 2816 + cc * 128, 128, ('wug', s))
                for j in range(31):
                    eng = 'dve' if j % 2 == 0 else 'pool'
                    e_ = nc.vector if eng == 'dve' else nc.gpsimd
                    c.op(eng, lambda j=j, e_=e_: e_.tensor_scalar(out=diag[s][:, j, :], in0=identf[:], scalar1=dwT[:, cc, j:j + 1], scalar2=None, op0=ALU.mult),
                         reads=['identf', ('dwT', cc)], writes=[('diag', 0, j)])
                for m, (hc, oi, n) in enumerate(UT):
                    pl = psv(0, n=n); pg = psv(1, n=n)
                    proj_T(pl, ('pb', 0), wul[s], ('wul', s), slice(hc, hc + n))
                    proj_T(pg, ('pb', 1), wug[s], ('wug', s), slice(hc, hc + n))
                    b_ = m % 2
                    c.op('act', lambda pg=pg, n=n, b_=b_: nc.scalar.activation(out=sgt[b_][:, 0:n], in_=pg, func=AF.Sigmoid), reads=[('pb', 1)], writes=[('sgt', b_)])
                    if oi < 0:
                        c.op('dve', lambda pl=pl, n=n, b_=b_: nc.vector.tensor_tensor(out=sgt[b_][:, 0:n], in0=pl, in1=sgt[b_][:, 0:n], op=ALU.mult),
                             reads=[('pb', 0), ('sgt', b_)], writes=[('sgt', b_)])
                        c.op('dve', lambda n=n, b_=b_: nc.vector.tensor_scalar(out=ucT[s][:, 0:30], in0=sgt[b_][:, 0:n], scalar1=hv[:, 0:1], scalar2=None, op0=ALU.mult),
                             reads=[('sgt', b_), 'hv'], writes=[('ucT', 0, 0)])
                    elif oi < NOWN:
                        c.op('dve', lambda pl=pl, n=n, b_=b_, oi=oi: nc.vector.tensor_tensor(out=ucT[s][:, 30 + oi:30 + oi + n], in0=pl, in1=sgt[b_][:, 0:n], op=ALU.mult),
                             reads=[('pb', 0), ('sgt', b_)], writes=[('ucT', 0, 1 + oi // 512)])
                        if oi == NOWN - 512:
                            c.op('dve', lambda pl=pl, b_=b_: nc.vector.tensor_tensor(out=uTf[:, cc, 0:30], in0=pl[:, 482:512], in1=sgt[b_][:, 482:512], op=ALU.mult),
                                 reads=[('pb', 0), ('sgt', b_)], writes=[('uTf', cc)])
                    else:
                        c.op('dve', lambda pl=pl, b_=b_: nc.vector.tensor_tensor(out=uTf[:, cc, 30:94], in0=pl, in1=sgt[b_][:, 0:64], op=ALU.mult),
                             reads=[('pb', 0), ('sgt', b_)], writes=[('uTf', cc)])
                        c.op('dve', lambda: nc.vector.tensor_copy(out=uxT[:, cc, :, 30:34], in_=uTf[:, cc, 30:94].rearrange("p (b t) -> p b t", t=4)),
                             reads=[('uTf', cc)], writes=['uxT'])
                dkeys = [('diag', 0, j) for j in range(31)]
                for m in range(8):
                    py = psv(2 + m % 2)
                    for j in range(31):
                        c.op('pe', lambda j=j, m=m, py=py: nc.tensor.matmul(py, lhsT=diag[s][:, j, :], rhs=ucT[s][:, 512 * m + j: 512 * m + j + 512], start=(j == 0), stop=(j == 30)),
                             reads=[dkeys[j], ('ucT', 0, 0), ('ucT', 0, 1 + m), ('ucT', 0, m)], writes=[('pb', 2 + m % 2)])
                    c.op('act', lambda m=m, py=py: nc.scalar.activation(out=yT[:, cc, 512 * m:512 * m + 512], in_=py, func=AF.Identity, bias=dwb[:, cc:cc + 1], scale=1.0),
                         reads=[('pb', 2 + m % 2), 'dwb'], writes=[('yT', m)])
                pys = PB[2][:, 0:64]
                for j in range(31):
                    c.op('pe', lambda j=j: nc.tensor.matmul(pys.rearrange("p (b t) -> p b t", t=4), lhsT=diag[s][:, j, :], rhs=uxT[:, cc, :, j:j + 4], start=(j == 0), stop=(j == 30)),
                         reads=[dkeys[j], 'uxT'], writes=[('pb', 2)])
                c.op('act', lambda: nc.scalar.activation(out=yT[:, cc, NOWN:NOT], in_=pys, func=AF.Identity, bias=dwb[:, cc:cc + 1], scale=1.0),
                     reads=[('pb', 2), 'dwb'], writes=[('yT', 8)])
            c.barrier()
            esC1.close()
            cpo = c.sb([94, 512], F32, esC)
            for cc in range(4):
                c.op('pe', lambda cc=cc: nc.tensor.transpose(out=PB[0][0:94, 0:128], in_=uTf[:, cc, :], identity=identf[:]), reads=[('uTf', cc), 'identf'], writes=[('pb', 0)])
                c.op('act', lambda cc=cc: nc.scalar.copy(out=cpo[:, cc * 128:(cc + 1) * 128], in_=PB[0][0:94, 0:128]), reads=[('pb', 0)], writes=['cpo'])
            c.dma('sp', lambda: nc.sync.dma_start(out=convp_t.ap(), in_=cpo[0:30, :]), reads=['cpo'], writes=['convp'])
            for b in range(NSQ):
                c.dma('sp', lambda b=b: nc.sync.dma_start(out=convs_t.ap()[b, 26:30, :], in_=cpo[30 + 4 * b:34 + 4 * b, :]), reads=['cpo'], writes=[('convs_b', b)])
            sq = c.sb([128, 4, 512], BF16, esC); mean = c.sb([128, 512], F32, esC); msq = c.sb([128, 512], F32, esC)
            var = c.sb([128, 512], F32, esC); rstd = c.sb([128, 512], F32, esC); tt = c.sb([128, 4, 512], F32, esC)
            for m, (hc, oi, n) in enumerate(OT):
                yk = ('yT', m)
                c.op('act', lambda oi=oi, n=n: nc.scalar.activation(out=sq[:, :, 0:n], in_=yT[:, :, oi:oi + n], func=AF.Square), reads=[yk], writes=['sq'])
                p1 = psv(4, n=n); p2 = psv(5, n=n)
                for cc in range(4):
                    c.op('pe', lambda cc=cc, p1=p1, oi=oi, n=n: nc.tensor.matmul(p1, lhsT=onesb[:], rhs=yT[:, cc, oi:oi + n], start=(cc == 0), stop=(cc == 3)),
                         reads=[yk, 'onesb'], writes=[('pb', 4)])
                for cc in range(4):
                    c.op('pe', lambda cc=cc, p2=p2, n=n: nc.tensor.matmul(p2, lhsT=onesb[:], rhs=sq[:, cc, 0:n], start=(cc == 0), stop=(cc == 3)),
                         reads=['sq', 'onesb'], writes=[('pb', 5)])
                c.op('dve', lambda p1=p1, n=n: nc.vector.tensor_scalar(out=mean[:, 0:n], in0=p1, scalar1=1.0 / 512, scalar2=None, op0=ALU.mult), reads=[('pb', 4)], writes=['mean'])
                c.op('pool', lambda n=n: nc.gpsimd.tensor_tensor(out=msq[:, 0:n], in0=mean[:, 0:n], in1=mean[:, 0:n], op=ALU.mult), reads=['mean'], writes=['msq'])
                c.op('dve', lambda p2=p2, n=n: nc.vector.scalar_tensor_tensor(out=var[:, 0:n], in0=p2, scalar=1.0 / 512, in1=msq[:, 0:n], op0=ALU.mult, op1=ALU.subtract),
                     reads=[('pb', 5), 'msq'], writes=['var'])
                c.op('act', lambda n=n: nc.scalar.activation(out=var[:, 0:n], in_=var[:, 0:n], func=AF.Sqrt, scale=1.0, bias=epsb[:, :]), reads=['var', 'epsb'], writes=['var'])
                c.op('dve', lambda n=n: nc.vector.reciprocal(out=rstd[:, 0:n], in_=var[:, 0:n]), reads=['var'], writes=['rstd'])
                c.op('dve', lambda oi=oi, n=n: nc.vector.tensor_tensor(out=tt[:, :, 0:n], in0=yT[:, :, oi:oi + n], in1=mean[:, 0:n].unsqueeze(1).to_broadcast([128, 4, n]), op=ALU.subtract),
                     reads=[yk, 'mean'], writes=['tt'])
                c.op('pool', lambda n=n: nc.gpsimd.tensor_tensor(out=tt[:, :, 0:n], in0=tt[:, :, 0:n], in1=rstd[:, 0:n].unsqueeze(1).to_broadcast([128, 4, n]), op=ALU.mult),
                     reads=['tt', 'rstd'], writes=['tt'])
                for cc in range(4):
                    c.op('act', lambda cc=cc, oi=oi, n=n: nc.scalar.activation(out=yT[:, cc, oi:oi + n], in_=tt[:, cc, 0:n], func=AF.Silu, bias=lnb[:, cc:cc + 1], scale=lng[:, cc:cc + 1]),
                         reads=['tt', 'lng', 'lnb'], writes=[yk])
            for cc in range(4):
                c.dma('sp', lambda cc=cc: nc.sync.dma_start(out=yts_t.ap()[cc], in_=yT[:, cc, :]), reads=[('yT', m) for m in range(9)], writes=[('yts', cc)])

        c.barrier()
        if STOP_AFTER == 'C':
            c.finish()
            return nc

        esD = ExitStack()
        with esD:
            ebf = c.sb([128, 1024], F32, esD)
            smk = c.sb([128, 9, 4, 4], F32, esD)
            nmk = c.sb([64, 3, 4, 64], F32, esD)
            bdm = c.sb([64, 128], F32, esD)
            pz = [c.sb([128, 4, 64], BF16, esD) for _ in range(NSQ)]
            v1n = c.sb([64, 3, 4, 65], BF16, esD)
            wkv = c.sb([128, 8, 512], BF16, esD)
            wq = c.sb([128, 8, 128], BF16, esD); wk = c.sb([128, 8, 128], BF16, esD)
            qT = c.sb([128, NOT], BF16, esD); kT = c.sb([128, NTOK], BF16, esD)
            qs = c.sb([128, 2, 64], BF16, esD); ks = c.sb([128, 2, 64], BF16, esD)
            v1 = c.sb([128, 48, 4, 65], BF16, esD)
            kvst = [c.sb([128, 512], F32, esD) for _ in range(2)]
            ef = [c.sb([128, 512], F32, esD) for _ in range(2)]
            pt = [c.sb([128, 512], BF16, esD) for _ in range(2)]
            oev = [c.sb([128, 130], F32, esD) for _ in range(2)]
            ctile = [c.sb([128, 512], F32, esD) for _ in range(2)]
            kTs = [c.sb([128, 2, 128], BF16, esD) for _ in range(2)]
            v1s = [c.sb([128, 4, 65], BF16, esD) for _ in range(2)]
            ess2 = [c.sb([128, 16], F32, esD) for _ in range(2)]
            en = c.sb([64, 256], F32, esD); pn = c.sb([64, 4, 64], BF16, esD)
            osv = c.sb([64, 260], F32, esD)
            c.dma('sp', lambda: nc.sync.dma_start(out=bdm[:], in_=bd_t.ap()), writes=['bdm'])
            c.op('pool', lambda: nc.gpsimd.memset(smk[:], 0.0), writes=['smk'])
            for b in range(NSQ):
                c.op('pool', lambda b=b: nc.gpsimd.memset(pz[b][:], 0.0), writes=[('pz', b)])
            for s in range(2):
                c.op('pool', lambda s=s: nc.gpsimd.memset(v1s[s][:], 1.0), writes=[('v1s', s)])
            c.op('pool', lambda: nc.gpsimd.memset(v1n[:], 1.0), writes=['v1n'])
            n_os = 1
            zl = c.sb([128, 64], BF16, esD); zr = c.sb([128, 260], BF16, esD)
            c.op('pool', lambda: nc.gpsimd.memset(zl[:], 0.0), writes=['zl'])
            c.op('pool', lambda: nc.gpsimd.memset(zr[:], 0.0), writes=['zr'])
            c.op('pe', lambda: nc.tensor.matmul(PB[7][0:64, 0:260], lhsT=zl[:], rhs=zr[:], start=True, stop=False), reads=['zl', 'zr'], writes=['pb7'])
            qb_i = 0
            for g, (wing, d) in enumerate(GROUPS):
                e0 = HALO - wing
                nbl = (NEXT - e0) // (128 * d)
                ebv = ebf[:].rearrange("p (h b q) -> p h b q", h=4, b=2)
                c.dma('sp', lambda g=g: nc.sync.dma_start(out=ebf[:], in_=ebd_t.ap()[g]), reads=[('ebd', g)], writes=['ebf'])
                if g == 0:
                    c.op('dve', lambda: nc.vector.tensor_copy(out=smk[:, 0, :, :], in_=ebv[:, :, 0, 0:4]), reads=['ebf'], writes=['smk'])
                else:
                    for r in range(4):
                        c.op('dve', lambda r=r, g=g: nc.vector.tensor_copy(out=smk[:, 1 + 4 * (g - 1) + r, :, r:r + 1], in_=ebv[:, :, 0, 0:1]), reads=['ebf'], writes=['smk'])
                bsel = bdm[:, 0:64] if g == 0 else bdm[:, 64:128]
                c.op('dve', lambda g=g, bsel=bsel: nc.vector.tensor_tensor(out=nmk[:, g, :, :], in0=ebv[0:64, :, 1, 0:64], in1=bsel.unsqueeze(1).to_broadcast([64, 4, 64]), op=ALU.mult),
                     reads=['ebf', 'bdm'], writes=['nmk'])
                c.dma('pool', lambda g=g: nc.gpsimd.dma_start(out=wkv[:, :, 0:256], in_=winr[:, :, 768 + 256 * g: 1024 + 256 * g]), writes=['wkv_k'])
                c.dma('pool', lambda g=g: nc.gpsimd.dma_start(out=wkv[:, :, 256:512], in_=winr[:, :, 1536 + 256 * g: 1792 + 256 * g]), writes=['wkv_v'])
                nkv = 0
                for r in range(d):
                    for i in range(nbl):
                        bi = r * nbl + i
                        cs0 = e0 + r + 128 * d * i
                        hsl = slice(cs0, cs0 + 127 * d + 1, d)
                        pi = 4 + nkv % 3
                        for k in range(8):
                            c.op('pe', lambda k=k, hsl=hsl, pi=pi: nc.tensor.matmul(PB[pi][:, :], lhsT=hT[:, k, hsl], rhs=wkv[:, k, :], start=(k == 0), stop=(k == 7)),
                                 reads=['hT', 'wkv_k', 'wkv_v'], writes=[('pb', pi)])
                        vc = hv if i == 0 else one1
                        c.op('dve', lambda bi=bi, pi=pi, vc=vc: nc.vector.tensor_scalar(out=v1[:, bi, :, 0:64], in0=PB[pi][:, 256:512].rearrange("p (h e) -> p h e", h=4),
                             scalar1=vc[:, 0:1], scalar2=None, op0=ALU.mult), reads=[('pb', pi), 'hv', 'one1'], writes=[('v1', bi)])
                        c.op('pool', lambda bi=bi, vc=vc: nc.gpsimd.tensor_copy(out=v1[:, bi, :, 64:65], in_=vc[:, 0:1].unsqueeze(1).to_broadcast([128, 4, 1])),
                             reads=['hv', 'one1'], writes=[('v1o', bi)])
                        if i == nbl - 1:
                            s = nkv % 2
                            c.op('act', lambda s=s, pi=pi: nc.scalar.copy(out=kvst[s][:], in_=PB[pi][:, :]), reads=[('pb', pi)], writes=[('kvst', s)])
                            c.dma('sp', lambda s=s, r=r, g=g, d=d, wing=wing: nc.sync.dma_start(out=kvp_t[g].ap()[r:wing:d, :], in_=kvst[s][:]), reads=[('kvst', s)], writes=[('kvp', g, r)])
                        nkv += 1
                pi = 4 + nkv % 3
                for k in range(8):
                    c.op('pe', lambda k=k, pi=pi: nc.tensor.matmul(PB[pi][0:64, :], lhsT=hT[:, k, NEXT:NTOK], rhs=wkv[:, k, :], start=(k == 0), stop=(k == 7)),
                         reads=['hT', 'wkv_k', 'wkv_v'], writes=[('pb', pi)])
                s = nkv % 2
                c.op('act', lambda s=s, pi=pi: nc.scalar.copy(out=kvst[s][0:64, :], in_=PB[pi][0:64, :]), reads=[('pb', pi)], writes=[('kvst', s)])
                c.op('dve', lambda g=g, pi=pi: nc.vector.tensor_copy(out=v1n[:, g, :, 0:64], in_=PB[pi][0:64, 256:512].rearrange("p (h e) -> p h e", h=4)), reads=[('pb', pi)], writes=['v1n'])
                for b in range(NSQ):
                    c.dma('sp', lambda b=b, s=s, g=g, wing=wing: nc.sync.dma_start(out=kvs_t[g].ap()[b, wing - 4:wing, :], in_=kvst[s][4 * b:4 * b + 4, :]), reads=[('kvst', s)], writes=[('kvsn', g, b)])
                for pair in range(2):
                    if _os_env('MK_SKIP_ATT'):
                        continue
                    c.dma('pool', lambda g=g, pair=pair: nc.gpsimd.dma_start(out=wq[:], in_=winr[:, :, 256 * g + 128 * pair: 256 * g + 128 * pair + 128]), writes=['wq'])
                    c.dma('pool', lambda g=g, pair=pair: nc.gpsimd.dma_start(out=wk[:], in_=winr[:, :, 768 + 256 * g + 128 * pair: 768 + 256 * g + 128 * pair + 128]), writes=['wk'])
                    npj = 0
                    for (hc, oi, n) in OT:
                        pi = 4 + npj % 3
                        proj_T(PB[pi][:, 0:n], ('pb', pi), wq, 'wq', slice(hc, hc + n))
                        c.op('act', lambda pi=pi, oi=oi, n=n: nc.scalar.copy(out=qT[:, oi:oi + n], in_=PB[pi][:, 0:n]), reads=[('pb', pi)], writes=['qT'])
                        npj += 1
                    cs_ = e0
                    while cs_ < NTOK:
                        n = min(512, NTOK - cs_)
                        pi = 4 + npj % 3
                        proj_T(PB[pi][:, 0:n], ('pb', pi), wk, 'wk', slice(cs_, cs_ + n))
                        if npj % 2 == 0:
                            c.op('dve', lambda pi=pi, cs_=cs_, n=n: nc.vector.tensor_copy(out=kT[:, cs_:cs_ + n], in_=PB[pi][:, 0:n]), reads=[('pb', pi)], writes=['kT'])
                        else:
                            c.op('act', lambda pi=pi, cs_=cs_, n=n: nc.scalar.copy(out=kT[:, cs_:cs_ + n], in_=PB[pi][:, 0:n]), reads=[('pb', pi)], writes=['kT_a'])
                        npj += 1
                        cs_ += n
                    c.op('dve', lambda pair=pair: nc.vector.tensor_copy(out=qs[:, pair, :], in_=qT[:, NOWN:NOT]), reads=['qT'], writes=['qs'])
                    c.op('dve', lambda pair=pair: nc.vector.tensor_copy(out=ks[:, pair, :], in_=kT[:, NEXT:NTOK]), reads=['kT', 'kT_a'], writes=['ks'])
                    for r in range(d):
                        for i in range(1, nbl):
                            s = qb_i % 2
                            qb_i += 1
                            q0 = r + 128 * d * (i - 1)
                            qsl = slice(q0, q0 + 127 * d + 1, d)
                            SBK = ((0, 1), (4, 5))[s]
                            for hh in range(2):
                                po = hh * 64
                                for blk in range(2):
                                    k0 = e0 + r + 128 * d * (i - 1 + blk)
                                    ksl = slice(k0, k0 + 127 * d + 1, d)
                                    c.op('pe', lambda bk=SBK[hh], po=po, ksl=ksl, qsl=qsl, blk=blk: nc.tensor.matmul(PB[bk][:, blk * 128:blk * 128 + 128], lhsT=kT[po:po + 64, ksl], rhs=qT[po:po + 64, qsl], start=True, stop=True),
                                         reads=['kT', 'kT_a', 'qT'], writes=[('pb', SBK[hh])])
                            for hh in range(2):
                                c.op('act', lambda s=s, hh=hh, bk=SBK[hh]: nc.scalar.activation(out=ef[s][:, hh * 256:(hh + 1) * 256], in_=PB[bk][:, 0:256], func=AF.Exp, scale=0.125),
                                     reads=[('pb', SBK[hh])], writes=[('ef', s, hh)])
                            c.op('dve', lambda s=s, pair=pair: nc.vector.tensor_tensor(out=pt[s][:], in0=ef[s][:], in1=ebf[:, pair * 512:(pair + 1) * 512], op=ALU.mult),
                                 reads=[('ef', s, 0), ('ef', s, 1), 'ebf'], writes=[('pt', s)])
                            for hh in range(2):
                                for blk in range(2):
                                    bi = r * nbl + i - 1 + blk
                                    col = (hh * 2 + blk) * 128
                                    c.op('pe', lambda s=s, hh=hh, blk=blk, bi=bi, col=col, pair=pair: nc.tensor.matmul(PB[2 + s][:, hh * 65:hh * 65 + 65], lhsT=pt[s][:, col:col + 128],
                                         rhs=v1[:, bi, 2 * pair + hh, :], start=(blk == 0), stop=(blk == 1)), reads=[('pt', s), ('v1', bi), ('v1o', bi)], writes=[('pb', 2 + s)])
                            c.op('dve', lambda s=s: nc.vector.tensor_copy(out=oev[s][:], in_=PB[2 + s][:, 0:130]), reads=[('pb', 2 + s)], writes=[('oev', s)])
                            c.dma('sp', lambda s=s, g=g, q0=q0, d=d, pair=pair: nc.sync.dma_start(out=acc_t.ap()[g, q0:q0 + 127 * d + 1:d, pair * 130:(pair + 1) * 130], in_=oev[s][:]),
                                  reads=[('oev', s)], writes=[('acc', g, q0, pair)])
                import os as _os
                ntile = 1 if g == 0 else 4
                if _os.environ.get('MK_SKIP_SAMPLE'):
                    continue
                for b in range(NSQ):
                    for r in range(ntile):
                        s = (b * ntile + r) % 2
                        tix = 0 if g == 0 else 1 + 4 * (g - 1) + r
                        c.dma('sp', lambda s=s, b=b, r=r, g=g, d=d, wing=wing: nc.sync.dma_start(out=ctile[s][:], in_=ck_t[g].ap()[b, r:wing:d, :]), writes=[('ctile', s)])
                        TB = (0, 1)[s]
                        for pr in range(2):
                            c.op('pe', lambda s=s, pr=pr, TB=TB: nc.tensor.transpose(out=PB[TB][:, pr * 128:(pr + 1) * 128], in_=ctile[s][:, pr * 128:(pr + 1) * 128], identity=identf[:]),
                                 reads=[('ctile', s), 'identf'], writes=[('pb', TB)])
                        c.op('act', lambda s=s, TB=TB: nc.scalar.copy(out=kTs[s][:].rearrange("p a k -> p (a k)"), in_=PB[TB][:, 0:256]), reads=[('pb', TB)], writes=[('kTs', s)])
                        c.op('pool', lambda s=s: nc.gpsimd.tensor_copy(out=v1s[s][:, :, 0:64], in_=ctile[s][:, 256:512].rearrange("p (h e) -> p h e", h=4)), reads=[('ctile', s)], writes=[('v1s', s)])
                        for h in range(4):
                            po = (h % 2) * 64
                            bk = (2, 3)[s] if h % 2 == 0 else (4, 5)[s]
                            c.op('pe', lambda s=s, h=h, po=po, b=b, bk=bk: nc.tensor.matmul(PB[bk][:, 256 + 4 * h:260 + 4 * h], lhsT=kTs[s][po:po + 64, h // 2, :], rhs=qs[po:po + 64, h // 2, 4 * b:4 * b + 4], start=True, stop=True),
                                 reads=[('kTs', s), 'qs'], writes=[('pb', bk)])
                        ess = ess2[s]
                        essv = ess[:].rearrange("p (h q) -> p h q", h=4)
                        for par, bk in ((0, (2, 3)[s]), (1, (4, 5)[s])):
                            c.op('act', lambda par=par, bk=bk, essv=essv: nc.scalar.activation(out=essv[:, par:4:2, :], in_=PB[bk][:, 256:272].rearrange("p (h q) -> p h q", h=4)[:, par:4:2, :], func=AF.Exp, scale=0.125),
                                 reads=[('pb', bk)], writes=[('ess', s, par)])
                        c.op('dve', lambda b=b, tix=tix, ess=ess: nc.vector.tensor_tensor(out=pz[b][:, :, 4 * b:4 * b + 4], in0=ess[:].rearrange("p (h q) -> p h q", h=4), in1=smk[:, tix, :, :], op=ALU.mult),
                             reads=[('ess', s, 0), ('ess', s, 1), 'smk'], writes=[('pz', b)])
                        for h in range(4):
                            c.op('pe', lambda s=s, h=h, b=b, st=(n_os == 0): nc.tensor.matmul(PB[7][0:64, 65 * h:65 * h + 65], lhsT=pz[b][:, h, :], rhs=v1s[s][:, h, :], start=st, stop=False),
                                 reads=[('pz', b), ('v1s', s)], writes=['pb7'])
                        n_os += 1
                for h in range(4):
                    po = (h % 2) * 64
                    bk = 6 if h % 2 == 0 else 5
                    c.op('pe', lambda h=h, po=po, bk=bk: nc.tensor.matmul(PB[bk][0:64, 64 * h:64 * h + 64], lhsT=ks[po:po + 64, h // 2, :], rhs=qs[po:po + 64, h // 2, :], start=True, stop=True),
                         reads=['ks', 'qs'], writes=[('pb', bk)])
                for h in range(4):
                    bk = 6 if h % 2 == 0 else 5
                    c.op('act', lambda h=h, bk=bk: nc.scalar.activation(out=en[:, 64 * h:64 * h + 64], in_=PB[bk][0:64, 64 * h:64 * h + 64], func=AF.Exp, scale=0.125), reads=[('pb', bk)], writes=[('en', h)])
                c.op('dve', lambda g=g: nc.vector.tensor_tensor(out=pn[:], in0=en[:].rearrange("p (h q) -> p h q", h=4), in1=nmk[:, g, :, :], op=ALU.mult), reads=[('en', 0), ('en', 1), ('en', 2), ('en', 3), 'nmk'], writes=['pn'])
                for h in range(4):
                    c.op('pe', lambda h=h, g=g: nc.tensor.matmul(PB[7][0:64, 65 * h:65 * h + 65], lhsT=pn[:, h, :], rhs=v1n[:, g, h, :], start=False, stop=(g == 2 and h == 3)),
                         reads=['pn', 'v1n'], writes=['pb7'])
            c.op('act', lambda: nc.scalar.copy(out=osv[:], in_=PB[7][0:64, 0:260]), reads=['pb7'], writes=['osv'])
            c.dma('sp', lambda: nc.sync.dma_start(out=accs_t.ap(), in_=osv[:]), reads=['osv'], writes=['accs'])
        c.barrier()
        es_hT.close()
        if STOP_AFTER == 'D':
            c.finish()
            return nc
        wcor = wco_t.ap().rearrange("(c p) n -> p c n", p=128)
        waor = wao_t.ap().rearrange("(c p) n -> p c n", p=128)
        wor = wo_t.ap().rearrange("(c p) n -> p c n", p=128)
        wrtr = wrt_t.ap().rearrange("(c p) n -> p c n", p=128)
        h2d_t = dscr("h2scr", [NOT, D], BF16)
        esE = ExitStack()
        es.enter_context(esE)
        lgall = c.sb([128, NTILE, 36], F32, esE)
        esE1 = ExitStack()
        with esE1:
            wco = c.sb([128, 4, D], BF16, esE1); wao = c.sb([128, 2, D], BF16, esE1); wo = c.sb([128, 8, D], BF16, esE1)
            wrt = c.sb([128, 8, 36], BF16, esE1); brtb = c.sb([128, 36], F32, esE1)
            c.dma('pool', lambda: nc.gpsimd.dma_start(out=wco[:], in_=wcor), writes=['wco'])
            c.dma('pool', lambda: nc.gpsimd.dma_start(out=wao[:], in_=waor), writes=['wao'])
            c.dma('pool', lambda: nc.gpsimd.dma_start(out=wo[:], in_=wor), writes=['wo'])
            c.dma('pool', lambda: nc.gpsimd.dma_start(out=wrt[:], in_=wrtr), writes=['wrt'])
            c.dma('sp', lambda: nc.sync.dma_start(out=brtb[:], in_=brt_t.ap().partition_broadcast(128)), writes=['brtb'])
            mrow = {}
            for kind, nm in ((0, 'g1'), (1, 'b2'), (2, 'a2')):
                for part, (c0, n) in enumerate(((0, 128), (128, 64))):
                    t_ = c.sb([n, D], F32, esE1)
                    c.dma('sp', lambda t_=t_, kind=kind, c0=c0, n=n: nc.sync.dma_start(out=t_[:], in_=modrows_t.ap()[kind, c0:c0 + n, :]),
                          reads=[('modrows', kind, part)], writes=[('mrow', nm, part)])
                    mrow[(nm, part)] = t_
            c.op('pool', lambda: nc.gpsimd.memset(lgall[:], 0.0), writes=['lgall'])
            ytile = [c.sb([128, 4, 512], BF16, esE1) for _ in range(2)]
            oT = c.sb([128, 2, 512], BF16, esE1)
            acc3 = [c.sb([128, 3, 260], F32, esE1) for _ in range(2)]
            osum = c.sb([128, 260], F32, esE1); rden = c.sb([128, 4], F32, esE1); ob = c.sb([128, 256], BF16, esE1)
            sga = [c.sb([128, 512], BF16, esE1) for _ in range(2)]; sgb = [c.sb([128, 512], BF16, esE1) for _ in range(2)]
            m1 = [c.sb([128, 512], F32, esE1) for _ in range(2)]; m2 = [c.sb([128, 512], F32, esE1) for _ in range(2)]
            mixT = c.sb([128, 8, 512], BF16, esE1)
            xt2 = [c.sb([128, D], F32, esE1) for _ in range(2)]; tmp = [c.sb([128, D], F32, esE1) for _ in range(2)]
            x1t = [c.sb([128, D], F32, esE1) for _ in range(2)]; t2 = [c.sb([128, D], F32, esE1) for _ in range(2)]
            h2b = [c.sb([128, D], BF16, esE1) for _ in range(2)]
            h2T = c.sb([128, 8, 128], BF16, esE1); st2 = [c.sb([128, 4], F32, esE1) for _ in range(2)]
            junk2 = c.sb([128, D], BF16, esE1)
            sub_i = 0
            for M, (hc, oi, n) in enumerate(OT):
                part = 0 if M < 8 else 1
                rr = 128 if M < 8 else 64
                nsub = n // rr
                ys_ = M % 2
                c.dma('sp', lambda ys_=ys_, oi=oi, n=n: nc.sync.dma_start(out=ytile[ys_][:, :, 0:n], in_=yts_t.ap()[:, :, oi:oi + n].rearrange("c p t -> p c t")),
                      reads=[('yts', cc) for cc in range(4)], writes=[('ytile', ys_)])
                for t in range(nsub):
                    a_ = (M * 4 + t) % 2
                    r0 = oi + rr * t
                    if M < 8:
                        c.dma('sp', lambda a_=a_, r0=r0: nc.sync.dma_start(out=acc3[a_][:], in_=acc_t.ap()[:, r0:r0 + 128, :].rearrange("g t c -> t g c")),
                              reads=[k for k in c.state if isinstance(k, tuple) and k[0] == 'acc'], writes=[('acc3', a_)])
                        c.op('dve', lambda a_=a_: nc.vector.tensor_tensor(out=osum[:], in0=acc3[a_][:, 0, :], in1=acc3[a_][:, 1, :], op=ALU.add), reads=[('acc3', a_)], writes=['osum'])
                        c.op('dve', lambda a_=a_: nc.vector.tensor_tensor(out=osum[:], in0=osum[:], in1=acc3[a_][:, 2, :], op=ALU.add), reads=[('acc3', a_), 'osum'], writes=['osum'])
                    else:
                        c.dma('sp', lambda a_=a_: nc.sync.dma_start(out=acc3[a_][0:64, 0, :], in_=accs_t.ap()), reads=['accs'], writes=[('acc3', a_)])
                        c.op('dve', lambda a_=a_: nc.vector.tensor_copy(out=osum[0:64, :], in_=acc3[a_][0:64, 0, :]), reads=[('acc3', a_)], writes=['osum'])
                    ov = osum[0:rr, :].rearrange("p (h e) -> p h e", h=4)
                    c.op('dve', lambda ov=ov, rr=rr: nc.vector.reciprocal(out=rden[0:rr, :].unsqueeze(2), in_=ov[:, :, 64:65]), reads=['osum'], writes=['rden'])
                    c.op('dve', lambda ov=ov, rr=rr: nc.vector.tensor_tensor(out=ob[0:rr, :].rearrange("p (h e) -> p h e", h=4), in0=ov[:, :, 0:64],
                         in1=rden[0:rr, :].unsqueeze(2).to_broadcast([rr, 4, 64]), op=ALU.mult), reads=['osum', 'rden'], writes=['ob'])
                    pv4 = pbf(4).rearrange("p (k t) -> p k t", k=8)
                    for k in range(2):
                        c.op('pe', lambda k=k, rr=rr, pv4=pv4: nc.tensor.transpose(out=pv4[:, k, 0:rr], in_=ob[0:rr, k * 128:(k + 1) * 128], identity=ident[0:rr, 0:rr]),
                             reads=['ob', 'ident'], writes=[('pb', 4)])
                    c.op('act', lambda t=t, rr=rr, pv4=pv4: nc.scalar.copy(out=oT[:, :, rr * t:rr * t + rr], in_=pv4[:, 0:2, 0:rr]), reads=[('pb', 4)], writes=['oT'])
                for j in range(8):
                    gs = j % 2
                    c.dma('sp', lambda gs=gs, j=j, oi=oi, n=n: nc.sync.dma_start(out=sga[gs][:, 0:n], in_=sg_t.ap()[j, :, oi:oi + n]), reads=[('sg', j)], writes=[('sga', gs)])
                    c.dma('sp', lambda gs=gs, j=j, oi=oi, n=n: nc.sync.dma_start(out=sgb[gs][:, 0:n], in_=sg_t.ap()[8 + j, :, oi:oi + n]), reads=[('sg', 8 + j)], writes=[('sgb', gs)])
                    pa_i, pb_i = (0, 1) if gs == 0 else (6, 7)
                    for cc in range(4):
                        c.op('pe', lambda cc=cc, j=j, pa_i=pa_i, ys_=ys_, n=n: nc.tensor.matmul(PB[pa_i][:, 0:n], lhsT=wco[:, cc, j * 128:(j + 1) * 128], rhs=ytile[ys_][:, cc, 0:n], start=(cc == 0), stop=(cc == 3)),
                             reads=['wco', ('ytile', ys_)], writes=[('pb', pa_i)])
                    for cc in range(2):
                        c.op('pe', lambda cc=cc, j=j, pb_i=pb_i, n=n: nc.tensor.matmul(PB[pb_i][:, 0:n], lhsT=wao[:, cc, j * 128:(j + 1) * 128], rhs=oT[:, cc, 0:n], start=(cc == 0), stop=(cc == 1)),
                             reads=['wao', 'oT'], writes=[('pb', pb_i)])
                    c.op('dve', lambda gs=gs, pa_i=pa_i, n=n: nc.vector.tensor_tensor(out=m1[gs][:, 0:n], in0=PB[pa_i][:, 0:n], in1=sga[gs][:, 0:n], op=ALU.mult), reads=[('pb', pa_i), ('sga', gs)], writes=[('m1', gs)])
                    c.op('dve', lambda gs=gs, pb_i=pb_i, n=n: nc.vector.tensor_tensor(out=m2[gs][:, 0:n], in0=PB[pb_i][:, 0:n], in1=sgb[gs][:, 0:n], op=ALU.mult), reads=[('pb', pb_i), ('sgb', gs)], writes=[('m2', gs)])
                    c.op('pool', lambda gs=gs, j=j, n=n: nc.gpsimd.tensor_tensor(out=mixT[:, j, 0:n], in0=m1[gs][:, 0:n], in1=m2[gs][:, 0:n], op=ALU.add), reads=[('m1', gs), ('m2', gs)], writes=[('mixT', j)])
                for t in range(nsub):
                    xs_ = sub_i % 2
                    sub_i += 1
                    r0 = oi + rr * t
                    tile_i = r0 // 128 if M < 8 else 32
                    src = xp[HALO + r0:HALO + r0 + 128, :] if M < 8 else xs
                    c.dma('sp', lambda xs_=xs_, src=src, rr=rr: nc.sync.dma_start(out=xt2[xs_][0:rr, :], in_=src), writes=[('xt2', xs_)])
                    for hf in range(2):
                        for j in range(8):
                            c.op('pe', lambda hf=hf, j=j, t=t, rr=rr: nc.tensor.matmul(PB[2 + hf][0:rr, :], lhsT=mixT[:, j, rr * t:rr * t + rr], rhs=wo[:, j, hf * 512:(hf + 1) * 512], start=(j == 0), stop=(j == 7)),
                                 reads=[('mixT', j), 'wo'], writes=[('pb', 2 + hf)])
                        c.op('dve', lambda hf=hf, xs_=xs_, rr=rr, part=part: nc.vector.tensor_tensor(out=tmp[xs_][0:rr, hf * 512:(hf + 1) * 512], in0=PB[2 + hf][0:rr, :],
                             in1=mrow[('g1', part)][0:rr, hf * 512:(hf + 1) * 512], op=ALU.mult), reads=[('pb', 2 + hf), ('mrow', 'g1', part)], writes=[('tmp', xs_)])
                    c.op('pool', lambda xs_=xs_, rr=rr: nc.gpsimd.tensor_tensor(out=x1t[xs_][0:rr, :], in0=tmp[xs_][0:rr, :], in1=xt2[xs_][0:rr, :], op=ALU.add),
                         reads=[('tmp', xs_), ('xt2', xs_)], writes=[('x1t', xs_)])
                    c.dma('act', lambda xs_=xs_, r0=r0, rr=rr: nc.scalar.dma_start(out=x1_t.ap()[r0:r0 + rr, :], in_=x1t[xs_][0:rr, :]), reads=[('x1t', xs_)], writes=[('x1', tile_i)])
                    c.op('act', lambda xs_=xs_, rr=rr: nc.scalar.activation(out=junk2[0:rr, :], in_=x1t[xs_][0:rr, :], func=AF.Square, accum_out=st2[xs_][0:rr, 0:1]),
                         reads=[('x1t', xs_)], writes=[('st2', xs_), 'junk2'])
                    c.op('act', lambda xs_=xs_, rr=rr: nc.scalar.activation(out=st2[xs_][0:rr, 1:2], in_=st2[xs_][0:rr, 0:1], func=AF.Sqrt, scale=1.0 / D, bias=epsb[0:rr, :]),
                         reads=[('st2', xs_), 'epsb'], writes=[('st2', xs_)])
                    c.op('dve', lambda xs_=xs_, rr=rr: nc.vector.reciprocal(out=st2[xs_][0:rr, 2:3], in_=st2[xs_][0:rr, 1:2]), reads=[('st2', xs_)], writes=[('st2', xs_)])
                    c.op('dve', lambda xs_=xs_, rr=rr, part=part: nc.vector.scalar_tensor_tensor(out=t2[xs_][0:rr, :], in0=x1t[xs_][0:rr, :], scalar=st2[xs_][0:rr, 2:3],
                         in1=mrow[('a2', part)][0:rr, :], op0=ALU.mult, op1=ALU.mult), reads=[('x1t', xs_), ('st2', xs_), ('mrow', 'a2', part)], writes=[('t2', xs_)])
                    c.op('pool', lambda xs_=xs_, rr=rr, part=part: nc.gpsimd.tensor_tensor(out=h2b[xs_][0:rr, :], in0=t2[xs_][0:rr, :], in1=mrow[('b2', part)][0:rr, :], op=ALU.add),
                         reads=[('t2', xs_), ('mrow', 'b2', part)], writes=[('h2b', xs_)])
                    c.dma('act', lambda xs_=xs_, r0=r0, rr=rr: nc.scalar.dma_start(out=h2d_t.ap()[r0:r0 + rr, :], in_=h2b[xs_][0:rr, :]), reads=[('h2b', xs_)], writes=[('h2d', tile_i)])
                    pv5 = pbf(5).rearrange("p (k t) -> p k t", k=8)
                    for k in range(8):
                        c.op('pe', lambda k=k, xs_=xs_, rr=rr, pv5=pv5: nc.tensor.transpose(out=pv5[:, k, 0:rr], in_=h2b[xs_][0:rr, k * 128:(k + 1) * 128], identity=ident[0:rr, 0:rr]),
                             reads=[('h2b', xs_), 'ident'], writes=[('pb', 5)])
                    c.op('act', lambda rr=rr, pv5=pv5: nc.scalar.copy(out=h2T[:, :, 0:rr], in_=pv5[:, :, 0:rr]), reads=[('pb', 5)], writes=['h2T'])
                    for k in range(8):
                        c.op('pe', lambda k=k, rr=rr: nc.tensor.matmul(PB[4][0:rr, 0:36], lhsT=h2T[:, k, 0:rr], rhs=wrt[:, k, :], start=(k == 0), stop=(k == 7)),
                             reads=['h2T', 'wrt'], writes=[('pb', 4)])
                    c.op('dve', lambda rr=rr, tile_i=tile_i: nc.vector.tensor_tensor(out=lgall[0:rr, tile_i, :], in0=PB[4][0:rr, 0:36], in1=brtb[0:rr, :], op=ALU.add),
                         reads=[('pb', 4), 'brtb', 'lgall'], writes=['lgall'])
        c.barrier()
        if STOP_AFTER == 'E':
            c.finish()
            return nc
        NT = NTILE
        esF = ExitStack()
        with esF:
            def ft(shape, dt=F32):
                return c.sb(shape, dt, esF)
            gmx = ft([128, NT]); ohg = ft([128, NT, 4]); gsh = ft([128, NT, 4]); gex = ft([128, NT, 4]); gsum = ft([128, NT]); pgr = ft([128, NT])
            pen = ft([128, NT, 4]); em = ft([128, NT, 32]); m8 = ft([128, NT, 8]); i8 = ft([128, NT, 8], U32)
            e0f = ft([128, NT]); e1f = ft([128, NT]); dv = ft([128, NT]); w0 = ft([128, NT]); w1 = ft([128, NT])
            oh0 = ft([128, NT, 32]); oh1 = ft([128, NT, 32]); mm_ = ft([128, NT, 32]); cs = ft([128, NT + 1, 32])
            base = ft([128, NT, 32]); prod = ft([128, NT, 32]); d0f = ft([128, NT]); d1f = ft([128, NT])
            d0i = ft([128, NT], I32); d1i = ft([128, NT], I32)
            io32i = ft([128, 32], I32); io32 = ft([128, 32]); thri = ft([128, NBLK], I32); thr = ft([128, NBLK])
            cnt = ft([128, 32]); cni = ft([128, 32], I32); pad = ft([128, 32]); pa_ = ft([128, 32]); pb_ = ft([128, 32]); pst = ft([128, 32])
            cmpb = ft([128, NBLK, 32]); bef = ft([128, NBLK])
            c.op('pool', lambda: nc.gpsimd.iota(io32i[:], pattern=[[1, 32]], base=0, channel_multiplier=0), writes=['io32i'])
            c.op('pool', lambda: nc.gpsimd.iota(thri[:], pattern=[[BLK, NBLK]], base=0, channel_multiplier=0), writes=['thri'])
            c.op('dve', lambda: nc.vector.tensor_copy(out=io32[:], in_=io32i[:]), reads=['io32i'], writes=['io32'])
            c.op('dve', lambda: nc.vector.tensor_copy(out=thr[:], in_=thri[:]), reads=['thri'], writes=['thr'])
            R = ['lgall']
            gl = lgall[:, :, 0:4]
            V = nc.vector
            c.op('dve', lambda: V.tensor_reduce(out=gmx[:], in_=gl, axis=AX.X, op=ALU.max), reads=R, writes=['gmx'])
            c.op('dve', lambda: V.tensor_tensor(out=ohg[:], in0=gl, in1=gmx[:].unsqueeze(2).to_broadcast([128, NT, 4]), op=ALU.is_equal), reads=R + ['gmx'], writes=['ohg'])
            c.op('dve', lambda: V.tensor_tensor(out=gsh[:], in0=gl, in1=gmx[:].unsqueeze(2).to_broadcast([128, NT, 4]), op=ALU.subtract), reads=R + ['gmx'], writes=['gsh'])
            c.op('act', lambda: nc.scalar.activation(out=gex[:], in_=gsh[:], func=AF.Exp), reads=['gsh'], writes=['gex'])
            c.op('dve', lambda: V.tensor_reduce(out=gsum[:], in_=gex[:], axis=AX.X, op=ALU.add), reads=['gex'], writes=['gsum'])
            c.op('dve', lambda: V.reciprocal(out=pgr[:], in_=gsum[:]), reads=['gsum'], writes=['pgr'])
            c.op('dve', lambda: V.tensor_scalar(out=pen[:], in0=ohg[:], scalar1=-1.0, scalar2=1e30, op0=ALU.add, op1=ALU.mult), reads=['ohg'], writes=['pen'])
            c.op('dve', lambda: V.tensor_tensor(out=em[:].rearrange("p t (g e) -> p t g e", g=4), in0=lgall[:, :, 4:36].rearrange("p t (g e) -> p t g e", g=4),
                 in1=pen[:].unsqueeze(3).to_broadcast([128, NT, 4, 8]), op=ALU.add), reads=R + ['pen'], writes=['em'])
            for i in range(NT):
                c.op('dve', lambda i=i: V.max(out=m8[:, i, :], in_=em[:, i, :]), reads=['em'], writes=['m8'])
                c.op('dve', lambda i=i: V.max_index(out=i8[:, i, :], in_max=m8[:, i, :], in_values=em[:, i, :]), reads=['em', 'm8'], writes=['i8'])
            c.op('dve', lambda: V.tensor_copy(out=e0f[:], in_=i8[:, :, 0]), reads=['i8'], writes=['e0f'])
            c.op('dve', lambda: V.tensor_copy(out=e1f[:], in_=i8[:, :, 1]), reads=['i8'], writes=['e1f'])
            c.op('dve', lambda: V.tensor_tensor(out=dv[:], in0=m8[:, :, 1], in1=m8[:, :, 0], op=ALU.subtract), reads=['m8'], writes=['dv'])
            c.op('act', lambda: nc.scalar.activation(out=dv[:], in_=dv[:], func=AF.Exp), reads=['dv'], writes=['dv'])
            c.op('dve', lambda: V.tensor_scalar(out=dv[:], in0=dv[:], scalar1=1.0, scalar2=None, op0=ALU.add), reads=['dv'], writes=['dv'])
            c.op('dve', lambda: V.reciprocal(out=w0[:], in_=dv[:]), reads=['dv'], writes=['w0'])
            c.op('dve', lambda: V.tensor_tensor(out=w0[:], in0=w0[:], in1=pgr[:], op=ALU.mult), reads=['w0', 'pgr'], writes=['w0'])
            c.op('dve', lambda: V.tensor_tensor(out=w1[:], in0=pgr[:], in1=w0[:], op=ALU.subtract), reads=['w0', 'pgr'], writes=['w1'])
            iob = io32[:].unsqueeze(1).to_broadcast([128, NT, 32])
            c.op('dve', lambda: V.tensor_tensor(out=oh0[:], in0=iob, in1=e0f[:].unsqueeze(2).to_broadcast([128, NT, 32]), op=ALU.is_equal), reads=['io32', 'e0f'], writes=['oh0'])
            c.op('dve', lambda: V.tensor_tensor(out=oh1[:], in0=iob, in1=e1f[:].unsqueeze(2).to_broadcast([128, NT, 32]), op=ALU.is_equal), reads=['io32', 'e1f'], writes=['oh1'])
            c.op('dve', lambda: V.memset(oh0[64:128, NT - 1, :], 0.0), reads=['oh0'], writes=['oh0'])
            c.op('dve', lambda: V.memset(oh1[64:128, NT - 1, :], 0.0), reads=['oh1'], writes=['oh1'])
            c.op('dve', lambda: V.tensor_tensor(out=mm_[:], in0=oh0[:], in1=oh1[:], op=ALU.add), reads=['oh0', 'oh1'], writes=['mm'])
            c.op('dve', lambda: V.memset(cs[:, 0, :], 0.0), writes=['cs'])
            for i in range(NT):
                c.op('dve', lambda i=i: V.tensor_tensor(out=cs[:, i + 1, :], in0=cs[:, i, :], in1=mm_[:, i, :], op=ALU.add), reads=['cs', 'mm'], writes=['cs'])
            for i in range(NT):
                bk = i // 16
                co = (i % 16) * 32
                c.op('pe', lambda i=i, bk=bk, co=co: nc.tensor.matmul(PB[bk][:, co:co + 32], lhsT=suf[:], rhs=mm_[:, i, :], start=True, stop=False), reads=['suf', 'mm'], writes=[('pb', bk)])
                c.op('pe', lambda i=i, bk=bk, co=co: nc.tensor.matmul(PB[bk][:, co:co + 32], lhsT=onesf[:], rhs=cs[:, i, :], start=False, stop=True), reads=['onesf', 'cs'], writes=[('pb', bk)])
            c.op('pe', lambda: nc.tensor.matmul(PB[3][:, 0:32], lhsT=onesf[:], rhs=cs[:, NT, :], start=True, stop=True), reads=['onesf', 'cs'], writes=[('pb', 3)])
            c.op('dve', lambda: V.tensor_scalar(out=cni[:], in0=PB[3][:, 0:32], scalar1=float(BLK - 1), scalar2=None, op0=ALU.add), reads=[('pb', 3)], writes=['cni'])
            c.op('dve', lambda: V.tensor_scalar(out=cni[:], in0=cni[:], scalar1=int(math.log2(BLK)), scalar2=int(math.log2(BLK)), op0=ALU.arith_shift_right, op1=ALU.logical_shift_left), reads=['cni'], writes=['cni'])
            c.op('dve', lambda: V.tensor_copy(out=pad[:], in_=cni[:]), reads=['cni'], writes=['pad'])
            src_, dst_ = pad, pa_
            for sft in (1, 2, 4, 8, 16):
                c.op('dve', lambda src_=src_, dst_=dst_, sft=sft: V.tensor_copy(out=dst_[:, 0:sft], in_=src_[:, 0:sft]), reads=['pfx', 'pad'], writes=['pfx'])
                c.op('dve', lambda src_=src_, dst_=dst_, sft=sft: V.tensor_tensor(out=dst_[:, sft:32], in0=src_[:, sft:32], in1=src_[:, 0:32 - sft], op=ALU.add), reads=['pfx', 'pad'], writes=['pfx'])
                src_, dst_ = dst_, (pb_ if dst_ is pa_ else pa_)
            pend = src_
            c.op('dve', lambda: V.tensor_tensor(out=pst[:], in0=pend[:], in1=pad[:], op=ALU.subtract), reads=['pfx', 'pad'], writes=['pst'])
            for bk in range(3):
                t0 = bk * 16
                nt_ = min(16, NT - t0)
                c.op('dve', lambda bk=bk, t0=t0, nt_=nt_: V.tensor_tensor(out=base[:, t0:t0 + nt_, :], in0=PB[bk][:, 0:nt_ * 32].rearrange("p (t e) -> p t e", e=32),
                     in1=pst[:].unsqueeze(1).to_broadcast([128, nt_, 32]), op=ALU.add), reads=[('pb', bk), 'pst'], writes=['base'])
            for oh_, df_, di_, nm in ((oh0, d0f, d0i, 'd0'), (oh1, d1f, d1i, 'd1')):
                c.op('dve', lambda oh_=oh_: V.tensor_tensor(out=prod[:], in0=oh_[:], in1=base[:], op=ALU.mult), reads=['oh0', 'oh1', 'base'], writes=['prod'])
                c.op('dve', lambda df_=df_: V.tensor_reduce(out=df_[:], in_=prod[:], axis=AX.X, op=ALU.add), reads=['prod'], writes=[nm + 'f'])
                c.op('dve', lambda df_=df_, di_=di_: V.tensor_copy(out=di_[:], in_=df_[:]), reads=[nm + 'f'], writes=[nm])
            c.op('dve', lambda: V.tensor_tensor(out=cmpb[:], in0=pend[:].unsqueeze(1).to_broadcast([128, NBLK, 32]), in1=thr[:].unsqueeze(2).to_broadcast([128, NBLK, 32]), op=ALU.is_le),
                 reads=['pfx', 'thr'], writes=['cmpb'])
            c.op('dve', lambda: V.tensor_reduce(out=bef[:], in_=cmpb[:], axis=AX.X, op=ALU.add), reads=['cmpb'], writes=['bef'])
            c.op('dve', lambda: V.tensor_scalar(out=bef[:], in0=bef[:], scalar1=31.0, scalar2=None, op0=ALU.min), reads=['bef'], writes=['bef'])
            pio = ft([128, 1], I32); piof = ft([128, 1]); widxf = ft([128, NBLK]); widx = ft([128, NBLK], I32)
            c.op('pool', lambda: nc.gpsimd.iota(pio[:], pattern=[[0, 1]], base=0, channel_multiplier=1), writes=['pio'])
            c.op('dve', lambda: V.tensor_copy(out=piof[:], in_=pio[:]), reads=['pio'], writes=['piof'])
            c.op('dve', lambda: V.tensor_scalar(out=widxf[:], in0=bef[:], scalar1=128.0, scalar2=piof[:, 0:1], op0=ALU.mult, op1=ALU.add), reads=['bef', 'piof'], writes=['widxf'])
            c.op('dve', lambda: V.tensor_copy(out=widx[:], in_=widxf[:]), reads=['widxf'], writes=['widx'])
            zt = ft([128, D], BF16)
            c.op('pool', lambda: nc.gpsimd.memset(zt[:], 0.0), writes=['zt'])
            xsv = xsd_t.ap().rearrange("(b p) d -> p b d", p=128)
            zkeys = []
            NRB = CAP // 128
            for q4 in range(8):
                b0 = q4 * (NRB // 8)
                nb_ = NRB // 8
                c.dma('act', lambda b0=b0, nb_=nb_: nc.scalar.dma_start(out=xsv[:, b0:b0 + nb_, :], in_=zt[:].unsqueeze(1).to_broadcast([128, nb_, D])), reads=['zt'], writes=[('xsz', q4)])
                zkeys.append(('xsz', q4))
            h2t = [ft([128, D], BF16) for _ in range(2)]
            skeys = []
            for i in range(NT):
                s = i % 2
                rr = 128 if i < NT - 1 else 64
                c.dma('sp', lambda s=s, i=i, rr=rr: nc.sync.dma_start(out=h2t[s][0:rr, :], in_=h2d_t.ap()[i * 128:i * 128 + rr, :]), reads=[('h2d', i)], writes=[('h2t', s)])
                for di_, nm in ((d0i, 'd0'), (d1i, 'd1')):
                    c.dma('pool', lambda s=s, i=i, rr=rr, di_=di_: nc.gpsimd.indirect_dma_start(out=xsd_t.ap(), out_offset=bass.IndirectOffsetOnAxis(ap=di_[0:rr, i:i + 1], axis=0),
                          in_=h2t[s][0:rr, :], in_offset=None), reads=[('h2t', s), nm] + zkeys, writes=[('xss', i, nm)])
                    skeys.append(('xss', i, nm))
            xsb = [ft([128, D], BF16) for _ in range(2)]; xsT = [ft([128, 8, 128], BF16) for _ in range(2)]
            wg = [ft([128, 8, 512], BF16) for _ in range(2)]; wu = [ft([128, 8, 512], BF16) for _ in range(2)]; wd = [ft([128, 4, D], BF16) for _ in range(2)]
            actt = [ft([128, 512]) for _ in range(2)]; ab = [ft([128, 512], BF16) for _ in range(2)]; aT = [ft([128, 4, 128], BF16) for _ in range(2)]
            yev = [ft([128, D]) for _ in range(2)]
            wegv = weg_t.ap().rearrange("e (p k) f -> (e p) (k f)", p=128)
            weuv = weu_t.ap().rearrange("e (p k) f -> (e p) (k f)", p=128)
            wedv = wed_t.ap().rearrange("e (p k) f -> (e p) (k f)", p=128)
            items = [(b, sub) for b in range(NBLK) for sub in range(SUBB)]

            def load_xsb(n):
                b, sub = items[n]
                xs_ = n % 2
                r0 = b * BLK + sub * 128
                c.dma('sp', lambda xs_=xs_, r0=r0: nc.sync.dma_start(out=xsb[xs_][:], in_=xsd_t.ap()[r0:r0 + 128, :]), reads=skeys + zkeys, writes=[('xsb', xs_)])

            load_xsb(0)
            for n, (b, sub) in enumerate(items):
                s = b % 2
                xs_ = n % 2
                if sub == 0:
                    for wt_, wv_, nm in ((wg, wegv, 'wg'), (wu, weuv, 'wu'), (wd, wedv, 'wd')):
                        c.dma('pool', lambda s=s, b=b, wt_=wt_, wv_=wv_: nc.gpsimd.indirect_dma_start(out=wt_[s][:].rearrange("p k f -> p (k f)"), out_offset=None, in_=wv_,
                              in_offset=bass.IndirectOffsetOnAxis(ap=widx[:, b:b + 1], axis=0)), reads=['widx'], writes=[(nm, s)])
                if n + 1 < len(items):
                    load_xsb(n + 1)
                pv0 = pbf(0).rearrange("p (k t) -> p k t", k=8)
                for k in range(8):
                    c.op('pe', lambda k=k, xs_=xs_, pv0=pv0: nc.tensor.transpose(out=pv0[:, k, :], in_=xsb[xs_][:, k:k + 8 * 127 + 1:8], identity=ident[:]), reads=[('xsb', xs_), 'ident'], writes=[('pb', 0)])
                c.op('act', lambda xs_=xs_, pv0=pv0: nc.scalar.copy(out=xsT[xs_][:], in_=pv0), reads=[('pb', 0)], writes=[('xsT', xs_)])
                gi, ui = (1, 2) if xs_ == 0 else (6, 7)
                for k in range(8):
                    c.op('pe', lambda k=k, s=s, xs_=xs_, gi=gi: nc.tensor.matmul(PB[gi][:, :], lhsT=xsT[xs_][:, k, :], rhs=wg[s][:, k, :], start=(k == 0), stop=(k == 7)), reads=[('xsT', xs_), ('wg', s)], writes=[('pb', gi)])
                for k in range(8):
                    c.op('pe', lambda k=k, s=s, xs_=xs_, ui=ui: nc.tensor.matmul(PB[ui][:, :], lhsT=xsT[xs_][:, k, :], rhs=wu[s][:, k, :], start=(k == 0), stop=(k == 7)), reads=[('xsT', xs_), ('wu', s)], writes=[('pb', ui)])
                c.op('act', lambda xs_=xs_, gi=gi: nc.scalar.activation(out=actt[xs_][:], in_=PB[gi][:, :], func=AF.Silu), reads=[('pb', gi)], writes=[('actt', xs_)])
                c.op('dve', lambda xs_=xs_, ui=ui: V.tensor_tensor(out=ab[xs_][:], in0=PB[ui][:, :], in1=actt[xs_][:], op=ALU.mult), reads=[('pb', ui), ('actt', xs_)], writes=[('ab', xs_)])
                pv3 = pbf(3).rearrange("p (k t) -> p k t", k=8)
                for k in range(4):
                    c.op('pe', lambda k=k, xs_=xs_, pv3=pv3: nc.tensor.transpose(out=pv3[:, k, :], in_=ab[xs_][:, k:k + 4 * 127 + 1:4], identity=ident[:]), reads=[('ab', xs_), 'ident'], writes=[('pb', 3)])
                c.op('dve', lambda xs_=xs_, pv3=pv3: V.tensor_copy(out=aT[xs_][:], in_=pv3[:, 0:4, :]), reads=[('pb', 3)], writes=[('aT', xs_)])
                for hf in range(2):
                    for k in range(4):
                        c.op('pe', lambda k=k, s=s, xs_=xs_, hf=hf: nc.tensor.matmul(PB[4 + hf][:, :], lhsT=aT[xs_][:, k, :], rhs=wd[s][:, k, hf * 512:(hf + 1) * 512], start=(k == 0), stop=(k == 3)),
                             reads=[('aT', xs_), ('wd', s)], writes=[('pb', 4 + hf)])
                c.op('act', lambda xs_=xs_: nc.scalar.copy(out=yev[xs_][:, 0:512], in_=PB[4][:, :]), reads=[('pb', 4)], writes=[('yev', xs_, 0)])
                c.op('dve', lambda xs_=xs_: V.tensor_copy(out=yev[xs_][:, 512:1024], in_=PB[5][:, :]), reads=[('pb', 5)], writes=[('yev', xs_, 1)])
                r0 = b * BLK + sub * 128
                c.dma('act', lambda xs_=xs_, r0=r0: nc.scalar.dma_start(out=ysd_t.ap()[r0:r0 + 128, :], in_=yev[xs_][:]), reads=[('yev', xs_, 0), ('yev', xs_, 1)], writes=[('ysd', n)])
            ykeys = [('ysd', n) for n in range(len(items))]
            g2r = {}
            for part, (c0, n) in enumerate(((0, 128), (128, 64))):
                t_ = ft([n, D])
                c.dma('sp', lambda t_=t_, c0=c0, n=n: nc.sync.dma_start(out=t_[:], in_=modrows_t.ap()[3, c0:c0 + n, :]), reads=[('modrows', 3, part)], writes=[('g2r', part)])
                g2r[part] = t_
            gfin = ft([128, D])
            c.dma('sp', lambda: nc.sync.dma_start(out=gfin[:], in_=gfin_t.ap().partition_broadcast(128)), writes=['gfin'])
            y0 = [ft([128, D]) for _ in range(2)]; y1 = [ft([128, D]) for _ in range(2)]; x1r = [ft([128, D]) for _ in range(2)]
            fa = [ft([128, D]) for _ in range(2)]; st3 = [ft([128, 4]) for _ in range(2)]
            junk3 = ft([128, D], BF16)
            for i in range(NT):
                s = i % 2
                rr = 128 if i < NT - 1 else 64
                part = 0 if i < NT - 1 else 1
                c.dma('pool', lambda s=s, i=i, rr=rr: nc.gpsimd.indirect_dma_start(out=y0[s][0:rr, :], out_offset=None, in_=ysd_t.ap(),
                      in_offset=bass.IndirectOffsetOnAxis(ap=d0i[0:rr, i:i + 1], axis=0)), reads=ykeys + ['d0'], writes=[('y0', s)])
                c.dma('pool', lambda s=s, i=i, rr=rr: nc.gpsimd.indirect_dma_start(out=y1[s][0:rr, :], out_offset=None, in_=ysd_t.ap(),
                      in_offset=bass.IndirectOffsetOnAxis(ap=d1i[0:rr, i:i + 1], axis=0)), reads=ykeys + ['d1'], writes=[('y1', s)])
                c.dma('sp', lambda s=s, i=i, rr=rr: nc.sync.dma_start(out=x1r[s][0:rr, :], in_=x1_t.ap()[i * 128:i * 128 + rr, :]), reads=[('x1', i)], writes=[('x1r', s)])
                c.op('act', lambda s=s, i=i, rr=rr: nc.scalar.activation(out=fa[s][0:rr, :], in_=y0[s][0:rr, :], func=AF.Copy, scale=w0[0:rr, i:i + 1]), reads=[('y0', s), 'w0'], writes=[('fa', s)])
                c.op('dve', lambda s=s, i=i, rr=rr: V.scalar_tensor_tensor(out=fa[s][0:rr, :], in0=y1[s][0:rr, :], scalar=w1[0:rr, i:i + 1], in1=fa[s][0:rr, :], op0=ALU.mult, op1=ALU.add),
                     reads=[('y1', s), 'w1', ('fa', s)], writes=[('fa', s)])
                c.op('dve', lambda s=s, rr=rr, part=part: V.tensor_tensor(out=fa[s][0:rr, :], in0=fa[s][0:rr, :], in1=g2r[part][0:rr, :], op=ALU.mult), reads=[('fa', s), ('g2r', part)], writes=[('fa', s)])
                c.op('pool', lambda s=s, rr=rr: nc.gpsimd.tensor_tensor(out=x1r[s][0:rr, :], in0=fa[s][0:rr, :], in1=x1r[s][0:rr, :], op=ALU.add), reads=[('fa', s), ('x1r', s)], writes=[('x1r', s)])
                c.op('act', lambda s=s, rr=rr: nc.scalar.activation(out=junk3[0:rr, :], in_=x1r[s][0:rr, :], func=AF.Square, accum_out=st3[s][0:rr, 0:1]), reads=[('x1r', s)], writes=[('st3', s), 'junk3'])
                c.op('act', lambda s=s, rr=rr: nc.scalar.activation(out=st3[s][0:rr, 1:2], in_=st3[s][0:rr, 0:1], func=AF.Sqrt, scale=1.0 / D, bias=epsb[0:rr, :]), reads=[('st3', s), 'epsb'], writes=[('st3', s)])
                c.op('dve', lambda s=s, rr=rr: V.reciprocal(out=st3[s][0:rr, 2:3], in_=st3[s][0:rr, 1:2]), reads=[('st3', s)], writes=[('st3', s)])
                c.op('dve', lambda s=s, rr=rr: V.scalar_tensor_tensor(out=y0[s][0:rr, :], in0=x1r[s][0:rr, :], scalar=st3[s][0:rr, 2:3], in1=gfin[0:rr, :], op0=ALU.mult, op1=ALU.mult),
                     reads=[('x1r', s), ('st3', s), 'gfin'], writes=[('y0', s)])
                dst = yp_t.ap()[i * 128:(i + 1) * 128, :] if i < NT - 1 else ys_t.ap()
                c.dma('act', lambda s=s, rr=rr, dst=dst: nc.scalar.dma_start(out=dst, in_=y0[s][0:rr, :]), reads=[('y0', s)], writes=[('yout', i)])
        c.finish()
    return nc


def build_two_pass():
    nc1 = build_nc(None)
    needed = set(nc1._mk_ctx.record)
    return build_nc(needed)


def _prep_inputs(inp):
    f = lambda a: np.ascontiguousarray(a, dtype=np.float32)
    ohw, vw, sel, bd = _structure_constants()
    shared = {
        "rel_bias": f(inp["rel_bias"]), "norm_mix_g": f(inp["norm_mix_g"][0][None]), "norm_ffn_g": f(inp["norm_ffn_g"][0][None]),
        "norm_final_g": f(inp["norm_final_g"][None]), "w_mod": f(inp["w_mod"][0]), "b_mod": f(inp["b_mod"][0][None]),
        "w_in": f(inp["w_in"][0]), "dw_w": f(inp["dw_w"][0]), "dw_b": f(inp["dw_b"][0][None]), "ln_g": f(inp["ln_conv_g"][0][None]),
        "ln_b": f(inp["ln_conv_b"][0][None]), "w_conv_out": f(inp["w_conv_out"][0]), "w_attn_out": f(inp["w_attn_out"][0]),
        "w_out": f(inp["w_out"][0]),
        "w_rt": f(np.concatenate([inp["w_router_group"][0], inp["w_router_expert"][0].reshape(D, 32)], axis=1)),
        "b_rt": f(np.concatenate([inp["b_router_group"][0], inp["b_router_expert"][0].reshape(32)])[None]),
        "w_eg": f(inp["w_exp_gate"][0]), "w_eu": f(inp["w_exp_up"][0]), "w_ed": f(inp["w_exp_down"][0]),
        "ohw": ohw, "vw": vw, "sel": sel, "bd": bd,
    }
    maps = []
    for cid in range(NCORE):
        b, half = cid // 2, cid % 2
        xp = np.zeros((NEXT, D), np.float32)
        xp[HALO:] = inp["x_prompt"][b, half * NOWN:(half + 1) * NOWN]
        if half == 1:
            xp[:HALO] = inp["x_prompt"][b, NOWN - HALO:NOWN]
        sl = slice(cid * NSQ, (cid + 1) * NSQ)
        m = dict(shared)
        m["xp"] = xp
        m["xs"] = f(inp["x_sample"][sl].reshape(NS, D))
        m["cmod"] = f(np.concatenate([inp["c_prompt"][b][None], inp["c_sample"][sl]], axis=0))
        m["hv"] = np.full((128, 1), float(half), np.float32)
        m["ck128"] = f(inp["cache_kv_w128"][0, sl].reshape(NSQ, 128, 512))
        m["ck512"] = f(inp["cache_kv_w512"][0, sl].reshape(NSQ, 512, 512))
        m["ck2048"] = f(inp["cache_kv_w2048"][0, sl].reshape(NSQ, 2048, 512))
        m["sconv"] = f(inp["state_conv"][0, sl])
        maps.append(m)
    return maps


_NC_CACHE = {}


def kernel(**inp):
    import time as _t
    t0 = _t.time()
    maps = _prep_inputs(inp)
    t1 = _t.time()
    if "nc" not in _NC_CACHE:
        _NC_CACHE["nc"] = build_two_pass()
    nc = _NC_CACHE["nc"]
    t2 = _t.time()
    if STOP_AFTER is not None:
        for m in maps:
            for k in ("w_eg", "w_eu", "w_ed"):
                m.pop(k, None)
    res = run_bass_kernel_spmd(nc, maps, core_ids=list(range(NCORE)))
    print("[kernel] prep %.1fs build %.1fs run %.1fs" % (t1 - t0, t2 - t1, _t.time() - t2), flush=True)
    R = res.results
    _NC_CACHE['last'] = R
    B = 4
    yp = np.zeros((B, 8192, D), np.float32); ys = np.zeros((128, 4, D), np.float32)
    kvp = [np.zeros((1, B, w, 2, 4, 64), np.float32) for (w, _) in GROUPS]
    convp = np.zeros((1, B, 30, 512), np.float32)
    kvs = [np.zeros((1, 128, w, 2, 4, 64), np.float32) for (w, _) in GROUPS]
    convs = np.zeros((1, 128, 30, 512), np.float32)
    for cid in range(NCORE):
        b, half = cid // 2, cid % 2
        r = R[cid]
        sl = slice(cid * NSQ, (cid + 1) * NSQ)
        if "yp" in r:
            yp[b, half * NOWN:(half + 1) * NOWN] = r["yp"]
            ys[sl] = r["ys"].reshape(NSQ, 4, D)
        for gi, (w, _) in enumerate(GROUPS):
            if half == 1:
                kvp[gi][0, b] = r["kvp%d" % w].reshape(w, 2, 4, 64)
            kvs[gi][0, sl] = r["kvs%d" % w].reshape(NSQ, w, 2, 4, 64)
        if half == 1:
            convp[0, b] = r["convp"]
        convs[0, sl] = r["convs"]
    return (yp, ys, kvp[0], kvp[1], kvp[2], convp, kvs[0], kvs[1], kvs[2], convs)
```

```python
import math
import numpy as np
from contextlib import ExitStack
import concourse.bass as bass
import concourse.mybir as mybir
from concourse.bass_utils import run_bass_kernel_spmd

F32 = mybir.dt.float32
BF16 = mybir.dt.bfloat16
I32 = mybir.dt.int32
U32 = mybir.dt.uint32
AF = mybir.ActivationFunctionType
ALU = mybir.AluOpType
AX = mybir.AxisListType

D = 1024
NCORE = 8
HALO = 2048
NOWN = 4096
NEXT = HALO + NOWN
NSQ = 16
NS = 64
NTOK = NEXT + NS
NOT = NOWN + NS
NTILE = 33
GROUPS = ((128, 1), (512, 4), (2048, 16))
EPS = 1e-6
NEXP = 32
BLK = 512
SUBB = BLK // 128
NBLK = (2 * NOT) // BLK + NEXP
CAP = NBLK * BLK
STOP_AFTER = None
DEBUG_SCR = False


class Ctx:
    KD = 8

    def __init__(self, nc, es, needed=None):
        self.nc = nc
        self.es = es
        self.needed = needed
        self.record = set()
        self.iidx = {e: 0 for e in ('pe', 'act', 'dve', 'pool')}
        self.eng = {'pe': nc.tensor, 'act': nc.scalar, 'dve': nc.vector, 'pool': nc.gpsimd, 'sp': nc.sync}
        self.csem = {e: es.enter_context(nc.semaphore('c_' + e)) for e in ('pe', 'act', 'dve', 'pool')}
        self.ccnt = {e: 0 for e in self.csem}
        self.dsem = {q: [es.enter_context(nc.semaphore('d_%s%d' % (q, i))) for i in range(self.KD)]
                     for q in ('sp', 'act', 'pool')}
        self.dcnt = {q: 0 for q in self.dsem}
        self.waited = {e: {} for e in self.eng}
        self.state = {}
        self.sbn = 0

    def sb(self, shape, dt, es=None):
        self.sbn += 1
        return (es or self.es).enter_context(self.nc.sbuf_tensor('sb%d' % self.sbn, list(shape), dt))

    def ps(self, shape, dt):
        self.sbn += 1
        return self.es.enter_context(self.nc.psum_tensor('ps%d' % self.sbn, list(shape), dt))

    def _wait(self, e, evs):
        best = {}
        for (sem, v, src) in evs:
            k = id(sem)
            if k not in best or best[k][1] < v:
                best[k] = (sem, v)
        for k, (sem, v) in best.items():
            if self.waited[e].get(k, 0) >= v:
                continue
            self.eng[e].wait_ge(sem, v)
            self.waited[e][k] = v
            if self.needed is None:
                for ce, cs in self.csem.items():
                    if cs is sem:
                        self.record.add((ce, v))

    def _deps(self, e, reads, writes):
        evs = []
        for k in reads:
            st = self.state.get(k)
            if st and st['w'] is not None:
                evs.append(st['w'])
        for k in writes:
            st = self.state.get(k)
            if st:
                if st['w'] is not None and (st['w'][2] != e or e != 'pe'):
                    evs.append(st['w'])
                for r in st['r']:
                    if r[2] != e or e != 'pe':
                        evs.append(r)
        return evs

    def _commit(self, ev, reads, writes):
        for k in reads:
            st = self.state.setdefault(k, {'w': None, 'r': []})
            st['r'] = [r for r in st['r'] if r[0] is not ev[0]] + [ev]
        for k in writes:
            self.state[k] = {'w': ev, 'r': []}

    @staticmethod
    def _psx(reads, writes):
        ps = [k for k in reads if k == 'pb7' or (isinstance(k, tuple) and k[0] == 'pb')]
        if not ps:
            return list(reads), list(writes)
        return [k for k in reads if k not in ps], list(writes) + [k for k in ps if k not in writes]

    def op(self, e, fn, reads=(), writes=()):
        reads, writes = self._psx(reads, writes)
        self._wait(e, self._deps(e, reads, writes))
        ins = fn()
        self.iidx[e] += 1
        if self.needed is None or (e, self.iidx[e]) in self.needed:
            self.ccnt[e] += 1
            ins.then_inc(self.csem[e], 1)
        ev = (self.csem[e], self.ccnt[e], e)
        self._commit(ev, reads, writes)
        return ev

    def dma(self, q, fn, reads=(), writes=()):
        j = self.dcnt[q]
        sem = self.dsem[q][j % self.KD]
        evs = self._deps(None, reads, writes)
        if j >= self.KD:
            evs.append((sem, 16 * (j // self.KD), 'dma_' + q))
        self._wait(q, evs)
        ins = fn()
        ins.then_inc(sem, 16)
        self.dcnt[q] += 1
        ev = (sem, 16 * (j // self.KD + 1), 'dma_' + q)
        self._commit(ev, reads, writes)
        return ev

    def wait_keys(self, e, keys):
        self._wait(e, self._deps(None, keys, ()))

    def barrier(self):
        evs = []
        for q in self.dsem:
            for i, sem in enumerate(self.dsem[q]):
                n = (self.dcnt[q] - i + self.KD - 1) // self.KD
                if n > 0:
                    evs.append((sem, 16 * n, 'x'))
        for e in self.csem:
            if self.ccnt[e]:
                evs.append((self.csem[e], self.ccnt[e], 'x'))
        for e in self.eng:
            self._wait(e, evs)

    def finish(self):
        evs = []
        for q in self.dsem:
            for i, sem in enumerate(self.dsem[q]):
                n = (self.dcnt[q] - i + self.KD - 1) // self.KD
                if n > 0:
                    evs.append((sem, 16 * n, 'x'))
        for e in self.csem:
            if self.ccnt[e]:
                evs.append((self.csem[e], self.ccnt[e], 'x'))
        self._wait('sp', evs)


def _t5_bucket_np(dist):
    dist = np.asarray(dist, np.int64)
    max_exact = 16
    d_f = np.maximum(dist, 1).astype(np.float32)
    large = max_exact + (np.log(d_f / np.float32(max_exact)) / np.float32(math.log(2048 / max_exact))
                         * np.float32(32 - max_exact)).astype(np.int32)
    large = np.minimum(large, 31)
    return np.where(dist < max_exact, dist, large)


def _structure_constants():
    ohw = np.zeros((32, 3 * 510), np.float32)
    vw = np.zeros((4, 3 * 510), np.float32)
    for g, (win, dil) in enumerate(GROUPS):
        for blk in range(2):
            for u in range(255):
                rel = u + 1 if blk == 0 else u - 127
                ok = (rel <= 128) if blk == 0 else (rel >= 0)
                if ok:
                    b = int(_t5_bucket_np(rel * dil))
                    ohw[b, g * 510 + blk * 255 + u] = 1.0
                    vw[:, g * 510 + blk * 255 + u] = 1.0
    sel = np.zeros((17, 192), np.float32)
    sel[0, 0:128] = 1.0
    for t in range(64):
        sel[1 + t // 4, 128 + t] = 1.0
    bd = np.zeros((64, 128), np.float32)
    for k in range(64):
        for q in range(64):
            if k // 4 == q // 4:
                bd[k, q] = 1.0
        bd[k, 64 + k] = 1.0
    return ohw, vw, sel, bd


def _os_env(k):
    import os
    return os.environ.get(k)


def build_nc(needed=None):
    nc = bass.Bass("TRN2", target_bir_lowering=False)

    def din(name, shape, dt=F32):
        return nc.dram_tensor(name, list(shape), dt, kind="ExternalInput")

    def dout(name, shape, dt=F32):
        return nc.dram_tensor(name, list(shape), dt, kind="ExternalOutput")

    def dscr(name, shape, dt=F32):
        return nc.dram_tensor(name, list(shape), dt, kind="ExternalOutput" if DEBUG_SCR else "Internal")

    xp_t = din("xp", [NEXT, D]); xs_t = din("xs", [NS, D]); cmod_t = din("cmod", [17, D]); hv_t = din("hv", [128, 1])
    ck_t = [din("ck%d" % w, [NSQ, w, 512]) for (w, _) in GROUPS]
    sconv_t = din("sconv", [NSQ, 30, 512])
    relb_t = din("rel_bias", [32, 12])
    gmix_t = din("norm_mix_g", [1, D]); gffn_t = din("norm_ffn_g", [1, D]); gfin_t = din("norm_final_g", [1, D])
    wmod_t = din("w_mod", [D, 6 * D]); bmod_t = din("b_mod", [1, 6 * D])
    win_t = din("w_in", [D, 5376])
    dww_t = din("dw_w", [31, 512]); dwb_t = din("dw_b", [1, 512]); lng_t = din("ln_g", [1, 512]); lnb_t = din("ln_b", [1, 512])
    wco_t = din("w_conv_out", [512, D]); wao_t = din("w_attn_out", [256, D]); wo_t = din("w_out", [D, D])
    wrt_t = din("w_rt", [D, 36]); brt_t = din("b_rt", [1, 36])
    if STOP_AFTER is None:
        weg_t = din("w_eg", [NEXP, D, 512]); weu_t = din("w_eu", [NEXP, D, 512]); wed_t = din("w_ed", [NEXP, 512, D])
    ohw_t = din("ohw", [32, 1530]); vw_t = din("vw", [4, 1530]); sel_t = din("sel", [17, 192]); bd_t = din("bd", [64, 128])

    yp_t = dout("yp", [NOWN, D]); ys_t = dout("ys", [NS, D])
    kvp_t = [dout("kvp%d" % w, [w, 512]) for (w, _) in GROUPS]
    convp_t = dout("convp", [30, 512])
    kvs_t = [dout("kvs%d" % w, [NSQ, w, 512]) for (w, _) in GROUPS]
    convs_t = dout("convs", [NSQ, 30, 512])

    modrows_t = dscr("modrows", [4, 192, D])
    wd_t = dscr("wdscr", [3, 4, 510])
    ebd_t = dscr("ebd", [3, 128, 1024])
    sg_t = dscr("sgscr", [16, 128, NOT], BF16)
    yts_t = dscr("ytscr", [4, 128, NOT], BF16)
    acc_t = dscr("accscr", [3, NOWN, 260])
    accs_t = dscr("accsscr", [NS, 260])
    x1_t = dscr("x1scr", [NOT, D])
    xsd_t = dscr("xsdisp", [CAP, D], BF16)
    ysd_t = dscr("ysdisp", [CAP, D])

    xp = xp_t.ap(); xs = xs_t.ap(); win = win_t.ap()

    with ExitStack() as es:
        c = Ctx(nc, es, needed)
        nc._mk_ctx = c
        PB = [c.ps([128, 512], F32) for _ in range(8)]

        def pbf(i):
            return PB[i][:].bitcast(BF16)

        identf = c.sb([128, 128], F32); ident = c.sb([128, 128], BF16)
        onesb = c.sb([128, 128], BF16); onesf = c.sb([128, 128], F32)
        suf = c.sb([128, 128], F32); jf = c.sb([128, 128], F32)
        epsb = c.sb([128, 1], F32); hv = c.sb([128, 1], F32); one1 = c.sb([128, 1], F32)
        c.op('pool', lambda: nc.gpsimd.memset(onesf[:], 1.0), writes=['onesf'])
        c.op('pool', lambda: nc.gpsimd.memset(onesb[:], 1.0), writes=['onesb'])
        c.op('pool', lambda: nc.gpsimd.memset(epsb[:], EPS), writes=['epsb'])
        c.op('pool', lambda: nc.gpsimd.memset(one1[:], 1.0), writes=['one1'])
        c.op('pool', lambda: nc.gpsimd.affine_select(out=identf[:], in_=onesf[:], pattern=[[-1, 128]], compare_op=ALU.is_equal,
                                                       fill=0.0, base=0, channel_multiplier=1), reads=['onesf'], writes=['identf'])
        c.op('pool', lambda: nc.gpsimd.affine_select(out=jf[:], in_=onesf[:], pattern=[[1, 128]], compare_op=ALU.is_equal,
                                                       fill=0.0, base=-127, channel_multiplier=1), reads=['onesf'], writes=['jf'])
        c.op('pool', lambda: nc.gpsimd.affine_select(out=suf[:], in_=onesf[:], pattern=[[1, 128]], compare_op=ALU.is_gt,
                                                       fill=0.0, base=0, channel_multiplier=-1), reads=['onesf'], writes=['suf'])
        c.op('dve', lambda: nc.vector.tensor_copy(out=ident[:], in_=identf[:]), reads=['identf'], writes=['ident'])
        c.dma('sp', lambda: nc.sync.dma_start(out=hv[:], in_=hv_t.ap()), writes=['hv'])


        es_hT = ExitStack()
        es.enter_context(es_hT)
        hT = c.sb([128, 8, NTOK], BF16, es_hT)
        es_mod = ExitStack()
        a1p = c.sb([128, D], F32, es_mod); b1p = c.sb([128, D], F32, es_mod); a1s = c.sb([64, D], F32, es_mod); b1s = c.sb([64, D], F32, es_mod)
        es0 = ExitStack()
        with es0:
            cm = c.sb([17, D], F32, es0); scm = c.sb([17, D], F32, es0); scT = c.sb([128, 8, 17], F32, es0)
            mtok = c.sb([17, 6 * D], F32, es0); selm = c.sb([17, 192], F32, es0)
            gmb = c.sb([128, D], F32, es0); gfb = c.sb([128, D], F32, es0)
            c.dma('sp', lambda: nc.sync.dma_start(out=cm[:], in_=cmod_t.ap()), writes=['cm'])
            c.dma('sp', lambda: nc.sync.dma_start(out=selm[:], in_=sel_t.ap()), writes=['selm'])
            c.dma('sp', lambda: nc.sync.dma_start(out=gmb[:], in_=gmix_t.ap().partition_broadcast(128)), writes=['gmb'])
            c.dma('sp', lambda: nc.sync.dma_start(out=gfb[:], in_=gffn_t.ap().partition_broadcast(128)), writes=['gfb'])
            c.op('act', lambda: nc.scalar.activation(out=scm[:], in_=cm[:], func=AF.Silu), reads=['cm'], writes=['scm'])
            for k in range(8):
                c.op('pe', lambda k=k: nc.tensor.transpose(out=PB[0][:, k * 17:(k + 1) * 17], in_=scm[0:17, k * 128:(k + 1) * 128],
                                                           identity=identf[0:17, 0:17]), reads=['scm', 'identf'], writes=['pb0'])
            c.op('dve', lambda: nc.vector.tensor_copy(out=scT[:].rearrange("p k s -> p (k s)"), in_=PB[0][:, 0:136]), reads=['pb0'], writes=['scT'])
            wmod = wmod_t.ap().rearrange("(k p) n -> p k n", p=128)
            wbs = [c.sb([128, 8, 512], F32, es0) for _ in range(2)]
            bbs = [c.sb([17, 512], F32, es0) for _ in range(2)]
            for nb in range(12):
                wb = wbs[nb % 2]
                bb = bbs[nb % 2]
                c.dma('sp', lambda wb=wb, nb=nb: nc.sync.dma_start(out=wb[:], in_=wmod[:, :, nb * 512:(nb + 1) * 512]), writes=[('wb', nb % 2)])
                c.dma('sp', lambda bb=bb, nb=nb: nc.sync.dma_start(out=bb[:], in_=bmod_t.ap()[:, nb * 512:(nb + 1) * 512].partition_broadcast(17)),
                      writes=[('bb', nb % 2)])
                pbk = 1 + nb % 2
                for k in range(8):
                    c.op('pe', lambda wb=wb, k=k, pbk=pbk: nc.tensor.matmul(PB[pbk][0:17, 0:512], lhsT=scT[:, k, :], rhs=wb[:, k, :], start=(k == 0), stop=(k == 7)),
                         reads=['scT', ('wb', nb % 2)], writes=[('pb', pbk)])
                c.op('dve', lambda bb=bb, nb=nb, pbk=pbk: nc.vector.tensor_tensor(out=mtok[:, nb * 512:(nb + 1) * 512], in0=PB[pbk][0:17, 0:512], in1=bb[:], op=ALU.add),
                     reads=[('pb', pbk), ('bb', nb % 2)], writes=['mtok'])
            rowst = [c.sb([128, D], F32, es0) for _ in range(2)]
            for kind in range(6):
                for part, (c0, n) in enumerate(((0, 128), (128, 64))):
                    rt = rowst[(kind * 2 + part) % 2]
                    rk = ('rowst', (kind * 2 + part) % 2)
                    for hf in range(2):
                        c.op('pe', lambda hf=hf, c0=c0, n=n, kind=kind: nc.tensor.matmul(PB[3 + hf][0:n, :], lhsT=selm[0:17, c0:c0 + n],
                             rhs=mtok[0:17, kind * D + hf * 512: kind * D + hf * 512 + 512], start=True, stop=True),
                             reads=['selm', 'mtok'], writes=[('pb', 3 + hf)])
                    dst = None; dk = 'nokey'
                    if kind == 0:
                        dst = (b1p, b1s)[part]; dk = ('b1', part)
                    elif kind == 1:
                        dst = (a1p, a1s)[part]; dk = ('a1', part)
                    for hf in range(2):
                        sl = slice(hf * 512, hf * 512 + 512)
                        if kind in (1, 4):
                            gb = gmb if kind == 1 else gfb
                            tgt = dst if dst is not None else rt
                            c.op('dve', lambda hf=hf, n=n, gb=gb, tgt=tgt, sl=sl: nc.vector.scalar_tensor_tensor(out=tgt[0:n, sl], in0=PB[3 + hf][0:n, :], scalar=1.0,
                                 in1=gb[0:n, sl], op0=ALU.add, op1=ALU.mult), reads=[('pb', 3 + hf), 'gmb', 'gfb'], writes=[rk, dk])
                        else:
                            tgt = dst if dst is not None else rt
                            c.op('act', lambda hf=hf, n=n, tgt=tgt, sl=sl: nc.scalar.copy(out=tgt[0:n, sl], in_=PB[3 + hf][0:n, :]),
                                 reads=[('pb', 3 + hf)], writes=[rk, dk])
                    if kind >= 2:
                        c.dma('sp', lambda rt=rt, c0=c0, n=n, kind=kind: nc.sync.dma_start(out=modrows_t.ap()[kind - 2, c0:c0 + n, :], in_=rt[0:n, :]),
                              reads=[rk], writes=[('modrows', kind - 2, part)])
        c.barrier()
        es0 = ExitStack()
        with es0:
            rb = c.sb([32, 12], F32, es0); ohw = c.sb([32, 1530], F32, es0); vw = c.sb([4, 1530], F32, es0)
            wsb = c.sb([4, 1530], F32, es0); hall = c.sb([128, 24, 128], F32, es0); ebst = c.sb([128, 3, 1024], F32, es0)
            c.dma('sp', lambda: nc.sync.dma_start(out=rb[:], in_=relb_t.ap()), writes=['rb'])
            c.dma('sp', lambda: nc.sync.dma_start(out=ohw[:], in_=ohw_t.ap()), writes=['ohw'])
            c.dma('sp', lambda: nc.sync.dma_start(out=vw[:], in_=vw_t.ap()), writes=['vw'])
            for g in range(3):
                c.op('pe', lambda g=g: nc.tensor.matmul(PB[5][0:4, 0:510], lhsT=rb[:, 4 * g:4 * g + 4], rhs=ohw[:, g * 510:(g + 1) * 510], start=True, stop=True),
                     reads=['rb', 'ohw'], writes=[('pb', 5)])
                c.op('act', lambda g=g: nc.scalar.activation(out=wsb[:, g * 510:(g + 1) * 510], in_=PB[5][0:4, 0:510], func=AF.Exp), reads=[('pb', 5)], writes=['wsb'])
            c.op('dve', lambda: nc.vector.tensor_tensor(out=wsb[:], in0=wsb[:], in1=vw[:], op=ALU.mult), reads=['wsb', 'vw'], writes=['wsb'])
            c.dma('sp', lambda: nc.sync.dma_start(out=wd_t.ap().rearrange("g h u -> h g u"), in_=wsb[:].rearrange("h (g u) -> h g u", g=3)), reads=['wsb'], writes=['wd'])
            for g in range(3):
                for h in range(4):
                    for blk in range(2):
                        idx = (g * 4 + h) * 2 + blk
                        src = bass.AP(wd_t, (g * 4 + h) * 510 + blk * 255, [[1, 128], [1, 128]])
                        c.dma('sp', lambda idx=idx, src=src: nc.sync.dma_start(out=hall[:, idx, :], in_=src), reads=['wd'], writes=[('hall', idx)])
            for g in range(3):
                for hf in range(2):
                    c.op('pe', lambda g=g, hf=hf: nc.tensor.matmul(PB[6 + hf][:, :], lhsT=jf[:], rhs=hall[:, g * 8 + hf * 4: g * 8 + hf * 4 + 4, :].rearrange("p a q -> p (a q)"),
                         start=True, stop=True), reads=['jf'] + [('hall', g * 8 + hf * 4 + i) for i in range(4)], writes=[('pb', 6 + hf)])
                    c.op('act', lambda g=g, hf=hf: nc.scalar.copy(out=ebst[:, g, hf * 512:(hf + 1) * 512], in_=PB[6 + hf][:, :]), reads=[('pb', 6 + hf)], writes=[('ebst', g)])
                c.dma('sp', lambda g=g: nc.sync.dma_start(out=ebd_t.ap()[g], in_=ebst[:, g, :]), reads=[('ebst', g)], writes=[('ebd', g)])

        c.barrier()
        def norm_to_T(tidx, src_ap, n, arow, brow, akey, bkey, bufs):
            xt, t1, hb, stt, junk = bufs
            s = tidx % 2
            c.dma('sp', lambda: nc.sync.dma_start(out=xt[s][0:n, :], in_=src_ap), writes=[('xt', s)])
            c.op('act', lambda: nc.scalar.activation(out=junk[0:n, :], in_=xt[s][0:n, :], func=AF.Square, accum_out=stt[s][0:n, 0:1]),
                 reads=[('xt', s)], writes=[('stt', s), 'junk'])
            c.op('act', lambda: nc.scalar.activation(out=stt[s][0:n, 1:2], in_=stt[s][0:n, 0:1], func=AF.Sqrt, scale=1.0 / D, bias=epsb[0:n, :]),
                 reads=[('stt', s), 'epsb'], writes=[('stt', s)])
            c.op('dve', lambda: nc.vector.reciprocal(out=stt[s][0:n, 2:3], in_=stt[s][0:n, 1:2]), reads=[('stt', s)], writes=[('stt', s)])
            c.op('dve', lambda: nc.vector.scalar_tensor_tensor(out=t1[s][0:n, :], in0=xt[s][0:n, :], scalar=stt[s][0:n, 2:3], in1=arow[0:n, :],
                                                                op0=ALU.mult, op1=ALU.mult), reads=[('xt', s), ('stt', s), akey], writes=[('t1', s)])
            c.op('pool', lambda: nc.gpsimd.tensor_tensor(out=hb[s][0:n, :], in0=t1[s][0:n, :], in1=brow[0:n, :], op=ALU.add),
                 reads=[('t1', s), bkey], writes=[('hb', s)])
            pv = pbf(s).rearrange("p (k t) -> p k t", k=8)
            for k in range(8):
                c.op('pe', lambda k=k: nc.tensor.transpose(out=pv[:, k, 0:n], in_=hb[s][0:n, k * 128:(k + 1) * 128], identity=ident[0:n, 0:n]),
                     reads=[('hb', s), 'ident'], writes=[('pb', s)])
            return s, pv

        esA = ExitStack()
        with esA:
            xt = [c.sb([128, D], F32, esA) for _ in range(2)]; t1 = [c.sb([128, D], F32, esA) for _ in range(2)]
            hb = [c.sb([128, D], BF16, esA) for _ in range(2)]; stt = [c.sb([128, 4], F32, esA) for _ in range(2)]
            junk = c.sb([128, D], BF16, esA)
            bufsA = (xt, t1, hb, stt, junk)
            for t in range(49):
                if t < 48:
                    n = 128; src = xp[t * 128:(t + 1) * 128, :]; ar, br = a1p, b1p; col = t * 128; pk = 0
                else:
                    n = 64; src = xs; ar, br = a1s, b1s; col = NEXT; pk = 1
                s, pv = norm_to_T(t, src, n, ar, br, ('a1', pk), ('b1', pk), bufsA)
                c.op('act', lambda pv=pv, col=col, n=n: nc.scalar.copy(out=hT[:, :, col:col + n], in_=pv[:, :, 0:n]), reads=[('pb', s)], writes=['hT'])
        c.barrier()
        es_mod.close()

        winr = win.rearrange("(k p) n -> p k n", p=128)

        def load_w(dst, c0, ncol, key):
            c.dma('pool', lambda: nc.gpsimd.dma_start(out=dst, in_=winr[:, :, c0:c0 + ncol]), writes=[key])

        def proj_T(ps_ap, pkey, wt, wkey, hcols):
            for k in range(8):
                c.op('pe', lambda k=k: nc.tensor.matmul(ps_ap, lhsT=wt[:, k, :], rhs=hT[:, k, hcols], start=(k == 0), stop=(k == 7)),
                     reads=['hT', wkey], writes=[pkey])

        def psv(i, sl=slice(None), n=512):
            return PB[i][sl, 0:n]

        OT = [(HALO + 512 * m, 512 * m, 512) for m in range(8)] + [(NEXT, NOWN, NS)]

        shiftq = []
        for g, (wing, d) in enumerate(GROUPS):
            r0 = 4
            while r0 < wing:
                nr = min(128, wing - r0)
                shiftq.append((g, r0, nr))
                r0 += nr

        def emit_shift(nmax):
            for _ in range(nmax):
                if not shiftq:
                    return
                g, r0, nr = shiftq.pop(0)
                c.dma('act', lambda g=g, r0=r0, nr=nr: nc.scalar.dma_start(out=kvs_t[g].ap()[:, r0 - 4:r0 - 4 + nr, :], in_=ck_t[g].ap()[:, r0:r0 + nr, :]), writes=[('kvs_shift', g, r0)])

        esG = ExitStack()
        with esG:
            wj = [c.sb([128, 8, 128], BF16, esG) for _ in range(2)]
            sgrow = [c.sb([128, NOT], BF16, esG) for _ in range(2)]
            for j in range(16):
                s = j % 2
                emit_shift(1)
                load_w(wj[s][:], 3328 + j * 128, 128, ('wj', s))
                for m, (hc, oi, n) in enumerate(OT):
                    pi = m % 2
                    pa = psv(pi, n=n)
                    proj_T(pa, ('pb', pi), wj[s], ('wj', s), slice(hc, hc + n))
                    c.op('act', lambda pa=pa, oi=oi, n=n, s=s: nc.scalar.activation(out=sgrow[s][:, oi:oi + n], in_=pa, func=AF.Sigmoid),
                         reads=[('pb', pi)], writes=[('sgrow', s)])
                c.dma('sp', lambda j=j, s=s: nc.sync.dma_start(out=sg_t.ap()[j], in_=sgrow[s][:]), reads=[('sgrow', s)], writes=[('sg', j)])

        c.barrier()
        if STOP_AFTER == 'A':
            c.finish()
            return nc

        esC = ExitStack()
        with esC:
            yT = c.sb([128, 4, NOT], BF16, esC)
            dwT = c.sb([128, 4, 31], F32, esC); dwb = c.sb([128, 4], F32, esC); lng = c.sb([128, 4], F32, esC); lnb = c.sb([128, 4], F32, esC)
            with nc.allow_non_contiguous_dma(reason="tiny per-channel parameter loads"):
                for cc in range(4):
                    c.dma('sp', lambda cc=cc: nc.sync.dma_start(out=dwT[:, cc, :], in_=dww_t.ap()[:, cc * 128:(cc + 1) * 128].rearrange("j p -> p j")), writes=[('dwT', cc)])
                c.dma('sp', lambda: nc.sync.dma_start(out=dwb[:], in_=dwb_t.ap().rearrange("o (c p) -> p (o c)", p=128)), writes=['dwb'])
                c.dma('sp', lambda: nc.sync.dma_start(out=lng[:], in_=lng_t.ap().rearrange("o (c p) -> p (o c)", p=128)), writes=['lng'])
                c.dma('sp', lambda: nc.sync.dma_start(out=lnb[:], in_=lnb_t.ap().rearrange("o (c p) -> p (o c)", p=128)), writes=['lnb'])
            uxT = c.sb([128, 4, NSQ, 34], BF16, esC)
            uTf = c.sb([128, 4, 94], F32, esC)
            sct = [c.sb([120, 512], F32, esC)] * 2
            for i4 in range(4):
                s = i4 % 2
                c.dma('sp', lambda i4=i4, s=s: nc.sync.dma_start(out=sct[s][:], in_=sconv_t.ap()[4 * i4:4 * i4 + 4].rearrange("b t c -> (b t) c")), writes=[('sct', 0)])
                for cc in range(4):
                    c.op('pe', lambda cc=cc, s=s: nc.tensor.transpose(out=PB[2][:, 0:120], in_=sct[s][:, cc * 128:(cc + 1) * 128], identity=identf[0:120, 0:120]),
                         reads=[('sct', 0), 'identf'], writes=[('pb', 2)])
                    c.op('act', lambda cc=cc, i4=i4: nc.scalar.copy(out=uxT[:, cc, 4 * i4:4 * i4 + 4, 0:30], in_=PB[2][:, 0:120].rearrange("p (b t) -> p b t", b=4)),
                         reads=[('pb', 2)], writes=['uxT'])
            c.dma('sp', lambda: nc.sync.dma_start(out=convs_t.ap()[:, 0:26, :], in_=sconv_t.ap()[:, 4:30, :]), writes=['convs_a'])
            esC1 = ExitStack()
            wul = [c.sb([128, 8, 128], BF16, esC1) for _ in range(2)]; wug = [c.sb([128, 8, 128], BF16, esC1) for _ in range(2)]
            diag = [c.sb([128, 31, 128], BF16, esC1)] * 2
            ucT = [c.sb([128, 30 + NOWN], BF16, esC1)] * 2
            sgt = [c.sb([128, 512], F32, esC1) for _ in range(2)]
            UT = [(HALO - 30, -30, 30)] + OT
            for cc in range(4):
                s = cc % 2
                emit_shift(2)
                load_w(wul[s][:], 2304 + cc * 128, 128, ('wul', s))
                load_w(wug[s][:], 2816 + cc * 128, 128, ('wug', s))
                for j in range(31):
                    eng = 'dve' if j % 2 == 0 else 'pool'
                    e_ = nc.vector if eng == 'dve' else nc.gpsimd
                    c.op(eng, lambda j=j, e_=e_: e_.tensor_scalar(out=diag[s][:, j, :], in0=identf[:], scalar1=dwT[:, cc, j:j + 1], scalar2=None, op0=ALU.mult),
                         reads=['identf', ('dwT', cc)], writes=[('diag', 0, j)])
                for m, (hc, oi, n) in enumerate(UT):
                    pl = psv(0, n=n); pg = psv(1, n=n)
                    proj_T(pl, ('pb', 0), wul[s], ('wul', s), slice(hc, hc + n))
                    proj_T(pg, ('pb', 1), wug[s], ('wug', s), slice(hc, hc + n))
                    b_ = m % 2
                    c.op('act', lambda pg=pg, n=n, b_=b_: nc.scalar.activation(out=sgt[b_][:, 0:n], in_=pg, func=AF.Sigmoid), reads=[('pb', 1)], writes=[('sgt', b_)])
                    if oi < 0:
                        c.op('dve', lambda pl=pl, n=n, b_=b_: nc.vector.tensor_tensor(out=sgt[b_][:, 0:n], in0=pl, in1=sgt[b_][:, 0:n], op=ALU.mult),
                             reads=[('pb', 0), ('sgt', b_)], writes=[('sgt', b_)])
                        c.op('dve', lambda n=n, b_=b_: nc.vector.tensor_scalar(out=ucT[s][:, 0:30], in0=sgt[b_][:, 0:n], scalar1=hv[:, 0:1], scalar2=None, op0=ALU.mult),
                             reads=[('sgt', b_), 'hv'], writes=[('ucT', 0, 0)])
                    elif oi < NOWN:
                        c.op('dve', lambda pl=pl, n=n, b_=b_, oi=oi: nc.vector.tensor_tensor(out=ucT[s][:, 30 + oi:30 + oi + n], in0=pl, in1=sgt[b_][:, 0:n], op=ALU.mult),
                             reads=[('pb', 0), ('sgt', b_)], writes=[('ucT', 0, 1 + oi // 512)])
                        if oi == NOWN - 512:
                            c.op('dve', lambda pl=pl, b_=b_: nc.vector.tensor_tensor(out=uTf[:, cc, 0:30], in0=pl[:, 482:512], in1=sgt[b_][:, 482:512], op=ALU.mult),
                                 reads=[('pb', 0), ('sgt', b_)], writes=[('uTf', cc)])
                    else:
                        c.op('dve', lambda pl=pl, b_=b_: nc.vector.tensor_tensor(out=uTf[:, cc, 30:94], in0=pl, in1=sgt[b_][:, 0:64], op=ALU.mult),
                             reads=[('pb', 0), ('sgt', b_)], writes=[('uTf', cc)])
                        c.op('dve', lambda: nc.vector.tensor_copy(out=uxT[:, cc, :, 30:34], in_=uTf[:, cc, 30:94].rearrange("p (b t) -> p b t", t=4)),
                             reads=[('uTf', cc)], writes=['uxT'])
                dkeys = [('diag', 0, j) for j in range(31)]
                for m in range(8):
                    py = psv(2 + m % 2)
                    for j in range(31):
                        c.op('pe', lambda j=j, m=m, py=py: nc.tensor.matmul(py, lhsT=diag[s][:, j, :], rhs=ucT[s][:, 512 * m + j: 512 * m + j + 512], start=(j == 0), stop=(j == 30)),
                             reads=[dkeys[j], ('ucT', 0, 0), ('ucT', 0, 1 + m), ('ucT', 0, m)], writes=[('pb', 2 + m % 2)])
                    c.op('act', lambda m=m, py=py: nc.scalar.activation(out=yT[:, cc, 512 * m:512 * m + 512], in_=py, func=AF.Identity, bias=dwb[:, cc:cc + 1], scale=1.0),
                         reads=[('pb', 2 + m % 2), 'dwb'], writes=[('yT', m)])
                pys = PB[2][:, 0:64]
                for j in range(31):
                    c.op('pe', lambda j=j: nc.tensor.matmul(pys.rearrange("p (b t) -> p b t", t=4), lhsT=diag[s][:, j, :], rhs=uxT[:, cc, :, j:j + 4], start=(j == 0), stop=(j == 30)),
                         reads=[dkeys[j], 'uxT'], writes=[('pb', 2)])
                c.op('act', lambda: nc.scalar.activation(out=yT[:, cc, NOWN:NOT], in_=pys, func=AF.Identity, bias=dwb[:, cc:cc + 1], scale=1.0),
                     reads=[('pb', 2), 'dwb'], writes=[('yT', 8)])
            c.barrier()
            esC1.close()
            cpo = c.sb([94, 512], F32, esC)
            for cc in range(4):
                c.op('pe', lambda cc=cc: nc.tensor.transpose(out=PB[0][0:94, 0:128], in_=uTf[:, cc, :], identity=identf[:]), reads=[('uTf', cc), 'identf'], writes=[('pb', 0)])
                c.op('act', lambda cc=cc: nc.scalar.copy(out=cpo[:, cc * 128:(cc + 1) * 128], in_=PB[0][0:94, 0:128]), reads=[('pb', 0)], writes=['cpo'])
            c.dma('sp', lambda: nc.sync.dma_start(out=convp_t.ap(), in_=cpo[0:30, :]), reads=['cpo'], writes=['convp'])
            for b in range(NSQ):
                c.dma('sp', lambda b=b: nc.sync.dma_start(out=convs_t.ap()[b, 26:30, :], in_=cpo[30 + 4 * b:34 + 4 * b, :]), reads=['cpo'], writes=[('convs_b', b)])
            sq = c.sb([128, 4, 512], BF16, esC); mean = c.sb([128, 512], F32, esC); msq = c.sb([128, 512], F32, esC)
            var = c.sb([128, 512], F32, esC); rstd = c.sb([128, 512], F32, esC); tt = c.sb([128, 4, 512], F32, esC)
            for m, (hc, oi, n) in enumerate(OT):
                yk = ('yT', m)
                c.op('act', lambda oi=oi, n=n: nc.scalar.activation(out=sq[:, :, 0:n], in_=yT[:, :, oi:oi + n], func=AF.Square), reads=[yk], writes=['sq'])
                p1 = psv(4, n=n); p2 = psv(5, n=n)
                for cc in range(4):
                    c.op('pe', lambda cc=cc, p1=p1, oi=oi, n=n: nc.tensor.matmul(p1, lhsT=onesb[:], rhs=yT[:, cc, oi:oi + n], start=(cc == 0), stop=(cc == 3)),
                         reads=[yk, 'onesb'], writes=[('pb', 4)])
                for cc in range(4):
                    c.op('pe', lambda cc=cc, p2=p2, n=n: nc.tensor.matmul(p2, lhsT=onesb[:], rhs=sq[:, cc, 0:n], start=(cc == 0), stop=(cc == 3)),
                         reads=['sq', 'onesb'], writes=[('pb', 5)])
                c.op('dve', lambda p1=p1, n=n: nc.vector.tensor_scalar(out=mean[:, 0:n], in0=p1, scalar1=1.0 / 512, scalar2=None, op0=ALU.mult), reads=[('pb', 4)], writes=['mean'])
                c.op('pool', lambda n=n: nc.gpsimd.tensor_tensor(out=msq[:, 0:n], in0=mean[:, 0:n], in1=mean[:, 0:n], op=ALU.mult), reads=['mean'], writes=['msq'])
                c.op('dve', lambda p2=p2, n=n: nc.vector.scalar_tensor_tensor(out=var[:, 0:n], in0=p2, scalar=1.0 / 512, in1=msq[:, 0:n], op0=ALU.mult, op1=ALU.subtract),
                     reads=[('pb', 5), 'msq'], writes=['var'])
                c.op('act', lambda n=n: nc.scalar.activation(out=var[:, 0:n], in_=var[:, 0:n], func=AF.Sqrt, scale=1.0, bias=epsb[:, :]), reads=['var', 'epsb'], writes=['var'])
                c.op('dve', lambda n=n: nc.vector.reciprocal(out=rstd[:, 0:n], in_=var[:, 0:n]), reads=['var'], writes=['rstd'])
                c.op('dve', lambda oi=oi, n=n: nc.vector.tensor_tensor(out=tt[:, :, 0:n], in0=yT[:, :, oi:oi + n], in1=mean[:, 0:n].unsqueeze(1).to_broadcast([128, 4, n]), op=ALU.subtract),
                     reads=[yk, 'mean'], writes=['tt'])
                c.op('pool', lambda n=n: nc.gpsimd.tensor_tensor(out=tt[:, :, 0:n], in0=tt[:, :, 0:n], in1=rstd[:, 0:n].unsqueeze(1).to_broadcast([128, 4, n]), op=ALU.mult),
                     reads=['tt', 'rstd'], writes=['tt'])
                for cc in range(4):
                    c.op('act', lambda cc=cc, oi=oi, n=n: nc.scalar.activation(out=yT[:, cc, oi:oi + n], in_=tt[:, cc, 0:n], func=AF.Silu, bias=lnb[:, cc:cc + 1], scale=lng[:, cc:cc + 1]),
                         reads=['tt', 'lng', 'lnb'], writes=[yk])
            for cc in range(4):
                c.dma('sp', lambda cc=cc: nc.sync.dma_start(out=yts_t.ap()[cc], in_=yT[:, cc, :]), reads=[('yT', m) for m in range(9)], writes=[('yts', cc)])

        c.barrier()
        if STOP_AFTER == 'C':
            c.finish()
            return nc

        emit_shift(99)
        esD = ExitStack()
        with esD:
            ebf = c.sb([128, 1024], F32, esD)
            smk = c.sb([128, 9, 4, 4], F32, esD)
            nmk = c.sb([64, 3, 4, 64], F32, esD)
            bdm = c.sb([64, 128], F32, esD)
            pz = [c.sb([128, 4, 64], BF16, esD) for _ in range(NSQ)]
            v1n = c.sb([64, 3, 4, 65], BF16, esD)
            wkv = c.sb([128, 8, 512], BF16, esD)
            wq = c.sb([128, 8, 128], BF16, esD); wk = c.sb([128, 8, 128], BF16, esD)
            qT = c.sb([128, NOT], BF16, esD); kT = c.sb([128, NTOK], BF16, esD)
            qs = c.sb([128, 2, 64], BF16, esD); ks = c.sb([128, 2, 64], BF16, esD)
            v1 = c.sb([128, 48, 4, 65], BF16, esD)
            kvst = [c.sb([128, 512], F32, esD) for _ in range(2)]
            ef = [c.sb([128, 512], F32, esD) for _ in range(2)]
            pt = [c.sb([128, 512], BF16, esD) for _ in range(2)]
            oev = [c.sb([128, 130], F32, esD) for _ in range(2)]
            ctile = [c.sb([128, 512], F32, esD) for _ in range(2)]
            kTs = [c.sb([128, 2, 128], BF16, esD) for _ in range(2)]
            v1s = [c.sb([128, 4, 65], BF16, esD) for _ in range(2)]
            ess2 = [c.sb([128, 16], F32, esD) for _ in range(2)]
            en = c.sb([64, 256], F32, esD); pn = c.sb([64, 4, 64], BF16, esD)
            osv = c.sb([64, 260], F32, esD)
            c.dma('sp', lambda: nc.sync.dma_start(out=bdm[:], in_=bd_t.ap()), writes=['bdm'])
            c.op('pool', lambda: nc.gpsimd.memset(smk[:], 0.0), writes=['smk'])
            for b in range(NSQ):
                c.op('pool', lambda b=b: nc.gpsimd.memset(pz[b][:], 0.0), writes=[('pz', b)])
            for s in range(2):
                c.op('pool', lambda s=s: nc.gpsimd.memset(v1s[s][:], 1.0), writes=[('v1s', s)])
            c.op('pool', lambda: nc.gpsimd.memset(v1n[:], 1.0), writes=['v1n'])
            n_os = 1
            zl = c.sb([128, 64], BF16, esD); zr = c.sb([128, 260], BF16, esD)
            c.op('pool', lambda: nc.gpsimd.memset(zl[:], 0.0), writes=['zl'])
            c.op('pool', lambda: nc.gpsimd.memset(zr[:], 0.0), writes=['zr'])
            c.op('pe', lambda: nc.tensor.matmul(PB[7][0:64, 0:260], lhsT=zl[:], rhs=zr[:], start=True, stop=False), reads=['zl', 'zr'], writes=['pb7'])
            qb_i = 0
            for g, (wing, d) in enumerate(GROUPS):
                e0 = HALO - wing
                nbl = (NEXT - e0) // (128 * d)
                ebv = ebf[:].rearrange("p (h b q) -> p h b q", h=4, b=2)
                c.dma('sp', lambda g=g: nc.sync.dma_start(out=ebf[:], in_=ebd_t.ap()[g]), reads=[('ebd', g)], writes=['ebf'])
                if g == 0:
                    c.op('dve', lambda: nc.vector.tensor_copy(out=smk[:, 0, :, :], in_=ebv[:, :, 0, 0:4]), reads=['ebf'], writes=['smk'])
                else:
                    for r in range(4):
                        c.op('dve', lambda r=r, g=g: nc.vector.tensor_copy(out=smk[:, 1 + 4 * (g - 1) + r, :, r:r + 1], in_=ebv[:, :, 0, 0:1]), reads=['ebf'], writes=['smk'])
                bsel = bdm[:, 0:64] if g == 0 else bdm[:, 64:128]
                c.op('dve', lambda g=g, bsel=bsel: nc.vector.tensor_tensor(out=nmk[:, g, :, :], in0=ebv[0:64, :, 1, 0:64], in1=bsel.unsqueeze(1).to_broadcast([64, 4, 64]), op=ALU.mult),
                     reads=['ebf', 'bdm'], writes=['nmk'])
                c.dma('pool', lambda g=g: nc.gpsimd.dma_start(out=wkv[:, :, 0:256], in_=winr[:, :, 768 + 256 * g: 1024 + 256 * g]), writes=['wkv_k'])
                c.dma('pool', lambda g=g: nc.gpsimd.dma_start(out=wkv[:, :, 256:512], in_=winr[:, :, 1536 + 256 * g: 1792 + 256 * g]), writes=['wkv_v'])
                nkv = 0
                for r in range(d):
                    for i in range(nbl):
                        bi = r * nbl + i
                        cs0 = e0 + r + 128 * d * i
                        hsl = slice(cs0, cs0 + 127 * d + 1, d)
                        pi = 4 + nkv % 3
                        for k in range(8):
                            c.op('pe', lambda k=k, hsl=hsl, pi=pi: nc.tensor.matmul(PB[pi][:, :], lhsT=hT[:, k, hsl], rhs=wkv[:, k, :], start=(k == 0), stop=(k == 7)),
                                 reads=['hT', 'wkv_k', 'wkv_v'], writes=[('pb', pi)])
                        vc = hv if i == 0 else one1
                        c.op('dve', lambda bi=bi, pi=pi, vc=vc: nc.vector.tensor_scalar(out=v1[:, bi, :, 0:64], in0=PB[pi][:, 256:512].rearrange("p (h e) -> p h e", h=4),
                             scalar1=vc[:, 0:1], scalar2=None, op0=ALU.mult), reads=[('pb', pi), 'hv', 'one1'], writes=[('v1', bi)])
                        c.op('pool', lambda bi=bi, vc=vc: nc.gpsimd.tensor_copy(out=v1[:, bi, :, 64:65], in_=vc[:, 0:1].unsqueeze(1).to_broadcast([128, 4, 1])),
                             reads=['hv', 'one1'], writes=[('v1o', bi)])
                        if i == nbl - 1:
                            s = nkv % 2
                            c.op('act', lambda s=s, pi=pi: nc.scalar.copy(out=kvst[s][:], in_=PB[pi][:, :]), reads=[('pb', pi)], writes=[('kvst', s)])
                            c.dma('sp', lambda s=s, r=r, g=g, d=d, wing=wing: nc.sync.dma_start(out=kvp_t[g].ap()[r:wing:d, :], in_=kvst[s][:]), reads=[('kvst', s)], writes=[('kvp', g, r)])
                        nkv += 1
                pi = 4 + nkv % 3
                for k in range(8):
                    c.op('pe', lambda k=k, pi=pi: nc.tensor.matmul(PB[pi][0:64, :], lhsT=hT[:, k, NEXT:NTOK], rhs=wkv[:, k, :], start=(k == 0), stop=(k == 7)),
                         reads=['hT', 'wkv_k', 'wkv_v'], writes=[('pb', pi)])
                s = nkv % 2
                c.op('act', lambda s=s, pi=pi: nc.scalar.copy(out=kvst[s][0:64, :], in_=PB[pi][0:64, :]), reads=[('pb', pi)], writes=[('kvst', s)])
                c.op('dve', lambda g=g, pi=pi: nc.vector.tensor_copy(out=v1n[:, g, :, 0:64], in_=PB[pi][0:64, 256:512].rearrange("p (h e) -> p h e", h=4)), reads=[('pb', pi)], writes=['v1n'])
                for b in range(NSQ):
                    c.dma('sp', lambda b=b, s=s, g=g, wing=wing: nc.sync.dma_start(out=kvs_t[g].ap()[b, wing - 4:wing, :], in_=kvst[s][4 * b:4 * b + 4, :]), reads=[('kvst', s)], writes=[('kvsn', g, b)])
                for pair in range(2):
                    if _os_env('MK_SKIP_ATT'):
                        continue
                    c.dma('pool', lambda g=g, pair=pair: nc.gpsimd.dma_start(out=wq[:], in_=winr[:, :, 256 * g + 128 * pair: 256 * g + 128 * pair + 128]), writes=['wq'])
                    c.dma('pool', lambda g=g, pair=pair: nc.gpsimd.dma_start(out=wk[:], in_=winr[:, :, 768 + 256 * g + 128 * pair: 768 + 256 * g + 128 * pair + 128]), writes=['wk'])
                    npj = 0
                    for (hc, oi, n) in OT:
                        pi = 4 + npj % 3
                        proj_T(PB[pi][:, 0:n], ('pb', pi), wq, 'wq', slice(hc, hc + n))
                        c.op('act', lambda pi=pi, oi=oi, n=n: nc.scalar.copy(out=qT[:, oi:oi + n], in_=PB[pi][:, 0:n]), reads=[('pb', pi)], writes=['qT'])
                        npj += 1
                    cs_ = e0
                    while cs_ < NTOK:
                        n = min(512, NTOK - cs_)
                        pi = 4 + npj % 3
                        proj_T(PB[pi][:, 0:n], ('pb', pi), wk, 'wk', slice(cs_, cs_ + n))
                        if npj % 2 == 0:
                            c.op('dve', lambda pi=pi, cs_=cs_, n=n: nc.vector.tensor_copy(out=kT[:, cs_:cs_ + n], in_=PB[pi][:, 0:n]), reads=[('pb', pi)], writes=['kT'])
                        else:
                            c.op('act', lambda pi=pi, cs_=cs_, n=n: nc.scalar.copy(out=kT[:, cs_:cs_ + n], in_=PB[pi][:, 0:n]), reads=[('pb', pi)], writes=['kT_a'])
                        npj += 1
                        cs_ += n
                    c.op('dve', lambda pair=pair: nc.vector.tensor_copy(out=qs[:, pair, :], in_=qT[:, NOWN:NOT]), reads=['qT'], writes=['qs'])
                    c.op('dve', lambda pair=pair: nc.vector.tensor_copy(out=ks[:, pair, :], in_=kT[:, NEXT:NTOK]), reads=['kT', 'kT_a'], writes=['ks'])
                    for r in range(d):
                        for i in range(1, nbl):
                            s = qb_i % 2
                            qb_i += 1
                            q0 = r + 128 * d * (i - 1)
                            qsl = slice(q0, q0 + 127 * d + 1, d)
                            SBK = ((0, 1), (4, 5))[s]
                            for hh in range(2):
                                po = hh * 64
                                for blk in range(2):
                                    k0 = e0 + r + 128 * d * (i - 1 + blk)
                                    ksl = slice(k0, k0 + 127 * d + 1, d)
                                    c.op('pe', lambda bk=SBK[hh], po=po, ksl=ksl, qsl=qsl, blk=blk: nc.tensor.matmul(PB[bk][:, blk * 128:blk * 128 + 128], lhsT=kT[po:po + 64, ksl], rhs=qT[po:po + 64, qsl], start=True, stop=True),
                                         reads=['kT', 'kT_a', 'qT'], writes=[('pb', SBK[hh])])
                            for hh in range(2):
                                c.op('act', lambda s=s, hh=hh, bk=SBK[hh]: nc.scalar.activation(out=ef[s][:, hh * 256:(hh + 1) * 256], in_=PB[bk][:, 0:256], func=AF.Exp, scale=0.125),
                                     reads=[('pb', SBK[hh])], writes=[('ef', s, hh)])
                            c.op('dve', lambda s=s, pair=pair: nc.vector.tensor_tensor(out=pt[s][:], in0=ef[s][:], in1=ebf[:, pair * 512:(pair + 1) * 512], op=ALU.mult),
                                 reads=[('ef', s, 0), ('ef', s, 1), 'ebf'], writes=[('pt', s)])
                            for hh in range(2):
                                for blk in range(2):
                                    bi = r * nbl + i - 1 + blk
                                    col = (hh * 2 + blk) * 128
                                    c.op('pe', lambda s=s, hh=hh, blk=blk, bi=bi, col=col, pair=pair: nc.tensor.matmul(PB[2 + s][:, hh * 65:hh * 65 + 65], lhsT=pt[s][:, col:col + 128],
                                         rhs=v1[:, bi, 2 * pair + hh, :], start=(blk == 0), stop=(blk == 1)), reads=[('pt', s), ('v1', bi), ('v1o', bi)], writes=[('pb', 2 + s)])
                            c.op('dve', lambda s=s: nc.vector.tensor_copy(out=oev[s][:], in_=PB[2 + s][:, 0:130]), reads=[('pb', 2 + s)], writes=[('oev', s)])
                            c.dma('sp', lambda s=s, g=g, q0=q0, d=d, pair=pair: nc.sync.dma_start(out=acc_t.ap()[g, q0:q0 + 127 * d + 1:d, pair * 130:(pair + 1) * 130], in_=oev[s][:]),
                                  reads=[('oev', s)], writes=[('acc', g, q0, pair)])
                import os as _os
                ntile = 1 if g == 0 else 4
                if _os.environ.get('MK_SKIP_SAMPLE'):
                    continue
                for b in range(NSQ):
                    for r in range(ntile):
                        s = (b * ntile + r) % 2
                        tix = 0 if g == 0 else 1 + 4 * (g - 1) + r
                        c.dma('sp', lambda s=s, b=b, r=r, g=g, d=d, wing=wing: nc.sync.dma_start(out=ctile[s][:], in_=ck_t[g].ap()[b, r:wing:d, :]), writes=[('ctile', s)])
                        TB = (0, 1)[s]
                        for pr in range(2):
                            c.op('pe', lambda s=s, pr=pr, TB=TB: nc.tensor.transpose(out=PB[TB][:, pr * 128:(pr + 1) * 128], in_=ctile[s][:, pr * 128:(pr + 1) * 128], identity=identf[:]),
                                 reads=[('ctile', s), 'identf'], writes=[('pb', TB)])
                        c.op('act', lambda s=s, TB=TB: nc.scalar.copy(out=kTs[s][:].rearrange("p a k -> p (a k)"), in_=PB[TB][:, 0:256]), reads=[('pb', TB)], writes=[('kTs', s)])
                        c.op('pool', lambda s=s: nc.gpsimd.tensor_copy(out=v1s[s][:, :, 0:64], in_=ctile[s][:, 256:512].rearrange("p (h e) -> p h e", h=4)), reads=[('ctile', s)], writes=[('v1s', s)])
                        for h in range(4):
                            po = (h % 2) * 64
                            bk = (2, 3)[s] if h % 2 == 0 else (4, 5)[s]
                            c.op('pe', lambda s=s, h=h, po=po, b=b, bk=bk: nc.tensor.matmul(PB[bk][:, 256 + 4 * h:260 + 4 * h], lhsT=kTs[s][po:po + 64, h // 2, :], rhs=qs[po:po + 64, h // 2, 4 * b:4 * b + 4], start=True, stop=True),
                                 reads=[('kTs', s), 'qs'], writes=[('pb', bk)])
                        ess = ess2[s]
                        essv = ess[:].rearrange("p (h q) -> p h q", h=4)
                        for par, bk in ((0, (2, 3)[s]), (1, (4, 5)[s])):
                            c.op('act', lambda par=par, bk=bk, essv=essv: nc.scalar.activation(out=essv[:, par:4:2, :], in_=PB[bk][:, 256:272].rearrange("p (h q) -> p h q", h=4)[:, par:4:2, :], func=AF.Exp, scale=0.125),
                                 reads=[('pb', bk)], writes=[('ess', s, par)])
                        c.op('dve', lambda b=b, tix=tix, ess=ess: nc.vector.tensor_tensor(out=pz[b][:, :, 4 * b:4 * b + 4], in0=ess[:].rearrange("p (h q) -> p h q", h=4), in1=smk[:, tix, :, :], op=ALU.mult),
                             reads=[('ess', s, 0), ('ess', s, 1), 'smk'], writes=[('pz', b)])
                        for h in range(4):
                            c.op('pe', lambda s=s, h=h, b=b, st=(n_os == 0): nc.tensor.matmul(PB[7][0:64, 65 * h:65 * h + 65], lhsT=pz[b][:, h, :], rhs=v1s[s][:, h, :], start=st, stop=False),
                                 reads=[('pz', b), ('v1s', s)], writes=['pb7'])
                        n_os += 1
                for h in range(4):
                    po = (h % 2) * 64
                    bk = 6 if h % 2 == 0 else 5
                    c.op('pe', lambda h=h, po=po, bk=bk: nc.tensor.matmul(PB[bk][0:64, 64 * h:64 * h + 64], lhsT=ks[po:po + 64, h // 2, :], rhs=qs[po:po + 64, h // 2, :], start=True, stop=True),
                         reads=['ks', 'qs'], writes=[('pb', bk)])
                for h in range(4):
                    bk = 6 if h % 2 == 0 else 5
                    c.op('act', lambda h=h, bk=bk: nc.scalar.activation(out=en[:, 64 * h:64 * h + 64], in_=PB[bk][0:64, 64 * h:64 * h + 64], func=AF.Exp, scale=0.125), reads=[('pb', bk)], writes=[('en', h)])
                c.op('dve', lambda g=g: nc.vector.tensor_tensor(out=pn[:], in0=en[:].rearrange("p (h q) -> p h q", h=4), in1=nmk[:, g, :, :], op=ALU.mult), reads=[('en', 0), ('en', 1), ('en', 2), ('en', 3), 'nmk'], writes=['pn'])
                for h in range(4):
                    c.op('pe', lambda h=h, g=g: nc.tensor.matmul(PB[7][0:64, 65 * h:65 * h + 65], lhsT=pn[:, h, :], rhs=v1n[:, g, h, :], start=False, stop=(g == 2 and h == 3)),
                         reads=['pn', 'v1n'], writes=['pb7'])
            c.op('act', lambda: nc.scalar.copy(out=osv[:], in_=PB[7][0:64, 0:260]), reads=['pb7'], writes=['osv'])
            c.dma('sp', lambda: nc.sync.dma_start(out=accs_t.ap(), in_=osv[:]), reads=['osv'], writes=['accs'])
        c.barrier()
        es_hT.close()
        if STOP_AFTER == 'D':
            c.finish()
            return nc
        wcor = wco_t.ap().rearrange("(c p) n -> p c n", p=128)
        waor = wao_t.ap().rearrange("(c p) n -> p c n", p=128)
        wor = wo_t.ap().rearrange("(c p) n -> p c n", p=128)
        wrtr = wrt_t.ap().rearrange("(c p) n -> p c n", p=128)
        h2d_t = dscr("h2scr", [NOT, D], BF16)
        esE = ExitStack()
        es.enter_context(esE)
        lgall = c.sb([128, NTILE, 36], F32, esE)
        esE1 = ExitStack()
        with esE1:
            wco = c.sb([128, 4, D], BF16, esE1); wao = c.sb([128, 2, D], BF16, esE1); wo = c.sb([128, 8, D], BF16, esE1)
            wrt = c.sb([128, 8, 36], BF16, esE1); brtb = c.sb([128, 36], F32, esE1)
            c.dma('pool', lambda: nc.gpsimd.dma_start(out=wco[:], in_=wcor), writes=['wco'])
            c.dma('pool', lambda: nc.gpsimd.dma_start(out=wao[:], in_=waor), writes=['wao'])
            c.dma('pool', lambda: nc.gpsimd.dma_start(out=wo[:], in_=wor), writes=['wo'])
            c.dma('pool', lambda: nc.gpsimd.dma_start(out=wrt[:], in_=wrtr), writes=['wrt'])
            c.dma('sp', lambda: nc.sync.dma_start(out=brtb[:], in_=brt_t.ap().partition_broadcast(128)), writes=['brtb'])
            mrow = {}
            for kind, nm in ((0, 'g1'), (1, 'b2'), (2, 'a2')):
                for part, (c0, n) in enumerate(((0, 128), (128, 64))):
                    t_ = c.sb([n, D], F32, esE1)
                    c.dma('sp', lambda t_=t_, kind=kind, c0=c0, n=n: nc.sync.dma_start(out=t_[:], in_=modrows_t.ap()[kind, c0:c0 + n, :]),
                          reads=[('modrows', kind, part)], writes=[('mrow', nm, part)])
                    mrow[(nm, part)] = t_
            c.op('pool', lambda: nc.gpsimd.memset(lgall[:], 0.0), writes=['lgall'])
            ytile = [c.sb([128, 4, 512], BF16, esE1) for _ in range(2)]
            oT = c.sb([128, 2, 512], BF16, esE1)
            acc3 = [c.sb([128, 3, 260], F32, esE1) for _ in range(2)]
            osum = c.sb([128, 260], F32, esE1); rden = c.sb([128, 4], F32, esE1); ob = c.sb([128, 256], BF16, esE1)
            sga = [c.sb([128, 512], BF16, esE1) for _ in range(2)]; sgb = [c.sb([128, 512], BF16, esE1) for _ in range(2)]
            m1 = [c.sb([128, 512], F32, esE1) for _ in range(2)]; m2 = [c.sb([128, 512], F32, esE1) for _ in range(2)]
            mixT = c.sb([128, 8, 512], BF16, esE1)
            xt2 = [c.sb([128, D], F32, esE1) for _ in range(2)]; tmp = [c.sb([128, D], F32, esE1) for _ in range(2)]
            x1t = [c.sb([128, D], F32, esE1) for _ in range(2)]; t2 = [c.sb([128, D], F32, esE1) for _ in range(2)]
            h2b = [c.sb([128, D], BF16, esE1) for _ in range(2)]
            h2T = c.sb([128, 8, 128], BF16, esE1); st2 = [c.sb([128, 4], F32, esE1) for _ in range(2)]
            junk2 = c.sb([128, D], BF16, esE1)
            sub_i = 0
            for M, (hc, oi, n) in enumerate(OT):
                part = 0 if M < 8 else 1
                rr = 128 if M < 8 else 64
                nsub = n // rr
                ys_ = M % 2
                c.dma('sp', lambda ys_=ys_, oi=oi, n=n: nc.sync.dma_start(out=ytile[ys_][:, :, 0:n], in_=yts_t.ap()[:, :, oi:oi + n].rearrange("c p t -> p c t")),
                      reads=[('yts', cc) for cc in range(4)], writes=[('ytile', ys_)])
                for t in range(nsub):
                    a_ = (M * 4 + t) % 2
                    r0 = oi + rr * t
                    if M < 8:
                        c.dma('sp', lambda a_=a_, r0=r0: nc.sync.dma_start(out=acc3[a_][:], in_=acc_t.ap()[:, r0:r0 + 128, :].rearrange("g t c -> t g c")),
                              reads=[k for k in c.state if isinstance(k, tuple) and k[0] == 'acc'], writes=[('acc3', a_)])
                        c.op('dve', lambda a_=a_: nc.vector.tensor_tensor(out=osum[:], in0=acc3[a_][:, 0, :], in1=acc3[a_][:, 1, :], op=ALU.add), reads=[('acc3', a_)], writes=['osum'])
                        c.op('dve', lambda a_=a_: nc.vector.tensor_tensor(out=osum[:], in0=osum[:], in1=acc3[a_][:, 2, :], op=ALU.add), reads=[('acc3', a_), 'osum'], writes=['osum'])
                    else:
                        c.dma('sp', lambda a_=a_: nc.sync.dma_start(out=acc3[a_][0:64, 0, :], in_=accs_t.ap()), reads=['accs'], writes=[('acc3', a_)])
                        c.op('dve', lambda a_=a_: nc.vector.tensor_copy(out=osum[0:64, :], in_=acc3[a_][0:64, 0, :]), reads=[('acc3', a_)], writes=['osum'])
                    ov = osum[0:rr, :].rearrange("p (h e) -> p h e", h=4)
                    c.op('dve', lambda ov=ov, rr=rr: nc.vector.reciprocal(out=rden[0:rr, :].unsqueeze(2), in_=ov[:, :, 64:65]), reads=['osum'], writes=['rden'])
                    c.op('dve', lambda ov=ov, rr=rr: nc.vector.tensor_tensor(out=ob[0:rr, :].rearrange("p (h e) -> p h e", h=4), in0=ov[:, :, 0:64],
                         in1=rden[0:rr, :].unsqueeze(2).to_broadcast([rr, 4, 64]), op=ALU.mult), reads=['osum', 'rden'], writes=['ob'])
                    pv4 = pbf(4).rearrange("p (k t) -> p k t", k=8)
                    for k in range(2):
                        c.op('pe', lambda k=k, rr=rr, pv4=pv4: nc.tensor.transpose(out=pv4[:, k, 0:rr], in_=ob[0:rr, k * 128:(k + 1) * 128], identity=ident[0:rr, 0:rr]),
                             reads=['ob', 'ident'], writes=[('pb', 4)])
                    c.op('act', lambda t=t, rr=rr, pv4=pv4: nc.scalar.copy(out=oT[:, :, rr * t:rr * t + rr], in_=pv4[:, 0:2, 0:rr]), reads=[('pb', 4)], writes=['oT'])
                for j in range(8):
                    gs = j % 2
                    c.dma('sp', lambda gs=gs, j=j, oi=oi, n=n: nc.sync.dma_start(out=sga[gs][:, 0:n], in_=sg_t.ap()[j, :, oi:oi + n]), reads=[('sg', j)], writes=[('sga', gs)])
                    c.dma('sp', lambda gs=gs, j=j, oi=oi, n=n: nc.sync.dma_start(out=sgb[gs][:, 0:n], in_=sg_t.ap()[8 + j, :, oi:oi + n]), reads=[('sg', 8 + j)], writes=[('sgb', gs)])
                    pa_i, pb_i = (0, 1) if gs == 0 else (6, 7)
                    for cc in range(4):
                        c.op('pe', lambda cc=cc, j=j, pa_i=pa_i, ys_=ys_, n=n: nc.tensor.matmul(PB[pa_i][:, 0:n], lhsT=wco[:, cc, j * 128:(j + 1) * 128], rhs=ytile[ys_][:, cc, 0:n], start=(cc == 0), stop=(cc == 3)),
                             reads=['wco', ('ytile', ys_)], writes=[('pb', pa_i)])
                    for cc in range(2):
                        c.op('pe', lambda cc=cc, j=j, pb_i=pb_i, n=n: nc.tensor.matmul(PB[pb_i][:, 0:n], lhsT=wao[:, cc, j * 128:(j + 1) * 128], rhs=oT[:, cc, 0:n], start=(cc == 0), stop=(cc == 1)),
                             reads=['wao', 'oT'], writes=[('pb', pb_i)])
                    c.op('dve', lambda gs=gs, pa_i=pa_i, n=n: nc.vector.tensor_tensor(out=m1[gs][:, 0:n], in0=PB[pa_i][:, 0:n], in1=sga[gs][:, 0:n], op=ALU.mult), reads=[('pb', pa_i), ('sga', gs)], writes=[('m1', gs)])
                    c.op('dve', lambda gs=gs, pb_i=pb_i, n=n: nc.vector.tensor_tensor(out=m2[gs][:, 0:n], in0=PB[pb_i][:, 0:n], in1=sgb[gs][:, 0:n], op=ALU.mult), reads=[('pb', pb_i), ('sgb', gs)], writes=[('m2', gs)])
                    c.op('pool', lambda gs=gs, j=j, n=n: nc.gpsimd.tensor_tensor(out=mixT[:, j, 0:n], in0=m1[gs][:, 0:n], in1=m2[gs][:, 0:n], op=ALU.add), reads=[('m1', gs), ('m2', gs)], writes=[('mixT', j)])
                for t in range(nsub):
                    xs_ = sub_i % 2
                    sub_i += 1
                    r0 = oi + rr * t
                    tile_i = r0 // 128 if M < 8 else 32
                    src = xp[HALO + r0:HALO + r0 + 128, :] if M < 8 else xs
                    c.dma('sp', lambda xs_=xs_, src=src, rr=rr: nc.sync.dma_start(out=xt2[xs_][0:rr, :], in_=src), writes=[('xt2', xs_)])
                    for hf in range(2):
                        for j in range(8):
                            c.op('pe', lambda hf=hf, j=j, t=t, rr=rr: nc.tensor.matmul(PB[2 + hf][0:rr, :], lhsT=mixT[:, j, rr * t:rr * t + rr], rhs=wo[:, j, hf * 512:(hf + 1) * 512], start=(j == 0), stop=(j == 7)),
                                 reads=[('mixT', j), 'wo'], writes=[('pb', 2 + hf)])
                        c.op('dve', lambda hf=hf, xs_=xs_, rr=rr, part=part: nc.vector.tensor_tensor(out=tmp[xs_][0:rr, hf * 512:(hf + 1) * 512], in0=PB[2 + hf][0:rr, :],
                             in1=mrow[('g1', part)][0:rr, hf * 512:(hf + 1) * 512], op=ALU.mult), reads=[('pb', 2 + hf), ('mrow', 'g1', part)], writes=[('tmp', xs_)])
                    c.op('pool', lambda xs_=xs_, rr=rr: nc.gpsimd.tensor_tensor(out=x1t[xs_][0:rr, :], in0=tmp[xs_][0:rr, :], in1=xt2[xs_][0:rr, :], op=ALU.add),
                         reads=[('tmp', xs_), ('xt2', xs_)], writes=[('x1t', xs_)])
                    c.dma('act', lambda xs_=xs_, r0=r0, rr=rr: nc.scalar.dma_start(out=x1_t.ap()[r0:r0 + rr, :], in_=x1t[xs_][0:rr, :]), reads=[('x1t', xs_)], writes=[('x1', tile_i)])
                    c.op('act', lambda xs_=xs_, rr=rr: nc.scalar.activation(out=junk2[0:rr, :], in_=x1t[xs_][0:rr, :], func=AF.Square, accum_out=st2[xs_][0:rr, 0:1]),
                         reads=[('x1t', xs_)], writes=[('st2', xs_), 'junk2'])
                    c.op('act', lambda xs_=xs_, rr=rr: nc.scalar.activation(out=st2[xs_][0:rr, 1:2], in_=st2[xs_][0:rr, 0:1], func=AF.Sqrt, scale=1.0 / D, bias=epsb[0:rr, :]),
                         reads=[('st2', xs_), 'epsb'], writes=[('st2', xs_)])
                    c.op('dve', lambda xs_=xs_, rr=rr: nc.vector.reciprocal(out=st2[xs_][0:rr, 2:3], in_=st2[xs_][0:rr, 1:2]), reads=[('st2', xs_)], writes=[('st2', xs_)])
                    c.op('dve', lambda xs_=xs_, rr=rr, part=part: nc.vector.scalar_tensor_tensor(out=t2[xs_][0:rr, :], in0=x1t[xs_][0:rr, :], scalar=st2[xs_][0:rr, 2:3],
                         in1=mrow[('a2', part)][0:rr, :], op0=ALU.mult, op1=ALU.mult), reads=[('x1t', xs_), ('st2', xs_), ('mrow', 'a2', part)], writes=[('t2', xs_)])
                    c.op('pool', lambda xs_=xs_, rr=rr, part=part: nc.gpsimd.tensor_tensor(out=h2b[xs_][0:rr, :], in0=t2[xs_][0:rr, :], in1=mrow[('b2', part)][0:rr, :], op=ALU.add),
                         reads=[('t2', xs_), ('mrow', 'b2', part)], writes=[('h2b', xs_)])
                    c.dma('act', lambda xs_=xs_, r0=r0, rr=rr: nc.scalar.dma_start(out=h2d_t.ap()[r0:r0 + rr, :], in_=h2b[xs_][0:rr, :]), reads=[('h2b', xs_)], writes=[('h2d', tile_i)])
                    pv5 = pbf(5).rearrange("p (k t) -> p k t", k=8)
                    for k in range(8):
                        c.op('pe', lambda k=k, xs_=xs_, rr=rr, pv5=pv5: nc.tensor.transpose(out=pv5[:, k, 0:rr], in_=h2b[xs_][0:rr, k * 128:(k + 1) * 128], identity=ident[0:rr, 0:rr]),
                             reads=[('h2b', xs_), 'ident'], writes=[('pb', 5)])
                    c.op('act', lambda rr=rr, pv5=pv5: nc.scalar.copy(out=h2T[:, :, 0:rr], in_=pv5[:, :, 0:rr]), reads=[('pb', 5)], writes=['h2T'])
                    for k in range(8):
                        c.op('pe', lambda k=k, rr=rr: nc.tensor.matmul(PB[4][0:rr, 0:36], lhsT=h2T[:, k, 0:rr], rhs=wrt[:, k, :], start=(k == 0), stop=(k == 7)),
                             reads=['h2T', 'wrt'], writes=[('pb', 4)])
                    c.op('dve', lambda rr=rr, tile_i=tile_i: nc.vector.tensor_tensor(out=lgall[0:rr, tile_i, :], in0=PB[4][0:rr, 0:36], in1=brtb[0:rr, :], op=ALU.add),
                         reads=[('pb', 4), 'brtb', 'lgall'], writes=['lgall'])
        c.barrier()
        if STOP_AFTER == 'E':
            c.finish()
            return nc
        NT = NTILE
        esF = ExitStack()
        with esF:
            def ft(shape, dt=F32):
                return c.sb(shape, dt, esF)
            gmx = ft([128, NT]); ohg = ft([128, NT, 4]); gsh = ft([128, NT, 4]); gex = ft([128, NT, 4]); gsum = ft([128, NT]); pgr = ft([128, NT])
            pen = ft([128, NT, 4]); em = ft([128, NT, 32]); m8 = ft([128, NT, 8]); i8 = ft([128, NT, 8], U32)
            e0f = ft([128, NT]); e1f = ft([128, NT]); dv = ft([128, NT]); w0 = ft([128, NT]); w1 = ft([128, NT])
            oh0 = ft([128, NT, 32]); oh1 = ft([128, NT, 32]); mm_ = ft([128, NT, 32]); cs = ft([128, NT + 1, 32])
            base = ft([128, NT, 32]); prod = ft([128, NT, 32]); d0f = ft([128, NT]); d1f = ft([128, NT])
            d0i = ft([128, NT], I32); d1i = ft([128, NT], I32)
            io32i = ft([128, 32], I32); io32 = ft([128, 32]); thri = ft([128, NBLK], I32); thr = ft([128, NBLK])
            cnt = ft([128, 32]); cni = ft([128, 32], I32); pad = ft([128, 32]); pa_ = ft([128, 32]); pb_ = ft([128, 32]); pst = ft([128, 32])
            cmpb = ft([128, NBLK, 32]); bef = ft([128, NBLK])
            c.op('pool', lambda: nc.gpsimd.iota(io32i[:], pattern=[[1, 32]], base=0, channel_multiplier=0), writes=['io32i'])
            c.op('pool', lambda: nc.gpsimd.iota(thri[:], pattern=[[BLK, NBLK]], base=0, channel_multiplier=0), writes=['thri'])
            c.op('dve', lambda: nc.vector.tensor_copy(out=io32[:], in_=io32i[:]), reads=['io32i'], writes=['io32'])
            c.op('dve', lambda: nc.vector.tensor_copy(out=thr[:], in_=thri[:]), reads=['thri'], writes=['thr'])
            R = ['lgall']
            gl = lgall[:, :, 0:4]
            V = nc.vector
            c.op('dve', lambda: V.tensor_reduce(out=gmx[:], in_=gl, axis=AX.X, op=ALU.max), reads=R, writes=['gmx'])
            c.op('dve', lambda: V.tensor_tensor(out=ohg[:], in0=gl, in1=gmx[:].unsqueeze(2).to_broadcast([128, NT, 4]), op=ALU.is_equal), reads=R + ['gmx'], writes=['ohg'])
            c.op('dve', lambda: V.tensor_tensor(out=gsh[:], in0=gl, in1=gmx[:].unsqueeze(2).to_broadcast([128, NT, 4]), op=ALU.subtract), reads=R + ['gmx'], writes=['gsh'])
            c.op('act', lambda: nc.scalar.activation(out=gex[:], in_=gsh[:], func=AF.Exp), reads=['gsh'], writes=['gex'])
            c.op('dve', lambda: V.tensor_reduce(out=gsum[:], in_=gex[:], axis=AX.X, op=ALU.add), reads=['gex'], writes=['gsum'])
            c.op('dve', lambda: V.reciprocal(out=pgr[:], in_=gsum[:]), reads=['gsum'], writes=['pgr'])
            c.op('dve', lambda: V.tensor_scalar(out=pen[:], in0=ohg[:], scalar1=-1.0, scalar2=1e30, op0=ALU.add, op1=ALU.mult), reads=['ohg'], writes=['pen'])
            c.op('dve', lambda: V.tensor_tensor(out=em[:].rearrange("p t (g e) -> p t g e", g=4), in0=lgall[:, :, 4:36].rearrange("p t (g e) -> p t g e", g=4),
                 in1=pen[:].unsqueeze(3).to_broadcast([128, NT, 4, 8]), op=ALU.add), reads=R + ['pen'], writes=['em'])
            for i in range(NT):
                c.op('dve', lambda i=i: V.max(out=m8[:, i, :], in_=em[:, i, :]), reads=['em'], writes=['m8'])
                c.op('dve', lambda i=i: V.max_index(out=i8[:, i, :], in_max=m8[:, i, :], in_values=em[:, i, :]), reads=['em', 'm8'], writes=['i8'])
            c.op('dve', lambda: V.tensor_copy(out=e0f[:], in_=i8[:, :, 0]), reads=['i8'], writes=['e0f'])
            c.op('dve', lambda: V.tensor_copy(out=e1f[:], in_=i8[:, :, 1]), reads=['i8'], writes=['e1f'])
            c.op('dve', lambda: V.tensor_tensor(out=dv[:], in0=m8[:, :, 1], in1=m8[:, :, 0], op=ALU.subtract), reads=['m8'], writes=['dv'])
            c.op('act', lambda: nc.scalar.activation(out=dv[:], in_=dv[:], func=AF.Exp), reads=['dv'], writes=['dv'])
            c.op('dve', lambda: V.tensor_scalar(out=dv[:], in0=dv[:], scalar1=1.0, scalar2=None, op0=ALU.add), reads=['dv'], writes=['dv'])
            c.op('dve', lambda: V.reciprocal(out=w0[:], in_=dv[:]), reads=['dv'], writes=['w0'])
            c.op('dve', lambda: V.tensor_tensor(out=w0[:], in0=w0[:], in1=pgr[:], op=ALU.mult), reads=['w0', 'pgr'], writes=['w0'])
            c.op('dve', lambda: V.tensor_tensor(out=w1[:], in0=pgr[:], in1=w0[:], op=ALU.subtract), reads=['w0', 'pgr'], writes=['w1'])
            iob = io32[:].unsqueeze(1).to_broadcast([128, NT, 32])
            c.op('dve', lambda: V.tensor_tensor(out=oh0[:], in0=iob, in1=e0f[:].unsqueeze(2).to_broadcast([128, NT, 32]), op=ALU.is_equal), reads=['io32', 'e0f'], writes=['oh0'])
            c.op('dve', lambda: V.tensor_tensor(out=oh1[:], in0=iob, in1=e1f[:].unsqueeze(2).to_broadcast([128, NT, 32]), op=ALU.is_equal), reads=['io32', 'e1f'], writes=['oh1'])
            c.op('dve', lambda: V.memset(oh0[64:128, NT - 1, :], 0.0), reads=['oh0'], writes=['oh0'])
            c.op('dve', lambda: V.memset(oh1[64:128, NT - 1, :], 0.0), reads=['oh1'], writes=['oh1'])
            c.op('dve', lambda: V.tensor_tensor(out=mm_[:], in0=oh0[:], in1=oh1[:], op=ALU.add), reads=['oh0', 'oh1'], writes=['mm'])
            c.op('dve', lambda: V.memset(cs[:, 0, :], 0.0), writes=['cs'])
            for i in range(NT):
                c.op('dve', lambda i=i: V.tensor_tensor(out=cs[:, i + 1, :], in0=cs[:, i, :], in1=mm_[:, i, :], op=ALU.add), reads=['cs', 'mm'], writes=['cs'])
            for i in range(NT):
                bk = i // 16
                co = (i % 16) * 32
                c.op('pe', lambda i=i, bk=bk, co=co: nc.tensor.matmul(PB[bk][:, co:co + 32], lhsT=suf[:], rhs=mm_[:, i, :], start=True, stop=False), reads=['suf', 'mm'], writes=[('pb', bk)])
                c.op('pe', lambda i=i, bk=bk, co=co: nc.tensor.matmul(PB[bk][:, co:co + 32], lhsT=onesf[:], rhs=cs[:, i, :], start=False, stop=True), reads=['onesf', 'cs'], writes=[('pb', bk)])
            c.op('pe', lambda: nc.tensor.matmul(PB[3][:, 0:32], lhsT=onesf[:], rhs=cs[:, NT, :], start=True, stop=True), reads=['onesf', 'cs'], writes=[('pb', 3)])
            c.op('dve', lambda: V.tensor_scalar(out=cni[:], in0=PB[3][:, 0:32], scalar1=float(BLK - 1), scalar2=None, op0=ALU.add), reads=[('pb', 3)], writes=['cni'])
            c.op('dve', lambda: V.tensor_scalar(out=cni[:], in0=cni[:], scalar1=int(math.log2(BLK)), scalar2=int(math.log2(BLK)), op0=ALU.arith_shift_right, op1=ALU.logical_shift_left), reads=['cni'], writes=['cni'])
            c.op('dve', lambda: V.tensor_copy(out=pad[:], in_=cni[:]), reads=['cni'], writes=['pad'])
            src_, dst_ = pad, pa_
            for sft in (1, 2, 4, 8, 16):
                c.op('dve', lambda src_=src_, dst_=dst_, sft=sft: V.tensor_copy(out=dst_[:, 0:sft], in_=src_[:, 0:sft]), reads=['pfx', 'pad'], writes=['pfx'])
                c.op('dve', lambda src_=src_, dst_=dst_, sft=sft: V.tensor_tensor(out=dst_[:, sft:32], in0=src_[:, sft:32], in1=src_[:, 0:32 - sft], op=ALU.add), reads=['pfx', 'pad'], writes=['pfx'])
                src_, dst_ = dst_, (pb_ if dst_ is pa_ else pa_)
            pend = src_
            c.op('dve', lambda: V.tensor_tensor(out=pst[:], in0=pend[:], in1=pad[:], op=ALU.subtract), reads=['pfx', 'pad'], writes=['pst'])
            for bk in range(3):
                t0 = bk * 16
                nt_ = min(16, NT - t0)
                c.op('dve', lambda bk=bk, t0=t0, nt_=nt_: V.tensor_tensor(out=base[:, t0:t0 + nt_, :], in0=PB[bk][:, 0:nt_ * 32].rearrange("p (t e) -> p t e", e=32),
                     in1=pst[:].unsqueeze(1).to_broadcast([128, nt_, 32]), op=ALU.add), reads=[('pb', bk), 'pst'], writes=['base'])
            for oh_, df_, di_, nm in ((oh0, d0f, d0i, 'd0'), (oh1, d1f, d1i, 'd1')):
                c.op('dve', lambda oh_=oh_: V.tensor_tensor(out=prod[:], in0=oh_[:], in1=base[:], op=ALU.mult), reads=['oh0', 'oh1', 'base'], writes=['prod'])
                c.op('dve', lambda df_=df_: V.tensor_reduce(out=df_[:], in_=prod[:], axis=AX.X, op=ALU.add), reads=['prod'], writes=[nm + 'f'])
                c.op('dve', lambda df_=df_, di_=di_: V.tensor_copy(out=di_[:], in_=df_[:]), reads=[nm + 'f'], writes=[nm])
            c.op('dve', lambda: V.tensor_tensor(out=cmpb[:], in0=pend[:].unsqueeze(1).to_broadcast([128, NBLK, 32]), in1=thr[:].unsqueeze(2).to_broadcast([128, NBLK, 32]), op=ALU.is_le),
                 reads=['pfx', 'thr'], writes=['cmpb'])
            c.op('dve', lambda: V.tensor_reduce(out=bef[:], in_=cmpb[:], axis=AX.X, op=ALU.add), reads=['cmpb'], writes=['bef'])
            c.op('dve', lambda: V.tensor_scalar(out=bef[:], in0=bef[:], scalar1=31.0, scalar2=None, op0=ALU.min), reads=['bef'], writes=['bef'])
            pio = ft([128, 1], I32); piof = ft([128, 1]); widxf = ft([128, NBLK]); widx = ft([128, NBLK], I32)
            c.op('pool', lambda: nc.gpsimd.iota(pio[:], pattern=[[0, 1]], base=0, channel_multiplier=1), writes=['pio'])
            c.op('dve', lambda: V.tensor_copy(out=piof[:], in_=pio[:]), reads=['pio'], writes=['piof'])
            c.op('dve', lambda: V.tensor_scalar(out=widxf[:], in0=bef[:], scalar1=128.0, scalar2=piof[:, 0:1], op0=ALU.mult, op1=ALU.add), reads=['bef', 'piof'], writes=['widxf'])
            c.op('dve', lambda: V.tensor_copy(out=widx[:], in_=widxf[:]), reads=['widxf'], writes=['widx'])
            zt = ft([128, D], BF16)
            c.op('pool', lambda: nc.gpsimd.memset(zt[:], 0.0), writes=['zt'])
            xsv = xsd_t.ap().rearrange("(b p) d -> p b d", p=128)
            zkeys = []
            NRB = CAP // 128
            for q4 in range(8):
                b0 = q4 * (NRB // 8)
                nb_ = NRB // 8
                c.dma('act', lambda b0=b0, nb_=nb_: nc.scalar.dma_start(out=xsv[:, b0:b0 + nb_, :], in_=zt[:].unsqueeze(1).to_broadcast([128, nb_, D])), reads=['zt'], writes=[('xsz', q4)])
                zkeys.append(('xsz', q4))
            h2t = [ft([128, D], BF16) for _ in range(2)]
            skeys = []
            for i in range(NT):
                s = i % 2
                rr = 128 if i < NT - 1 else 64
                c.dma('sp', lambda s=s, i=i, rr=rr: nc.sync.dma_start(out=h2t[s][0:rr, :], in_=h2d_t.ap()[i * 128:i * 128 + rr, :]), reads=[('h2d', i)], writes=[('h2t', s)])
                for di_, nm in ((d0i, 'd0'), (d1i, 'd1')):
                    c.dma('pool', lambda s=s, i=i, rr=rr, di_=di_: nc.gpsimd.indirect_dma_start(out=xsd_t.ap(), out_offset=bass.IndirectOffsetOnAxis(ap=di_[0:rr, i:i + 1], axis=0),
                          in_=h2t[s][0:rr, :], in_offset=None), reads=[('h2t', s), nm] + zkeys, writes=[('xss', i, nm)])
                    skeys.append(('xss', i, nm))
            xsb = [ft([128, D], BF16) for _ in range(2)]; xsT = [ft([128, 8, 128], BF16) for _ in range(2)]
            wg = [ft([128, 8, 512], BF16) for _ in range(2)]; wu = [ft([128, 8, 512], BF16) for _ in range(2)]; wd = [ft([128, 4, D], BF16) for _ in range(2)]
            actt = [ft([128, 512]) for _ in range(2)]; ab = [ft([128, 512], BF16) for _ in range(2)]; aT = [ft([128, 4, 128], BF16) for _ in range(2)]
            yev = [ft([128, D]) for _ in range(2)]
            wegv = weg_t.ap().rearrange("e (p k) f -> (e p) (k f)", p=128)
            weuv = weu_t.ap().rearrange("e (p k) f -> (e p) (k f)", p=128)
            wedv = wed_t.ap().rearrange("e (p k) f -> (e p) (k f)", p=128)
            items = [(b, sub) for b in range(NBLK) for sub in range(SUBB)]

            def load_xsb(n):
                b, sub = items[n]
                xs_ = n % 2
                r0 = b * BLK + sub * 128
                c.dma('sp', lambda xs_=xs_, r0=r0: nc.sync.dma_start(out=xsb[xs_][:], in_=xsd_t.ap()[r0:r0 + 128, :]), reads=skeys + zkeys, writes=[('xsb', xs_)])

            load_xsb(0)
            for n, (b, sub) in enumerate(items):
                s = b % 2
                xs_ = n % 2
                if sub == 0:
                    for wt_, wv_, nm in ((wg, wegv, 'wg'), (wu, weuv, 'wu'), (wd, wedv, 'wd')):
                        c.dma('pool', lambda s=s, b=b, wt_=wt_, wv_=wv_: nc.gpsimd.indirect_dma_start(out=wt_[s][:].rearrange("p k f -> p (k f)"), out_offset=None, in_=wv_,
                              in_offset=bass.IndirectOffsetOnAxis(ap=widx[:, b:b + 1], axis=0)), reads=['widx'], writes=[(nm, s)])
                if n + 1 < len(items):
                    load_xsb(n + 1)
                pv0 = pbf(0).rearrange("p (k t) -> p k t", k=8)
                for k in range(8):
                    c.op('pe', lambda k=k, xs_=xs_, pv0=pv0: nc.tensor.transpose(out=pv0[:, k, :], in_=xsb[xs_][:, k:k + 8 * 127 + 1:8], identity=ident[:]), reads=[('xsb', xs_), 'ident'], writes=[('pb', 0)])
                c.op('act', lambda xs_=xs_, pv0=pv0: nc.scalar.copy(out=xsT[xs_][:], in_=pv0), reads=[('pb', 0)], writes=[('xsT', xs_)])
                gi, ui = (1, 2) if xs_ == 0 else (6, 7)
                for k in range(8):
                    c.op('pe', lambda k=k, s=s, xs_=xs_, gi=gi: nc.tensor.matmul(PB[gi][:, :], lhsT=xsT[xs_][:, k, :], rhs=wg[s][:, k, :], start=(k == 0), stop=(k == 7)), reads=[('xsT', xs_), ('wg', s)], writes=[('pb', gi)])
                for k in range(8):
                    c.op('pe', lambda k=k, s=s, xs_=xs_, ui=ui: nc.tensor.matmul(PB[ui][:, :], lhsT=xsT[xs_][:, k, :], rhs=wu[s][:, k, :], start=(k == 0), stop=(k == 7)), reads=[('xsT', xs_), ('wu', s)], writes=[('pb', ui)])
                c.op('act', lambda xs_=xs_, gi=gi: nc.scalar.activation(out=actt[xs_][:], in_=PB[gi][:, :], func=AF.Silu), reads=[('pb', gi)], writes=[('actt', xs_)])
                c.op('dve', lambda xs_=xs_, ui=ui: V.tensor_tensor(out=ab[xs_][:], in0=PB[ui][:, :], in1=actt[xs_][:], op=ALU.mult), reads=[('pb', ui), ('actt', xs_)], writes=[('ab', xs_)])
                pv3 = pbf(3).rearrange("p (k t) -> p k t", k=8)
                for k in range(4):
                    c.op('pe', lambda k=k, xs_=xs_, pv3=pv3: nc.tensor.transpose(out=pv3[:, k, :], in_=ab[xs_][:, k:k + 4 * 127 + 1:4], identity=ident[:]), reads=[('ab', xs_), 'ident'], writes=[('pb', 3)])
                c.op('dve', lambda xs_=xs_, pv3=pv3: V.tensor_copy(out=aT[xs_][:], in_=pv3[:, 0:4, :]), reads=[('pb', 3)], writes=[('aT', xs_)])
                for hf in range(2):
                    for k in range(4):
                        c.op('pe', lambda k=k, s=s, xs_=xs_, hf=hf: nc.tensor.matmul(PB[4 + hf][:, :], lhsT=aT[xs_][:, k, :], rhs=wd[s][:, k, hf * 512:(hf + 1) * 512], start=(k == 0), stop=(k == 3)),
                             reads=[('aT', xs_), ('wd', s)], writes=[('pb', 4 + hf)])
                c.op('act', lambda xs_=xs_: nc.scalar.copy(out=yev[xs_][:, 0:512], in_=PB[4][:, :]), reads=[('pb', 4)], writes=[('yev', xs_, 0)])
                c.op('dve', lambda xs_=xs_: V.tensor_copy(out=yev[xs_][:, 512:1024], in_=PB[5][:, :]), reads=[('pb', 5)], writes=[('yev', xs_, 1)])
                r0 = b * BLK + sub * 128
                c.dma('act', lambda xs_=xs_, r0=r0: nc.scalar.dma_start(out=ysd_t.ap()[r0:r0 + 128, :], in_=yev[xs_][:]), reads=[('yev', xs_, 0), ('yev', xs_, 1)], writes=[('ysd', n)])
            ykeys = [('ysd', n) for n in range(len(items))]
            g2r = {}
            for part, (c0, n) in enumerate(((0, 128), (128, 64))):
                t_ = ft([n, D])
                c.dma('sp', lambda t_=t_, c0=c0, n=n: nc.sync.dma_start(out=t_[:], in_=modrows_t.ap()[3, c0:c0 + n, :]), reads=[('modrows', 3, part)], writes=[('g2r', part)])
                g2r[part] = t_
            gfin = ft([128, D])
            c.dma('sp', lambda: nc.sync.dma_start(out=gfin[:], in_=gfin_t.ap().partition_broadcast(128)), writes=['gfin'])
            NBF = 3
            y0 = [ft([128, D]) for _ in range(NBF)]; y1 = [ft([128, D]) for _ in range(NBF)]; x1r = [ft([128, D]) for _ in range(NBF)]
            fa = [ft([128, D]) for _ in range(NBF)]; st3 = [ft([128, 4]) for _ in range(NBF)]
            junk3 = ft([128, D], BF16)

            def comb_stage1(i):
                s = i % NBF
                rr = 128 if i < NT - 1 else 64
                part = 0 if i < NT - 1 else 1
                c.dma('pool', lambda: nc.gpsimd.indirect_dma_start(out=y0[s][0:rr, :], out_offset=None, in_=ysd_t.ap(),
                      in_offset=bass.IndirectOffsetOnAxis(ap=d0i[0:rr, i:i + 1], axis=0)), reads=ykeys + ['d0'], writes=[('y0', s)])
                c.dma('pool', lambda: nc.gpsimd.indirect_dma_start(out=y1[s][0:rr, :], out_offset=None, in_=ysd_t.ap(),
                      in_offset=bass.IndirectOffsetOnAxis(ap=d1i[0:rr, i:i + 1], axis=0)), reads=ykeys + ['d1'], writes=[('y1', s)])
                c.dma('sp', lambda: nc.sync.dma_start(out=x1r[s][0:rr, :], in_=x1_t.ap()[i * 128:i * 128 + rr, :]), reads=[('x1', i)], writes=[('x1r', s)])
                c.op('act', lambda: nc.scalar.activation(out=fa[s][0:rr, :], in_=y0[s][0:rr, :], func=AF.Copy, scale=w0[0:rr, i:i + 1]), reads=[('y0', s), 'w0'], writes=[('fa', s)])
                c.op('dve', lambda: V.scalar_tensor_tensor(out=fa[s][0:rr, :], in0=y1[s][0:rr, :], scalar=w1[0:rr, i:i + 1], in1=fa[s][0:rr, :], op0=ALU.mult, op1=ALU.add),
                     reads=[('y1', s), 'w1', ('fa', s)], writes=[('fa', s)])
                c.op('dve', lambda: V.tensor_tensor(out=fa[s][0:rr, :], in0=fa[s][0:rr, :], in1=g2r[part][0:rr, :], op=ALU.mult), reads=[('fa', s), ('g2r', part)], writes=[('fa', s)])
                c.op('pool', lambda: nc.gpsimd.tensor_tensor(out=x1r[s][0:rr, :], in0=fa[s][0:rr, :], in1=x1r[s][0:rr, :], op=ALU.add), reads=[('fa', s), ('x1r', s)], writes=[('x1r', s)])

            def comb_stage2(i):
                s = i % NBF
                rr = 128 if i < NT - 1 else 64
                c.op('act', lambda: nc.scalar.activation(out=junk3[0:rr, :], in_=x1r[s][0:rr, :], func=AF.Square, accum_out=st3[s][0:rr, 0:1]), reads=[('x1r', s)], writes=[('st3', s), 'junk3'])
                c.op('act', lambda: nc.scalar.activation(out=st3[s][0:rr, 1:2], in_=st3[s][0:rr, 0:1], func=AF.Sqrt, scale=1.0 / D, bias=epsb[0:rr, :]), reads=[('st3', s), 'epsb'], writes=[('st3', s)])
                c.op('dve', lambda: V.reciprocal(out=st3[s][0:rr, 2:3], in_=st3[s][0:rr, 1:2]), reads=[('st3', s)], writes=[('st3', s)])
                c.op('dve', lambda: V.scalar_tensor_tensor(out=y0[s][0:rr, :], in0=x1r[s][0:rr, :], scalar=st3[s][0:rr, 2:3], in1=gfin[0:rr, :], op0=ALU.mult, op1=ALU.mult),
                     reads=[('x1r', s), ('st3', s), 'gfin'], writes=[('y0', s)])
                dst = yp_t.ap()[i * 128:(i + 1) * 128, :] if i < NT - 1 else ys_t.ap()
                c.dma('act', lambda: nc.scalar.dma_start(out=dst, in_=y0[s][0:rr, :]), reads=[('y0', s)], writes=[('yout', i)])

            for i in range(NT + 1):
                if i < NT:
                    comb_stage1(i)
                if i >= 1:
                    comb_stage2(i - 1)
        c.finish()
    return nc


def build_two_pass():
    nc1 = build_nc(None)
    needed = set(nc1._mk_ctx.record)
    return build_nc(needed)


def _prep_inputs(inp):
    f = lambda a: np.ascontiguousarray(a, dtype=np.float32)
    ohw, vw, sel, bd = _structure_constants()
    shared = {
        "rel_bias": f(inp["rel_bias"]), "norm_mix_g": f(inp["norm_mix_g"][0][None]), "norm_ffn_g": f(inp["norm_ffn_g"][0][None]),
        "norm_final_g": f(inp["norm_final_g"][None]), "w_mod": f(inp["w_mod"][0]), "b_mod": f(inp["b_mod"][0][None]),
        "w_in": f(inp["w_in"][0]), "dw_w": f(inp["dw_w"][0]), "dw_b": f(inp["dw_b"][0][None]), "ln_g": f(inp["ln_conv_g"][0][None]),
        "ln_b": f(inp["ln_conv_b"][0][None]), "w_conv_out": f(inp["w_conv_out"][0]), "w_attn_out": f(inp["w_attn_out"][0]),
        "w_out": f(inp["w_out"][0]),
        "w_rt": f(np.concatenate([inp["w_router_group"][0], inp["w_router_expert"][0].reshape(D, 32)], axis=1)),
        "b_rt": f(np.concatenate([inp["b_router_group"][0], inp["b_router_expert"][0].reshape(32)])[None]),
        "w_eg": f(inp["w_exp_gate"][0]), "w_eu": f(inp["w_exp_up"][0]), "w_ed": f(inp["w_exp_down"][0]),
        "ohw": ohw, "vw": vw, "sel": sel, "bd": bd,
    }
    maps = []
    for cid in range(NCORE):
        b, half = cid // 2, cid % 2
        xp = np.zeros((NEXT, D), np.float32)
        xp[HALO:] = inp["x_prompt"][b, half * NOWN:(half + 1) * NOWN]
        if half == 1:
            xp[:HALO] = inp["x_prompt"][b, NOWN - HALO:NOWN]
        sl = slice(cid * NSQ, (cid + 1) * NSQ)
        m = dict(shared)
        m["xp"] = xp
        m["xs"] = f(inp["x_sample"][sl].reshape(NS, D))
        m["cmod"] = f(np.concatenate([inp["c_prompt"][b][None], inp["c_sample"][sl]], axis=0))
        m["hv"] = np.full((128, 1), float(half), np.float32)
        m["ck128"] = f(inp["cache_kv_w128"][0, sl].reshape(NSQ, 128, 512))
        m["ck512"] = f(inp["cache_kv_w512"][0, sl].reshape(NSQ, 512, 512))
        m["ck2048"] = f(inp["cache_kv_w2048"][0, sl].reshape(NSQ, 2048, 512))
        m["sconv"] = f(inp["state_conv"][0, sl])
        maps.append(m)
    return maps


_NC_CACHE = {}


def kernel(**inp):
    import time as _t
    t0 = _t.time()
    maps = _prep_inputs(inp)
    t1 = _t.time()
    if "nc" not in _NC_CACHE:
        _NC_CACHE["nc"] = build_two_pass()
    nc = _NC_CACHE["nc"]
    t2 = _t.time()
    if STOP_AFTER is not None:
        for m in maps:
            for k in ("w_eg", "w_eu", "w_ed"):
                m.pop(k, None)
    res = run_bass_kernel_spmd(nc, maps, core_ids=list(range(NCORE)))
    print("[kernel] prep %.1fs build %.1fs run %.1fs" % (t1 - t0, t2 - t1, _t.time() - t2), flush=True)
    R = res.results
    _NC_CACHE['last'] = R
    B = 4
    yp = np.zeros((B, 8192, D), np.float32); ys = np.zeros((128, 4, D), np.float32)
    kvp = [np.zeros((1, B, w, 2, 4, 64), np.float32) for (w, _) in GROUPS]
    convp = np.zeros((1, B, 30, 512), np.float32)
    kvs = [np.zeros((1, 128, w, 2, 4, 64), np.float32) for (w, _) in GROUPS]
    convs = np.zeros((1, 128, 30, 512), np.float32)
    for cid in range(NCORE):
        b, half = cid // 2, cid % 2
        r = R[cid]
        sl = slice(cid * NSQ, (cid + 1) * NSQ)
        if "yp" in r:
            yp[b, half * NOWN:(half + 1) * NOWN] = r["yp"]
            ys[sl] = r["ys"].reshape(NSQ, 4, D)
        for gi, (w, _) in enumerate(GROUPS):
            if half == 1:
                kvp[gi][0, b] = r["kvp%d" % w].reshape(w, 2, 4, 64)
            kvs[gi][0, sl] = r["kvs%d" % w].reshape(NSQ, w, 2, 4, 64)
        if half == 1:
            convp[0, b] = r["convp"]
        convs[0, sl] = r["convs"]
    return (yp, ys, kvp[0], kvp[1], kvp[2], convp, kvs[0], kvs[1], kvs[2], convs)
```

```python
import math
import numpy as np
from contextlib import ExitStack
import concourse.bass as bass
import concourse.mybir as mybir
from concourse.bass_utils import run_bass_kernel_spmd

F32 = mybir.dt.float32
BF16 = mybir.dt.bfloat16
I32 = mybir.dt.int32
U32 = mybir.dt.uint32
AF = mybir.ActivationFunctionType
ALU = mybir.AluOpType
AX = mybir.AxisListType

D = 1024
NCORE = 8
HALO = 2048
NOWN = 4096
NEXT = HALO + NOWN
NSQ = 16
NS = 64
NTOK = NEXT + NS
NOT = NOWN + NS
NTILE = 33
GROUPS = ((128, 1), (512, 4), (2048, 16))
EPS = 1e-6
NEXP = 32
BLK = 512
SUBB = BLK // 128
NBLK = (2 * NOT) // BLK + NEXP
CAP = NBLK * BLK
STOP_AFTER = None
DEBUG_SCR = False


class Ctx:
    KD = 8

    def __init__(self, nc, es, needed=None):
        self.nc = nc
        self.es = es
        self.needed = needed
        self.record = set()
        self.iidx = {e: 0 for e in ('pe', 'act', 'dve', 'pool')}
        self.eng = {'pe': nc.tensor, 'act': nc.scalar, 'dve': nc.vector, 'pool': nc.gpsimd, 'sp': nc.sync}
        self.csem = {e: es.enter_context(nc.semaphore('c_' + e)) for e in ('pe', 'act', 'dve', 'pool')}
        self.ccnt = {e: 0 for e in self.csem}
        self.dsem = {q: [es.enter_context(nc.semaphore('d_%s%d' % (q, i))) for i in range(self.KD)]
                     for q in ('sp', 'act', 'pool')}
        self.dcnt = {q: 0 for q in self.dsem}
        self.waited = {e: {} for e in self.eng}
        self.state = {}
        self.sbn = 0

    def sb(self, shape, dt, es=None):
        self.sbn += 1
        return (es or self.es).enter_context(self.nc.sbuf_tensor('sb%d' % self.sbn, list(shape), dt))

    def ps(self, shape, dt):
        self.sbn += 1
        return self.es.enter_context(self.nc.psum_tensor('ps%d' % self.sbn, list(shape), dt))

    def _wait(self, e, evs):
        best = {}
        for (sem, v, src) in evs:
            k = id(sem)
            if k not in best or best[k][1] < v:
                best[k] = (sem, v)
        for k, (sem, v) in best.items():
            if self.waited[e].get(k, 0) >= v:
                continue
            self.eng[e].wait_ge(sem, v)
            self.waited[e][k] = v
            if self.needed is None:
                for ce, cs in self.csem.items():
                    if cs is sem:
                        self.record.add((ce, v))

    def _deps(self, e, reads, writes):
        evs = []
        for k in reads:
            st = self.state.get(k)
            if st and st['w'] is not None:
                evs.append(st['w'])
        for k in writes:
            st = self.state.get(k)
            if st:
                if st['w'] is not None and (st['w'][2] != e or e != 'pe'):
                    evs.append(st['w'])
                for r in st['r']:
                    if r[2] != e or e != 'pe':
                        evs.append(r)
        return evs

    def _commit(self, ev, reads, writes):
        for k in reads:
            st = self.state.setdefault(k, {'w': None, 'r': []})
            st['r'] = [r for r in st['r'] if r[0] is not ev[0]] + [ev]
        for k in writes:
            self.state[k] = {'w': ev, 'r': []}

    @staticmethod
    def _psx(reads, writes):
        ps = [k for k in reads if k == 'pb7' or (isinstance(k, tuple) and k[0] == 'pb')]
        if not ps:
            return list(reads), list(writes)
        return [k for k in reads if k not in ps], list(writes) + [k for k in ps if k not in writes]

    def op(self, e, fn, reads=(), writes=()):
        reads, writes = self._psx(reads, writes)
        self._wait(e, self._deps(e, reads, writes))
        ins = fn()
        self.iidx[e] += 1
        if self.needed is None or (e, self.iidx[e]) in self.needed:
            self.ccnt[e] += 1
            ins.then_inc(self.csem[e], 1)
        ev = (self.csem[e], self.ccnt[e], e)
        self._commit(ev, reads, writes)
        return ev

    def dma(self, q, fn, reads=(), writes=()):
        j = self.dcnt[q]
        sem = self.dsem[q][j % self.KD]
        evs = self._deps(None, reads, writes)
        if j >= self.KD:
            evs.append((sem, 16 * (j // self.KD), 'dma_' + q))
        self._wait(q, evs)
        ins = fn()
        ins.then_inc(sem, 16)
        self.dcnt[q] += 1
        ev = (sem, 16 * (j // self.KD + 1), 'dma_' + q)
        self._commit(ev, reads, writes)
        return ev

    def wait_keys(self, e, keys):
        self._wait(e, self._deps(None, keys, ()))

    def barrier(self):
        evs = []
        for q in self.dsem:
            for i, sem in enumerate(self.dsem[q]):
                n = (self.dcnt[q] - i + self.KD - 1) // self.KD
                if n > 0:
                    evs.append((sem, 16 * n, 'x'))
        for e in self.csem:
            if self.ccnt[e]:
                evs.append((self.csem[e], self.ccnt[e], 'x'))
        for e in self.eng:
            self._wait(e, evs)

    def finish(self):
        evs = []
        for q in self.dsem:
            for i, sem in enumerate(self.dsem[q]):
                n = (self.dcnt[q] - i + self.KD - 1) // self.KD
                if n > 0:
                    evs.append((sem, 16 * n, 'x'))
        for e in self.csem:
            if self.ccnt[e]:
                evs.append((self.csem[e], self.ccnt[e], 'x'))
        self._wait('sp', evs)


def _t5_bucket_np(dist):
    dist = np.asarray(dist, np.int64)
    max_exact = 16
    d_f = np.maximum(dist, 1).astype(np.float32)
    large = max_exact + (np.log(d_f / np.float32(max_exact)) / np.float32(math.log(2048 / max_exact))
                         * np.float32(32 - max_exact)).astype(np.int32)
    large = np.minimum(large, 31)
    return np.where(dist < max_exact, dist, large)


def _structure_constants():
    ohw = np.zeros((32, 3 * 510), np.float32)
    vw = np.zeros((4, 3 * 510), np.float32)
    for g, (win, dil) in enumerate(GROUPS):
        for blk in range(2):
            for u in range(255):
                rel = u + 1 if blk == 0 else u - 127
                ok = (rel <= 128) if blk == 0 else (rel >= 0)
                if ok:
                    b = int(_t5_bucket_np(rel * dil))
                    ohw[b, g * 510 + blk * 255 + u] = 1.0
                    vw[:, g * 510 + blk * 255 + u] = 1.0
    sel = np.zeros((17, 192), np.float32)
    sel[0, 0:128] = 1.0
    for t in range(64):
        sel[1 + t // 4, 128 + t] = 1.0
    bd = np.zeros((64, 128), np.float32)
    for k in range(64):
        for q in range(64):
            if k // 4 == q // 4:
                bd[k, q] = 1.0
        bd[k, 64 + k] = 1.0
    return ohw, vw, sel, bd


def _os_env(k):
    import os
    return os.environ.get(k)


def build_nc(needed=None):
    nc = bass.Bass("TRN2", target_bir_lowering=False)

    def din(name, shape, dt=F32):
        return nc.dram_tensor(name, list(shape), dt, kind="ExternalInput")

    def dout(name, shape, dt=F32):
        return nc.dram_tensor(name, list(shape), dt, kind="ExternalOutput")

    def dscr(name, shape, dt=F32):
        return nc.dram_tensor(name, list(shape), dt, kind="ExternalOutput" if DEBUG_SCR else "Internal")

    xp_t = din("xp", [NEXT, D]); xs_t = din("xs", [NS, D]); cmod_t = din("cmod", [17, D]); hv_t = din("hv", [128, 1])
    ck_t = [din("ck%d" % w, [NSQ, w, 512]) for (w, _) in GROUPS]
    sconv_t = din("sconv", [NSQ, 30, 512])
    relb_t = din("rel_bias", [32, 12])
    gmix_t = din("norm_mix_g", [1, D]); gffn_t = din("norm_ffn_g", [1, D]); gfin_t = din("norm_final_g", [1, D])
    wmod_t = din("w_mod", [D, 6 * D]); bmod_t = din("b_mod", [1, 6 * D])
    win_t = din("w_in", [D, 5376])
    dww_t = din("dw_w", [31, 512]); dwb_t = din("dw_b", [1, 512]); lng_t = din("ln_g", [1, 512]); lnb_t = din("ln_b", [1, 512])
    wco_t = din("w_conv_out", [512, D]); wao_t = din("w_attn_out", [256, D]); wo_t = din("w_out", [D, D])
    wrt_t = din("w_rt", [D, 36]); brt_t = din("b_rt", [1, 36])
    if STOP_AFTER is None:
        weg_t = din("w_eg", [NEXP, D, 512]); weu_t = din("w_eu", [NEXP, D, 512]); wed_t = din("w_ed", [NEXP, 512, D])
    ohw_t = din("ohw", [32, 1530]); vw_t = din("vw", [4, 1530]); sel_t = din("sel", [17, 192]); bd_t = din("bd", [64, 128])

    yp_t = dout("yp", [NOWN, D]); ys_t = dout("ys", [NS, D])
    kvp_t = [dout("kvp%d" % w, [w, 512]) for (w, _) in GROUPS]
    convp_t = dout("convp", [30, 512])
    kvs_t = [dout("kvs%d" % w, [NSQ, w, 512]) for (w, _) in GROUPS]
    convs_t = dout("convs", [NSQ, 30, 512])

    modrows_t = dscr("modrows", [4, 192, D])
    wd_t = dscr("wdscr", [3, 4, 510])
    ebd_t = dscr("ebd", [3, 128, 1024])
    sg_t = dscr("sgscr", [16, 128, NOT], BF16)
    yts_t = dscr("ytscr", [4, 128, NOT], BF16)
    acc_t = dscr("accscr", [3, NOWN, 260])
    accs_t = dscr("accsscr", [NS, 260])
    x1_t = dscr("x1scr", [NOT, D])
    xsd_t = dscr("xsdisp", [CAP, D], BF16)
    ysd_t = dscr("ysdisp", [CAP, D])

    xp = xp_t.ap(); xs = xs_t.ap(); win = win_t.ap()

    with ExitStack() as es:
        c = Ctx(nc, es, needed)
        nc._mk_ctx = c
        PB = [c.ps([128, 512], F32) for _ in range(8)]

        def pbf(i):
            return PB[i][:].bitcast(BF16)

        identf = c.sb([128, 128], F32); ident = c.sb([128, 128], BF16)
        onesb = c.sb([128, 128], BF16); onesf = c.sb([128, 128], F32)
        suf = c.sb([128, 128], F32); jf = c.sb([128, 128], F32)
        epsb = c.sb([128, 1], F32); hv = c.sb([128, 1], F32); one1 = c.sb([128, 1], F32)
        c.op('pool', lambda: nc.gpsimd.memset(onesf[:], 1.0), writes=['onesf'])
        c.op('pool', lambda: nc.gpsimd.memset(onesb[:], 1.0), writes=['onesb'])
        c.op('pool', lambda: nc.gpsimd.memset(epsb[:], EPS), writes=['epsb'])
        c.op('pool', lambda: nc.gpsimd.memset(one1[:], 1.0), writes=['one1'])
        c.op('pool', lambda: nc.gpsimd.affine_select(out=identf[:], in_=onesf[:], pattern=[[-1, 128]], compare_op=ALU.is_equal,
                                                       fill=0.0, base=0, channel_multiplier=1), reads=['onesf'], writes=['identf'])
        c.op('pool', lambda: nc.gpsimd.affine_select(out=jf[:], in_=onesf[:], pattern=[[1, 128]], compare_op=ALU.is_equal,
                                                       fill=0.0, base=-127, channel_multiplier=1), reads=['onesf'], writes=['jf'])
        c.op('pool', lambda: nc.gpsimd.affine_select(out=suf[:], in_=onesf[:], pattern=[[1, 128]], compare_op=ALU.is_gt,
                                                       fill=0.0, base=0, channel_multiplier=-1), reads=['onesf'], writes=['suf'])
        c.op('dve', lambda: nc.vector.tensor_copy(out=ident[:], in_=identf[:]), reads=['identf'], writes=['ident'])
        c.dma('sp', lambda: nc.sync.dma_start(out=hv[:], in_=hv_t.ap()), writes=['hv'])


        es_hT = ExitStack()
        es.enter_context(es_hT)
        hT = c.sb([128, 8, NTOK], BF16, es_hT)
        es_mod = ExitStack()
        a1p = c.sb([128, D], F32, es_mod); b1p = c.sb([128, D], F32, es_mod); a1s = c.sb([64, D], F32, es_mod); b1s = c.sb([64, D], F32, es_mod)
        es0 = ExitStack()
        with es0:
            cm = c.sb([17, D], F32, es0); scm = c.sb([17, D], F32, es0); scT = c.sb([128, 8, 17], F32, es0)
            mtok = c.sb([17, 6 * D], F32, es0); selm = c.sb([17, 192], F32, es0)
            gmb = c.sb([128, D], F32, es0); gfb = c.sb([128, D], F32, es0)
            c.dma('sp', lambda: nc.sync.dma_start(out=cm[:], in_=cmod_t.ap()), writes=['cm'])
            c.dma('sp', lambda: nc.sync.dma_start(out=selm[:], in_=sel_t.ap()), writes=['selm'])
            c.dma('sp', lambda: nc.sync.dma_start(out=gmb[:], in_=gmix_t.ap().partition_broadcast(128)), writes=['gmb'])
            c.dma('sp', lambda: nc.sync.dma_start(out=gfb[:], in_=gffn_t.ap().partition_broadcast(128)), writes=['gfb'])
            c.op('act', lambda: nc.scalar.activation(out=scm[:], in_=cm[:], func=AF.Silu), reads=['cm'], writes=['scm'])
            for k in range(8):
                c.op('pe', lambda k=k: nc.tensor.transpose(out=PB[0][:, k * 17:(k + 1) * 17], in_=scm[0:17, k * 128:(k + 1) * 128],
                                                           identity=identf[0:17, 0:17]), reads=['scm', 'identf'], writes=['pb0'])
            c.op('dve', lambda: nc.vector.tensor_copy(out=scT[:].rearrange("p k s -> p (k s)"), in_=PB[0][:, 0:136]), reads=['pb0'], writes=['scT'])
            wmod = wmod_t.ap().rearrange("(k p) n -> p k n", p=128)
            wbs = [c.sb([128, 8, 512], F32, es0) for _ in range(2)]
            bbs = [c.sb([17, 512], F32, es0) for _ in range(2)]
            for nb in range(12):
                wb = wbs[nb % 2]
                bb = bbs[nb % 2]
                c.dma('sp', lambda wb=wb, nb=nb: nc.sync.dma_start(out=wb[:], in_=wmod[:, :, nb * 512:(nb + 1) * 512]), writes=[('wb', nb % 2)])
                c.dma('sp', lambda bb=bb, nb=nb: nc.sync.dma_start(out=bb[:], in_=bmod_t.ap()[:, nb * 512:(nb + 1) * 512].partition_broadcast(17)),
                      writes=[('bb', nb % 2)])
                pbk = 1 + nb % 2
                for k in range(8):
                    c.op('pe', lambda wb=wb, k=k, pbk=pbk: nc.tensor.matmul(PB[pbk][0:17, 0:512], lhsT=scT[:, k, :], rhs=wb[:, k, :], start=(k == 0), stop=(k == 7)),
                         reads=['scT', ('wb', nb % 2)], writes=[('pb', pbk)])
                c.op('dve', lambda bb=bb, nb=nb, pbk=pbk: nc.vector.tensor_tensor(out=mtok[:, nb * 512:(nb + 1) * 512], in0=PB[pbk][0:17, 0:512], in1=bb[:], op=ALU.add),
                     reads=[('pb', pbk), ('bb', nb % 2)], writes=['mtok'])
            rowst = [c.sb([128, D], F32, es0) for _ in range(2)]
            for kind in range(6):
                for part, (c0, n) in enumerate(((0, 128), (128, 64))):
                    rt = rowst[(kind * 2 + part) % 2]
                    rk = ('rowst', (kind * 2 + part) % 2)
                    for hf in range(2):
                        c.op('pe', lambda hf=hf, c0=c0, n=n, kind=kind: nc.tensor.matmul(PB[3 + hf][0:n, :], lhsT=selm[0:17, c0:c0 + n],
                             rhs=mtok[0:17, kind * D + hf * 512: kind * D + hf * 512 + 512], start=True, stop=True),
                             reads=['selm', 'mtok'], writes=[('pb', 3 + hf)])
                    dst = None; dk = 'nokey'
                    if kind == 0:
                        dst = (b1p, b1s)[part]; dk = ('b1', part)
                    elif kind == 1:
                        dst = (a1p, a1s)[part]; dk = ('a1', part)
                    for hf in range(2):
                        sl = slice(hf * 512, hf * 512 + 512)
                        if kind in (1, 4):
                            gb = gmb if kind == 1 else gfb
                            tgt = dst if dst is not None else rt
                            c.op('dve', lambda hf=hf, n=n, gb=gb, tgt=tgt, sl=sl: nc.vector.scalar_tensor_tensor(out=tgt[0:n, sl], in0=PB[3 + hf][0:n, :], scalar=1.0,
                                 in1=gb[0:n, sl], op0=ALU.add, op1=ALU.mult), reads=[('pb', 3 + hf), 'gmb', 'gfb'], writes=[rk, dk])
                        else:
                            tgt = dst if dst is not None else rt
                            c.op('act', lambda hf=hf, n=n, tgt=tgt, sl=sl: nc.scalar.copy(out=tgt[0:n, sl], in_=PB[3 + hf][0:n, :]),
                                 reads=[('pb', 3 + hf)], writes=[rk, dk])
                    if kind >= 2:
                        c.dma('sp', lambda rt=rt, c0=c0, n=n, kind=kind: nc.sync.dma_start(out=modrows_t.ap()[kind - 2, c0:c0 + n, :], in_=rt[0:n, :]),
                              reads=[rk], writes=[('modrows', kind - 2, part)])
        c.barrier()
        es0 = ExitStack()
        with es0:
            rb = c.sb([32, 12], F32, es0); ohw = c.sb([32, 1530], F32, es0); vw = c.sb([4, 1530], F32, es0)
            wsb = c.sb([4, 1530], F32, es0); hall = c.sb([128, 24, 128], F32, es0); ebst = c.sb([128, 3, 1024], F32, es0)
            c.dma('sp', lambda: nc.sync.dma_start(out=rb[:], in_=relb_t.ap()), writes=['rb'])
            c.dma('sp', lambda: nc.sync.dma_start(out=ohw[:], in_=ohw_t.ap()), writes=['ohw'])
            c.dma('sp', lambda: nc.sync.dma_start(out=vw[:], in_=vw_t.ap()), writes=['vw'])
            for g in range(3):
                c.op('pe', lambda g=g: nc.tensor.matmul(PB[5][0:4, 0:510], lhsT=rb[:, 4 * g:4 * g + 4], rhs=ohw[:, g * 510:(g + 1) * 510], start=True, stop=True),
                     reads=['rb', 'ohw'], writes=[('pb', 5)])
                c.op('act', lambda g=g: nc.scalar.activation(out=wsb[:, g * 510:(g + 1) * 510], in_=PB[5][0:4, 0:510], func=AF.Exp), reads=[('pb', 5)], writes=['wsb'])
            c.op('dve', lambda: nc.vector.tensor_tensor(out=wsb[:], in0=wsb[:], in1=vw[:], op=ALU.mult), reads=['wsb', 'vw'], writes=['wsb'])
            c.dma('sp', lambda: nc.sync.dma_start(out=wd_t.ap().rearrange("g h u -> h g u"), in_=wsb[:].rearrange("h (g u) -> h g u", g=3)), reads=['wsb'], writes=['wd'])
            for g in range(3):
                for h in range(4):
                    for blk in range(2):
                        idx = (g * 4 + h) * 2 + blk
                        src = bass.AP(wd_t, (g * 4 + h) * 510 + blk * 255, [[1, 128], [1, 128]])
                        c.dma('sp', lambda idx=idx, src=src: nc.sync.dma_start(out=hall[:, idx, :], in_=src), reads=['wd'], writes=[('hall', idx)])
            for g in range(3):
                for hf in range(2):
                    c.op('pe', lambda g=g, hf=hf: nc.tensor.matmul(PB[6 + hf][:, :], lhsT=jf[:], rhs=hall[:, g * 8 + hf * 4: g * 8 + hf * 4 + 4, :].rearrange("p a q -> p (a q)"),
                         start=True, stop=True), reads=['jf'] + [('hall', g * 8 + hf * 4 + i) for i in range(4)], writes=[('pb', 6 + hf)])
                    c.op('act', lambda g=g, hf=hf: nc.scalar.copy(out=ebst[:, g, hf * 512:(hf + 1) * 512], in_=PB[6 + hf][:, :]), reads=[('pb', 6 + hf)], writes=[('ebst', g)])
                c.dma('sp', lambda g=g: nc.sync.dma_start(out=ebd_t.ap()[g], in_=ebst[:, g, :]), reads=[('ebst', g)], writes=[('ebd', g)])

        c.barrier()
        def norm_to_T(tidx, src_ap, n, arow, brow, akey, bkey, bufs):
            xt, t1, hb, stt, junk = bufs
            s = tidx % 2
            c.dma('sp', lambda: nc.sync.dma_start(out=xt[s][0:n, :], in_=src_ap), writes=[('xt', s)])
            c.op('act', lambda: nc.scalar.activation(out=junk[0:n, :], in_=xt[s][0:n, :], func=AF.Square, accum_out=stt[s][0:n, 0:1]),
                 reads=[('xt', s)], writes=[('stt', s), 'junk'])
            c.op('act', lambda: nc.scalar.activation(out=stt[s][0:n, 1:2], in_=stt[s][0:n, 0:1], func=AF.Sqrt, scale=1.0 / D, bias=epsb[0:n, :]),
                 reads=[('stt', s), 'epsb'], writes=[('stt', s)])
            c.op('dve', lambda: nc.vector.reciprocal(out=stt[s][0:n, 2:3], in_=stt[s][0:n, 1:2]), reads=[('stt', s)], writes=[('stt', s)])
            c.op('dve', lambda: nc.vector.scalar_tensor_tensor(out=t1[s][0:n, :], in0=xt[s][0:n, :], scalar=stt[s][0:n, 2:3], in1=arow[0:n, :],
                                                                op0=ALU.mult, op1=ALU.mult), reads=[('xt', s), ('stt', s), akey], writes=[('t1', s)])
            c.op('pool', lambda: nc.gpsimd.tensor_tensor(out=hb[s][0:n, :], in0=t1[s][0:n, :], in1=brow[0:n, :], op=ALU.add),
                 reads=[('t1', s), bkey], writes=[('hb', s)])
            pv = pbf(s).rearrange("p (k t) -> p k t", k=8)
            for k in range(8):
                c.op('pe', lambda k=k: nc.tensor.transpose(out=pv[:, k, 0:n], in_=hb[s][0:n, k * 128:(k + 1) * 128], identity=ident[0:n, 0:n]),
                     reads=[('hb', s), 'ident'], writes=[('pb', s)])
            return s, pv

        esA = ExitStack()
        with esA:
            xt = [c.sb([128, D], F32, esA) for _ in range(2)]; t1 = [c.sb([128, D], F32, esA) for _ in range(2)]
            hb = [c.sb([128, D], BF16, esA) for _ in range(2)]; stt = [c.sb([128, 4], F32, esA) for _ in range(2)]
            junk = c.sb([128, D], BF16, esA)
            bufsA = (xt, t1, hb, stt, junk)
            for t in range(49):
                if t < 48:
                    n = 128; src = xp[t * 128:(t + 1) * 128, :]; ar, br = a1p, b1p; col = t * 128; pk = 0
                else:
                    n = 64; src = xs; ar, br = a1s, b1s; col = NEXT; pk = 1
                s, pv = norm_to_T(t, src, n, ar, br, ('a1', pk), ('b1', pk), bufsA)
                c.op('act', lambda pv=pv, col=col, n=n: nc.scalar.copy(out=hT[:, :, col:col + n], in_=pv[:, :, 0:n]), reads=[('pb', s)], writes=['hT'])
        c.barrier()
        es_mod.close()

        winr = win.rearrange("(k p) n -> p k n", p=128)

        def load_w(dst, c0, ncol, key):
            c.dma('pool', lambda: nc.gpsimd.dma_start(out=dst, in_=winr[:, :, c0:c0 + ncol]), writes=[key])

        def proj_T(ps_ap, pkey, wt, wkey, hcols):
            for k in range(8):
                c.op('pe', lambda k=k: nc.tensor.matmul(ps_ap, lhsT=wt[:, k, :], rhs=hT[:, k, hcols], start=(k == 0), stop=(k == 7)),
                     reads=['hT', wkey], writes=[pkey])

        def psv(i, sl=slice(None), n=512):
            return PB[i][sl, 0:n]

        OT = [(HALO + 512 * m, 512 * m, 512) for m in range(8)] + [(NEXT, NOWN, NS)]

        shiftq = []
        for g, (wing, d) in enumerate(GROUPS):
            A_ = (1, 4, 28)[g]
            npart = 4 if g == 2 else 1
            for q4 in range(npart):
                shiftq.append((g, A_, q4 * (A_ // npart), A_ // npart))
        shiftq = [shiftq[2], shiftq[3], shiftq[4], shiftq[5], shiftq[0], shiftq[1]]

        def emit_shift(nmax):
            for _ in range(nmax):
                if not shiftq:
                    return
                g, A_, a0, na = shiftq.pop(0)
                wing = GROUPS[g][0]
                c.dma('act', lambda g=g, A_=A_, a0=a0, na=na, wing=wing: nc.scalar.dma_start(
                      out=kvs_t[g].ap()[:, 0:wing - 4, :].rearrange("b (a r) c -> b a (r c)", a=A_)[:, a0:a0 + na, :],
                      in_=ck_t[g].ap()[:, 4:wing, :].rearrange("b (a r) c -> b a (r c)", a=A_)[:, a0:a0 + na, :]), writes=[('kvs_shift', g, a0)])

        esG = ExitStack()
        with esG:
            wj = [c.sb([128, 8, 128], BF16, esG) for _ in range(2)]
            sgrow = [c.sb([128, NOT], BF16, esG) for _ in range(2)]
            for j in range(16):
                s = j % 2
                load_w(wj[s][:], 3328 + j * 128, 128, ('wj', s))
                for m, (hc, oi, n) in enumerate(OT):
                    pi = m % 2
                    pa = psv(pi, n=n)
                    proj_T(pa, ('pb', pi), wj[s], ('wj', s), slice(hc, hc + n))
                    c.op('act', lambda pa=pa, oi=oi, n=n, s=s: nc.scalar.activation(out=sgrow[s][:, oi:oi + n], in_=pa, func=AF.Sigmoid),
                         reads=[('pb', pi)], writes=[('sgrow', s)])
                c.dma('sp', lambda j=j, s=s: nc.sync.dma_start(out=sg_t.ap()[j], in_=sgrow[s][:]), reads=[('sgrow', s)], writes=[('sg', j)])

        c.barrier()
        if STOP_AFTER == 'A':
            c.finish()
            return nc

        esC = ExitStack()
        with esC:
            yT = c.sb([128, 4, NOT], BF16, esC)
            dwT = c.sb([128, 4, 31], F32, esC); dwb = c.sb([128, 4], F32, esC); lng = c.sb([128, 4], F32, esC); lnb = c.sb([128, 4], F32, esC)
            with nc.allow_non_contiguous_dma(reason="tiny per-channel parameter loads"):
                for cc in range(4):
                    c.dma('sp', lambda cc=cc: nc.sync.dma_start(out=dwT[:, cc, :], in_=dww_t.ap()[:, cc * 128:(cc + 1) * 128].rearrange("j p -> p j")), writes=[('dwT', cc)])
                c.dma('sp', lambda: nc.sync.dma_start(out=dwb[:], in_=dwb_t.ap().rearrange("o (c p) -> p (o c)", p=128)), writes=['dwb'])
                c.dma('sp', lambda: nc.sync.dma_start(out=lng[:], in_=lng_t.ap().rearrange("o (c p) -> p (o c)", p=128)), writes=['lng'])
                c.dma('sp', lambda: nc.sync.dma_start(out=lnb[:], in_=lnb_t.ap().rearrange("o (c p) -> p (o c)", p=128)), writes=['lnb'])
            uxT = c.sb([128, 4, NSQ, 34], BF16, esC)
            uTf = c.sb([128, 4, 94], F32, esC)
            sct = [c.sb([120, 512], F32, esC)] * 2
            for i4 in range(4):
                s = i4 % 2
                c.dma('sp', lambda i4=i4, s=s: nc.sync.dma_start(out=sct[s][:], in_=sconv_t.ap()[4 * i4:4 * i4 + 4].rearrange("b t c -> (b t) c")), writes=[('sct', 0)])
                for cc in range(4):
                    c.op('pe', lambda cc=cc, s=s: nc.tensor.transpose(out=PB[2][:, 0:120], in_=sct[s][:, cc * 128:(cc + 1) * 128], identity=identf[0:120, 0:120]),
                         reads=[('sct', 0), 'identf'], writes=[('pb', 2)])
                    c.op('act', lambda cc=cc, i4=i4: nc.scalar.copy(out=uxT[:, cc, 4 * i4:4 * i4 + 4, 0:30], in_=PB[2][:, 0:120].rearrange("p (b t) -> p b t", b=4)),
                         reads=[('pb', 2)], writes=['uxT'])
            c.dma('sp', lambda: nc.sync.dma_start(out=convs_t.ap()[:, 0:26, :], in_=sconv_t.ap()[:, 4:30, :]), writes=['convs_a'])
            esC1 = ExitStack()
            wul = [c.sb([128, 8, 128], BF16, esC1) for _ in range(2)]; wug = [c.sb([128, 8, 128], BF16, esC1) for _ in range(2)]
            diag = [c.sb([128, 31, 128], BF16, esC1)] * 2
            ucT = [c.sb([128, 30 + NOWN], BF16, esC1)] * 2
            sgt = [c.sb([128, 512], F32, esC1) for _ in range(2)]
            UT = [(HALO - 30, -30, 30)] + OT
            for cc in range(4):
                s = cc % 2
                load_w(wul[s][:], 2304 + cc * 128, 128, ('wul', s))
                load_w(wug[s][:], 2816 + cc * 128, 128, ('wug', s))
                emit_shift(1)
                for j in range(31):
                    eng = 'dve' if j % 2 == 0 else 'pool'
                    e_ = nc.vector if eng == 'dve' else nc.gpsimd
                    c.op(eng, lambda j=j, e_=e_: e_.tensor_scalar(out=diag[s][:, j, :], in0=identf[:], scalar1=dwT[:, cc, j:j + 1], scalar2=None, op0=ALU.mult),
                         reads=['identf', ('dwT', cc)], writes=[('diag', 0, j)])
                for m, (hc, oi, n) in enumerate(UT):
                    pl = psv(0, n=n); pg = psv(1, n=n)
                    proj_T(pl, ('pb', 0), wul[s], ('wul', s), slice(hc, hc + n))
                    proj_T(pg, ('pb', 1), wug[s], ('wug', s), slice(hc, hc + n))
                    b_ = m % 2
                    c.op('act', lambda pg=pg, n=n, b_=b_: nc.scalar.activation(out=sgt[b_][:, 0:n], in_=pg, func=AF.Sigmoid), reads=[('pb', 1)], writes=[('sgt', b_)])
                    if oi < 0:
                        c.op('dve', lambda pl=pl, n=n, b_=b_: nc.vector.tensor_tensor(out=sgt[b_][:, 0:n], in0=pl, in1=sgt[b_][:, 0:n], op=ALU.mult),
                             reads=[('pb', 0), ('sgt', b_)], writes=[('sgt', b_)])
                        c.op('dve', lambda n=n, b_=b_: nc.vector.tensor_scalar(out=ucT[s][:, 0:30], in0=sgt[b_][:, 0:n], scalar1=hv[:, 0:1], scalar2=None, op0=ALU.mult),
                             reads=[('sgt', b_), 'hv'], writes=[('ucT', 0, 0)])
                    elif oi < NOWN:
                        c.op('dve', lambda pl=pl, n=n, b_=b_, oi=oi: nc.vector.tensor_tensor(out=ucT[s][:, 30 + oi:30 + oi + n], in0=pl, in1=sgt[b_][:, 0:n], op=ALU.mult),
                             reads=[('pb', 0), ('sgt', b_)], writes=[('ucT', 0, 1 + oi // 512)])
                        if oi == NOWN - 512:
                            c.op('dve', lambda pl=pl, b_=b_: nc.vector.tensor_tensor(out=uTf[:, cc, 0:30], in0=pl[:, 482:512], in1=sgt[b_][:, 482:512], op=ALU.mult),
                                 reads=[('pb', 0), ('sgt', b_)], writes=[('uTf', cc)])
                    else:
                        c.op('dve', lambda pl=pl, b_=b_: nc.vector.tensor_tensor(out=uTf[:, cc, 30:94], in0=pl, in1=sgt[b_][:, 0:64], op=ALU.mult),
                             reads=[('pb', 0), ('sgt', b_)], writes=[('uTf', cc)])
                        c.op('dve', lambda: nc.vector.tensor_copy(out=uxT[:, cc, :, 30:34], in_=uTf[:, cc, 30:94].rearrange("p (b t) -> p b t", t=4)),
                             reads=[('uTf', cc)], writes=['uxT'])
                dkeys = [('diag', 0, j) for j in range(31)]
                for m in range(8):
                    py = psv(2 + m % 2)
                    for j in range(31):
                        c.op('pe', lambda j=j, m=m, py=py: nc.tensor.matmul(py, lhsT=diag[s][:, j, :], rhs=ucT[s][:, 512 * m + j: 512 * m + j + 512], start=(j == 0), stop=(j == 30)),
                             reads=[dkeys[j], ('ucT', 0, 0), ('ucT', 0, 1 + m), ('ucT', 0, m)], writes=[('pb', 2 + m % 2)])
                    c.op('act', lambda m=m, py=py: nc.scalar.activation(out=yT[:, cc, 512 * m:512 * m + 512], in_=py, func=AF.Identity, bias=dwb[:, cc:cc + 1], scale=1.0),
                         reads=[('pb', 2 + m % 2), 'dwb'], writes=[('yT', m)])
                pys = PB[2][:, 0:64]
                for j in range(31):
                    c.op('pe', lambda j=j: nc.tensor.matmul(pys.rearrange("p (b t) -> p b t", t=4), lhsT=diag[s][:, j, :], rhs=uxT[:, cc, :, j:j + 4], start=(j == 0), stop=(j == 30)),
                         reads=[dkeys[j], 'uxT'], writes=[('pb', 2)])
                c.op('act', lambda: nc.scalar.activation(out=yT[:, cc, NOWN:NOT], in_=pys, func=AF.Identity, bias=dwb[:, cc:cc + 1], scale=1.0),
                     reads=[('pb', 2), 'dwb'], writes=[('yT', 8)])
            c.barrier()
            esC1.close()
            cpo = c.sb([94, 512], F32, esC)
            for cc in range(4):
                c.op('pe', lambda cc=cc: nc.tensor.transpose(out=PB[0][0:94, 0:128], in_=uTf[:, cc, :], identity=identf[:]), reads=[('uTf', cc), 'identf'], writes=[('pb', 0)])
                c.op('act', lambda cc=cc: nc.scalar.copy(out=cpo[:, cc * 128:(cc + 1) * 128], in_=PB[0][0:94, 0:128]), reads=[('pb', 0)], writes=['cpo'])
            c.dma('sp', lambda: nc.sync.dma_start(out=convp_t.ap(), in_=cpo[0:30, :]), reads=['cpo'], writes=['convp'])
            for b in range(NSQ):
                c.dma('sp', lambda b=b: nc.sync.dma_start(out=convs_t.ap()[b, 26:30, :], in_=cpo[30 + 4 * b:34 + 4 * b, :]), reads=['cpo'], writes=[('convs_b', b)])
            emit_shift(2)
            sq = c.sb([128, 4, 512], BF16, esC); mean = c.sb([128, 512], F32, esC); msq = c.sb([128, 512], F32, esC)
            var = c.sb([128, 512], F32, esC); rstd = c.sb([128, 512], F32, esC); tt = c.sb([128, 4, 512], F32, esC)
            for m, (hc, oi, n) in enumerate(OT):
                yk = ('yT', m)
                c.op('act', lambda oi=oi, n=n: nc.scalar.activation(out=sq[:, :, 0:n], in_=yT[:, :, oi:oi + n], func=AF.Square), reads=[yk], writes=['sq'])
                p1 = psv(4, n=n); p2 = psv(5, n=n)
                for cc in range(4):
                    c.op('pe', lambda cc=cc, p1=p1, oi=oi, n=n: nc.tensor.matmul(p1, lhsT=onesb[:], rhs=yT[:, cc, oi:oi + n], start=(cc == 0), stop=(cc == 3)),
                         reads=[yk, 'onesb'], writes=[('pb', 4)])
                for cc in range(4):
                    c.op('pe', lambda cc=cc, p2=p2, n=n: nc.tensor.matmul(p2, lhsT=onesb[:], rhs=sq[:, cc, 0:n], start=(cc == 0), stop=(cc == 3)),
                         reads=['sq', 'onesb'], writes=[('pb', 5)])
                c.op('dve', lambda p1=p1, n=n: nc.vector.tensor_scalar(out=mean[:, 0:n], in0=p1, scalar1=1.0 / 512, scalar2=None, op0=ALU.mult), reads=[('pb', 4)], writes=['mean'])
                c.op('pool', lambda n=n: nc.gpsimd.tensor_tensor(out=msq[:, 0:n], in0=mean[:, 0:n], in1=mean[:, 0:n], op=ALU.mult), reads=['mean'], writes=['msq'])
                c.op('dve', lambda p2=p2, n=n: nc.vector.scalar_tensor_tensor(out=var[:, 0:n], in0=p2, scalar=1.0 / 512, in1=msq[:, 0:n], op0=ALU.mult, op1=ALU.subtract),
                     reads=[('pb', 5), 'msq'], writes=['var'])
                c.op('act', lambda n=n: nc.scalar.activation(out=var[:, 0:n], in_=var[:, 0:n], func=AF.Sqrt, scale=1.0, bias=epsb[:, :]), reads=['var', 'epsb'], writes=['var'])
                c.op('dve', lambda n=n: nc.vector.reciprocal(out=rstd[:, 0:n], in_=var[:, 0:n]), reads=['var'], writes=['rstd'])
                c.op('dve', lambda oi=oi, n=n: nc.vector.tensor_tensor(out=tt[:, :, 0:n], in0=yT[:, :, oi:oi + n], in1=mean[:, 0:n].unsqueeze(1).to_broadcast([128, 4, n]), op=ALU.subtract),
                     reads=[yk, 'mean'], writes=['tt'])
                c.op('pool', lambda n=n: nc.gpsimd.tensor_tensor(out=tt[:, :, 0:n], in0=tt[:, :, 0:n], in1=rstd[:, 0:n].unsqueeze(1).to_broadcast([128, 4, n]), op=ALU.mult),
                     reads=['tt', 'rstd'], writes=['tt'])
                for cc in range(4):
                    c.op('act', lambda cc=cc, oi=oi, n=n: nc.scalar.activation(out=yT[:, cc, oi:oi + n], in_=tt[:, cc, 0:n], func=AF.Silu, bias=lnb[:, cc:cc + 1], scale=lng[:, cc:cc + 1]),
                         reads=['tt', 'lng', 'lnb'], writes=[yk])
            for cc in range(4):
                c.dma('sp', lambda cc=cc: nc.sync.dma_start(out=yts_t.ap()[cc], in_=yT[:, cc, :]), reads=[('yT', m) for m in range(9)], writes=[('yts', cc)])

        c.barrier()
        if STOP_AFTER == 'C':
            c.finish()
            return nc

        emit_shift(99)
        esD = ExitStack()
        with esD:
            ebf = c.sb([128, 1024], F32, esD)
            smk = c.sb([128, 9, 4, 4], F32, esD)
            nmk = c.sb([64, 3, 4, 64], F32, esD)
            bdm = c.sb([64, 128], F32, esD)
            pz = [c.sb([128, 4, 64], BF16, esD) for _ in range(NSQ)]
            v1n = c.sb([64, 3, 4, 65], BF16, esD)
            wkv = c.sb([128, 8, 512], BF16, esD)
            wq = c.sb([128, 8, 128], BF16, esD); wk = c.sb([128, 8, 128], BF16, esD)
            qT = c.sb([128, NOT], BF16, esD); kT = c.sb([128, NTOK], BF16, esD)
            qs = c.sb([128, 2, 64], BF16, esD); ks = c.sb([128, 2, 64], BF16, esD)
            v1 = c.sb([128, 48, 4, 65], BF16, esD)
            kvst = [c.sb([128, 512], F32, esD) for _ in range(2)]
            ef = [c.sb([128, 512], F32, esD) for _ in range(2)]
            pt = [c.sb([128, 512], BF16, esD) for _ in range(2)]
            oev = [c.sb([128, 130], F32, esD) for _ in range(2)]
            ctile = [c.sb([128, 512], F32, esD) for _ in range(2)]
            kTs = [c.sb([128, 2, 128], BF16, esD) for _ in range(2)]
            v1s = [c.sb([128, 4, 65], BF16, esD) for _ in range(2)]
            ess2 = [c.sb([128, 16], F32, esD) for _ in range(2)]
            en = c.sb([64, 256], F32, esD); pn = c.sb([64, 4, 64], BF16, esD)
            osv = c.sb([64, 260], F32, esD)
            c.dma('sp', lambda: nc.sync.dma_start(out=bdm[:], in_=bd_t.ap()), writes=['bdm'])
            c.op('pool', lambda: nc.gpsimd.memset(smk[:], 0.0), writes=['smk'])
            for b in range(NSQ):
                c.op('pool', lambda b=b: nc.gpsimd.memset(pz[b][:], 0.0), writes=[('pz', b)])
            for s in range(2):
                c.op('pool', lambda s=s: nc.gpsimd.memset(v1s[s][:], 1.0), writes=[('v1s', s)])
            c.op('pool', lambda: nc.gpsimd.memset(v1n[:], 1.0), writes=['v1n'])
            n_os = 1
            zl = c.sb([128, 64], BF16, esD); zr = c.sb([128, 260], BF16, esD)
            c.op('pool', lambda: nc.gpsimd.memset(zl[:], 0.0), writes=['zl'])
            c.op('pool', lambda: nc.gpsimd.memset(zr[:], 0.0), writes=['zr'])
            c.op('pe', lambda: nc.tensor.matmul(PB[7][0:64, 0:260], lhsT=zl[:], rhs=zr[:], start=True, stop=False), reads=['zl', 'zr'], writes=['pb7'])
            qb_i = 0
            for g, (wing, d) in enumerate(GROUPS):
                e0 = HALO - wing
                nbl = (NEXT - e0) // (128 * d)
                ebv = ebf[:].rearrange("p (h b q) -> p h b q", h=4, b=2)
                c.dma('sp', lambda g=g: nc.sync.dma_start(out=ebf[:], in_=ebd_t.ap()[g]), reads=[('ebd', g)], writes=['ebf'])
                if g == 0:
                    c.op('dve', lambda: nc.vector.tensor_copy(out=smk[:, 0, :, :], in_=ebv[:, :, 0, 0:4]), reads=['ebf'], writes=['smk'])
                else:
                    for r in range(4):
                        c.op('dve', lambda r=r, g=g: nc.vector.tensor_copy(out=smk[:, 1 + 4 * (g - 1) + r, :, r:r + 1], in_=ebv[:, :, 0, 0:1]), reads=['ebf'], writes=['smk'])
                bsel = bdm[:, 0:64] if g == 0 else bdm[:, 64:128]
                c.op('dve', lambda g=g, bsel=bsel: nc.vector.tensor_tensor(out=nmk[:, g, :, :], in0=ebv[0:64, :, 1, 0:64], in1=bsel.unsqueeze(1).to_broadcast([64, 4, 64]), op=ALU.mult),
                     reads=['ebf', 'bdm'], writes=['nmk'])
                c.dma('pool', lambda g=g: nc.gpsimd.dma_start(out=wkv[:, :, 0:256], in_=winr[:, :, 768 + 256 * g: 1024 + 256 * g]), writes=['wkv_k'])
                c.dma('pool', lambda g=g: nc.gpsimd.dma_start(out=wkv[:, :, 256:512], in_=winr[:, :, 1536 + 256 * g: 1792 + 256 * g]), writes=['wkv_v'])
                nkv = 0
                for r in range(d):
                    for i in range(nbl):
                        bi = r * nbl + i
                        cs0 = e0 + r + 128 * d * i
                        hsl = slice(cs0, cs0 + 127 * d + 1, d)
                        pi = 4 + nkv % 3
                        for k in range(8):
                            c.op('pe', lambda k=k, hsl=hsl, pi=pi: nc.tensor.matmul(PB[pi][:, :], lhsT=hT[:, k, hsl], rhs=wkv[:, k, :], start=(k == 0), stop=(k == 7)),
                                 reads=['hT', 'wkv_k', 'wkv_v'], writes=[('pb', pi)])
                        vc = hv if i == 0 else one1
                        c.op('dve', lambda bi=bi, pi=pi, vc=vc: nc.vector.tensor_scalar(out=v1[:, bi, :, 0:64], in0=PB[pi][:, 256:512].rearrange("p (h e) -> p h e", h=4),
                             scalar1=vc[:, 0:1], scalar2=None, op0=ALU.mult), reads=[('pb', pi), 'hv', 'one1'], writes=[('v1', bi)])
                        c.op('pool', lambda bi=bi, vc=vc: nc.gpsimd.tensor_copy(out=v1[:, bi, :, 64:65], in_=vc[:, 0:1].unsqueeze(1).to_broadcast([128, 4, 1])),
                             reads=['hv', 'one1'], writes=[('v1o', bi)])
                        if i == nbl - 1:
                            s = nkv % 2
                            c.op('act', lambda s=s, pi=pi: nc.scalar.copy(out=kvst[s][:], in_=PB[pi][:, :]), reads=[('pb', pi)], writes=[('kvst', s)])
                            c.dma('sp', lambda s=s, r=r, g=g, d=d, wing=wing: nc.sync.dma_start(out=kvp_t[g].ap()[r:wing:d, :], in_=kvst[s][:]), reads=[('kvst', s)], writes=[('kvp', g, r)])
                        nkv += 1
                pi = 4 + nkv % 3
                for k in range(8):
                    c.op('pe', lambda k=k, pi=pi: nc.tensor.matmul(PB[pi][0:64, :], lhsT=hT[:, k, NEXT:NTOK], rhs=wkv[:, k, :], start=(k == 0), stop=(k == 7)),
                         reads=['hT', 'wkv_k', 'wkv_v'], writes=[('pb', pi)])
                s = nkv % 2
                c.op('act', lambda s=s, pi=pi: nc.scalar.copy(out=kvst[s][0:64, :], in_=PB[pi][0:64, :]), reads=[('pb', pi)], writes=[('kvst', s)])
                c.op('dve', lambda g=g, pi=pi: nc.vector.tensor_copy(out=v1n[:, g, :, 0:64], in_=PB[pi][0:64, 256:512].rearrange("p (h e) -> p h e", h=4)), reads=[('pb', pi)], writes=['v1n'])
                for b in range(NSQ):
                    c.dma('sp', lambda b=b, s=s, g=g, wing=wing: nc.sync.dma_start(out=kvs_t[g].ap()[b, wing - 4:wing, :], in_=kvst[s][4 * b:4 * b + 4, :]), reads=[('kvst', s)], writes=[('kvsn', g, b)])
                for pair in range(2):
                    if _os_env('MK_SKIP_ATT'):
                        continue
                    c.dma('pool', lambda g=g, pair=pair: nc.gpsimd.dma_start(out=wq[:], in_=winr[:, :, 256 * g + 128 * pair: 256 * g + 128 * pair + 128]), writes=['wq'])
                    c.dma('pool', lambda g=g, pair=pair: nc.gpsimd.dma_start(out=wk[:], in_=winr[:, :, 768 + 256 * g + 128 * pair: 768 + 256 * g + 128 * pair + 128]), writes=['wk'])
                    npj = 0
                    for (hc, oi, n) in OT:
                        pi = 4 + npj % 3
                        proj_T(PB[pi][:, 0:n], ('pb', pi), wq, 'wq', slice(hc, hc + n))
                        c.op('act', lambda pi=pi, oi=oi, n=n: nc.scalar.copy(out=qT[:, oi:oi + n], in_=PB[pi][:, 0:n]), reads=[('pb', pi)], writes=['qT'])
                        npj += 1
                    cs_ = e0
                    while cs_ < NTOK:
                        n = min(512, NTOK - cs_)
                        pi = 4 + npj % 3
                        proj_T(PB[pi][:, 0:n], ('pb', pi), wk, 'wk', slice(cs_, cs_ + n))
                        if npj % 2 == 0:
                            c.op('dve', lambda pi=pi, cs_=cs_, n=n: nc.vector.tensor_copy(out=kT[:, cs_:cs_ + n], in_=PB[pi][:, 0:n]), reads=[('pb', pi)], writes=['kT'])
                        else:
                            c.op('act', lambda pi=pi, cs_=cs_, n=n: nc.scalar.copy(out=kT[:, cs_:cs_ + n], in_=PB[pi][:, 0:n]), reads=[('pb', pi)], writes=['kT_a'])
                        npj += 1
                        cs_ += n
                    c.op('dve', lambda pair=pair: nc.vector.tensor_copy(out=qs[:, pair, :], in_=qT[:, NOWN:NOT]), reads=['qT'], writes=['qs'])
                    c.op('dve', lambda pair=pair: nc.vector.tensor_copy(out=ks[:, pair, :], in_=kT[:, NEXT:NTOK]), reads=['kT', 'kT_a'], writes=['ks'])
                    for r in range(d):
                        for i in range(1, nbl):
                            s = qb_i % 2
                            qb_i += 1
                            q0 = r + 128 * d * (i - 1)
                            qsl = slice(q0, q0 + 127 * d + 1, d)
                            SBK = ((0, 1), (4, 5))[s]
                            for hh in range(2):
                                po = hh * 64
                                for blk in range(2):
                                    k0 = e0 + r + 128 * d * (i - 1 + blk)
                                    ksl = slice(k0, k0 + 127 * d + 1, d)
                                    c.op('pe', lambda bk=SBK[hh], po=po, ksl=ksl, qsl=qsl, blk=blk: nc.tensor.matmul(PB[bk][:, blk * 128:blk * 128 + 128], lhsT=kT[po:po + 64, ksl], rhs=qT[po:po + 64, qsl], start=True, stop=True),
                                         reads=['kT', 'kT_a', 'qT'], writes=[('pb', SBK[hh])])
                            for hh in range(2):
                                c.op('act', lambda s=s, hh=hh, bk=SBK[hh]: nc.scalar.activation(out=ef[s][:, hh * 256:(hh + 1) * 256], in_=PB[bk][:, 0:256], func=AF.Exp, scale=0.125),
                                     reads=[('pb', SBK[hh])], writes=[('ef', s, hh)])
                            c.op('dve', lambda s=s, pair=pair: nc.vector.tensor_tensor(out=pt[s][:], in0=ef[s][:], in1=ebf[:, pair * 512:(pair + 1) * 512], op=ALU.mult),
                                 reads=[('ef', s, 0), ('ef', s, 1), 'ebf'], writes=[('pt', s)])
                            for hh in range(2):
                                for blk in range(2):
                                    bi = r * nbl + i - 1 + blk
                                    col = (hh * 2 + blk) * 128
                                    c.op('pe', lambda s=s, hh=hh, blk=blk, bi=bi, col=col, pair=pair: nc.tensor.matmul(PB[2 + s][:, hh * 65:hh * 65 + 65], lhsT=pt[s][:, col:col + 128],
                                         rhs=v1[:, bi, 2 * pair + hh, :], start=(blk == 0), stop=(blk == 1)), reads=[('pt', s), ('v1', bi), ('v1o', bi)], writes=[('pb', 2 + s)])
                            c.op('dve', lambda s=s: nc.vector.tensor_copy(out=oev[s][:], in_=PB[2 + s][:, 0:130]), reads=[('pb', 2 + s)], writes=[('oev', s)])
                            c.dma('sp', lambda s=s, g=g, q0=q0, d=d, pair=pair: nc.sync.dma_start(out=acc_t.ap()[g, q0:q0 + 127 * d + 1:d, pair * 130:(pair + 1) * 130], in_=oev[s][:]),
                                  reads=[('oev', s)], writes=[('acc', g, q0, pair)])
                import os as _os
                ntile = 1 if g == 0 else 4
                if _os.environ.get('MK_SKIP_SAMPLE'):
                    continue
                for b in range(NSQ):
                    for r in range(ntile):
                        s = (b * ntile + r) % 2
                        tix = 0 if g == 0 else 1 + 4 * (g - 1) + r
                        c.dma('sp', lambda s=s, b=b, r=r, g=g, d=d, wing=wing: nc.sync.dma_start(out=ctile[s][:], in_=ck_t[g].ap()[b, r:wing:d, :]), writes=[('ctile', s)])
                        TB = (0, 1)[s]
                        for pr in range(2):
                            c.op('pe', lambda s=s, pr=pr, TB=TB: nc.tensor.transpose(out=PB[TB][:, pr * 128:(pr + 1) * 128], in_=ctile[s][:, pr * 128:(pr + 1) * 128], identity=identf[:]),
                                 reads=[('ctile', s), 'identf'], writes=[('pb', TB)])
                        c.op('act', lambda s=s, TB=TB: nc.scalar.copy(out=kTs[s][:].rearrange("p a k -> p (a k)"), in_=PB[TB][:, 0:256]), reads=[('pb', TB)], writes=[('kTs', s)])
                        c.op('pool', lambda s=s: nc.gpsimd.tensor_copy(out=v1s[s][:, :, 0:64], in_=ctile[s][:, 256:512].rearrange("p (h e) -> p h e", h=4)), reads=[('ctile', s)], writes=[('v1s', s)])
                        for h in range(4):
                            po = (h % 2) * 64
                            bk = (2, 3)[s] if h % 2 == 0 else (4, 5)[s]
                            c.op('pe', lambda s=s, h=h, po=po, b=b, bk=bk: nc.tensor.matmul(PB[bk][:, 256 + 4 * h:260 + 4 * h], lhsT=kTs[s][po:po + 64, h // 2, :], rhs=qs[po:po + 64, h // 2, 4 * b:4 * b + 4], start=True, stop=True),
                                 reads=[('kTs', s), 'qs'], writes=[('pb', bk)])
                        ess = ess2[s]
                        essv = ess[:].rearrange("p (h q) -> p h q", h=4)
                        for par, bk in ((0, (2, 3)[s]), (1, (4, 5)[s])):
                            c.op('act', lambda par=par, bk=bk, essv=essv: nc.scalar.activation(out=essv[:, par:4:2, :], in_=PB[bk][:, 256:272].rearrange("p (h q) -> p h q", h=4)[:, par:4:2, :], func=AF.Exp, scale=0.125),
                                 reads=[('pb', bk)], writes=[('ess', s, par)])
                        c.op('dve', lambda b=b, tix=tix, ess=ess: nc.vector.tensor_tensor(out=pz[b][:, :, 4 * b:4 * b + 4], in0=ess[:].rearrange("p (h q) -> p h q", h=4), in1=smk[:, tix, :, :], op=ALU.mult),
                             reads=[('ess', s, 0), ('ess', s, 1), 'smk'], writes=[('pz', b)])
                        for h in range(4):
                            c.op('pe', lambda s=s, h=h, b=b, st=(n_os == 0): nc.tensor.matmul(PB[7][0:64, 65 * h:65 * h + 65], lhsT=pz[b][:, h, :], rhs=v1s[s][:, h, :], start=st, stop=False),
                                 reads=[('pz', b), ('v1s', s)], writes=['pb7'])
                        n_os += 1
                for h in range(4):
                    po = (h % 2) * 64
                    bk = 6 if h % 2 == 0 else 5
                    c.op('pe', lambda h=h, po=po, bk=bk: nc.tensor.matmul(PB[bk][0:64, 64 * h:64 * h + 64], lhsT=ks[po:po + 64, h // 2, :], rhs=qs[po:po + 64, h // 2, :], start=True, stop=True),
                         reads=['ks', 'qs'], writes=[('pb', bk)])
                for h in range(4):
                    bk = 6 if h % 2 == 0 else 5
                    c.op('act', lambda h=h, bk=bk: nc.scalar.activation(out=en[:, 64 * h:64 * h + 64], in_=PB[bk][0:64, 64 * h:64 * h + 64], func=AF.Exp, scale=0.125), reads=[('pb', bk)], writes=[('en', h)])
                c.op('dve', lambda g=g: nc.vector.tensor_tensor(out=pn[:], in0=en[:].rearrange("p (h q) -> p h q", h=4), in1=nmk[:, g, :, :], op=ALU.mult), reads=[('en', 0), ('en', 1), ('en', 2), ('en', 3), 'nmk'], writes=['pn'])
                for h in range(4):
                    c.op('pe', lambda h=h, g=g: nc.tensor.matmul(PB[7][0:64, 65 * h:65 * h + 65], lhsT=pn[:, h, :], rhs=v1n[:, g, h, :], start=False, stop=(g == 2 and h == 3)),
                         reads=['pn', 'v1n'], writes=['pb7'])
            c.op('act', lambda: nc.scalar.copy(out=osv[:], in_=PB[7][0:64, 0:260]), reads=['pb7'], writes=['osv'])
            c.dma('sp', lambda: nc.sync.dma_start(out=accs_t.ap(), in_=osv[:]), reads=['osv'], writes=['accs'])
        c.barrier()
        es_hT.close()
        if STOP_AFTER == 'D':
            c.finish()
            return nc
        wcor = wco_t.ap().rearrange("(c p) n -> p c n", p=128)
        waor = wao_t.ap().rearrange("(c p) n -> p c n", p=128)
        wor = wo_t.ap().rearrange("(c p) n -> p c n", p=128)
        wrtr = wrt_t.ap().rearrange("(c p) n -> p c n", p=128)
        h2d_t = dscr("h2scr", [NOT, D], BF16)
        esE = ExitStack()
        es.enter_context(esE)
        lgall = c.sb([128, NTILE, 36], F32, esE)
        esE1 = ExitStack()
        with esE1:
            wco = c.sb([128, 4, D], BF16, esE1); wao = c.sb([128, 2, D], BF16, esE1); wo = c.sb([128, 8, D], BF16, esE1)
            wrt = c.sb([128, 8, 36], BF16, esE1); brtb = c.sb([128, 36], F32, esE1)
            c.dma('pool', lambda: nc.gpsimd.dma_start(out=wco[:], in_=wcor), writes=['wco'])
            c.dma('pool', lambda: nc.gpsimd.dma_start(out=wao[:], in_=waor), writes=['wao'])
            c.dma('pool', lambda: nc.gpsimd.dma_start(out=wo[:], in_=wor), writes=['wo'])
            c.dma('pool', lambda: nc.gpsimd.dma_start(out=wrt[:], in_=wrtr), writes=['wrt'])
            c.dma('sp', lambda: nc.sync.dma_start(out=brtb[:], in_=brt_t.ap().partition_broadcast(128)), writes=['brtb'])
            mrow = {}
            for kind, nm in ((0, 'g1'), (1, 'b2'), (2, 'a2')):
                for part, (c0, n) in enumerate(((0, 128), (128, 64))):
                    t_ = c.sb([n, D], F32, esE1)
                    c.dma('sp', lambda t_=t_, kind=kind, c0=c0, n=n: nc.sync.dma_start(out=t_[:], in_=modrows_t.ap()[kind, c0:c0 + n, :]),
                          reads=[('modrows', kind, part)], writes=[('mrow', nm, part)])
                    mrow[(nm, part)] = t_
            c.op('pool', lambda: nc.gpsimd.memset(lgall[:], 0.0), writes=['lgall'])
            ytile = [c.sb([128, 4, 512], BF16, esE1) for _ in range(2)]
            oT = c.sb([128, 2, 512], BF16, esE1)
            acc3 = [c.sb([128, 3, 260], F32, esE1) for _ in range(2)]
            osum = c.sb([128, 260], F32, esE1); rden = c.sb([128, 4], F32, esE1); ob = c.sb([128, 256], BF16, esE1)
            sga = [c.sb([128, 512], BF16, esE1) for _ in range(2)]; sgb = [c.sb([128, 512], BF16, esE1) for _ in range(2)]
            m1 = [c.sb([128, 512], F32, esE1) for _ in range(2)]; m2 = [c.sb([128, 512], F32, esE1) for _ in range(2)]
            mixT = c.sb([128, 8, 512], BF16, esE1)
            xt2 = [c.sb([128, D], F32, esE1) for _ in range(2)]; tmp = [c.sb([128, D], F32, esE1) for _ in range(2)]
            x1t = [c.sb([128, D], F32, esE1) for _ in range(2)]; t2 = [c.sb([128, D], F32, esE1) for _ in range(2)]
            h2b = [c.sb([128, D], BF16, esE1) for _ in range(2)]
            h2T = c.sb([128, 8, 128], BF16, esE1); st2 = [c.sb([128, 4], F32, esE1) for _ in range(2)]
            junk2 = c.sb([128, D], BF16, esE1)
            sub_i = 0
            for M, (hc, oi, n) in enumerate(OT):
                part = 0 if M < 8 else 1
                rr = 128 if M < 8 else 64
                nsub = n // rr
                ys_ = M % 2
                c.dma('sp', lambda ys_=ys_, oi=oi, n=n: nc.sync.dma_start(out=ytile[ys_][:, :, 0:n], in_=yts_t.ap()[:, :, oi:oi + n].rearrange("c p t -> p c t")),
                      reads=[('yts', cc) for cc in range(4)], writes=[('ytile', ys_)])
                for t in range(nsub):
                    a_ = (M * 4 + t) % 2
                    r0 = oi + rr * t
                    if M < 8:
                        c.dma('sp', lambda a_=a_, r0=r0: nc.sync.dma_start(out=acc3[a_][:], in_=acc_t.ap()[:, r0:r0 + 128, :].rearrange("g t c -> t g c")),
                              reads=[k for k in c.state if isinstance(k, tuple) and k[0] == 'acc'], writes=[('acc3', a_)])
                        c.op('dve', lambda a_=a_: nc.vector.tensor_tensor(out=osum[:], in0=acc3[a_][:, 0, :], in1=acc3[a_][:, 1, :], op=ALU.add), reads=[('acc3', a_)], writes=['osum'])
                        c.op('dve', lambda a_=a_: nc.vector.tensor_tensor(out=osum[:], in0=osum[:], in1=acc3[a_][:, 2, :], op=ALU.add), reads=[('acc3', a_), 'osum'], writes=['osum'])
                    else:
                        c.dma('sp', lambda a_=a_: nc.sync.dma_start(out=acc3[a_][0:64, 0, :], in_=accs_t.ap()), reads=['accs'], writes=[('acc3', a_)])
                        c.op('dve', lambda a_=a_: nc.vector.tensor_copy(out=osum[0:64, :], in_=acc3[a_][0:64, 0, :]), reads=[('acc3', a_)], writes=['osum'])
                    ov = osum[0:rr, :].rearrange("p (h e) -> p h e", h=4)
                    c.op('dve', lambda ov=ov, rr=rr: nc.vector.reciprocal(out=rden[0:rr, :].unsqueeze(2), in_=ov[:, :, 64:65]), reads=['osum'], writes=['rden'])
                    c.op('dve', lambda ov=ov, rr=rr: nc.vector.tensor_tensor(out=ob[0:rr, :].rearrange("p (h e) -> p h e", h=4), in0=ov[:, :, 0:64],
                         in1=rden[0:rr, :].unsqueeze(2).to_broadcast([rr, 4, 64]), op=ALU.mult), reads=['osum', 'rden'], writes=['ob'])
                    pv4 = pbf(4).rearrange("p (k t) -> p k t", k=8)
                    for k in range(2):
                        c.op('pe', lambda k=k, rr=rr, pv4=pv4: nc.tensor.transpose(out=pv4[:, k, 0:rr], in_=ob[0:rr, k * 128:(k + 1) * 128], identity=ident[0:rr, 0:rr]),
                             reads=['ob', 'ident'], writes=[('pb', 4)])
                    c.op('act', lambda t=t, rr=rr, pv4=pv4: nc.scalar.copy(out=oT[:, :, rr * t:rr * t + rr], in_=pv4[:, 0:2, 0:rr]), reads=[('pb', 4)], writes=['oT'])
                for j in range(8):
                    gs = j % 2
                    c.dma('sp', lambda gs=gs, j=j, oi=oi, n=n: nc.sync.dma_start(out=sga[gs][:, 0:n], in_=sg_t.ap()[j, :, oi:oi + n]), reads=[('sg', j)], writes=[('sga', gs)])
                    c.dma('sp', lambda gs=gs, j=j, oi=oi, n=n: nc.sync.dma_start(out=sgb[gs][:, 0:n], in_=sg_t.ap()[8 + j, :, oi:oi + n]), reads=[('sg', 8 + j)], writes=[('sgb', gs)])
                    pa_i, pb_i = (0, 1) if gs == 0 else (6, 7)
                    for cc in range(4):
                        c.op('pe', lambda cc=cc, j=j, pa_i=pa_i, ys_=ys_, n=n: nc.tensor.matmul(PB[pa_i][:, 0:n], lhsT=wco[:, cc, j * 128:(j + 1) * 128], rhs=ytile[ys_][:, cc, 0:n], start=(cc == 0), stop=(cc == 3)),
                             reads=['wco', ('ytile', ys_)], writes=[('pb', pa_i)])
                    for cc in range(2):
                        c.op('pe', lambda cc=cc, j=j, pb_i=pb_i, n=n: nc.tensor.matmul(PB[pb_i][:, 0:n], lhsT=wao[:, cc, j * 128:(j + 1) * 128], rhs=oT[:, cc, 0:n], start=(cc == 0), stop=(cc == 1)),
                             reads=['wao', 'oT'], writes=[('pb', pb_i)])
                    c.op('dve', lambda gs=gs, pa_i=pa_i, n=n: nc.vector.tensor_tensor(out=m1[gs][:, 0:n], in0=PB[pa_i][:, 0:n], in1=sga[gs][:, 0:n], op=ALU.mult), reads=[('pb', pa_i), ('sga', gs)], writes=[('m1', gs)])
                    c.op('dve', lambda gs=gs, pb_i=pb_i, n=n: nc.vector.tensor_tensor(out=m2[gs][:, 0:n], in0=PB[pb_i][:, 0:n], in1=sgb[gs][:, 0:n], op=ALU.mult), reads=[('pb', pb_i), ('sgb', gs)], writes=[('m2', gs)])
                    c.op('pool', lambda gs=gs, j=j, n=n: nc.gpsimd.tensor_tensor(out=mixT[:, j, 0:n], in0=m1[gs][:, 0:n], in1=m2[gs][:, 0:n], op=ALU.add), reads=[('m1', gs), ('m2', gs)], writes=[('mixT', j)])
                for t in range(nsub):
                    xs_ = sub_i % 2
                    sub_i += 1
                    r0 = oi + rr * t
                    tile_i = r0 // 128 if M < 8 else 32
                    src = xp[HALO + r0:HALO + r0 + 128, :] if M < 8 else xs
                    c.dma('sp', lambda xs_=xs_, src=src, rr=rr: nc.sync.dma_start(out=xt2[xs_][0:rr, :], in_=src), writes=[('xt2', xs_)])
                    for hf in range(2):
                        for j in range(8):
                            c.op('pe', lambda hf=hf, j=j, t=t, rr=rr: nc.tensor.matmul(PB[2 + hf][0:rr, :], lhsT=mixT[:, j, rr * t:rr * t + rr], rhs=wo[:, j, hf * 512:(hf + 1) * 512], start=(j == 0), stop=(j == 7)),
                                 reads=[('mixT', j), 'wo'], writes=[('pb', 2 + hf)])
                        c.op('dve', lambda hf=hf, xs_=xs_, rr=rr, part=part: nc.vector.tensor_tensor(out=tmp[xs_][0:rr, hf * 512:(hf + 1) * 512], in0=PB[2 + hf][0:rr, :],
                             in1=mrow[('g1', part)][0:rr, hf * 512:(hf + 1) * 512], op=ALU.mult), reads=[('pb', 2 + hf), ('mrow', 'g1', part)], writes=[('tmp', xs_)])
                    c.op('pool', lambda xs_=xs_, rr=rr: nc.gpsimd.tensor_tensor(out=x1t[xs_][0:rr, :], in0=tmp[xs_][0:rr, :], in1=xt2[xs_][0:rr, :], op=ALU.add),
                         reads=[('tmp', xs_), ('xt2', xs_)], writes=[('x1t', xs_)])
                    c.dma('act', lambda xs_=xs_, r0=r0, rr=rr: nc.scalar.dma_start(out=x1_t.ap()[r0:r0 + rr, :], in_=x1t[xs_][0:rr, :]), reads=[('x1t', xs_)], writes=[('x1', tile_i)])
                    c.op('act', lambda xs_=xs_, rr=rr: nc.scalar.activation(out=junk2[0:rr, :], in_=x1t[xs_][0:rr, :], func=AF.Square, accum_out=st2[xs_][0:rr, 0:1]),
                         reads=[('x1t', xs_)], writes=[('st2', xs_), 'junk2'])
                    c.op('act', lambda xs_=xs_, rr=rr: nc.scalar.activation(out=st2[xs_][0:rr, 1:2], in_=st2[xs_][0:rr, 0:1], func=AF.Sqrt, scale=1.0 / D, bias=epsb[0:rr, :]),
                         reads=[('st2', xs_), 'epsb'], writes=[('st2', xs_)])
                    c.op('dve', lambda xs_=xs_, rr=rr: nc.vector.reciprocal(out=st2[xs_][0:rr, 2:3], in_=st2[xs_][0:rr, 1:2]), reads=[('st2', xs_)], writes=[('st2', xs_)])
                    c.op('dve', lambda xs_=xs_, rr=rr, part=part: nc.vector.scalar_tensor_tensor(out=t2[xs_][0:rr, :], in0=x1t[xs_][0:rr, :], scalar=st2[xs_][0:rr, 2:3],
                         in1=mrow[('a2', part)][0:rr, :], op0=ALU.mult, op1=ALU.mult), reads=[('x1t', xs_), ('st2', xs_), ('mrow', 'a2', part)], writes=[('t2', xs_)])
                    c.op('pool', lambda xs_=xs_, rr=rr, part=part: nc.gpsimd.tensor_tensor(out=h2b[xs_][0:rr, :], in0=t2[xs_][0:rr, :], in1=mrow[('b2', part)][0:rr, :], op=ALU.add),
                         reads=[('t2', xs_), ('mrow', 'b2', part)], writes=[('h2b', xs_)])
                    c.dma('act', lambda xs_=xs_, r0=r0, rr=rr: nc.scalar.dma_start(out=h2d_t.ap()[r0:r0 + rr, :], in_=h2b[xs_][0:rr, :]), reads=[('h2b', xs_)], writes=[('h2d', tile_i)])
                    pv5 = pbf(5).rearrange("p (k t) -> p k t", k=8)
                    for k in range(8):
                        c.op('pe', lambda k=k, xs_=xs_, rr=rr, pv5=pv5: nc.tensor.transpose(out=pv5[:, k, 0:rr], in_=h2b[xs_][0:rr, k * 128:(k + 1) * 128], identity=ident[0:rr, 0:rr]),
                             reads=[('h2b', xs_), 'ident'], writes=[('pb', 5)])
                    c.op('act', lambda rr=rr, pv5=pv5: nc.scalar.copy(out=h2T[:, :, 0:rr], in_=pv5[:, :, 0:rr]), reads=[('pb', 5)], writes=['h2T'])
                    for k in range(8):
                        c.op('pe', lambda k=k, rr=rr: nc.tensor.matmul(PB[4][0:rr, 0:36], lhsT=h2T[:, k, 0:rr], rhs=wrt[:, k, :], start=(k == 0), stop=(k == 7)),
                             reads=['h2T', 'wrt'], writes=[('pb', 4)])
                    c.op('dve', lambda rr=rr, tile_i=tile_i: nc.vector.tensor_tensor(out=lgall[0:rr, tile_i, :], in0=PB[4][0:rr, 0:36], in1=brtb[0:rr, :], op=ALU.add),
                         reads=[('pb', 4), 'brtb', 'lgall'], writes=['lgall'])
        c.barrier()
        if STOP_AFTER == 'E':
            c.finish()
            return nc
        NT = NTILE
        esF = ExitStack()
        with esF:
            def ft(shape, dt=F32):
                return c.sb(shape, dt, esF)
            gmx = ft([128, NT]); ohg = ft([128, NT, 4]); gsh = ft([128, NT, 4]); gex = ft([128, NT, 4]); gsum = ft([128, NT]); pgr = ft([128, NT])
            pen = ft([128, NT, 4]); em = ft([128, NT, 32]); m8 = ft([128, NT, 8]); i8 = ft([128, NT, 8], U32)
            e0f = ft([128, NT]); e1f = ft([128, NT]); dv = ft([128, NT]); w0 = ft([128, NT]); w1 = ft([128, NT])
            oh0 = ft([128, NT, 32]); oh1 = ft([128, NT, 32]); mm_ = ft([128, NT, 32]); cs = ft([128, NT + 1, 32])
            base = ft([128, NT, 32]); prod = ft([128, NT, 32]); d0f = ft([128, NT]); d1f = ft([128, NT])
            d0i = ft([128, NT], I32); d1i = ft([128, NT], I32)
            io32i = ft([128, 32], I32); io32 = ft([128, 32]); thri = ft([128, NBLK], I32); thr = ft([128, NBLK])
            cnt = ft([128, 32]); cni = ft([128, 32], I32); pad = ft([128, 32]); pa_ = ft([128, 32]); pb_ = ft([128, 32]); pst = ft([128, 32])
            cmpb = ft([128, NBLK, 32]); bef = ft([128, NBLK])
            c.op('pool', lambda: nc.gpsimd.iota(io32i[:], pattern=[[1, 32]], base=0, channel_multiplier=0), writes=['io32i'])
            c.op('pool', lambda: nc.gpsimd.iota(thri[:], pattern=[[BLK, NBLK]], base=0, channel_multiplier=0), writes=['thri'])
            c.op('dve', lambda: nc.vector.tensor_copy(out=io32[:], in_=io32i[:]), reads=['io32i'], writes=['io32'])
            c.op('dve', lambda: nc.vector.tensor_copy(out=thr[:], in_=thri[:]), reads=['thri'], writes=['thr'])
            R = ['lgall']
            gl = lgall[:, :, 0:4]
            V = nc.vector
            c.op('dve', lambda: V.tensor_reduce(out=gmx[:], in_=gl, axis=AX.X, op=ALU.max), reads=R, writes=['gmx'])
            c.op('dve', lambda: V.tensor_tensor(out=ohg[:], in0=gl, in1=gmx[:].unsqueeze(2).to_broadcast([128, NT, 4]), op=ALU.is_equal), reads=R + ['gmx'], writes=['ohg'])
            c.op('dve', lambda: V.tensor_tensor(out=gsh[:], in0=gl, in1=gmx[:].unsqueeze(2).to_broadcast([128, NT, 4]), op=ALU.subtract), reads=R + ['gmx'], writes=['gsh'])
            c.op('act', lambda: nc.scalar.activation(out=gex[:], in_=gsh[:], func=AF.Exp), reads=['gsh'], writes=['gex'])
            c.op('dve', lambda: V.tensor_reduce(out=gsum[:], in_=gex[:], axis=AX.X, op=ALU.add), reads=['gex'], writes=['gsum'])
            c.op('dve', lambda: V.reciprocal(out=pgr[:], in_=gsum[:]), reads=['gsum'], writes=['pgr'])
            c.op('dve', lambda: V.tensor_scalar(out=pen[:], in0=ohg[:], scalar1=-1.0, scalar2=1e30, op0=ALU.add, op1=ALU.mult), reads=['ohg'], writes=['pen'])
            c.op('dve', lambda: V.tensor_tensor(out=em[:].rearrange("p t (g e) -> p t g e", g=4), in0=lgall[:, :, 4:36].rearrange("p t (g e) -> p t g e", g=4),
                 in1=pen[:].unsqueeze(3).to_broadcast([128, NT, 4, 8]), op=ALU.add), reads=R + ['pen'], writes=['em'])
            for i in range(NT):
                c.op('dve', lambda i=i: V.max(out=m8[:, i, :], in_=em[:, i, :]), reads=['em'], writes=['m8'])
                c.op('dve', lambda i=i: V.max_index(out=i8[:, i, :], in_max=m8[:, i, :], in_values=em[:, i, :]), reads=['em', 'm8'], writes=['i8'])
            c.op('dve', lambda: V.tensor_copy(out=e0f[:], in_=i8[:, :, 0]), reads=['i8'], writes=['e0f'])
            c.op('dve', lambda: V.tensor_copy(out=e1f[:], in_=i8[:, :, 1]), reads=['i8'], writes=['e1f'])
            c.op('dve', lambda: V.tensor_tensor(out=dv[:], in0=m8[:, :, 1], in1=m8[:, :, 0], op=ALU.subtract), reads=['m8'], writes=['dv'])
            c.op('act', lambda: nc.scalar.activation(out=dv[:], in_=dv[:], func=AF.Exp), reads=['dv'], writes=['dv'])
            c.op('dve', lambda: V.tensor_scalar(out=dv[:], in0=dv[:], scalar1=1.0, scalar2=None, op0=ALU.add), reads=['dv'], writes=['dv'])
            c.op('dve', lambda: V.reciprocal(out=w0[:], in_=dv[:]), reads=['dv'], writes=['w0'])
            c.op('dve', lambda: V.tensor_tensor(out=w0[:], in0=w0[:], in1=pgr[:], op=ALU.mult), reads=['w0', 'pgr'], writes=['w0'])
            c.op('dve', lambda: V.tensor_tensor(out=w1[:], in0=pgr[:], in1=w0[:], op=ALU.subtract), reads=['w0', 'pgr'], writes=['w1'])
            iob = io32[:].unsqueeze(1).to_broadcast([128, NT, 32])
            c.op('dve', lambda: V.tensor_tensor(out=oh0[:], in0=iob, in1=e0f[:].unsqueeze(2).to_broadcast([128, NT, 32]), op=ALU.is_equal), reads=['io32', 'e0f'], writes=['oh0'])
            c.op('dve', lambda: V.tensor_tensor(out=oh1[:], in0=iob, in1=e1f[:].unsqueeze(2).to_broadcast([128, NT, 32]), op=ALU.is_equal), reads=['io32', 'e1f'], writes=['oh1'])
            c.op('dve', lambda: V.memset(oh0[64:128, NT - 1, :], 0.0), reads=['oh0'], writes=['oh0'])
            c.op('dve', lambda: V.memset(oh1[64:128, NT - 1, :], 0.0), reads=['oh1'], writes=['oh1'])
            c.op('dve', lambda: V.tensor_tensor(out=mm_[:], in0=oh0[:], in1=oh1[:], op=ALU.add), reads=['oh0', 'oh1'], writes=['mm'])
            c.op('dve', lambda: V.memset(cs[:, 0, :], 0.0), writes=['cs'])
            for i in range(NT):
                c.op('dve', lambda i=i: V.tensor_tensor(out=cs[:, i + 1, :], in0=cs[:, i, :], in1=mm_[:, i, :], op=ALU.add), reads=['cs', 'mm'], writes=['cs'])
            for i in range(NT):
                bk = i // 16
                co = (i % 16) * 32
                c.op('pe', lambda i=i, bk=bk, co=co: nc.tensor.matmul(PB[bk][:, co:co + 32], lhsT=suf[:], rhs=mm_[:, i, :], start=True, stop=False), reads=['suf', 'mm'], writes=[('pb', bk)])
                c.op('pe', lambda i=i, bk=bk, co=co: nc.tensor.matmul(PB[bk][:, co:co + 32], lhsT=onesf[:], rhs=cs[:, i, :], start=False, stop=True), reads=['onesf', 'cs'], writes=[('pb', bk)])
            c.op('pe', lambda: nc.tensor.matmul(PB[3][:, 0:32], lhsT=onesf[:], rhs=cs[:, NT, :], start=True, stop=True), reads=['onesf', 'cs'], writes=[('pb', 3)])
            c.op('dve', lambda: V.tensor_scalar(out=cni[:], in0=PB[3][:, 0:32], scalar1=float(BLK - 1), scalar2=None, op0=ALU.add), reads=[('pb', 3)], writes=['cni'])
            c.op('dve', lambda: V.tensor_scalar(out=cni[:], in0=cni[:], scalar1=int(math.log2(BLK)), scalar2=int(math.log2(BLK)), op0=ALU.arith_shift_right, op1=ALU.logical_shift_left), reads=['cni'], writes=['cni'])
            c.op('dve', lambda: V.tensor_copy(out=pad[:], in_=cni[:]), reads=['cni'], writes=['pad'])
            src_, dst_ = pad, pa_
            for sft in (1, 2, 4, 8, 16):
                c.op('dve', lambda src_=src_, dst_=dst_, sft=sft: V.tensor_copy(out=dst_[:, 0:sft], in_=src_[:, 0:sft]), reads=['pfx', 'pad'], writes=['pfx'])
                c.op('dve', lambda src_=src_, dst_=dst_, sft=sft: V.tensor_tensor(out=dst_[:, sft:32], in0=src_[:, sft:32], in1=src_[:, 0:32 - sft], op=ALU.add), reads=['pfx', 'pad'], writes=['pfx'])
                src_, dst_ = dst_, (pb_ if dst_ is pa_ else pa_)
            pend = src_
            c.op('dve', lambda: V.tensor_tensor(out=pst[:], in0=pend[:], in1=pad[:], op=ALU.subtract), reads=['pfx', 'pad'], writes=['pst'])
            for bk in range(3):
                t0 = bk * 16
                nt_ = min(16, NT - t0)
                c.op('dve', lambda bk=bk, t0=t0, nt_=nt_: V.tensor_tensor(out=base[:, t0:t0 + nt_, :], in0=PB[bk][:, 0:nt_ * 32].rearrange("p (t e) -> p t e", e=32),
                     in1=pst[:].unsqueeze(1).to_broadcast([128, nt_, 32]), op=ALU.add), reads=[('pb', bk), 'pst'], writes=['base'])
            for oh_, df_, di_, nm in ((oh0, d0f, d0i, 'd0'), (oh1, d1f, d1i, 'd1')):
                c.op('dve', lambda oh_=oh_: V.tensor_tensor(out=prod[:], in0=oh_[:], in1=base[:], op=ALU.mult), reads=['oh0', 'oh1', 'base'], writes=['prod'])
                c.op('dve', lambda df_=df_: V.tensor_reduce(out=df_[:], in_=prod[:], axis=AX.X, op=ALU.add), reads=['prod'], writes=[nm + 'f'])
                c.op('dve', lambda df_=df_, di_=di_: V.tensor_copy(out=di_[:], in_=df_[:]), reads=[nm + 'f'], writes=[nm])
            c.op('dve', lambda: V.tensor_tensor(out=cmpb[:], in0=pend[:].unsqueeze(1).to_broadcast([128, NBLK, 32]), in1=thr[:].unsqueeze(2).to_broadcast([128, NBLK, 32]), op=ALU.is_le),
                 reads=['pfx', 'thr'], writes=['cmpb'])
            c.op('dve', lambda: V.tensor_reduce(out=bef[:], in_=cmpb[:], axis=AX.X, op=ALU.add), reads=['cmpb'], writes=['bef'])
            c.op('dve', lambda: V.tensor_scalar(out=bef[:], in0=bef[:], scalar1=31.0, scalar2=None, op0=ALU.min), reads=['bef'], writes=['bef'])
            pio = ft([128, 1], I32); piof = ft([128, 1]); widxf = ft([128, NBLK]); widx = ft([128, NBLK], I32)
            c.op('pool', lambda: nc.gpsimd.iota(pio[:], pattern=[[0, 1]], base=0, channel_multiplier=1), writes=['pio'])
            c.op('dve', lambda: V.tensor_copy(out=piof[:], in_=pio[:]), reads=['pio'], writes=['piof'])
            c.op('dve', lambda: V.tensor_scalar(out=widxf[:], in0=bef[:], scalar1=128.0, scalar2=piof[:, 0:1], op0=ALU.mult, op1=ALU.add), reads=['bef', 'piof'], writes=['widxf'])
            c.op('dve', lambda: V.tensor_copy(out=widx[:], in_=widxf[:]), reads=['widxf'], writes=['widx'])
            zt = ft([128, D], BF16)
            c.op('pool', lambda: nc.gpsimd.memset(zt[:], 0.0), writes=['zt'])
            xsv = xsd_t.ap().rearrange("(b p) d -> p b d", p=128)
            zkeys = []
            NRB = CAP // 128
            for q4 in range(8):
                b0 = q4 * (NRB // 8)
                nb_ = NRB // 8
                c.dma('act', lambda b0=b0, nb_=nb_: nc.scalar.dma_start(out=xsv[:, b0:b0 + nb_, :], in_=zt[:].unsqueeze(1).to_broadcast([128, nb_, D])), reads=['zt'], writes=[('xsz', q4)])
                zkeys.append(('xsz', q4))
            h2t = [ft([128, D], BF16) for _ in range(2)]
            skeys = []
            for i in range(NT):
                s = i % 2
                rr = 128 if i < NT - 1 else 64
                c.dma('sp', lambda s=s, i=i, rr=rr: nc.sync.dma_start(out=h2t[s][0:rr, :], in_=h2d_t.ap()[i * 128:i * 128 + rr, :]), reads=[('h2d', i)], writes=[('h2t', s)])
                for di_, nm in ((d0i, 'd0'), (d1i, 'd1')):
                    c.dma('pool', lambda s=s, i=i, rr=rr, di_=di_: nc.gpsimd.indirect_dma_start(out=xsd_t.ap(), out_offset=bass.IndirectOffsetOnAxis(ap=di_[0:rr, i:i + 1], axis=0),
                          in_=h2t[s][0:rr, :], in_offset=None), reads=[('h2t', s), nm] + zkeys, writes=[('xss', i, nm)])
                    skeys.append(('xss', i, nm))
            xsb = [ft([128, D], BF16) for _ in range(2)]; xsT = [ft([128, 8, 128], BF16) for _ in range(2)]
            wg = [ft([128, 8, 512], BF16) for _ in range(2)]; wu = [ft([128, 8, 512], BF16) for _ in range(2)]; wd = [ft([128, 4, D], BF16) for _ in range(2)]
            actt = [ft([128, 512]) for _ in range(2)]; ab = [ft([128, 512], BF16) for _ in range(2)]; aT = [ft([128, 4, 128], BF16) for _ in range(2)]
            yev = [ft([128, D]) for _ in range(2)]
            wegv = weg_t.ap().rearrange("e (p k) f -> (e p) (k f)", p=128)
            weuv = weu_t.ap().rearrange("e (p k) f -> (e p) (k f)", p=128)
            wedv = wed_t.ap().rearrange("e (p k) f -> (e p) (k f)", p=128)
            items = [(b, sub) for b in range(NBLK) for sub in range(SUBB)]

            def load_xsb(n):
                b, sub = items[n]
                xs_ = n % 2
                r0 = b * BLK + sub * 128
                c.dma('sp', lambda xs_=xs_, r0=r0: nc.sync.dma_start(out=xsb[xs_][:], in_=xsd_t.ap()[r0:r0 + 128, :]), reads=skeys + zkeys, writes=[('xsb', xs_)])

            load_xsb(0)
            for n, (b, sub) in enumerate(items):
                s = b % 2
                xs_ = n % 2
                if sub == 0:
                    for wt_, wv_, nm in ((wg, wegv, 'wg'), (wu, weuv, 'wu'), (wd, wedv, 'wd')):
                        c.dma('pool', lambda s=s, b=b, wt_=wt_, wv_=wv_: nc.gpsimd.indirect_dma_start(out=wt_[s][:].rearrange("p k f -> p (k f)"), out_offset=None, in_=wv_,
                              in_offset=bass.IndirectOffsetOnAxis(ap=widx[:, b:b + 1], axis=0)), reads=['widx'], writes=[(nm, s)])
                if n + 1 < len(items):
                    load_xsb(n + 1)
                pv0 = pbf(0).rearrange("p (k t) -> p k t", k=8)
                for k in range(8):
                    c.op('pe', lambda k=k, xs_=xs_, pv0=pv0: nc.tensor.transpose(out=pv0[:, k, :], in_=xsb[xs_][:, k:k + 8 * 127 + 1:8], identity=ident[:]), reads=[('xsb', xs_), 'ident'], writes=[('pb', 0)])
                c.op('act', lambda xs_=xs_, pv0=pv0: nc.scalar.copy(out=xsT[xs_][:], in_=pv0), reads=[('pb', 0)], writes=[('xsT', xs_)])
                gi, ui = (1, 2) if xs_ == 0 else (6, 7)
                for k in range(8):
                    c.op('pe', lambda k=k, s=s, xs_=xs_, gi=gi: nc.tensor.matmul(PB[gi][:, :], lhsT=xsT[xs_][:, k, :], rhs=wg[s][:, k, :], start=(k == 0), stop=(k == 7)), reads=[('xsT', xs_), ('wg', s)], writes=[('pb', gi)])
                for k in range(8):
                    c.op('pe', lambda k=k, s=s, xs_=xs_, ui=ui: nc.tensor.matmul(PB[ui][:, :], lhsT=xsT[xs_][:, k, :], rhs=wu[s][:, k, :], start=(k == 0), stop=(k == 7)), reads=[('xsT', xs_), ('wu', s)], writes=[('pb', ui)])
                c.op('act', lambda xs_=xs_, gi=gi: nc.scalar.activation(out=actt[xs_][:], in_=PB[gi][:, :], func=AF.Silu), reads=[('pb', gi)], writes=[('actt', xs_)])
                c.op('dve', lambda xs_=xs_, ui=ui: V.tensor_tensor(out=ab[xs_][:], in0=PB[ui][:, :], in1=actt[xs_][:], op=ALU.mult), reads=[('pb', ui), ('actt', xs_)], writes=[('ab', xs_)])
                pv3 = pbf(3).rearrange("p (k t) -> p k t", k=8)
                for k in range(4):
                    c.op('pe', lambda k=k, xs_=xs_, pv3=pv3: nc.tensor.transpose(out=pv3[:, k, :], in_=ab[xs_][:, k:k + 4 * 127 + 1:4], identity=ident[:]), reads=[('ab', xs_), 'ident'], writes=[('pb', 3)])
                c.op('dve', lambda xs_=xs_, pv3=pv3: V.tensor_copy(out=aT[xs_][:], in_=pv3[:, 0:4, :]), reads=[('pb', 3)], writes=[('aT', xs_)])
                for hf in range(2):
                    for k in range(4):
                        c.op('pe', lambda k=k, s=s, xs_=xs_, hf=hf: nc.tensor.matmul(PB[4 + hf][:, :], lhsT=aT[xs_][:, k, :], rhs=wd[s][:, k, hf * 512:(hf + 1) * 512], start=(k == 0), stop=(k == 3)),
                             reads=[('aT', xs_), ('wd', s)], writes=[('pb', 4 + hf)])
                c.op('act', lambda xs_=xs_: nc.scalar.copy(out=yev[xs_][:, 0:512], in_=PB[4][:, :]), reads=[('pb', 4)], writes=[('yev', xs_, 0)])
                c.op('dve', lambda xs_=xs_: V.tensor_copy(out=yev[xs_][:, 512:1024], in_=PB[5][:, :]), reads=[('pb', 5)], writes=[('yev', xs_, 1)])
                r0 = b * BLK + sub * 128
                c.dma('act', lambda xs_=xs_, r0=r0: nc.scalar.dma_start(out=ysd_t.ap()[r0:r0 + 128, :], in_=yev[xs_][:]), reads=[('yev', xs_, 0), ('yev', xs_, 1)], writes=[('ysd', n)])
            ykeys = [('ysd', n) for n in range(len(items))]
            g2r = {}
            for part, (c0, n) in enumerate(((0, 128), (128, 64))):
                t_ = ft([n, D])
                c.dma('sp', lambda t_=t_, c0=c0, n=n: nc.sync.dma_start(out=t_[:], in_=modrows_t.ap()[3, c0:c0 + n, :]), reads=[('modrows', 3, part)], writes=[('g2r', part)])
                g2r[part] = t_
            gfin = ft([128, D])
            c.dma('sp', lambda: nc.sync.dma_start(out=gfin[:], in_=gfin_t.ap().partition_broadcast(128)), writes=['gfin'])
            NBF = 3
            y0 = [ft([128, D]) for _ in range(NBF)]; y1 = [ft([128, D]) for _ in range(NBF)]; x1r = [ft([128, D]) for _ in range(NBF)]
            fa = [ft([128, D]) for _ in range(NBF)]; st3 = [ft([128, 4]) for _ in range(NBF)]
            junk3 = ft([128, D], BF16)

            def comb_stage1(i):
                s = i % NBF
                rr = 128 if i < NT - 1 else 64
                part = 0 if i < NT - 1 else 1
                c.dma('pool', lambda: nc.gpsimd.indirect_dma_start(out=y0[s][0:rr, :], out_offset=None, in_=ysd_t.ap(),
                      in_offset=bass.IndirectOffsetOnAxis(ap=d0i[0:rr, i:i + 1], axis=0)), reads=ykeys + ['d0'], writes=[('y0', s)])
                c.dma('pool', lambda: nc.gpsimd.indirect_dma_start(out=y1[s][0:rr, :], out_offset=None, in_=ysd_t.ap(),
                      in_offset=bass.IndirectOffsetOnAxis(ap=d1i[0:rr, i:i + 1], axis=0)), reads=ykeys + ['d1'], writes=[('y1', s)])
                c.dma('sp', lambda: nc.sync.dma_start(out=x1r[s][0:rr, :], in_=x1_t.ap()[i * 128:i * 128 + rr, :]), reads=[('x1', i)], writes=[('x1r', s)])
                c.op('act', lambda: nc.scalar.activation(out=fa[s][0:rr, :], in_=y0[s][0:rr, :], func=AF.Copy, scale=w0[0:rr, i:i + 1]), reads=[('y0', s), 'w0'], writes=[('fa', s)])
                c.op('dve', lambda: V.scalar_tensor_tensor(out=fa[s][0:rr, :], in0=y1[s][0:rr, :], scalar=w1[0:rr, i:i + 1], in1=fa[s][0:rr, :], op0=ALU.mult, op1=ALU.add),
                     reads=[('y1', s), 'w1', ('fa', s)], writes=[('fa', s)])
                c.op('dve', lambda: V.tensor_tensor(out=fa[s][0:rr, :], in0=fa[s][0:rr, :], in1=g2r[part][0:rr, :], op=ALU.mult), reads=[('fa', s), ('g2r', part)], writes=[('fa', s)])
                c.op('pool', lambda: nc.gpsimd.tensor_tensor(out=x1r[s][0:rr, :], in0=fa[s][0:rr, :], in1=x1r[s][0:rr, :], op=ALU.add), reads=[('fa', s), ('x1r', s)], writes=[('x1r', s)])

            def comb_stage2(i):
                s = i % NBF
                rr = 128 if i < NT - 1 else 64
                c.op('act', lambda: nc.scalar.activation(out=junk3[0:rr, :], in_=x1r[s][0:rr, :], func=AF.Square, accum_out=st3[s][0:rr, 0:1]), reads=[('x1r', s)], writes=[('st3', s), 'junk3'])
                c.op('act', lambda: nc.scalar.activation(out=st3[s][0:rr, 1:2], in_=st3[s][0:rr, 0:1], func=AF.Sqrt, scale=1.0 / D, bias=epsb[0:rr, :]), reads=[('st3', s), 'epsb'], writes=[('st3', s)])
                c.op('dve', lambda: V.reciprocal(out=st3[s][0:rr, 2:3], in_=st3[s][0:rr, 1:2]), reads=[('st3', s)], writes=[('st3', s)])
                c.op('dve', lambda: V.scalar_tensor_tensor(out=y0[s][0:rr, :], in0=x1r[s][0:rr, :], scalar=st3[s][0:rr, 2:3], in1=gfin[0:rr, :], op0=ALU.mult, op1=ALU.mult),
                     reads=[('x1r', s), ('st3', s), 'gfin'], writes=[('y0', s)])
                dst = yp_t.ap()[i * 128:(i + 1) * 128, :] if i < NT - 1 else ys_t.ap()
                c.dma('act', lambda: nc.scalar.dma_start(out=dst, in_=y0[s][0:rr, :]), reads=[('y0', s)], writes=[('yout', i)])

            for i in range(NT + 1):
                if i < NT:
                    comb_stage1(i)
                if i >= 1:
                    comb_stage2(i - 1)
        c.finish()
    return nc


def build_two_pass():
    nc1 = build_nc(None)
    needed = set(nc1._mk_ctx.record)
    return build_nc(needed)


def _prep_inputs(inp):
    f = lambda a: np.ascontiguousarray(a, dtype=np.float32)
    ohw, vw, sel, bd = _structure_constants()
    shared = {
        "rel_bias": f(inp["rel_bias"]), "norm_mix_g": f(inp["norm_mix_g"][0][None]), "norm_ffn_g": f(inp["norm_ffn_g"][0][None]),
        "norm_final_g": f(inp["norm_final_g"][None]), "w_mod": f(inp["w_mod"][0]), "b_mod": f(inp["b_mod"][0][None]),
        "w_in": f(inp["w_in"][0]), "dw_w": f(inp["dw_w"][0]), "dw_b": f(inp["dw_b"][0][None]), "ln_g": f(inp["ln_conv_g"][0][None]),
        "ln_b": f(inp["ln_conv_b"][0][None]), "w_conv_out": f(inp["w_conv_out"][0]), "w_attn_out": f(inp["w_attn_out"][0]),
        "w_out": f(inp["w_out"][0]),
        "w_rt": f(np.concatenate([inp["w_router_group"][0], inp["w_router_expert"][0].reshape(D, 32)], axis=1)),
        "b_rt": f(np.concatenate([inp["b_router_group"][0], inp["b_router_expert"][0].reshape(32)])[None]),
        "w_eg": f(inp["w_exp_gate"][0]), "w_eu": f(inp["w_exp_up"][0]), "w_ed": f(inp["w_exp_down"][0]),
        "ohw": ohw, "vw": vw, "sel": sel, "bd": bd,
    }
    maps = []
    for cid in range(NCORE):
        b, half = cid // 2, cid % 2
        xp = np.zeros((NEXT, D), np.float32)
        xp[HALO:] = inp["x_prompt"][b, half * NOWN:(half + 1) * NOWN]
        if half == 1:
            xp[:HALO] = inp["x_prompt"][b, NOWN - HALO:NOWN]
        sl = slice(cid * NSQ, (cid + 1) * NSQ)
        m = dict(shared)
        m["xp"] = xp
        m["xs"] = f(inp["x_sample"][sl].reshape(NS, D))
        m["cmod"] = f(np.concatenate([inp["c_prompt"][b][None], inp["c_sample"][sl]], axis=0))
        m["hv"] = np.full((128, 1), float(half), np.float32)
        m["ck128"] = f(inp["cache_kv_w128"][0, sl].reshape(NSQ, 128, 512))
        m["ck512"] = f(inp["cache_kv_w512"][0, sl].reshape(NSQ, 512, 512))
        m["ck2048"] = f(inp["cache_kv_w2048"][0, sl].reshape(NSQ, 2048, 512))
        m["sconv"] = f(inp["state_conv"][0, sl])
        maps.append(m)
    return maps


_NC_CACHE = {}


def kernel(**inp):
    import time as _t
    t0 = _t.time()
    maps = _prep_inputs(inp)
    t1 = _t.time()
    if "nc" not in _NC_CACHE:
        _NC_CACHE["nc"] = build_two_pass()
    nc = _NC_CACHE["nc"]
    t2 = _t.time()
    if STOP_AFTER is not None:
        for m in maps:
            for k in ("w_eg", "w_eu", "w_ed"):
                m.pop(k, None)
    res = run_bass_kernel_spmd(nc, maps, core_ids=list(range(NCORE)))
    print("[kernel] prep %.1fs build %.1fs run %.1fs" % (t1 - t0, t2 - t1, _t.time() - t2), flush=True)
    R = res.results
    _NC_CACHE['last'] = R
    B = 4
    yp = np.zeros((B, 8192, D), np.float32); ys = np.zeros((128, 4, D), np.float32)
    kvp = [np.zeros((1, B, w, 2, 4, 64), np.float32) for (w, _) in GROUPS]
    convp = np.zeros((1, B, 30, 512), np.float32)
    kvs = [np.zeros((1, 128, w, 2, 4, 64), np.float32) for (w, _) in GROUPS]
    convs = np.zeros((1, 128, 30, 512), np.float32)
    for cid in range(NCORE):
        b, half = cid // 2, cid % 2
        r = R[cid]
        sl = slice(cid * NSQ, (cid + 1) * NSQ)
        if "yp" in r:
            yp[b, half * NOWN:(half + 1) * NOWN] = r["yp"]
            ys[sl] = r["ys"].reshape(NSQ, 4, D)
        for gi, (w, _) in enumerate(GROUPS):
            if half == 1:
                kvp[gi][0, b] = r["kvp%d" % w].reshape(w, 2, 4, 64)
            kvs[gi][0, sl] = r["kvs%d" % w].reshape(NSQ, w, 2, 4, 64)
        if half == 1:
            convp[0, b] = r["convp"]
        convs[0, sl] = r["convs"]
    return (yp, ys, kvp[0], kvp[1], kvp[2], convp, kvs[0], kvs[1], kvs[2], convs)
```

```python
import math
import numpy as np
from contextlib import ExitStack
import concourse.bass as bass
import concourse.mybir as mybir
from concourse.bass_utils import run_bass_kernel_spmd

F32 = mybir.dt.float32
BF16 = mybir.dt.bfloat16
I32 = mybir.dt.int32
U32 = mybir.dt.uint32
AF = mybir.ActivationFunctionType
ALU = mybir.AluOpType
AX = mybir.AxisListType

D = 1024
NCORE = 8
HALO = 2048
NOWN = 4096
NEXT = HALO + NOWN
NSQ = 16
NS = 64
NTOK = NEXT + NS
NOT = NOWN + NS
NTILE = 33
GROUPS = ((128, 1), (512, 4), (2048, 16))
EPS = 1e-6
NEXP = 32
BLK = 512
SUBB = BLK // 128
NBLK = (2 * NOT) // BLK + NEXP
CAP = NBLK * BLK
STOP_AFTER = None
DEBUG_SCR = False


class Ctx:
    KD = 8

    def __init__(self, nc, es, needed=None):
        self.nc = nc
        self.es = es
        self.needed = needed
        self.record = set()
        self.iidx = {e: 0 for e in ('pe', 'act', 'dve', 'pool')}
        self.eng = {'pe': nc.tensor, 'act': nc.scalar, 'dve': nc.vector, 'pool': nc.gpsimd, 'sp': nc.sync}
        self.csem = {e: es.enter_context(nc.semaphore('c_' + e)) for e in ('pe', 'act', 'dve', 'pool')}
        self.ccnt = {e: 0 for e in self.csem}
        self.dsem = {q: [es.enter_context(nc.semaphore('d_%s%d' % (q, i))) for i in range(self.KD)]
                     for q in ('sp', 'act', 'pool')}
        self.dcnt = {q: 0 for q in self.dsem}
        self.waited = {e: {} for e in self.eng}
        self.state = {}
        self.sbn = 0

    def sb(self, shape, dt, es=None):
        self.sbn += 1
        return (es or self.es).enter_context(self.nc.sbuf_tensor('sb%d' % self.sbn, list(shape), dt))

    def ps(self, shape, dt):
        self.sbn += 1
        return self.es.enter_context(self.nc.psum_tensor('ps%d' % self.sbn, list(shape), dt))

    def _wait(self, e, evs):
        best = {}
        for (sem, v, src) in evs:
            k = id(sem)
            if k not in best or best[k][1] < v:
                best[k] = (sem, v)
        for k, (sem, v) in best.items():
            if self.waited[e].get(k, 0) >= v:
                continue
            self.eng[e].wait_ge(sem, v)
            self.waited[e][k] = v
            if self.needed is None:
                for ce, cs in self.csem.items():
                    if cs is sem:
                        self.record.add((ce, v))

    def _deps(self, e, reads, writes):
        evs = []
        for k in reads:
            st = self.state.get(k)
            if st and st['w'] is not None:
                evs.append(st['w'])
        for k in writes:
            st = self.state.get(k)
            if st:
                if st['w'] is not None and (st['w'][2] != e or e != 'pe'):
                    evs.append(st['w'])
                for r in st['r']:
                    if r[2] != e or e != 'pe':
                        evs.append(r)
        return evs

    def _commit(self, ev, reads, writes):
        for k in reads:
            st = self.state.setdefault(k, {'w': None, 'r': []})
            st['r'] = [r for r in st['r'] if r[0] is not ev[0]] + [ev]
        for k in writes:
            self.state[k] = {'w': ev, 'r': []}

    @staticmethod
    def _psx(reads, writes):
        ps = [k for k in reads if k == 'pb7' or (isinstance(k, tuple) and k[0] == 'pb')]
        if not ps:
            return list(reads), list(writes)
        return [k for k in reads if k not in ps], list(writes) + [k for k in ps if k not in writes]

    def op(self, e, fn, reads=(), writes=()):
        reads, writes = self._psx(reads, writes)
        self._wait(e, self._deps(e, reads, writes))
        ins = fn()
        self.iidx[e] += 1
        if self.needed is None or (e, self.iidx[e]) in self.needed:
            self.ccnt[e] += 1
            ins.then_inc(self.csem[e], 1)
        ev = (self.csem[e], self.ccnt[e], e)
        self._commit(ev, reads, writes)
        return ev

    def dma(self, q, fn, reads=(), writes=()):
        j = self.dcnt[q]
        sem = self.dsem[q][j % self.KD]
        evs = self._deps(None, reads, writes)
        if j >= self.KD:
            evs.append((sem, 16 * (j // self.KD), 'dma_' + q))
        self._wait(q, evs)
        ins = fn()
        ins.then_inc(sem, 16)
        self.dcnt[q] += 1
        ev = (sem, 16 * (j // self.KD + 1), 'dma_' + q)
        self._commit(ev, reads, writes)
        return ev

    def wait_keys(self, e, keys):
        self._wait(e, self._deps(None, keys, ()))

    def barrier(self):
        evs = []
        for q in self.dsem:
            for i, sem in enumerate(self.dsem[q]):
                n = (self.dcnt[q] - i + self.KD - 1) // self.KD
                if n > 0:
                    evs.append((sem, 16 * n, 'x'))
        for e in self.csem:
            if self.ccnt[e]:
                evs.append((self.csem[e], self.ccnt[e], 'x'))
        for e in self.eng:
            self._wait(e, evs)

    def finish(self):
        evs = []
        for q in self.dsem:
            for i, sem in enumerate(self.dsem[q]):
                n = (self.dcnt[q] - i + self.KD - 1) // self.KD
                if n > 0:
                    evs.append((sem, 16 * n, 'x'))
        for e in self.csem:
            if self.ccnt[e]:
                evs.append((self.csem[e], self.ccnt[e], 'x'))
        self._wait('sp', evs)


def _t5_bucket_np(dist):
    dist = np.asarray(dist, np.int64)
    max_exact = 16
    d_f = np.maximum(dist, 1).astype(np.float32)
    large = max_exact + (np.log(d_f / np.float32(max_exact)) / np.float32(math.log(2048 / max_exact))
                         * np.float32(32 - max_exact)).astype(np.int32)
    large = np.minimum(large, 31)
    return np.where(dist < max_exact, dist, large)


def _structure_constants():
    ohw = np.zeros((32, 3 * 510), np.float32)
    vw = np.zeros((4, 3 * 510), np.float32)
    for g, (win, dil) in enumerate(GROUPS):
        for blk in range(2):
            for u in range(255):
                rel = u + 1 if blk == 0 else u - 127
                ok = (rel <= 128) if blk == 0 else (rel >= 0)
                if ok:
                    b = int(_t5_bucket_np(rel * dil))
                    ohw[b, g * 510 + blk * 255 + u] = 1.0
                    vw[:, g * 510 + blk * 255 + u] = 1.0
    sel = np.zeros((17, 192), np.float32)
    sel[0, 0:128] = 1.0
    for t in range(64):
        sel[1 + t // 4, 128 + t] = 1.0
    bd = np.zeros((64, 128), np.float32)
    for k in range(64):
        for q in range(64):
            if k // 4 == q // 4:
                bd[k, q] = 1.0
        bd[k, 64 + k] = 1.0
    return ohw, vw, sel, bd


def _os_env(k):
    import os
    return os.environ.get(k)


def build_nc(needed=None):
    nc = bass.Bass("TRN2", target_bir_lowering=False)

    def din(name, shape, dt=F32):
        return nc.dram_tensor(name, list(shape), dt, kind="ExternalInput")

    def dout(name, shape, dt=F32):
        return nc.dram_tensor(name, list(shape), dt, kind="ExternalOutput")

    def dscr(name, shape, dt=F32):
        return nc.dram_tensor(name, list(shape), dt, kind="ExternalOutput" if DEBUG_SCR else "Internal")

    xp_t = din("xp", [NEXT, D]); xs_t = din("xs", [NS, D]); cmod_t = din("cmod", [17, D]); hv_t = din("hv", [128, 1])
    ck_t = [din("ck%d" % w, [NSQ, w, 512]) for (w, _) in GROUPS]
    sconv_t = din("sconv", [NSQ, 30, 512])
    relb_t = din("rel_bias", [32, 12])
    gmix_t = din("norm_mix_g", [1, D]); gffn_t = din("norm_ffn_g", [1, D]); gfin_t = din("norm_final_g", [1, D])
    wmod_t = din("w_mod", [D, 6 * D]); bmod_t = din("b_mod", [1, 6 * D])
    win_t = din("w_in", [D, 5376])
    dww_t = din("dw_w", [31, 512]); dwb_t = din("dw_b", [1, 512]); lng_t = din("ln_g", [1, 512]); lnb_t = din("ln_b", [1, 512])
    wco_t = din("w_conv_out", [512, D]); wao_t = din("w_attn_out", [256, D]); wo_t = din("w_out", [D, D])
    wrt_t = din("w_rt", [D, 36]); brt_t = din("b_rt", [1, 36])
    if STOP_AFTER is None:
        weg_t = din("w_eg", [NEXP, D, 512]); weu_t = din("w_eu", [NEXP, D, 512]); wed_t = din("w_ed", [NEXP, 512, D])
    ohw_t = din("ohw", [32, 1530]); vw_t = din("vw", [4, 1530]); sel_t = din("sel", [17, 192]); bd_t = din("bd", [64, 128])

    yp_t = dout("yp", [NOWN, D]); ys_t = dout("ys", [NS, D])
    kvp_t = [dout("kvp%d" % w, [w, 512]) for (w, _) in GROUPS]
    convp_t = dout("convp", [30, 512])
    kvs_t = [dout("kvs%d" % w, [NSQ, w, 512]) for (w, _) in GROUPS]
    convs_t = dout("convs", [NSQ, 30, 512])

    modrows_t = dscr("modrows", [4, 192, D])
    wd_t = dscr("wdscr", [3, 4, 510])
    ebd_t = dscr("ebd", [3, 128, 1024])
    sg_t = dscr("sgscr", [16, 128, NOT], BF16)
    yts_t = dscr("ytscr", [4, 128, NOT], BF16)
    acc_t = dscr("accscr", [3, NOWN, 260])
    accs_t = dscr("accsscr", [NS, 260])
    x1_t = dscr("x1scr", [NOT, D])
    xsd_t = dscr("xsdisp", [CAP, D], BF16)
    ysd_t = dscr("ysdisp", [CAP, D])

    xp = xp_t.ap(); xs = xs_t.ap(); win = win_t.ap()

    with ExitStack() as es:
        c = Ctx(nc, es, needed)
        nc._mk_ctx = c
        PB = [c.ps([128, 512], F32) for _ in range(8)]

        def pbf(i):
            return PB[i][:].bitcast(BF16)

        identf = c.sb([128, 128], F32); ident = c.sb([128, 128], BF16)
        onesb = c.sb([128, 128], BF16); onesf = c.sb([128, 128], F32)
        suf = c.sb([128, 128], F32); jf = c.sb([128, 128], F32)
        epsb = c.sb([128, 1], F32); hv = c.sb([128, 1], F32); one1 = c.sb([128, 1], F32)
        c.op('pool', lambda: nc.gpsimd.memset(onesf[:], 1.0), writes=['onesf'])
        c.op('pool', lambda: nc.gpsimd.memset(onesb[:], 1.0), writes=['onesb'])
        c.op('pool', lambda: nc.gpsimd.memset(epsb[:], EPS), writes=['epsb'])
        c.op('pool', lambda: nc.gpsimd.memset(one1[:], 1.0), writes=['one1'])
        c.op('pool', lambda: nc.gpsimd.affine_select(out=identf[:], in_=onesf[:], pattern=[[-1, 128]], compare_op=ALU.is_equal,
                                                       fill=0.0, base=0, channel_multiplier=1), reads=['onesf'], writes=['identf'])
        c.op('pool', lambda: nc.gpsimd.affine_select(out=jf[:], in_=onesf[:], pattern=[[1, 128]], compare_op=ALU.is_equal,
                                                       fill=0.0, base=-127, channel_multiplier=1), reads=['onesf'], writes=['jf'])
        c.op('pool', lambda: nc.gpsimd.affine_select(out=suf[:], in_=onesf[:], pattern=[[1, 128]], compare_op=ALU.is_gt,
                                                       fill=0.0, base=0, channel_multiplier=-1), reads=['onesf'], writes=['suf'])
        c.op('dve', lambda: nc.vector.tensor_copy(out=ident[:], in_=identf[:]), reads=['identf'], writes=['ident'])
        c.dma('sp', lambda: nc.sync.dma_start(out=hv[:], in_=hv_t.ap()), writes=['hv'])


        es_hT = ExitStack()
        es.enter_context(es_hT)
        hT = c.sb([128, 8, NTOK], BF16, es_hT)
        es_mod = ExitStack()
        a1p = c.sb([128, D], F32, es_mod); b1p = c.sb([128, D], F32, es_mod); a1s = c.sb([64, D], F32, es_mod); b1s = c.sb([64, D], F32, es_mod)
        es0 = ExitStack()
        with es0:
            cm = c.sb([17, D], F32, es0); scm = c.sb([17, D], F32, es0); scT = c.sb([128, 8, 17], F32, es0)
            mtok = c.sb([17, 6 * D], F32, es0); selm = c.sb([17, 192], F32, es0)
            gmb = c.sb([128, D], F32, es0); gfb = c.sb([128, D], F32, es0)
            c.dma('sp', lambda: nc.sync.dma_start(out=cm[:], in_=cmod_t.ap()), writes=['cm'])
            c.dma('sp', lambda: nc.sync.dma_start(out=selm[:], in_=sel_t.ap()), writes=['selm'])
            c.dma('sp', lambda: nc.sync.dma_start(out=gmb[:], in_=gmix_t.ap().partition_broadcast(128)), writes=['gmb'])
            c.dma('sp', lambda: nc.sync.dma_start(out=gfb[:], in_=gffn_t.ap().partition_broadcast(128)), writes=['gfb'])
            c.op('act', lambda: nc.scalar.activation(out=scm[:], in_=cm[:], func=AF.Silu), reads=['cm'], writes=['scm'])
            for k in range(8):
                c.op('pe', lambda k=k: nc.tensor.transpose(out=PB[0][:, k * 17:(k + 1) * 17], in_=scm[0:17, k * 128:(k + 1) * 128],
                                                           identity=identf[0:17, 0:17]), reads=['scm', 'identf'], writes=['pb0'])
            c.op('dve', lambda: nc.vector.tensor_copy(out=scT[:].rearrange("p k s -> p (k s)"), in_=PB[0][:, 0:136]), reads=['pb0'], writes=['scT'])
            wmod = wmod_t.ap().rearrange("(k p) n -> p k n", p=128)
            wbs = [c.sb([128, 8, 512], F32, es0) for _ in range(2)]
            bbs = [c.sb([17, 512], F32, es0) for _ in range(2)]
            for nb in range(12):
                wb = wbs[nb % 2]
                bb = bbs[nb % 2]
                c.dma('sp', lambda wb=wb, nb=nb: nc.sync.dma_start(out=wb[:], in_=wmod[:, :, nb * 512:(nb + 1) * 512]), writes=[('wb', nb % 2)])
                c.dma('sp', lambda bb=bb, nb=nb: nc.sync.dma_start(out=bb[:], in_=bmod_t.ap()[:, nb * 512:(nb + 1) * 512].partition_broadcast(17)),
                      writes=[('bb', nb % 2)])
                pbk = 1 + nb % 2
                for k in range(8):
                    c.op('pe', lambda wb=wb, k=k, pbk=pbk: nc.tensor.matmul(PB[pbk][0:17, 0:512], lhsT=scT[:, k, :], rhs=wb[:, k, :], start=(k == 0), stop=(k == 7)),
                         reads=['scT', ('wb', nb % 2)], writes=[('pb', pbk)])
                c.op('dve', lambda bb=bb, nb=nb, pbk=pbk: nc.vector.tensor_tensor(out=mtok[:, nb * 512:(nb + 1) * 512], in0=PB[pbk][0:17, 0:512], in1=bb[:], op=ALU.add),
                     reads=[('pb', pbk), ('bb', nb % 2)], writes=['mtok'])
            rowst = [c.sb([128, D], F32, es0) for _ in range(2)]
            for kind in range(6):
                for part, (c0, n) in enumerate(((0, 128), (128, 64))):
                    rt = rowst[(kind * 2 + part) % 2]
                    rk = ('rowst', (kind * 2 + part) % 2)
                    for hf in range(2):
                        c.op('pe', lambda hf=hf, c0=c0, n=n, kind=kind: nc.tensor.matmul(PB[3 + hf][0:n, :], lhsT=selm[0:17, c0:c0 + n],
                             rhs=mtok[0:17, kind * D + hf * 512: kind * D + hf * 512 + 512], start=True, stop=True),
                             reads=['selm', 'mtok'], writes=[('pb', 3 + hf)])
                    dst = None; dk = 'nokey'
                    if kind == 0:
                        dst = (b1p, b1s)[part]; dk = ('b1', part)
                    elif kind == 1:
                        dst = (a1p, a1s)[part]; dk = ('a1', part)
                    for hf in range(2):
                        sl = slice(hf * 512, hf * 512 + 512)
                        if kind in (1, 4):
                            gb = gmb if kind == 1 else gfb
                            tgt = dst if dst is not None else rt
                            c.op('dve', lambda hf=hf, n=n, gb=gb, tgt=tgt, sl=sl: nc.vector.scalar_tensor_tensor(out=tgt[0:n, sl], in0=PB[3 + hf][0:n, :], scalar=1.0,
                                 in1=gb[0:n, sl], op0=ALU.add, op1=ALU.mult), reads=[('pb', 3 + hf), 'gmb', 'gfb'], writes=[rk, dk])
                        else:
                            tgt = dst if dst is not None else rt
                            c.op('act', lambda hf=hf, n=n, tgt=tgt, sl=sl: nc.scalar.copy(out=tgt[0:n, sl], in_=PB[3 + hf][0:n, :]),
                                 reads=[('pb', 3 + hf)], writes=[rk, dk])
                    if kind >= 2:
                        c.dma('sp', lambda rt=rt, c0=c0, n=n, kind=kind: nc.sync.dma_start(out=modrows_t.ap()[kind - 2, c0:c0 + n, :], in_=rt[0:n, :]),
                              reads=[rk], writes=[('modrows', kind - 2, part)])
        c.barrier()
        es0 = ExitStack()
        with es0:
            rb = c.sb([32, 12], F32, es0); ohw = c.sb([32, 1530], F32, es0); vw = c.sb([4, 1530], F32, es0)
            wsb = c.sb([4, 1530], F32, es0); hall = c.sb([128, 24, 128], F32, es0); ebst = c.sb([128, 3, 1024], F32, es0)
            c.dma('sp', lambda: nc.sync.dma_start(out=rb[:], in_=relb_t.ap()), writes=['rb'])
            c.dma('sp', lambda: nc.sync.dma_start(out=ohw[:], in_=ohw_t.ap()), writes=['ohw'])
            c.dma('sp', lambda: nc.sync.dma_start(out=vw[:], in_=vw_t.ap()), writes=['vw'])
            for g in range(3):
                c.op('pe', lambda g=g: nc.tensor.matmul(PB[5][0:4, 0:510], lhsT=rb[:, 4 * g:4 * g + 4], rhs=ohw[:, g * 510:(g + 1) * 510], start=True, stop=True),
                     reads=['rb', 'ohw'], writes=[('pb', 5)])
                c.op('act', lambda g=g: nc.scalar.activation(out=wsb[:, g * 510:(g + 1) * 510], in_=PB[5][0:4, 0:510], func=AF.Exp), reads=[('pb', 5)], writes=['wsb'])
            c.op('dve', lambda: nc.vector.tensor_tensor(out=wsb[:], in0=wsb[:], in1=vw[:], op=ALU.mult), reads=['wsb', 'vw'], writes=['wsb'])
            c.dma('sp', lambda: nc.sync.dma_start(out=wd_t.ap().rearrange("g h u -> h g u"), in_=wsb[:].rearrange("h (g u) -> h g u", g=3)), reads=['wsb'], writes=['wd'])
            for g in range(3):
                for h in range(4):
                    for blk in range(2):
                        idx = (g * 4 + h) * 2 + blk
                        src = bass.AP(wd_t, (g * 4 + h) * 510 + blk * 255, [[1, 128], [1, 128]])
                        c.dma('sp', lambda idx=idx, src=src: nc.sync.dma_start(out=hall[:, idx, :], in_=src), reads=['wd'], writes=[('hall', idx)])
            for g in range(3):
                for hf in range(2):
                    c.op('pe', lambda g=g, hf=hf: nc.tensor.matmul(PB[6 + hf][:, :], lhsT=jf[:], rhs=hall[:, g * 8 + hf * 4: g * 8 + hf * 4 + 4, :].rearrange("p a q -> p (a q)"),
                         start=True, stop=True), reads=['jf'] + [('hall', g * 8 + hf * 4 + i) for i in range(4)], writes=[('pb', 6 + hf)])
                    c.op('act', lambda g=g, hf=hf: nc.scalar.copy(out=ebst[:, g, hf * 512:(hf + 1) * 512], in_=PB[6 + hf][:, :]), reads=[('pb', 6 + hf)], writes=[('ebst', g)])
                c.dma('sp', lambda g=g: nc.sync.dma_start(out=ebd_t.ap()[g], in_=ebst[:, g, :]), reads=[('ebst', g)], writes=[('ebd', g)])

        c.barrier()
        def norm_to_T(tidx, src_ap, n, arow, brow, akey, bkey, bufs):
            xt, t1, hb, stt, junk = bufs
            s = tidx % 2
            c.dma('sp', lambda: nc.sync.dma_start(out=xt[s][0:n, :], in_=src_ap), writes=[('xt', s)])
            c.op('act', lambda: nc.scalar.activation(out=junk[0:n, :], in_=xt[s][0:n, :], func=AF.Square, accum_out=stt[s][0:n, 0:1]),
                 reads=[('xt', s)], writes=[('stt', s), 'junk'])
            c.op('act', lambda: nc.scalar.activation(out=stt[s][0:n, 1:2], in_=stt[s][0:n, 0:1], func=AF.Sqrt, scale=1.0 / D, bias=epsb[0:n, :]),
                 reads=[('stt', s), 'epsb'], writes=[('stt', s)])
            c.op('dve', lambda: nc.vector.reciprocal(out=stt[s][0:n, 2:3], in_=stt[s][0:n, 1:2]), reads=[('stt', s)], writes=[('stt', s)])
            c.op('dve', lambda: nc.vector.scalar_tensor_tensor(out=t1[s][0:n, :], in0=xt[s][0:n, :], scalar=stt[s][0:n, 2:3], in1=arow[0:n, :],
                                                                op0=ALU.mult, op1=ALU.mult), reads=[('xt', s), ('stt', s), akey], writes=[('t1', s)])
            c.op('pool', lambda: nc.gpsimd.tensor_tensor(out=hb[s][0:n, :], in0=t1[s][0:n, :], in1=brow[0:n, :], op=ALU.add),
                 reads=[('t1', s), bkey], writes=[('hb', s)])
            pv = pbf(s).rearrange("p (k t) -> p k t", k=8)
            for k in range(8):
                c.op('pe', lambda k=k: nc.tensor.transpose(out=pv[:, k, 0:n], in_=hb[s][0:n, k * 128:(k + 1) * 128], identity=ident[0:n, 0:n]),
                     reads=[('hb', s), 'ident'], writes=[('pb', s)])
            return s, pv

        esA = ExitStack()
        with esA:
            xt = [c.sb([128, D], F32, esA) for _ in range(2)]; t1 = [c.sb([128, D], F32, esA) for _ in range(2)]
            hb = [c.sb([128, D], BF16, esA) for _ in range(2)]; stt = [c.sb([128, 4], F32, esA) for _ in range(2)]
            junk = c.sb([128, D], BF16, esA)
            bufsA = (xt, t1, hb, stt, junk)
            for t in range(49):
                if t < 48:
                    n = 128; src = xp[t * 128:(t + 1) * 128, :]; ar, br = a1p, b1p; col = t * 128; pk = 0
                else:
                    n = 64; src = xs; ar, br = a1s, b1s; col = NEXT; pk = 1
                s, pv = norm_to_T(t, src, n, ar, br, ('a1', pk), ('b1', pk), bufsA)
                c.op('act', lambda pv=pv, col=col, n=n: nc.scalar.copy(out=hT[:, :, col:col + n], in_=pv[:, :, 0:n]), reads=[('pb', s)], writes=['hT'])
        c.barrier()
        es_mod.close()

        winr = win.rearrange("(k p) n -> p k n", p=128)

        def load_w(dst, c0, ncol, key):
            c.dma('pool', lambda: nc.gpsimd.dma_start(out=dst, in_=winr[:, :, c0:c0 + ncol]), writes=[key])

        def proj_T(ps_ap, pkey, wt, wkey, hcols):
            for k in range(8):
                c.op('pe', lambda k=k: nc.tensor.matmul(ps_ap, lhsT=wt[:, k, :], rhs=hT[:, k, hcols], start=(k == 0), stop=(k == 7)),
                     reads=['hT', wkey], writes=[pkey])

        def psv(i, sl=slice(None), n=512):
            return PB[i][sl, 0:n]

        OT = [(HALO + 512 * m, 512 * m, 512) for m in range(8)] + [(NEXT, NOWN, NS)]

        shiftq = []
        for g, (wing, d) in enumerate(GROUPS):
            A_ = (1, 4, 28)[g]
            npart = 4 if g == 2 else 1
            for q4 in range(npart):
                shiftq.append((g, A_, q4 * (A_ // npart), A_ // npart))
        shiftq = [shiftq[2], shiftq[3], shiftq[4], shiftq[5], shiftq[0], shiftq[1]]

        def emit_shift(nmax):
            for _ in range(nmax):
                if not shiftq:
                    return
                g, A_, a0, na = shiftq.pop(0)
                wing = GROUPS[g][0]
                c.dma('act', lambda g=g, A_=A_, a0=a0, na=na, wing=wing: nc.scalar.dma_start(
                      out=kvs_t[g].ap()[:, 0:wing - 4, :].rearrange("b (a r) c -> b a (r c)", a=A_)[:, a0:a0 + na, :],
                      in_=ck_t[g].ap()[:, 4:wing, :].rearrange("b (a r) c -> b a (r c)", a=A_)[:, a0:a0 + na, :]), writes=[('kvs_shift', g, a0)])

        esG = ExitStack()
        with esG:
            wj = [c.sb([128, 8, 128], BF16, esG) for _ in range(2)]
            sgrow = [c.sb([128, NOT], BF16, esG) for _ in range(2)]
            for j in range(16):
                s = j % 2
                load_w(wj[s][:], 3328 + j * 128, 128, ('wj', s))
                for m, (hc, oi, n) in enumerate(OT):
                    pi = m % 4
                    pa = psv(pi, n=n)
                    proj_T(pa, ('pb', pi), wj[s], ('wj', s), slice(hc, hc + n))
                    c.op('act', lambda pa=pa, oi=oi, n=n, s=s: nc.scalar.activation(out=sgrow[s][:, oi:oi + n], in_=pa, func=AF.Sigmoid),
                         reads=[('pb', pi)], writes=[('sgrow', s)])
                c.dma('sp', lambda j=j, s=s: nc.sync.dma_start(out=sg_t.ap()[j], in_=sgrow[s][:]), reads=[('sgrow', s)], writes=[('sg', j)])

        c.barrier()
        if STOP_AFTER == 'A':
            c.finish()
            return nc

        esC = ExitStack()
        with esC:
            yT = c.sb([128, 4, NOT], BF16, esC)
            dwT = c.sb([128, 4, 31], F32, esC); dwb = c.sb([128, 4], F32, esC); lng = c.sb([128, 4], F32, esC); lnb = c.sb([128, 4], F32, esC)
            with nc.allow_non_contiguous_dma(reason="tiny per-channel parameter loads"):
                for cc in range(4):
                    c.dma('sp', lambda cc=cc: nc.sync.dma_start(out=dwT[:, cc, :], in_=dww_t.ap()[:, cc * 128:(cc + 1) * 128].rearrange("j p -> p j")), writes=[('dwT', cc)])
                c.dma('sp', lambda: nc.sync.dma_start(out=dwb[:], in_=dwb_t.ap().rearrange("o (c p) -> p (o c)", p=128)), writes=['dwb'])
                c.dma('sp', lambda: nc.sync.dma_start(out=lng[:], in_=lng_t.ap().rearrange("o (c p) -> p (o c)", p=128)), writes=['lng'])
                c.dma('sp', lambda: nc.sync.dma_start(out=lnb[:], in_=lnb_t.ap().rearrange("o (c p) -> p (o c)", p=128)), writes=['lnb'])
            uxT = c.sb([128, 4, NSQ, 34], BF16, esC)
            uTf = c.sb([128, 4, 94], F32, esC)
            sct = [c.sb([120, 512], F32, esC)] * 2
            for i4 in range(4):
                s = i4 % 2
                c.dma('sp', lambda i4=i4, s=s: nc.sync.dma_start(out=sct[s][:], in_=sconv_t.ap()[4 * i4:4 * i4 + 4].rearrange("b t c -> (b t) c")), writes=[('sct', 0)])
                for cc in range(4):
                    c.op('pe', lambda cc=cc, s=s: nc.tensor.transpose(out=PB[2][:, 0:120], in_=sct[s][:, cc * 128:(cc + 1) * 128], identity=identf[0:120, 0:120]),
                         reads=[('sct', 0), 'identf'], writes=[('pb', 2)])
                    c.op('act', lambda cc=cc, i4=i4: nc.scalar.copy(out=uxT[:, cc, 4 * i4:4 * i4 + 4, 0:30], in_=PB[2][:, 0:120].rearrange("p (b t) -> p b t", b=4)),
                         reads=[('pb', 2)], writes=['uxT'])
            c.dma('sp', lambda: nc.sync.dma_start(out=convs_t.ap()[:, 0:26, :], in_=sconv_t.ap()[:, 4:30, :]), writes=['convs_a'])
            esC1 = ExitStack()
            wul = [c.sb([128, 8, 128], BF16, esC1) for _ in range(2)]; wug = [c.sb([128, 8, 128], BF16, esC1) for _ in range(2)]
            diag = [c.sb([128, 31, 128], BF16, esC1)] * 2
            ucT = [c.sb([128, 30 + NOWN], BF16, esC1)] * 2
            sgt = [c.sb([128, 512], F32, esC1) for _ in range(2)]
            UT = [(HALO - 30, -30, 30)] + OT
            for cc in range(4):
                s = cc % 2
                load_w(wul[s][:], 2304 + cc * 128, 128, ('wul', s))
                load_w(wug[s][:], 2816 + cc * 128, 128, ('wug', s))
                emit_shift(1)
                for j in range(31):
                    eng = 'dve' if j % 2 == 0 else 'pool'
                    e_ = nc.vector if eng == 'dve' else nc.gpsimd
                    c.op(eng, lambda j=j, e_=e_: e_.tensor_scalar(out=diag[s][:, j, :], in0=identf[:], scalar1=dwT[:, cc, j:j + 1], scalar2=None, op0=ALU.mult),
                         reads=['identf', ('dwT', cc)], writes=[('diag', 0, j)])
                for m, (hc, oi, n) in enumerate(UT):
                    pl = psv(0, n=n); pg = psv(1, n=n)
                    proj_T(pl, ('pb', 0), wul[s], ('wul', s), slice(hc, hc + n))
                    proj_T(pg, ('pb', 1), wug[s], ('wug', s), slice(hc, hc + n))
                    b_ = m % 2
                    c.op('act', lambda pg=pg, n=n, b_=b_: nc.scalar.activation(out=sgt[b_][:, 0:n], in_=pg, func=AF.Sigmoid), reads=[('pb', 1)], writes=[('sgt', b_)])
                    if oi < 0:
                        c.op('dve', lambda pl=pl, n=n, b_=b_: nc.vector.tensor_tensor(out=sgt[b_][:, 0:n], in0=pl, in1=sgt[b_][:, 0:n], op=ALU.mult),
                             reads=[('pb', 0), ('sgt', b_)], writes=[('sgt', b_)])
                        c.op('dve', lambda n=n, b_=b_: nc.vector.tensor_scalar(out=ucT[s][:, 0:30], in0=sgt[b_][:, 0:n], scalar1=hv[:, 0:1], scalar2=None, op0=ALU.mult),
                             reads=[('sgt', b_), 'hv'], writes=[('ucT', 0, 0)])
                    elif oi < NOWN:
                        c.op('dve', lambda pl=pl, n=n, b_=b_, oi=oi: nc.vector.tensor_tensor(out=ucT[s][:, 30 + oi:30 + oi + n], in0=pl, in1=sgt[b_][:, 0:n], op=ALU.mult),
                             reads=[('pb', 0), ('sgt', b_)], writes=[('ucT', 0, 1 + oi // 512)])
                        if oi == NOWN - 512:
                            c.op('dve', lambda pl=pl, b_=b_: nc.vector.tensor_tensor(out=uTf[:, cc, 0:30], in0=pl[:, 482:512], in1=sgt[b_][:, 482:512], op=ALU.mult),
                                 reads=[('pb', 0), ('sgt', b_)], writes=[('uTf', cc)])
                    else:
                        c.op('dve', lambda pl=pl, b_=b_: nc.vector.tensor_tensor(out=uTf[:, cc, 30:94], in0=pl, in1=sgt[b_][:, 0:64], op=ALU.mult),
                             reads=[('pb', 0), ('sgt', b_)], writes=[('uTf', cc)])
                        c.op('dve', lambda: nc.vector.tensor_copy(out=uxT[:, cc, :, 30:34], in_=uTf[:, cc, 30:94].rearrange("p (b t) -> p b t", t=4)),
                             reads=[('uTf', cc)], writes=['uxT'])
                dkeys = [('diag', 0, j) for j in range(31)]
                for m in range(8):
                    py = psv(2 + m % 2)
                    for j in range(31):
                        c.op('pe', lambda j=j, m=m, py=py: nc.tensor.matmul(py, lhsT=diag[s][:, j, :], rhs=ucT[s][:, 512 * m + j: 512 * m + j + 512], start=(j == 0), stop=(j == 30)),
                             reads=[dkeys[j], ('ucT', 0, 0), ('ucT', 0, 1 + m), ('ucT', 0, m)], writes=[('pb', 2 + m % 2)])
                    c.op('act', lambda m=m, py=py: nc.scalar.activation(out=yT[:, cc, 512 * m:512 * m + 512], in_=py, func=AF.Identity, bias=dwb[:, cc:cc + 1], scale=1.0),
                         reads=[('pb', 2 + m % 2), 'dwb'], writes=[('yT', m)])
                pys = PB[2][:, 0:64]
                for j in range(31):
                    c.op('pe', lambda j=j: nc.tensor.matmul(pys.rearrange("p (b t) -> p b t", t=4), lhsT=diag[s][:, j, :], rhs=uxT[:, cc, :, j:j + 4], start=(j == 0), stop=(j == 30)),
                         reads=[dkeys[j], 'uxT'], writes=[('pb', 2)])
                c.op('act', lambda: nc.scalar.activation(out=yT[:, cc, NOWN:NOT], in_=pys, func=AF.Identity, bias=dwb[:, cc:cc + 1], scale=1.0),
                     reads=[('pb', 2), 'dwb'], writes=[('yT', 8)])
            c.barrier()
            esC1.close()
            cpo = c.sb([94, 512], F32, esC)
            for cc in range(4):
                c.op('pe', lambda cc=cc: nc.tensor.transpose(out=PB[0][0:94, 0:128], in_=uTf[:, cc, :], identity=identf[:]), reads=[('uTf', cc), 'identf'], writes=[('pb', 0)])
                c.op('act', lambda cc=cc: nc.scalar.copy(out=cpo[:, cc * 128:(cc + 1) * 128], in_=PB[0][0:94, 0:128]), reads=[('pb', 0)], writes=['cpo'])
            c.dma('sp', lambda: nc.sync.dma_start(out=convp_t.ap(), in_=cpo[0:30, :]), reads=['cpo'], writes=['convp'])
            for b in range(NSQ):
                c.dma('sp', lambda b=b: nc.sync.dma_start(out=convs_t.ap()[b, 26:30, :], in_=cpo[30 + 4 * b:34 + 4 * b, :]), reads=['cpo'], writes=[('convs_b', b)])
            emit_shift(2)
            sq = c.sb([128, 4, 512], BF16, esC); mean = c.sb([128, 512], F32, esC); msq = c.sb([128, 512], F32, esC)
            var = c.sb([128, 512], F32, esC); rstd = c.sb([128, 512], F32, esC); tt = c.sb([128, 4, 512], F32, esC)
            for m, (hc, oi, n) in enumerate(OT):
                yk = ('yT', m)
                c.op('act', lambda oi=oi, n=n: nc.scalar.activation(out=sq[:, :, 0:n], in_=yT[:, :, oi:oi + n], func=AF.Square), reads=[yk], writes=['sq'])
                p1 = psv(4, n=n); p2 = psv(5, n=n)
                for cc in range(4):
                    c.op('pe', lambda cc=cc, p1=p1, oi=oi, n=n: nc.tensor.matmul(p1, lhsT=onesb[:], rhs=yT[:, cc, oi:oi + n], start=(cc == 0), stop=(cc == 3)),
                         reads=[yk, 'onesb'], writes=[('pb', 4)])
                for cc in range(4):
                    c.op('pe', lambda cc=cc, p2=p2, n=n: nc.tensor.matmul(p2, lhsT=onesb[:], rhs=sq[:, cc, 0:n], start=(cc == 0), stop=(cc == 3)),
                         reads=['sq', 'onesb'], writes=[('pb', 5)])
                c.op('dve', lambda p1=p1, n=n: nc.vector.tensor_scalar(out=mean[:, 0:n], in0=p1, scalar1=1.0 / 512, scalar2=None, op0=ALU.mult), reads=[('pb', 4)], writes=['mean'])
                c.op('pool', lambda n=n: nc.gpsimd.tensor_tensor(out=msq[:, 0:n], in0=mean[:, 0:n], in1=mean[:, 0:n], op=ALU.mult), reads=['mean'], writes=['msq'])
                c.op('dve', lambda p2=p2, n=n: nc.vector.scalar_tensor_tensor(out=var[:, 0:n], in0=p2, scalar=1.0 / 512, in1=msq[:, 0:n], op0=ALU.mult, op1=ALU.subtract),
                     reads=[('pb', 5), 'msq'], writes=['var'])
                c.op('act', lambda n=n: nc.scalar.activation(out=var[:, 0:n], in_=var[:, 0:n], func=AF.Sqrt, scale=1.0, bias=epsb[:, :]), reads=['var', 'epsb'], writes=['var'])
                c.op('dve', lambda n=n: nc.vector.reciprocal(out=rstd[:, 0:n], in_=var[:, 0:n]), reads=['var'], writes=['rstd'])
                c.op('dve', lambda oi=oi, n=n: nc.vector.tensor_tensor(out=tt[:, :, 0:n], in0=yT[:, :, oi:oi + n], in1=mean[:, 0:n].unsqueeze(1).to_broadcast([128, 4, n]), op=ALU.subtract),
                     reads=[yk, 'mean'], writes=['tt'])
                c.op('pool', lambda n=n: nc.gpsimd.tensor_tensor(out=tt[:, :, 0:n], in0=tt[:, :, 0:n], in1=rstd[:, 0:n].unsqueeze(1).to_broadcast([128, 4, n]), op=ALU.mult),
                     reads=['tt', 'rstd'], writes=['tt'])
                for cc in range(4):
                    c.op('act', lambda cc=cc, oi=oi, n=n: nc.scalar.activation(out=yT[:, cc, oi:oi + n], in_=tt[:, cc, 0:n], func=AF.Silu, bias=lnb[:, cc:cc + 1], scale=lng[:, cc:cc + 1]),
                         reads=['tt', 'lng', 'lnb'], writes=[yk])
            for cc in range(4):
                c.dma('sp', lambda cc=cc: nc.sync.dma_start(out=yts_t.ap()[cc], in_=yT[:, cc, :]), reads=[('yT', m) for m in range(9)], writes=[('yts', cc)])

        c.barrier()
        if STOP_AFTER == 'C':
            c.finish()
            return nc

        emit_shift(99)
        esD = ExitStack()
        with esD:
            ebf = c.sb([128, 1024], F32, esD)
            smk = c.sb([128, 9, 4, 4], F32, esD)
            nmk = c.sb([64, 3, 4, 64], F32, esD)
            bdm = c.sb([64, 128], F32, esD)
            pz = [c.sb([128, 4, 64], BF16, esD) for _ in range(NSQ)]
            v1n = c.sb([64, 3, 4, 65], BF16, esD)
            wkv = c.sb([128, 8, 512], BF16, esD)
            wq = c.sb([128, 8, 128], BF16, esD); wk = c.sb([128, 8, 128], BF16, esD)
            qT = c.sb([128, NOT], BF16, esD); kT = c.sb([128, NTOK], BF16, esD)
            qs = c.sb([128, 2, 64], BF16, esD); ks = c.sb([128, 2, 64], BF16, esD)
            v1 = c.sb([128, 48, 4, 65], BF16, esD)
            kvst = [c.sb([128, 512], F32, esD) for _ in range(2)]
            ef = [c.sb([128, 512], F32, esD) for _ in range(2)]
            pt = [c.sb([128, 512], BF16, esD) for _ in range(2)]
            oev = [c.sb([128, 130], F32, esD) for _ in range(2)]
            ctile = [c.sb([128, 512], F32, esD) for _ in range(2)]
            kTs = [c.sb([128, 2, 128], BF16, esD) for _ in range(2)]
            v1s = [c.sb([128, 4, 65], BF16, esD) for _ in range(2)]
            ess2 = [c.sb([128, 16], F32, esD) for _ in range(2)]
            en = c.sb([64, 256], F32, esD); pn = c.sb([64, 4, 64], BF16, esD)
            osv = c.sb([64, 260], F32, esD)
            c.dma('sp', lambda: nc.sync.dma_start(out=bdm[:], in_=bd_t.ap()), writes=['bdm'])
            c.op('pool', lambda: nc.gpsimd.memset(smk[:], 0.0), writes=['smk'])
            for b in range(NSQ):
                c.op('pool', lambda b=b: nc.gpsimd.memset(pz[b][:], 0.0), writes=[('pz', b)])
            for s in range(2):
                c.op('pool', lambda s=s: nc.gpsimd.memset(v1s[s][:], 1.0), writes=[('v1s', s)])
            c.op('pool', lambda: nc.gpsimd.memset(v1n[:], 1.0), writes=['v1n'])
            n_os = 1
            zl = c.sb([128, 64], BF16, esD); zr = c.sb([128, 260], BF16, esD)
            c.op('pool', lambda: nc.gpsimd.memset(zl[:], 0.0), writes=['zl'])
            c.op('pool', lambda: nc.gpsimd.memset(zr[:], 0.0), writes=['zr'])
            c.op('pe', lambda: nc.tensor.matmul(PB[7][0:64, 0:260], lhsT=zl[:], rhs=zr[:], start=True, stop=False), reads=['zl', 'zr'], writes=['pb7'])
            qb_i = 0
            for g, (wing, d) in enumerate(GROUPS):
                e0 = HALO - wing
                nbl = (NEXT - e0) // (128 * d)
                ebv = ebf[:].rearrange("p (h b q) -> p h b q", h=4, b=2)
                c.dma('sp', lambda g=g: nc.sync.dma_start(out=ebf[:], in_=ebd_t.ap()[g]), reads=[('ebd', g)], writes=['ebf'])
                if g == 0:
                    c.op('dve', lambda: nc.vector.tensor_copy(out=smk[:, 0, :, :], in_=ebv[:, :, 0, 0:4]), reads=['ebf'], writes=['smk'])
                else:
                    for r in range(4):
                        c.op('dve', lambda r=r, g=g: nc.vector.tensor_copy(out=smk[:, 1 + 4 * (g - 1) + r, :, r:r + 1], in_=ebv[:, :, 0, 0:1]), reads=['ebf'], writes=['smk'])
                bsel = bdm[:, 0:64] if g == 0 else bdm[:, 64:128]
                c.op('dve', lambda g=g, bsel=bsel: nc.vector.tensor_tensor(out=nmk[:, g, :, :], in0=ebv[0:64, :, 1, 0:64], in1=bsel.unsqueeze(1).to_broadcast([64, 4, 64]), op=ALU.mult),
                     reads=['ebf', 'bdm'], writes=['nmk'])
                c.dma('pool', lambda g=g: nc.gpsimd.dma_start(out=wkv[:, :, 0:256], in_=winr[:, :, 768 + 256 * g: 1024 + 256 * g]), writes=['wkv_k'])
                c.dma('pool', lambda g=g: nc.gpsimd.dma_start(out=wkv[:, :, 256:512], in_=winr[:, :, 1536 + 256 * g: 1792 + 256 * g]), writes=['wkv_v'])
                nkv = 0
                for r in range(d):
                    for i in range(nbl):
                        bi = r * nbl + i
                        cs0 = e0 + r + 128 * d * i
                        hsl = slice(cs0, cs0 + 127 * d + 1, d)
                        pi = 4 + nkv % 3
                        for k in range(8):
                            c.op('pe', lambda k=k, hsl=hsl, pi=pi: nc.tensor.matmul(PB[pi][:, :], lhsT=hT[:, k, hsl], rhs=wkv[:, k, :], start=(k == 0), stop=(k == 7)),
                                 reads=['hT', 'wkv_k', 'wkv_v'], writes=[('pb', pi)])
                        vc = hv if i == 0 else one1
                        c.op('dve', lambda bi=bi, pi=pi, vc=vc: nc.vector.tensor_scalar(out=v1[:, bi, :, 0:64], in0=PB[pi][:, 256:512].rearrange("p (h e) -> p h e", h=4),
                             scalar1=vc[:, 0:1], scalar2=None, op0=ALU.mult), reads=[('pb', pi), 'hv', 'one1'], writes=[('v1', bi)])
                        c.op('pool', lambda bi=bi, vc=vc: nc.gpsimd.tensor_copy(out=v1[:, bi, :, 64:65], in_=vc[:, 0:1].unsqueeze(1).to_broadcast([128, 4, 1])),
                             reads=['hv', 'one1'], writes=[('v1o', bi)])
                        if i == nbl - 1:
                            s = nkv % 2
                            c.op('act', lambda s=s, pi=pi: nc.scalar.copy(out=kvst[s][:], in_=PB[pi][:, :]), reads=[('pb', pi)], writes=[('kvst', s)])
                            c.dma('sp', lambda s=s, r=r, g=g, d=d, wing=wing: nc.sync.dma_start(out=kvp_t[g].ap()[r:wing:d, :], in_=kvst[s][:]), reads=[('kvst', s)], writes=[('kvp', g, r)])
                        nkv += 1
                pi = 4 + nkv % 3
                for k in range(8):
                    c.op('pe', lambda k=k, pi=pi: nc.tensor.matmul(PB[pi][0:64, :], lhsT=hT[:, k, NEXT:NTOK], rhs=wkv[:, k, :], start=(k == 0), stop=(k == 7)),
                         reads=['hT', 'wkv_k', 'wkv_v'], writes=[('pb', pi)])
                s = nkv % 2
                c.op('act', lambda s=s, pi=pi: nc.scalar.copy(out=kvst[s][0:64, :], in_=PB[pi][0:64, :]), reads=[('pb', pi)], writes=[('kvst', s)])
                c.op('dve', lambda g=g, pi=pi: nc.vector.tensor_copy(out=v1n[:, g, :, 0:64], in_=PB[pi][0:64, 256:512].rearrange("p (h e) -> p h e", h=4)), reads=[('pb', pi)], writes=['v1n'])
                for b in range(NSQ):
                    c.dma('sp', lambda b=b, s=s, g=g, wing=wing: nc.sync.dma_start(out=kvs_t[g].ap()[b, wing - 4:wing, :], in_=kvst[s][4 * b:4 * b + 4, :]), reads=[('kvst', s)], writes=[('kvsn', g, b)])
                for pair in range(2):
                    if _os_env('MK_SKIP_ATT'):
                        continue
                    c.dma('pool', lambda g=g, pair=pair: nc.gpsimd.dma_start(out=wq[:], in_=winr[:, :, 256 * g + 128 * pair: 256 * g + 128 * pair + 128]), writes=['wq'])
                    c.dma('pool', lambda g=g, pair=pair: nc.gpsimd.dma_start(out=wk[:], in_=winr[:, :, 768 + 256 * g + 128 * pair: 768 + 256 * g + 128 * pair + 128]), writes=['wk'])
                    npj = 0
                    for (hc, oi, n) in OT:
                        pi = 4 + npj % 3
                        proj_T(PB[pi][:, 0:n], ('pb', pi), wq, 'wq', slice(hc, hc + n))
                        c.op('act', lambda pi=pi, oi=oi, n=n: nc.scalar.copy(out=qT[:, oi:oi + n], in_=PB[pi][:, 0:n]), reads=[('pb', pi)], writes=['qT'])
                        npj += 1
                    cs_ = e0
                    while cs_ < NTOK:
                        n = min(512, NTOK - cs_)
                        pi = 4 + npj % 3
                        proj_T(PB[pi][:, 0:n], ('pb', pi), wk, 'wk', slice(cs_, cs_ + n))
                        if npj % 2 == 0:
                            c.op('dve', lambda pi=pi, cs_=cs_, n=n: nc.vector.tensor_copy(out=kT[:, cs_:cs_ + n], in_=PB[pi][:, 0:n]), reads=[('pb', pi)], writes=['kT'])
                        else:
                            c.op('act', lambda pi=pi, cs_=cs_, n=n: nc.scalar.copy(out=kT[:, cs_:cs_ + n], in_=PB[pi][:, 0:n]), reads=[('pb', pi)], writes=['kT_a'])
                        npj += 1
                        cs_ += n
                    c.op('dve', lambda pair=pair: nc.vector.tensor_copy(out=qs[:, pair, :], in_=qT[:, NOWN:NOT]), reads=['qT'], writes=['qs'])
                    c.op('dve', lambda pair=pair: nc.vector.tensor_copy(out=ks[:, pair, :], in_=kT[:, NEXT:NTOK]), reads=['kT', 'kT_a'], writes=['ks'])
                    for r in range(d):
                        for i in range(1, nbl):
                            s = qb_i % 2
                            qb_i += 1
                            q0 = r + 128 * d * (i - 1)
                            qsl = slice(q0, q0 + 127 * d + 1, d)
                            SBK = ((0, 1), (4, 5))[s]
                            for hh in range(2):
                                po = hh * 64
                                for blk in range(2):
                                    k0 = e0 + r + 128 * d * (i - 1 + blk)
                                    ksl = slice(k0, k0 + 127 * d + 1, d)
                                    c.op('pe', lambda bk=SBK[hh], po=po, ksl=ksl, qsl=qsl, blk=blk: nc.tensor.matmul(PB[bk][:, blk * 128:blk * 128 + 128], lhsT=kT[po:po + 64, ksl], rhs=qT[po:po + 64, qsl], start=True, stop=True),
                                         reads=['kT', 'kT_a', 'qT'], writes=[('pb', SBK[hh])])
                            for hh in range(2):
                                c.op('act', lambda s=s, hh=hh, bk=SBK[hh]: nc.scalar.activation(out=ef[s][:, hh * 256:(hh + 1) * 256], in_=PB[bk][:, 0:256], func=AF.Exp, scale=0.125),
                                     reads=[('pb', SBK[hh])], writes=[('ef', s, hh)])
                            c.op('dve', lambda s=s, pair=pair: nc.vector.tensor_tensor(out=pt[s][:], in0=ef[s][:], in1=ebf[:, pair * 512:(pair + 1) * 512], op=ALU.mult),
                                 reads=[('ef', s, 0), ('ef', s, 1), 'ebf'], writes=[('pt', s)])
                            for hh in range(2):
                                for blk in range(2):
                                    bi = r * nbl + i - 1 + blk
                                    col = (hh * 2 + blk) * 128
                                    c.op('pe', lambda s=s, hh=hh, blk=blk, bi=bi, col=col, pair=pair: nc.tensor.matmul(PB[2 + s][:, hh * 65:hh * 65 + 65], lhsT=pt[s][:, col:col + 128],
                                         rhs=v1[:, bi, 2 * pair + hh, :], start=(blk == 0), stop=(blk == 1)), reads=[('pt', s), ('v1', bi), ('v1o', bi)], writes=[('pb', 2 + s)])
                            c.op('dve', lambda s=s: nc.vector.tensor_copy(out=oev[s][:], in_=PB[2 + s][:, 0:130]), reads=[('pb', 2 + s)], writes=[('oev', s)])
                            c.dma('sp', lambda s=s, g=g, q0=q0, d=d, pair=pair: nc.sync.dma_start(out=acc_t.ap()[g, q0:q0 + 127 * d + 1:d, pair * 130:(pair + 1) * 130], in_=oev[s][:]),
                                  reads=[('oev', s)], writes=[('acc', g, q0, pair)])
                import os as _os
                ntile = 1 if g == 0 else 4
                if _os.environ.get('MK_SKIP_SAMPLE'):
                    continue
                for b in range(NSQ):
                    for r in range(ntile):
                        s = (b * ntile + r) % 2
                        tix = 0 if g == 0 else 1 + 4 * (g - 1) + r
                        c.dma('sp', lambda s=s, b=b, r=r, g=g, d=d, wing=wing: nc.sync.dma_start(out=ctile[s][:], in_=ck_t[g].ap()[b, r:wing:d, :]), writes=[('ctile', s)])
                        TB = (0, 1)[s]
                        for pr in range(2):
                            c.op('pe', lambda s=s, pr=pr, TB=TB: nc.tensor.transpose(out=PB[TB][:, pr * 128:(pr + 1) * 128], in_=ctile[s][:, pr * 128:(pr + 1) * 128], identity=identf[:]),
                                 reads=[('ctile', s), 'identf'], writes=[('pb', TB)])
                        c.op('act', lambda s=s, TB=TB: nc.scalar.copy(out=kTs[s][:].rearrange("p a k -> p (a k)"), in_=PB[TB][:, 0:256]), reads=[('pb', TB)], writes=[('kTs', s)])
                        c.op('pool', lambda s=s: nc.gpsimd.tensor_copy(out=v1s[s][:, :, 0:64], in_=ctile[s][:, 256:512].rearrange("p (h e) -> p h e", h=4)), reads=[('ctile', s)], writes=[('v1s', s)])
                        for h in range(4):
                            po = (h % 2) * 64
                            bk = (2, 3)[s] if h % 2 == 0 else (4, 5)[s]
                            c.op('pe', lambda s=s, h=h, po=po, b=b, bk=bk: nc.tensor.matmul(PB[bk][:, 256 + 4 * h:260 + 4 * h], lhsT=kTs[s][po:po + 64, h // 2, :], rhs=qs[po:po + 64, h // 2, 4 * b:4 * b + 4], start=True, stop=True),
                                 reads=[('kTs', s), 'qs'], writes=[('pb', bk)])
                        ess = ess2[s]
                        essv = ess[:].rearrange("p (h q) -> p h q", h=4)
                        for par, bk in ((0, (2, 3)[s]), (1, (4, 5)[s])):
                            c.op('act', lambda par=par, bk=bk, essv=essv: nc.scalar.activation(out=essv[:, par:4:2, :], in_=PB[bk][:, 256:272].rearrange("p (h q) -> p h q", h=4)[:, par:4:2, :], func=AF.Exp, scale=0.125),
                                 reads=[('pb', bk)], writes=[('ess', s, par)])
                        c.op('dve', lambda b=b, tix=tix, ess=ess: nc.vector.tensor_tensor(out=pz[b][:, :, 4 * b:4 * b + 4], in0=ess[:].rearrange("p (h q) -> p h q", h=4), in1=smk[:, tix, :, :], op=ALU.mult),
                             reads=[('ess', s, 0), ('ess', s, 1), 'smk'], writes=[('pz', b)])
                        for h in range(4):
                            c.op('pe', lambda s=s, h=h, b=b, st=(n_os == 0): nc.tensor.matmul(PB[7][0:64, 65 * h:65 * h + 65], lhsT=pz[b][:, h, :], rhs=v1s[s][:, h, :], start=st, stop=False),
                                 reads=[('pz', b), ('v1s', s)], writes=['pb7'])
                        n_os += 1
                for h in range(4):
                    po = (h % 2) * 64
                    bk = 6 if h % 2 == 0 else 5
                    c.op('pe', lambda h=h, po=po, bk=bk: nc.tensor.matmul(PB[bk][0:64, 64 * h:64 * h + 64], lhsT=ks[po:po + 64, h // 2, :], rhs=qs[po:po + 64, h // 2, :], start=True, stop=True),
                         reads=['ks', 'qs'], writes=[('pb', bk)])
                for h in range(4):
                    bk = 6 if h % 2 == 0 else 5
                    c.op('act', lambda h=h, bk=bk: nc.scalar.activation(out=en[:, 64 * h:64 * h + 64], in_=PB[bk][0:64, 64 * h:64 * h + 64], func=AF.Exp, scale=0.125), reads=[('pb', bk)], writes=[('en', h)])
                c.op('dve', lambda g=g: nc.vector.tensor_tensor(out=pn[:], in0=en[:].rearrange("p (h q) -> p h q", h=4), in1=nmk[:, g, :, :], op=ALU.mult), reads=[('en', 0), ('en', 1), ('en', 2), ('en', 3), 'nmk'], writes=['pn'])
                for h in range(4):
                    c.op('pe', lambda h=h, g=g: nc.tensor.matmul(PB[7][0:64, 65 * h:65 * h + 65], lhsT=pn[:, h, :], rhs=v1n[:, g, h, :], start=False, stop=(g == 2 and h == 3)),
                         reads=['pn', 'v1n'], writes=['pb7'])
            c.op('act', lambda: nc.scalar.copy(out=osv[:], in_=PB[7][0:64, 0:260]), reads=['pb7'], writes=['osv'])
            c.dma('sp', lambda: nc.sync.dma_start(out=accs_t.ap(), in_=osv[:]), reads=['osv'], writes=['accs'])
        c.barrier()
        es_hT.close()
        if STOP_AFTER == 'D':
            c.finish()
            return nc
        wcor = wco_t.ap().rearrange("(c p) n -> p c n", p=128)
        waor = wao_t.ap().rearrange("(c p) n -> p c n", p=128)
        wor = wo_t.ap().rearrange("(c p) n -> p c n", p=128)
        wrtr = wrt_t.ap().rearrange("(c p) n -> p c n", p=128)
        h2d_t = dscr("h2scr", [NOT, D], BF16)
        esE = ExitStack()
        es.enter_context(esE)
        lgall = c.sb([128, NTILE, 36], F32, esE)
        esE1 = ExitStack()
        with esE1:
            wco = c.sb([128, 4, D], BF16, esE1); wao = c.sb([128, 2, D], BF16, esE1); wo = c.sb([128, 8, D], BF16, esE1)
            wrt = c.sb([128, 8, 36], BF16, esE1); brtb = c.sb([128, 36], F32, esE1)
            c.dma('pool', lambda: nc.gpsimd.dma_start(out=wco[:], in_=wcor), writes=['wco'])
            c.dma('pool', lambda: nc.gpsimd.dma_start(out=wao[:], in_=waor), writes=['wao'])
            c.dma('pool', lambda: nc.gpsimd.dma_start(out=wo[:], in_=wor), writes=['wo'])
            c.dma('pool', lambda: nc.gpsimd.dma_start(out=wrt[:], in_=wrtr), writes=['wrt'])
            c.dma('sp', lambda: nc.sync.dma_start(out=brtb[:], in_=brt_t.ap().partition_broadcast(128)), writes=['brtb'])
            mrow = {}
            for kind, nm in ((0, 'g1'), (1, 'b2'), (2, 'a2')):
                for part, (c0, n) in enumerate(((0, 128), (128, 64))):
                    t_ = c.sb([n, D], F32, esE1)
                    c.dma('sp', lambda t_=t_, kind=kind, c0=c0, n=n: nc.sync.dma_start(out=t_[:], in_=modrows_t.ap()[kind, c0:c0 + n, :]),
                          reads=[('modrows', kind, part)], writes=[('mrow', nm, part)])
                    mrow[(nm, part)] = t_
            c.op('pool', lambda: nc.gpsimd.memset(lgall[:], 0.0), writes=['lgall'])
            ytile = [c.sb([128, 4, 512], BF16, esE1) for _ in range(2)]
            oT = c.sb([128, 2, 512], BF16, esE1)
            acc3 = [c.sb([128, 3, 260], F32, esE1) for _ in range(2)]
            osum = c.sb([128, 260], F32, esE1); rden = c.sb([128, 4], F32, esE1); ob = c.sb([128, 256], BF16, esE1)
            sga = [c.sb([128, 512], BF16, esE1) for _ in range(2)]; sgb = [c.sb([128, 512], BF16, esE1) for _ in range(2)]
            m1 = [c.sb([128, 512], F32, esE1) for _ in range(2)]; m2 = [c.sb([128, 512], F32, esE1) for _ in range(2)]
            mixT = c.sb([128, 8, 512], BF16, esE1)
            xt2 = [c.sb([128, D], F32, esE1) for _ in range(2)]; tmp = [c.sb([128, D], F32, esE1) for _ in range(2)]
            x1t = [c.sb([128, D], F32, esE1) for _ in range(2)]; t2 = [c.sb([128, D], F32, esE1) for _ in range(2)]
            h2b = [c.sb([128, D], BF16, esE1) for _ in range(2)]
            h2T = c.sb([128, 8, 128], BF16, esE1); st2 = [c.sb([128, 4], F32, esE1) for _ in range(2)]
            junk2 = c.sb([128, D], BF16, esE1)
            sub_i = 0
            for M, (hc, oi, n) in enumerate(OT):
                part = 0 if M < 8 else 1
                rr = 128 if M < 8 else 64
                nsub = n // rr
                ys_ = M % 2
                c.dma('sp', lambda ys_=ys_, oi=oi, n=n: nc.sync.dma_start(out=ytile[ys_][:, :, 0:n], in_=yts_t.ap()[:, :, oi:oi + n].rearrange("c p t -> p c t")),
                      reads=[('yts', cc) for cc in range(4)], writes=[('ytile', ys_)])
                for t in range(nsub):
                    a_ = (M * 4 + t) % 2
                    r0 = oi + rr * t
                    if M < 8:
                        c.dma('sp', lambda a_=a_, r0=r0: nc.sync.dma_start(out=acc3[a_][:], in_=acc_t.ap()[:, r0:r0 + 128, :].rearrange("g t c -> t g c")),
                              reads=[k for k in c.state if isinstance(k, tuple) and k[0] == 'acc'], writes=[('acc3', a_)])
                        c.op('dve', lambda a_=a_: nc.vector.tensor_tensor(out=osum[:], in0=acc3[a_][:, 0, :], in1=acc3[a_][:, 1, :], op=ALU.add), reads=[('acc3', a_)], writes=['osum'])
                        c.op('dve', lambda a_=a_: nc.vector.tensor_tensor(out=osum[:], in0=osum[:], in1=acc3[a_][:, 2, :], op=ALU.add), reads=[('acc3', a_), 'osum'], writes=['osum'])
                    else:
                        c.dma('sp', lambda a_=a_: nc.sync.dma_start(out=acc3[a_][0:64, 0, :], in_=accs_t.ap()), reads=['accs'], writes=[('acc3', a_)])
                        c.op('dve', lambda a_=a_: nc.vector.tensor_copy(out=osum[0:64, :], in_=acc3[a_][0:64, 0, :]), reads=[('acc3', a_)], writes=['osum'])
                    ov = osum[0:rr, :].rearrange("p (h e) -> p h e", h=4)
                    c.op('dve', lambda ov=ov, rr=rr: nc.vector.reciprocal(out=rden[0:rr, :].unsqueeze(2), in_=ov[:, :, 64:65]), reads=['osum'], writes=['rden'])
                    c.op('dve', lambda ov=ov, rr=rr: nc.vector.tensor_tensor(out=ob[0:rr, :].rearrange("p (h e) -> p h e", h=4), in0=ov[:, :, 0:64],
                         in1=rden[0:rr, :].unsqueeze(2).to_broadcast([rr, 4, 64]), op=ALU.mult), reads=['osum', 'rden'], writes=['ob'])
                    pv4 = pbf(4).rearrange("p (k t) -> p k t", k=8)
                    for k in range(2):
                        c.op('pe', lambda k=k, rr=rr, pv4=pv4: nc.tensor.transpose(out=pv4[:, k, 0:rr], in_=ob[0:rr, k * 128:(k + 1) * 128], identity=ident[0:rr, 0:rr]),
                             reads=['ob', 'ident'], writes=[('pb', 4)])
                    c.op('act', lambda t=t, rr=rr, pv4=pv4: nc.scalar.copy(out=oT[:, :, rr * t:rr * t + rr], in_=pv4[:, 0:2, 0:rr]), reads=[('pb', 4)], writes=['oT'])
                for j in range(8):
                    gs = j % 2
                    c.dma('sp', lambda gs=gs, j=j, oi=oi, n=n: nc.sync.dma_start(out=sga[gs][:, 0:n], in_=sg_t.ap()[j, :, oi:oi + n]), reads=[('sg', j)], writes=[('sga', gs)])
                    c.dma('sp', lambda gs=gs, j=j, oi=oi, n=n: nc.sync.dma_start(out=sgb[gs][:, 0:n], in_=sg_t.ap()[8 + j, :, oi:oi + n]), reads=[('sg', 8 + j)], writes=[('sgb', gs)])
                    pa_i, pb_i = (0, 1) if gs == 0 else (6, 7)
                    for cc in range(4):
                        c.op('pe', lambda cc=cc, j=j, pa_i=pa_i, ys_=ys_, n=n: nc.tensor.matmul(PB[pa_i][:, 0:n], lhsT=wco[:, cc, j * 128:(j + 1) * 128], rhs=ytile[ys_][:, cc, 0:n], start=(cc == 0), stop=(cc == 3)),
                             reads=['wco', ('ytile', ys_)], writes=[('pb', pa_i)])
                    for cc in range(2):
                        c.op('pe', lambda cc=cc, j=j, pb_i=pb_i, n=n: nc.tensor.matmul(PB[pb_i][:, 0:n], lhsT=wao[:, cc, j * 128:(j + 1) * 128], rhs=oT[:, cc, 0:n], start=(cc == 0), stop=(cc == 1)),
                             reads=['wao', 'oT'], writes=[('pb', pb_i)])
                    c.op('dve', lambda gs=gs, pa_i=pa_i, n=n: nc.vector.tensor_tensor(out=m1[gs][:, 0:n], in0=PB[pa_i][:, 0:n], in1=sga[gs][:, 0:n], op=ALU.mult), reads=[('pb', pa_i), ('sga', gs)], writes=[('m1', gs)])
                    c.op('dve', lambda gs=gs, pb_i=pb_i, n=n: nc.vector.tensor_tensor(out=m2[gs][:, 0:n], in0=PB[pb_i][:, 0:n], in1=sgb[gs][:, 0:n], op=ALU.mult), reads=[('pb', pb_i), ('sgb', gs)], writes=[('m2', gs)])
                    c.op('pool', lambda gs=gs, j=j, n=n: nc.gpsimd.tensor_tensor(out=mixT[:, j, 0:n], in0=m1[gs][:, 0:n], in1=m2[gs][:, 0:n], op=ALU.add), reads=[('m1', gs), ('m2', gs)], writes=[('mixT', j)])
                for t in range(nsub):
                    xs_ = sub_i % 2
                    sub_i += 1
                    r0 = oi + rr * t
                    tile_i = r0 // 128 if M < 8 else 32
                    src = xp[HALO + r0:HALO + r0 + 128, :] if M < 8 else xs
                    c.dma('sp', lambda xs_=xs_, src=src, rr=rr: nc.sync.dma_start(out=xt2[xs_][0:rr, :], in_=src), writes=[('xt2', xs_)])
                    for hf in range(2):
                        for j in range(8):
                            c.op('pe', lambda hf=hf, j=j, t=t, rr=rr: nc.tensor.matmul(PB[2 + hf][0:rr, :], lhsT=mixT[:, j, rr * t:rr * t + rr], rhs=wo[:, j, hf * 512:(hf + 1) * 512], start=(j == 0), stop=(j == 7)),
                                 reads=[('mixT', j), 'wo'], writes=[('pb', 2 + hf)])
                        c.op('dve', lambda hf=hf, xs_=xs_, rr=rr, part=part: nc.vector.tensor_tensor(out=tmp[xs_][0:rr, hf * 512:(hf + 1) * 512], in0=PB[2 + hf][0:rr, :],
                             in1=mrow[('g1', part)][0:rr, hf * 512:(hf + 1) * 512], op=ALU.mult), reads=[('pb', 2 + hf), ('mrow', 'g1', part)], writes=[('tmp', xs_)])
                    c.op('pool', lambda xs_=xs_, rr=rr: nc.gpsimd.tensor_tensor(out=x1t[xs_][0:rr, :], in0=tmp[xs_][0:rr, :], in1=xt2[xs_][0:rr, :], op=ALU.add),
                         reads=[('tmp', xs_), ('xt2', xs_)], writes=[('x1t', xs_)])
                    c.dma('act', lambda xs_=xs_, r0=r0, rr=rr: nc.scalar.dma_start(out=x1_t.ap()[r0:r0 + rr, :], in_=x1t[xs_][0:rr, :]), reads=[('x1t', xs_)], writes=[('x1', tile_i)])
                    c.op('act', lambda xs_=xs_, rr=rr: nc.scalar.activation(out=junk2[0:rr, :], in_=x1t[xs_][0:rr, :], func=AF.Square, accum_out=st2[xs_][0:rr, 0:1]),
                         reads=[('x1t', xs_)], writes=[('st2', xs_), 'junk2'])
                    c.op('act', lambda xs_=xs_, rr=rr: nc.scalar.activation(out=st2[xs_][0:rr, 1:2], in_=st2[xs_][0:rr, 0:1], func=AF.Sqrt, scale=1.0 / D, bias=epsb[0:rr, :]),
                         reads=[('st2', xs_), 'epsb'], writes=[('st2', xs_)])
                    c.op('dve', lambda xs_=xs_, rr=rr: nc.vector.reciprocal(out=st2[xs_][0:rr, 2:3], in_=st2[xs_][0:rr, 1:2]), reads=[('st2', xs_)], writes=[('st2', xs_)])
                    c.op('dve', lambda xs_=xs_, rr=rr, part=part: nc.vector.scalar_tensor_tensor(out=t2[xs_][0:rr, :], in0=x1t[xs_][0:rr, :], scalar=st2[xs_][0:rr, 2:3],
                         in1=mrow[('a2', part)][0:rr, :], op0=ALU.mult, op1=ALU.mult), reads=[('x1t', xs_), ('st2', xs_), ('mrow', 'a2', part)], writes=[('t2', xs_)])
                    c.op('pool', lambda xs_=xs_, rr=rr, part=part: nc.gpsimd.tensor_tensor(out=h2b[xs_][0:rr, :], in0=t2[xs_][0:rr, :], in1=mrow[('b2', part)][0:rr, :], op=ALU.add),
                         reads=[('t2', xs_), ('mrow', 'b2', part)], writes=[('h2b', xs_)])
                    c.dma('act', lambda xs_=xs_, r0=r0, rr=rr: nc.scalar.dma_start(out=h2d_t.ap()[r0:r0 + rr, :], in_=h2b[xs_][0:rr, :]), reads=[('h2b', xs_)], writes=[('h2d', tile_i)])
                    pv5 = pbf(5).rearrange("p (k t) -> p k t", k=8)
                    for k in range(8):
                        c.op('pe', lambda k=k, xs_=xs_, rr=rr, pv5=pv5: nc.tensor.transpose(out=pv5[:, k, 0:rr], in_=h2b[xs_][0:rr, k * 128:(k + 1) * 128], identity=ident[0:rr, 0:rr]),
                             reads=[('h2b', xs_), 'ident'], writes=[('pb', 5)])
                    c.op('act', lambda rr=rr, pv5=pv5: nc.scalar.copy(out=h2T[:, :, 0:rr], in_=pv5[:, :, 0:rr]), reads=[('pb', 5)], writes=['h2T'])
                    for k in range(8):
                        c.op('pe', lambda k=k, rr=rr: nc.tensor.matmul(PB[4][0:rr, 0:36], lhsT=h2T[:, k, 0:rr], rhs=wrt[:, k, :], start=(k == 0), stop=(k == 7)),
                             reads=['h2T', 'wrt'], writes=[('pb', 4)])
                    c.op('dve', lambda rr=rr, tile_i=tile_i: nc.vector.tensor_tensor(out=lgall[0:rr, tile_i, :], in0=PB[4][0:rr, 0:36], in1=brtb[0:rr, :], op=ALU.add),
                         reads=[('pb', 4), 'brtb', 'lgall'], writes=['lgall'])
        c.barrier()
        if STOP_AFTER == 'E':
            c.finish()
            return nc
        NT = NTILE
        esF = ExitStack()
        with esF:
            def ft(shape, dt=F32):
                return c.sb(shape, dt, esF)
            gmx = ft([128, NT]); ohg = ft([128, NT, 4]); gsh = ft([128, NT, 4]); gex = ft([128, NT, 4]); gsum = ft([128, NT]); pgr = ft([128, NT])
            pen = ft([128, NT, 4]); em = ft([128, NT, 32]); m8 = ft([128, NT, 8]); i8 = ft([128, NT, 8], U32)
            e0f = ft([128, NT]); e1f = ft([128, NT]); dv = ft([128, NT]); w0 = ft([128, NT]); w1 = ft([128, NT])
            oh0 = ft([128, NT, 32]); oh1 = ft([128, NT, 32]); mm_ = ft([128, NT, 32]); cs = ft([128, NT + 1, 32])
            base = ft([128, NT, 32]); prod = ft([128, NT, 32]); d0f = ft([128, NT]); d1f = ft([128, NT])
            d0i = ft([128, NT], I32); d1i = ft([128, NT], I32)
            io32i = ft([128, 32], I32); io32 = ft([128, 32]); thri = ft([128, NBLK], I32); thr = ft([128, NBLK])
            cnt = ft([128, 32]); cni = ft([128, 32], I32); pad = ft([128, 32]); pa_ = ft([128, 32]); pb_ = ft([128, 32]); pst = ft([128, 32])
            cmpb = ft([128, NBLK, 32]); bef = ft([128, NBLK])
            c.op('pool', lambda: nc.gpsimd.iota(io32i[:], pattern=[[1, 32]], base=0, channel_multiplier=0), writes=['io32i'])
            c.op('pool', lambda: nc.gpsimd.iota(thri[:], pattern=[[BLK, NBLK]], base=0, channel_multiplier=0), writes=['thri'])
            c.op('dve', lambda: nc.vector.tensor_copy(out=io32[:], in_=io32i[:]), reads=['io32i'], writes=['io32'])
            c.op('dve', lambda: nc.vector.tensor_copy(out=thr[:], in_=thri[:]), reads=['thri'], writes=['thr'])
            R = ['lgall']
            gl = lgall[:, :, 0:4]
            V = nc.vector
            c.op('dve', lambda: V.tensor_reduce(out=gmx[:], in_=gl, axis=AX.X, op=ALU.max), reads=R, writes=['gmx'])
            c.op('dve', lambda: V.tensor_tensor(out=ohg[:], in0=gl, in1=gmx[:].unsqueeze(2).to_broadcast([128, NT, 4]), op=ALU.is_equal), reads=R + ['gmx'], writes=['ohg'])
            c.op('dve', lambda: V.tensor_tensor(out=gsh[:], in0=gl, in1=gmx[:].unsqueeze(2).to_broadcast([128, NT, 4]), op=ALU.subtract), reads=R + ['gmx'], writes=['gsh'])
            c.op('act', lambda: nc.scalar.activation(out=gex[:], in_=gsh[:], func=AF.Exp), reads=['gsh'], writes=['gex'])
            c.op('dve', lambda: V.tensor_reduce(out=gsum[:], in_=gex[:], axis=AX.X, op=ALU.add), reads=['gex'], writes=['gsum'])
            c.op('dve', lambda: V.reciprocal(out=pgr[:], in_=gsum[:]), reads=['gsum'], writes=['pgr'])
            c.op('dve', lambda: V.tensor_scalar(out=pen[:], in0=ohg[:], scalar1=-1.0, scalar2=1e30, op0=ALU.add, op1=ALU.mult), reads=['ohg'], writes=['pen'])
            c.op('dve', lambda: V.tensor_tensor(out=em[:].rearrange("p t (g e) -> p t g e", g=4), in0=lgall[:, :, 4:36].rearrange("p t (g e) -> p t g e", g=4),
                 in1=pen[:].unsqueeze(3).to_broadcast([128, NT, 4, 8]), op=ALU.add), reads=R + ['pen'], writes=['em'])
            for i in range(NT):
                c.op('dve', lambda i=i: V.max(out=m8[:, i, :], in_=em[:, i, :]), reads=['em'], writes=['m8'])
                c.op('dve', lambda i=i: V.max_index(out=i8[:, i, :], in_max=m8[:, i, :], in_values=em[:, i, :]), reads=['em', 'm8'], writes=['i8'])
            c.op('dve', lambda: V.tensor_copy(out=e0f[:], in_=i8[:, :, 0]), reads=['i8'], writes=['e0f'])
            c.op('dve', lambda: V.tensor_copy(out=e1f[:], in_=i8[:, :, 1]), reads=['i8'], writes=['e1f'])
            c.op('dve', lambda: V.tensor_tensor(out=dv[:], in0=m8[:, :, 1], in1=m8[:, :, 0], op=ALU.subtract), reads=['m8'], writes=['dv'])
            c.op('act', lambda: nc.scalar.activation(out=dv[:], in_=dv[:], func=AF.Exp), reads=['dv'], writes=['dv'])
            c.op('dve', lambda: V.tensor_scalar(out=dv[:], in0=dv[:], scalar1=1.0, scalar2=None, op0=ALU.add), reads=['dv'], writes=['dv'])
            c.op('dve', lambda: V.reciprocal(out=w0[:], in_=dv[:]), reads=['dv'], writes=['w0'])
            c.op('dve', lambda: V.tensor_tensor(out=w0[:], in0=w0[:], in1=pgr[:], op=ALU.mult), reads=['w0', 'pgr'], writes=['w0'])
            c.op('dve', lambda: V.tensor_tensor(out=w1[:], in0=pgr[:], in1=w0[:], op=ALU.subtract), reads=['w0', 'pgr'], writes=['w1'])
            iob = io32[:].unsqueeze(1).to_broadcast([128, NT, 32])
            c.op('dve', lambda: V.tensor_tensor(out=oh0[:], in0=iob, in1=e0f[:].unsqueeze(2).to_broadcast([128, NT, 32]), op=ALU.is_equal), reads=['io32', 'e0f'], writes=['oh0'])
            c.op('dve', lambda: V.tensor_tensor(out=oh1[:], in0=iob, in1=e1f[:].unsqueeze(2).to_broadcast([128, NT, 32]), op=ALU.is_equal), reads=['io32', 'e1f'], writes=['oh1'])
            c.op('dve', lambda: V.memset(oh0[64:128, NT - 1, :], 0.0), reads=['oh0'], writes=['oh0'])
            c.op('dve', lambda: V.memset(oh1[64:128, NT - 1, :], 0.0), reads=['oh1'], writes=['oh1'])
            c.op('dve', lambda: V.tensor_tensor(out=mm_[:], in0=oh0[:], in1=oh1[:], op=ALU.add), reads=['oh0', 'oh1'], writes=['mm'])
            c.op('dve', lambda: V.memset(cs[:, 0, :], 0.0), writes=['cs'])
            for i in range(NT):
                c.op('dve', lambda i=i: V.tensor_tensor(out=cs[:, i + 1, :], in0=cs[:, i, :], in1=mm_[:, i, :], op=ALU.add), reads=['cs', 'mm'], writes=['cs'])
            for i in range(NT):
                bk = i // 16
                co = (i % 16) * 32
                c.op('pe', lambda i=i, bk=bk, co=co: nc.tensor.matmul(PB[bk][:, co:co + 32], lhsT=suf[:], rhs=mm_[:, i, :], start=True, stop=False), reads=['suf', 'mm'], writes=[('pb', bk)])
                c.op('pe', lambda i=i, bk=bk, co=co: nc.tensor.matmul(PB[bk][:, co:co + 32], lhsT=onesf[:], rhs=cs[:, i, :], start=False, stop=True), reads=['onesf', 'cs'], writes=[('pb', bk)])
            c.op('pe', lambda: nc.tensor.matmul(PB[3][:, 0:32], lhsT=onesf[:], rhs=cs[:, NT, :], start=True, stop=True), reads=['onesf', 'cs'], writes=[('pb', 3)])
            c.op('dve', lambda: V.tensor_scalar(out=cni[:], in0=PB[3][:, 0:32], scalar1=float(BLK - 1), scalar2=None, op0=ALU.add), reads=[('pb', 3)], writes=['cni'])
            c.op('dve', lambda: V.tensor_scalar(out=cni[:], in0=cni[:], scalar1=int(math.log2(BLK)), scalar2=int(math.log2(BLK)), op0=ALU.arith_shift_right, op1=ALU.logical_shift_left), reads=['cni'], writes=['cni'])
            c.op('dve', lambda: V.tensor_copy(out=pad[:], in_=cni[:]), reads=['cni'], writes=['pad'])
            src_, dst_ = pad, pa_
            for sft in (1, 2, 4, 8, 16):
                c.op('dve', lambda src_=src_, dst_=dst_, sft=sft: V.tensor_copy(out=dst_[:, 0:sft], in_=src_[:, 0:sft]), reads=['pfx', 'pad'], writes=['pfx'])
                c.op('dve', lambda src_=src_, dst_=dst_, sft=sft: V.tensor_tensor(out=dst_[:, sft:32], in0=src_[:, sft:32], in1=src_[:, 0:32 - sft], op=ALU.add), reads=['pfx', 'pad'], writes=['pfx'])
                src_, dst_ = dst_, (pb_ if dst_ is pa_ else pa_)
            pend = src_
            c.op('dve', lambda: V.tensor_tensor(out=pst[:], in0=pend[:], in1=pad[:], op=ALU.subtract), reads=['pfx', 'pad'], writes=['pst'])
            for bk in range(3):
                t0 = bk * 16
                nt_ = min(16, NT - t0)
                c.op('dve', lambda bk=bk, t0=t0, nt_=nt_: V.tensor_tensor(out=base[:, t0:t0 + nt_, :], in0=PB[bk][:, 0:nt_ * 32].rearrange("p (t e) -> p t e", e=32),
                     in1=pst[:].unsqueeze(1).to_broadcast([128, nt_, 32]), op=ALU.add), reads=[('pb', bk), 'pst'], writes=['base'])
            for oh_, df_, di_, nm in ((oh0, d0f, d0i, 'd0'), (oh1, d1f, d1i, 'd1')):
                c.op('dve', lambda oh_=oh_: V.tensor_tensor(out=prod[:], in0=oh_[:], in1=base[:], op=ALU.mult), reads=['oh0', 'oh1', 'base'], writes=['prod'])
                c.op('dve', lambda df_=df_: V.tensor_reduce(out=df_[:], in_=prod[:], axis=AX.X, op=ALU.add), reads=['prod'], writes=[nm + 'f'])
                c.op('dve', lambda df_=df_, di_=di_: V.tensor_copy(out=di_[:], in_=df_[:]), reads=[nm + 'f'], writes=[nm])
            c.op('dve', lambda: V.tensor_tensor(out=cmpb[:], in0=pend[:].unsqueeze(1).to_broadcast([128, NBLK, 32]), in1=thr[:].unsqueeze(2).to_broadcast([128, NBLK, 32]), op=ALU.is_le),
                 reads=['pfx', 'thr'], writes=['cmpb'])
            c.op('dve', lambda: V.tensor_reduce(out=bef[:], in_=cmpb[:], axis=AX.X, op=ALU.add), reads=['cmpb'], writes=['bef'])
            c.op('dve', lambda: V.tensor_scalar(out=bef[:], in0=bef[:], scalar1=31.0, scalar2=None, op0=ALU.min), reads=['bef'], writes=['bef'])
            pio = ft([128, 1], I32); piof = ft([128, 1]); widxf = ft([128, NBLK]); widx = ft([128, NBLK], I32)
            c.op('pool', lambda: nc.gpsimd.iota(pio[:], pattern=[[0, 1]], base=0, channel_multiplier=1), writes=['pio'])
            c.op('dve', lambda: V.tensor_copy(out=piof[:], in_=pio[:]), reads=['pio'], writes=['piof'])
            c.op('dve', lambda: V.tensor_scalar(out=widxf[:], in0=bef[:], scalar1=128.0, scalar2=piof[:, 0:1], op0=ALU.mult, op1=ALU.add), reads=['bef', 'piof'], writes=['widxf'])
            c.op('dve', lambda: V.tensor_copy(out=widx[:], in_=widxf[:]), reads=['widxf'], writes=['widx'])
            zt = ft([128, D], BF16)
            c.op('pool', lambda: nc.gpsimd.memset(zt[:], 0.0), writes=['zt'])
            xsv = xsd_t.ap().rearrange("(b p) d -> p b d", p=128)
            zkeys = []
            NRB = CAP // 128
            for q4 in range(8):
                b0 = q4 * (NRB // 8)
                nb_ = NRB // 8
                c.dma('act', lambda b0=b0, nb_=nb_: nc.scalar.dma_start(out=xsv[:, b0:b0 + nb_, :], in_=zt[:].unsqueeze(1).to_broadcast([128, nb_, D])), reads=['zt'], writes=[('xsz', q4)])
                zkeys.append(('xsz', q4))
            h2t = [ft([128, D], BF16) for _ in range(2)]
            skeys = []
            for i in range(NT):
                s = i % 2
                rr = 128 if i < NT - 1 else 64
                c.dma('sp', lambda s=s, i=i, rr=rr: nc.sync.dma_start(out=h2t[s][0:rr, :], in_=h2d_t.ap()[i * 128:i * 128 + rr, :]), reads=[('h2d', i)], writes=[('h2t', s)])
                for di_, nm in ((d0i, 'd0'), (d1i, 'd1')):
                    c.dma('pool', lambda s=s, i=i, rr=rr, di_=di_: nc.gpsimd.indirect_dma_start(out=xsd_t.ap(), out_offset=bass.IndirectOffsetOnAxis(ap=di_[0:rr, i:i + 1], axis=0),
                          in_=h2t[s][0:rr, :], in_offset=None), reads=[('h2t', s), nm] + zkeys, writes=[('xss', i, nm)])
                    skeys.append(('xss', i, nm))
            xsb = [ft([128, D], BF16) for _ in range(2)]; xsT = [ft([128, 8, 128], BF16) for _ in range(2)]
            wg = [ft([128, 8, 512], BF16) for _ in range(2)]; wu = [ft([128, 8, 512], BF16) for _ in range(2)]; wd = [ft([128, 4, D], BF16) for _ in range(2)]
            actt = [ft([128, 512]) for _ in range(2)]; ab = [ft([128, 512], BF16) for _ in range(2)]; aT = [ft([128, 4, 128], BF16) for _ in range(2)]
            yev = [ft([128, D]) for _ in range(2)]
            wegv = weg_t.ap().rearrange("e (p k) f -> (e p) (k f)", p=128)
            weuv = weu_t.ap().rearrange("e (p k) f -> (e p) (k f)", p=128)
            wedv = wed_t.ap().rearrange("e (p k) f -> (e p) (k f)", p=128)
            items = [(b, sub) for b in range(NBLK) for sub in range(SUBB)]

            def load_xsb(n):
                b, sub = items[n]
                xs_ = n % 2
                r0 = b * BLK + sub * 128
                c.dma('sp', lambda xs_=xs_, r0=r0: nc.sync.dma_start(out=xsb[xs_][:], in_=xsd_t.ap()[r0:r0 + 128, :]), reads=skeys + zkeys, writes=[('xsb', xs_)])

            load_xsb(0)
            for n, (b, sub) in enumerate(items):
                s = b % 2
                xs_ = n % 2
                if sub == 0:
                    for wt_, wv_, nm in ((wg, wegv, 'wg'), (wu, weuv, 'wu'), (wd, wedv, 'wd')):
                        c.dma('pool', lambda s=s, b=b, wt_=wt_, wv_=wv_: nc.gpsimd.indirect_dma_start(out=wt_[s][:].rearrange("p k f -> p (k f)"), out_offset=None, in_=wv_,
                              in_offset=bass.IndirectOffsetOnAxis(ap=widx[:, b:b + 1], axis=0)), reads=['widx'], writes=[(nm, s)])
                if n + 1 < len(items):
                    load_xsb(n + 1)
                pv0 = pbf(0).rearrange("p (k t) -> p k t", k=8)
                for k in range(8):
                    c.op('pe', lambda k=k, xs_=xs_, pv0=pv0: nc.tensor.transpose(out=pv0[:, k, :], in_=xsb[xs_][:, k:k + 8 * 127 + 1:8], identity=ident[:]), reads=[('xsb', xs_), 'ident'], writes=[('pb', 0)])
                c.op('act', lambda xs_=xs_, pv0=pv0: nc.scalar.copy(out=xsT[xs_][:], in_=pv0), reads=[('pb', 0)], writes=[('xsT', xs_)])
                gi, ui = (1, 2) if xs_ == 0 else (6, 7)
                for k in range(8):
                    c.op('pe', lambda k=k, s=s, xs_=xs_, gi=gi: nc.tensor.matmul(PB[gi][:, :], lhsT=xsT[xs_][:, k, :], rhs=wg[s][:, k, :], start=(k == 0), stop=(k == 7)), reads=[('xsT', xs_), ('wg', s)], writes=[('pb', gi)])
                for k in range(8):
                    c.op('pe', lambda k=k, s=s, xs_=xs_, ui=ui: nc.tensor.matmul(PB[ui][:, :], lhsT=xsT[xs_][:, k, :], rhs=wu[s][:, k, :], start=(k == 0), stop=(k == 7)), reads=[('xsT', xs_), ('wu', s)], writes=[('pb', ui)])
                c.op('act', lambda xs_=xs_, gi=gi: nc.scalar.activation(out=actt[xs_][:], in_=PB[gi][:, :], func=AF.Silu), reads=[('pb', gi)], writes=[('actt', xs_)])
                c.op('dve', lambda xs_=xs_, ui=ui: V.tensor_tensor(out=ab[xs_][:], in0=PB[ui][:, :], in1=actt[xs_][:], op=ALU.mult), reads=[('pb', ui), ('actt', xs_)], writes=[('ab', xs_)])
                pv3 = pbf(3).rearrange("p (k t) -> p k t", k=8)
                for k in range(4):
                    c.op('pe', lambda k=k, xs_=xs_, pv3=pv3: nc.tensor.transpose(out=pv3[:, k, :], in_=ab[xs_][:, k:k + 4 * 127 + 1:4], identity=ident[:]), reads=[('ab', xs_), 'ident'], writes=[('pb', 3)])
                c.op('dve', lambda xs_=xs_, pv3=pv3: V.tensor_copy(out=aT[xs_][:], in_=pv3[:, 0:4, :]), reads=[('pb', 3)], writes=[('aT', xs_)])
                for hf in range(2):
                    for k in range(4):
                        c.op('pe', lambda k=k, s=s, xs_=xs_, hf=hf: nc.tensor.matmul(PB[4 + hf][:, :], lhsT=aT[xs_][:, k, :], rhs=wd[s][:, k, hf * 512:(hf + 1) * 512], start=(k == 0), stop=(k == 3)),
                             reads=[('aT', xs_), ('wd', s)], writes=[('pb', 4 + hf)])
                c.op('act', lambda xs_=xs_: nc.scalar.copy(out=yev[xs_][:, 0:512], in_=PB[4][:, :]), reads=[('pb', 4)], writes=[('yev', xs_, 0)])
                c.op('dve', lambda xs_=xs_: V.tensor_copy(out=yev[xs_][:, 512:1024], in_=PB[5][:, :]), reads=[('pb', 5)], writes=[('yev', xs_, 1)])
                r0 = b * BLK + sub * 128
                c.dma('act', lambda xs_=xs_, r0=r0: nc.scalar.dma_start(out=ysd_t.ap()[r0:r0 + 128, :], in_=yev[xs_][:]), reads=[('yev', xs_, 0), ('yev', xs_, 1)], writes=[('ysd', n)])
            ykeys = [('ysd', n) for n in range(len(items))]
            g2r = {}
            for part, (c0, n) in enumerate(((0, 128), (128, 64))):
                t_ = ft([n, D])
                c.dma('sp', lambda t_=t_, c0=c0, n=n: nc.sync.dma_start(out=t_[:], in_=modrows_t.ap()[3, c0:c0 + n, :]), reads=[('modrows', 3, part)], writes=[('g2r', part)])
                g2r[part] = t_
            gfin = ft([128, D])
            c.dma('sp', lambda: nc.sync.dma_start(out=gfin[:], in_=gfin_t.ap().partition_broadcast(128)), writes=['gfin'])
            NBF = 3
            y0 = [ft([128, D]) for _ in range(NBF)]; y1 = [ft([128, D]) for _ in range(NBF)]; x1r = [ft([128, D]) for _ in range(NBF)]
            fa = [ft([128, D]) for _ in range(NBF)]; st3 = [ft([128, 4]) for _ in range(NBF)]
            junk3 = ft([128, D], BF16)

            def comb_stage1(i):
                s = i % NBF
                rr = 128 if i < NT - 1 else 64
                part = 0 if i < NT - 1 else 1
                c.dma('pool', lambda: nc.gpsimd.indirect_dma_start(out=y0[s][0:rr, :], out_offset=None, in_=ysd_t.ap(),
                      in_offset=bass.IndirectOffsetOnAxis(ap=d0i[0:rr, i:i + 1], axis=0)), reads=ykeys + ['d0'], writes=[('y0', s)])
                c.dma('pool', lambda: nc.gpsimd.indirect_dma_start(out=y1[s][0:rr, :], out_offset=None, in_=ysd_t.ap(),
                      in_offset=bass.IndirectOffsetOnAxis(ap=d1i[0:rr, i:i + 1], axis=0)), reads=ykeys + ['d1'], writes=[('y1', s)])
                c.dma('sp', lambda: nc.sync.dma_start(out=x1r[s][0:rr, :], in_=x1_t.ap()[i * 128:i * 128 + rr, :]), reads=[('x1', i)], writes=[('x1r', s)])
                c.op('act', lambda: nc.scalar.activation(out=fa[s][0:rr, :], in_=y0[s][0:rr, :], func=AF.Copy, scale=w0[0:rr, i:i + 1]), reads=[('y0', s), 'w0'], writes=[('fa', s)])
                c.op('dve', lambda: V.scalar_tensor_tensor(out=fa[s][0:rr, :], in0=y1[s][0:rr, :], scalar=w1[0:rr, i:i + 1], in1=fa[s][0:rr, :], op0=ALU.mult, op1=ALU.add),
                     reads=[('y1', s), 'w1', ('fa', s)], writes=[('fa', s)])
                c.op('dve', lambda: V.tensor_tensor(out=fa[s][0:rr, :], in0=fa[s][0:rr, :], in1=g2r[part][0:rr, :], op=ALU.mult), reads=[('fa', s), ('g2r', part)], writes=[('fa', s)])
                c.op('dve', lambda: V.tensor_tensor(out=x1r[s][0:rr, :], in0=fa[s][0:rr, :], in1=x1r[s][0:rr, :], op=ALU.add), reads=[('fa', s), ('x1r', s)], writes=[('x1r', s)])

            def comb_stage2(i):
                s = i % NBF
                rr = 128 if i < NT - 1 else 64
                c.op('act', lambda: nc.scalar.activation(out=junk3[0:rr, :], in_=x1r[s][0:rr, :], func=AF.Square, accum_out=st3[s][0:rr, 0:1]), reads=[('x1r', s)], writes=[('st3', s), 'junk3'])
                c.op('act', lambda: nc.scalar.activation(out=st3[s][0:rr, 1:2], in_=st3[s][0:rr, 0:1], func=AF.Sqrt, scale=1.0 / D, bias=epsb[0:rr, :]), reads=[('st3', s), 'epsb'], writes=[('st3', s)])
                c.op('dve', lambda: V.reciprocal(out=st3[s][0:rr, 2:3], in_=st3[s][0:rr, 1:2]), reads=[('st3', s)], writes=[('st3', s)])
                c.op('dve', lambda: V.scalar_tensor_tensor(out=y0[s][0:rr, :], in0=x1r[s][0:rr, :], scalar=st3[s][0:rr, 2:3], in1=gfin[0:rr, :], op0=ALU.mult, op1=ALU.mult),
                     reads=[('x1r', s), ('st3', s), 'gfin'], writes=[('y0', s)])
                dst = yp_t.ap()[i * 128:(i + 1) * 128, :] if i < NT - 1 else ys_t.ap()
                c.dma('act', lambda: nc.scalar.dma_start(out=dst, in_=y0[s][0:rr, :]), reads=[('y0', s)], writes=[('yout', i)])

            for i in range(NT + 1):
                if i < NT:
                    comb_stage1(i)
                if i >= 1:
                    comb_stage2(i - 1)
        c.finish()
    return nc


def build_two_pass():
    nc1 = build_nc(None)
    needed = set(nc1._mk_ctx.record)
    return build_nc(needed)


def _prep_inputs(inp):
    f = lambda a: np.ascontiguousarray(a, dtype=np.float32)
    ohw, vw, sel, bd = _structure_constants()
    shared = {
        "rel_bias": f(inp["rel_bias"]), "norm_mix_g": f(inp["norm_mix_g"][0][None]), "norm_ffn_g": f(inp["norm_ffn_g"][0][None]),
        "norm_final_g": f(inp["norm_final_g"][None]), "w_mod": f(inp["w_mod"][0]), "b_mod": f(inp["b_mod"][0][None]),
        "w_in": f(inp["w_in"][0]), "dw_w": f(inp["dw_w"][0]), "dw_b": f(inp["dw_b"][0][None]), "ln_g": f(inp["ln_conv_g"][0][None]),
        "ln_b": f(inp["ln_conv_b"][0][None]), "w_conv_out": f(inp["w_conv_out"][0]), "w_attn_out": f(inp["w_attn_out"][0]),
        "w_out": f(inp["w_out"][0]),
        "w_rt": f(np.concatenate([inp["w_router_group"][0], inp["w_router_expert"][0].reshape(D, 32)], axis=1)),
        "b_rt": f(np.concatenate([inp["b_router_group"][0], inp["b_router_expert"][0].reshape(32)])[None]),
        "w_eg": f(inp["w_exp_gate"][0]), "w_eu": f(inp["w_exp_up"][0]), "w_ed": f(inp["w_exp_down"][0]),
        "ohw": ohw, "vw": vw, "sel": sel, "bd": bd,
    }
    maps = []
    for cid in range(NCORE):
        b, half = cid // 2, cid % 2
        xp = np.zeros((NEXT, D), np.float32)
        xp[HALO:] = inp["x_prompt"][b, half * NOWN:(half + 1) * NOWN]
        if half == 1:
            xp[:HALO] = inp["x_prompt"][b, NOWN - HALO:NOWN]
        sl = slice(cid * NSQ, (cid + 1) * NSQ)
        m = dict(shared)
        m["xp"] = xp
        m["xs"] = f(inp["x_sample"][sl].reshape(NS, D))
        m["cmod"] = f(np.concatenate([inp["c_prompt"][b][None], inp["c_sample"][sl]], axis=0))
        m["hv"] = np.full((128, 1), float(half), np.float32)
        m["ck128"] = f(inp["cache_kv_w128"][0, sl].reshape(NSQ, 128, 512))
        m["ck512"] = f(inp["cache_kv_w512"][0, sl].reshape(NSQ, 512, 512))
        m["ck2048"] = f(inp["cache_kv_w2048"][0, sl].reshape(NSQ, 2048, 512))
        m["sconv"] = f(inp["state_conv"][0, sl])
        maps.append(m)
    return maps


_NC_CACHE = {}


def kernel(**inp):
    import time as _t
    t0 = _t.time()
    maps = _prep_inputs(inp)
    t1 = _t.time()
    if "nc" not in _NC_CACHE:
        _NC_CACHE["nc"] = build_two_pass()
    nc = _NC_CACHE["nc"]
    t2 = _t.time()
    if STOP_AFTER is not None:
        for m in maps:
            for k in ("w_eg", "w_eu", "w_ed"):
                m.pop(k, None)
    res = run_bass_kernel_spmd(nc, maps, core_ids=list(range(NCORE)))
    print("[kernel] prep %.1fs build %.1fs run %.1fs" % (t1 - t0, t2 - t1, _t.time() - t2), flush=True)
    R = res.results
    _NC_CACHE['last'] = R
    B = 4
    yp = np.zeros((B, 8192, D), np.float32); ys = np.zeros((128, 4, D), np.float32)
    kvp = [np.zeros((1, B, w, 2, 4, 64), np.float32) for (w, _) in GROUPS]
    convp = np.zeros((1, B, 30, 512), np.float32)
    kvs = [np.zeros((1, 128, w, 2, 4, 64), np.float32) for (w, _) in GROUPS]
    convs = np.zeros((1, 128, 30, 512), np.float32)
    for cid in range(NCORE):
        b, half = cid // 2, cid % 2
        r = R[cid]
        sl = slice(cid * NSQ, (cid + 1) * NSQ)
        if "yp" in r:
            yp[b, half * NOWN:(half + 1) * NOWN] = r["yp"]
            ys[sl] = r["ys"].reshape(NSQ, 4, D)
        for gi, (w, _) in enumerate(GROUPS):
            if half == 1:
                kvp[gi][0, b] = r["kvp%d" % w].reshape(w, 2, 4, 64)
            kvs[gi][0, sl] = r["kvs%d" % w].reshape(NSQ, w, 2, 4, 64)
        if half == 1:
            convp[0, b] = r["convp"]
        convs[0, sl] = r["convs"]
    return (yp, ys, kvp[0], kvp[1], kvp[2], convp, kvs[0], kvs[1], kvs[2], convs)
```

```python
import math
import numpy as np
from contextlib import ExitStack
import concourse.bass as bass
import concourse.mybir as mybir
from concourse.bass_utils import run_bass_kernel_spmd

F32 = mybir.dt.float32
BF16 = mybir.dt.bfloat16
I32 = mybir.dt.int32
U32 = mybir.dt.uint32
AF = mybir.ActivationFunctionType
ALU = mybir.AluOpType
AX = mybir.AxisListType

D = 1024
NCORE = 8
HALO = 2048
NOWN = 4096
NEXT = HALO + NOWN
NSQ = 16
NS = 64
NTOK = NEXT + NS
NOT = NOWN + NS
NTILE = 33
GROUPS = ((128, 1), (512, 4), (2048, 16))
EPS = 1e-6
NEXP = 32
BLK = 512
SUBB = BLK // 128
NBLK = (2 * NOT) // BLK + NEXP
CAP = NBLK * BLK
STOP_AFTER = None
DEBUG_SCR = False


class Ctx:
    KD = 8

    def __init__(self, nc, es, needed=None):
        self.nc = nc
        self.es = es
        self.needed = needed
        self.record = set()
        self.iidx = {e: 0 for e in ('pe', 'act', 'dve', 'pool')}
        self.eng = {'pe': nc.tensor, 'act': nc.scalar, 'dve': nc.vector, 'pool': nc.gpsimd, 'sp': nc.sync}
        self.csem = {e: es.enter_context(nc.semaphore('c_' + e)) for e in ('pe', 'act', 'dve', 'pool')}
        self.ccnt = {e: 0 for e in self.csem}
        self.dsem = {q: [es.enter_context(nc.semaphore('d_%s%d' % (q, i))) for i in range(self.KD)]
                     for q in ('sp', 'act', 'pool')}
        self.dcnt = {q: 0 for q in self.dsem}
        self.waited = {e: {} for e in self.eng}
        self.state = {}
        self.sbn = 0

    def sb(self, shape, dt, es=None):
        self.sbn += 1
        return (es or self.es).enter_context(self.nc.sbuf_tensor('sb%d' % self.sbn, list(shape), dt))

    def ps(self, shape, dt):
        self.sbn += 1
        return self.es.enter_context(self.nc.psum_tensor('ps%d' % self.sbn, list(shape), dt))

    def _wait(self, e, evs):
        best = {}
        for (sem, v, src) in evs:
            k = id(sem)
            if k not in best or best[k][1] < v:
                best[k] = (sem, v)
        for k, (sem, v) in best.items():
            if self.waited[e].get(k, 0) >= v:
                continue
            self.eng[e].wait_ge(sem, v)
            self.waited[e][k] = v
            if self.needed is None:
                for ce, cs in self.csem.items():
                    if cs is sem:
                        self.record.add((ce, v))

    def _deps(self, e, reads, writes):
        evs = []
        for k in reads:
            st = self.state.get(k)
            if st and st['w'] is not None:
                evs.append(st['w'])
        for k in writes:
            st = self.state.get(k)
            if st:
                if st['w'] is not None and (st['w'][2] != e or e != 'pe'):
                    evs.append(st['w'])
                for r in st['r']:
                    if r[2] != e or e != 'pe':
                        evs.append(r)
        return evs

    def _commit(self, ev, reads, writes):
        for k in reads:
            st = self.state.setdefault(k, {'w': None, 'r': []})
            st['r'] = [r for r in st['r'] if r[0] is not ev[0]] + [ev]
        for k in writes:
            self.state[k] = {'w': ev, 'r': []}

    @staticmethod
    def _psx(reads, writes):
        ps = [k for k in reads if k == 'pb7' or (isinstance(k, tuple) and k[0] == 'pb')]
        if not ps:
            return list(reads), list(writes)
        return [k for k in reads if k not in ps], list(writes) + [k for k in ps if k not in writes]

    def op(self, e, fn, reads=(), writes=()):
        reads, writes = self._psx(reads, writes)
        self._wait(e, self._deps(e, reads, writes))
        ins = fn()
        self.iidx[e] += 1
        if self.needed is None or (e, self.iidx[e]) in self.needed:
            self.ccnt[e] += 1
            ins.then_inc(self.csem[e], 1)
        ev = (self.csem[e], self.ccnt[e], e)
        self._commit(ev, reads, writes)
        return ev

    def dma(self, q, fn, reads=(), writes=()):
        j = self.dcnt[q]
        sem = self.dsem[q][j % self.KD]
        evs = self._deps(None, reads, writes)
        if j >= self.KD:
            evs.append((sem, 16 * (j // self.KD), 'dma_' + q))
        self._wait(q, evs)
        ins = fn()
        ins.then_inc(sem, 16)
        self.dcnt[q] += 1
        ev = (sem, 16 * (j // self.KD + 1), 'dma_' + q)
        self._commit(ev, reads, writes)
        return ev

    def wait_keys(self, e, keys):
        self._wait(e, self._deps(None, keys, ()))

    def barrier(self):
        evs = []
        for q in self.dsem:
            for i, sem in enumerate(self.dsem[q]):
                n = (self.dcnt[q] - i + self.KD - 1) // self.KD
                if n > 0:
                    evs.append((sem, 16 * n, 'x'))
        for e in self.csem:
            if self.ccnt[e]:
                evs.append((self.csem[e], self.ccnt[e], 'x'))
        for e in self.eng:
            self._wait(e, evs)

    def finish(self):
        evs = []
        for q in self.dsem:
            for i, sem in enumerate(self.dsem[q]):
                n = (self.dcnt[q] - i + self.KD - 1) // self.KD
                if n > 0:
                    evs.append((sem, 16 * n, 'x'))
        for e in self.csem:
            if self.ccnt[e]:
                evs.append((self.csem[e], self.ccnt[e], 'x'))
        self._wait('sp', evs)


def _t5_bucket_np(dist):
    dist = np.asarray(dist, np.int64)
    max_exact = 16
    d_f = np.maximum(dist, 1).astype(np.float32)
    large = max_exact + (np.log(d_f / np.float32(max_exact)) / np.float32(math.log(2048 / max_exact))
                         * np.float32(32 - max_exact)).astype(np.int32)
    large = np.minimum(large, 31)
    return np.where(dist < max_exact, dist, large)


def _structure_constants():
    ohw = np.zeros((32, 3 * 510), np.float32)
    vw = np.zeros((4, 3 * 510), np.float32)
    for g, (win, dil) in enumerate(GROUPS):
        for blk in range(2):
            for u in range(255):
                rel = u + 1 if blk == 0 else u - 127
                ok = (rel <= 128) if blk == 0 else (rel >= 0)
                if ok:
                    b = int(_t5_bucket_np(rel * dil))
                    ohw[b, g * 510 + blk * 255 + u] = 1.0
                    vw[:, g * 510 + blk * 255 + u] = 1.0
    sel = np.zeros((17, 192), np.float32)
    sel[0, 0:128] = 1.0
    for t in range(64):
        sel[1 + t // 4, 128 + t] = 1.0
    bd = np.zeros((64, 128), np.float32)
    for k in range(64):
        for q in range(64):
            if k // 4 == q // 4:
                bd[k, q] = 1.0
        bd[k, 64 + k] = 1.0
    return ohw, vw, sel, bd


def _os_env(k):
    import os
    return os.environ.get(k)


def build_nc(needed=None):
    nc = bass.Bass("TRN2", target_bir_lowering=False)

    def din(name, shape, dt=F32):
        return nc.dram_tensor(name, list(shape), dt, kind="ExternalInput")

    def dout(name, shape, dt=F32):
        return nc.dram_tensor(name, list(shape), dt, kind="ExternalOutput")

    def dscr(name, shape, dt=F32):
        return nc.dram_tensor(name, list(shape), dt, kind="ExternalOutput" if DEBUG_SCR else "Internal")

    xp_t = din("xp", [NEXT, D]); xs_t = din("xs", [NS, D]); cmod_t = din("cmod", [17, D]); hv_t = din("hv", [128, 1])
    ck_t = [din("ck%d" % w, [NSQ, w, 512]) for (w, _) in GROUPS]
    sconv_t = din("sconv", [NSQ, 30, 512])
    relb_t = din("rel_bias", [32, 12])
    gmix_t = din("norm_mix_g", [1, D]); gffn_t = din("norm_ffn_g", [1, D]); gfin_t = din("norm_final_g", [1, D])
    wmod_t = din("w_mod", [D, 6 * D]); bmod_t = din("b_mod", [1, 6 * D])
    win_t = din("w_in", [D, 5376])
    dww_t = din("dw_w", [31, 512]); dwb_t = din("dw_b", [1, 512]); lng_t = din("ln_g", [1, 512]); lnb_t = din("ln_b", [1, 512])
    wco_t = din("w_conv_out", [512, D]); wao_t = din("w_attn_out", [256, D]); wo_t = din("w_out", [D, D])
    wrt_t = din("w_rt", [D, 36]); brt_t = din("b_rt", [1, 36])
    if STOP_AFTER is None:
        weg_t = din("w_eg", [NEXP, D, 512]); weu_t = din("w_eu", [NEXP, D, 512]); wed_t = din("w_ed", [NEXP, 512, D])
    ohw_t = din("ohw", [32, 1530]); vw_t = din("vw", [4, 1530]); sel_t = din("sel", [17, 192]); bd_t = din("bd", [64, 128])

    yp_t = dout("yp", [NOWN, D]); ys_t = dout("ys", [NS, D])
    kvp_t = [dout("kvp%d" % w, [w, 512]) for (w, _) in GROUPS]
    convp_t = dout("convp", [30, 512])
    kvs_t = [dout("kvs%d" % w, [NSQ, w, 512]) for (w, _) in GROUPS]
    convs_t = dout("convs", [NSQ, 30, 512])

    modrows_t = dscr("modrows", [4, 192, D])
    wd_t = dscr("wdscr", [3, 4, 510])
    ebd_t = dscr("ebd", [3, 128, 1024])
    sg_t = dscr("sgscr", [16, 128, NOT], BF16)
    yts_t = dscr("ytscr", [4, 128, NOT], BF16)
    acc_t = dscr("accscr", [3, NOWN, 260])
    accs_t = dscr("accsscr", [NS, 260])
    x1_t = dscr("x1scr", [NOT, D])
    xsd_t = dscr("xsdisp", [CAP, D], BF16)
    ysd_t = dscr("ysdisp", [CAP, D])

    xp = xp_t.ap(); xs = xs_t.ap(); win = win_t.ap()

    with ExitStack() as es:
        c = Ctx(nc, es, needed)
        nc._mk_ctx = c
        PB = [c.ps([128, 512], F32) for _ in range(8)]

        def pbf(i):
            return PB[i][:].bitcast(BF16)

        identf = c.sb([128, 128], F32); ident = c.sb([128, 128], BF16)
        onesb = c.sb([128, 128], BF16); onesf = c.sb([128, 128], F32)
        suf = c.sb([128, 128], F32); jf = c.sb([128, 128], F32)
        epsb = c.sb([128, 1], F32); hv = c.sb([128, 1], F32); one1 = c.sb([128, 1], F32)
        c.op('pool', lambda: nc.gpsimd.memset(onesf[:], 1.0), writes=['onesf'])
        c.op('pool', lambda: nc.gpsimd.memset(onesb[:], 1.0), writes=['onesb'])
        c.op('pool', lambda: nc.gpsimd.memset(epsb[:], EPS), writes=['epsb'])
        c.op('pool', lambda: nc.gpsimd.memset(one1[:], 1.0), writes=['one1'])
        c.op('pool', lambda: nc.gpsimd.affine_select(out=identf[:], in_=onesf[:], pattern=[[-1, 128]], compare_op=ALU.is_equal,
                                                       fill=0.0, base=0, channel_multiplier=1), reads=['onesf'], writes=['identf'])
        c.op('pool', lambda: nc.gpsimd.affine_select(out=jf[:], in_=onesf[:], pattern=[[1, 128]], compare_op=ALU.is_equal,
                                                       fill=0.0, base=-127, channel_multiplier=1), reads=['onesf'], writes=['jf'])
        c.op('pool', lambda: nc.gpsimd.affine_select(out=suf[:], in_=onesf[:], pattern=[[1, 128]], compare_op=ALU.is_gt,
                                                       fill=0.0, base=0, channel_multiplier=-1), reads=['onesf'], writes=['suf'])
        c.op('dve', lambda: nc.vector.tensor_copy(out=ident[:], in_=identf[:]), reads=['identf'], writes=['ident'])
        c.dma('sp', lambda: nc.sync.dma_start(out=hv[:], in_=hv_t.ap()), writes=['hv'])


        es_hT = ExitStack()
        es.enter_context(es_hT)
        hT = c.sb([128, 8, NTOK], BF16, es_hT)
        es_mod = ExitStack()
        a1p = c.sb([128, D], F32, es_mod); b1p = c.sb([128, D], F32, es_mod); a1s = c.sb([64, D], F32, es_mod); b1s = c.sb([64, D], F32, es_mod)
        es0 = ExitStack()
        with es0:
            cm = c.sb([17, D], F32, es0); scm = c.sb([17, D], F32, es0); scT = c.sb([128, 8, 17], F32, es0)
            mtok = c.sb([17, 6 * D], F32, es0); selm = c.sb([17, 192], F32, es0)
            gmb = c.sb([128, D], F32, es0); gfb = c.sb([128, D], F32, es0)
            c.dma('sp', lambda: nc.sync.dma_start(out=cm[:], in_=cmod_t.ap()), writes=['cm'])
            c.dma('sp', lambda: nc.sync.dma_start(out=selm[:], in_=sel_t.ap()), writes=['selm'])
            c.dma('sp', lambda: nc.sync.dma_start(out=gmb[:], in_=gmix_t.ap().partition_broadcast(128)), writes=['gmb'])
            c.dma('sp', lambda: nc.sync.dma_start(out=gfb[:], in_=gffn_t.ap().partition_broadcast(128)), writes=['gfb'])
            c.op('act', lambda: nc.scalar.activation(out=scm[:], in_=cm[:], func=AF.Silu), reads=['cm'], writes=['scm'])
            for k in range(8):
                c.op('pe', lambda k=k: nc.tensor.transpose(out=PB[0][:, k * 17:(k + 1) * 17], in_=scm[0:17, k * 128:(k + 1) * 128],
                                                           identity=identf[0:17, 0:17]), reads=['scm', 'identf'], writes=['pb0'])
            c.op('dve', lambda: nc.vector.tensor_copy(out=scT[:].rearrange("p k s -> p (k s)"), in_=PB[0][:, 0:136]), reads=['pb0'], writes=['scT'])
            wmod = wmod_t.ap().rearrange("(k p) n -> p k n", p=128)
            wbs = [c.sb([128, 8, 512], F32, es0) for _ in range(2)]
            bbs = [c.sb([17, 512], F32, es0) for _ in range(2)]
            for nb in range(12):
                wb = wbs[nb % 2]
                bb = bbs[nb % 2]
                c.dma('sp', lambda wb=wb, nb=nb: nc.sync.dma_start(out=wb[:], in_=wmod[:, :, nb * 512:(nb + 1) * 512]), writes=[('wb', nb % 2)])
                c.dma('sp', lambda bb=bb, nb=nb: nc.sync.dma_start(out=bb[:], in_=bmod_t.ap()[:, nb * 512:(nb + 1) * 512].partition_broadcast(17)),
                      writes=[('bb', nb % 2)])
                pbk = 1 + nb % 2
                for k in range(8):
                    c.op('pe', lambda wb=wb, k=k, pbk=pbk: nc.tensor.matmul(PB[pbk][0:17, 0:512], lhsT=scT[:, k, :], rhs=wb[:, k, :], start=(k == 0), stop=(k == 7)),
                         reads=['scT', ('wb', nb % 2)], writes=[('pb', pbk)])
                c.op('dve', lambda bb=bb, nb=nb, pbk=pbk: nc.vector.tensor_tensor(out=mtok[:, nb * 512:(nb + 1) * 512], in0=PB[pbk][0:17, 0:512], in1=bb[:], op=ALU.add),
                     reads=[('pb', pbk), ('bb', nb % 2)], writes=['mtok'])
            rowst = [c.sb([128, D], F32, es0) for _ in range(2)]
            for kind in range(6):
                for part, (c0, n) in enumerate(((0, 128), (128, 64))):
                    rt = rowst[(kind * 2 + part) % 2]
                    rk = ('rowst', (kind * 2 + part) % 2)
                    for hf in range(2):
                        c.op('pe', lambda hf=hf, c0=c0, n=n, kind=kind: nc.tensor.matmul(PB[3 + hf][0:n, :], lhsT=selm[0:17, c0:c0 + n],
                             rhs=mtok[0:17, kind * D + hf * 512: kind * D + hf * 512 + 512], start=True, stop=True),
                             reads=['selm', 'mtok'], writes=[('pb', 3 + hf)])
                    dst = None; dk = 'nokey'
                    if kind == 0:
                        dst = (b1p, b1s)[part]; dk = ('b1', part)
                    elif kind == 1:
                        dst = (a1p, a1s)[part]; dk = ('a1', part)
                    for hf in range(2):
                        sl = slice(hf * 512, hf * 512 + 512)
                        if kind in (1, 4):
                            gb = gmb if kind == 1 else gfb
                            tgt = dst if dst is not None else rt
                            c.op('dve', lambda hf=hf, n=n, gb=gb, tgt=tgt, sl=sl: nc.vector.scalar_tensor_tensor(out=tgt[0:n, sl], in0=PB[3 + hf][0:n, :], scalar=1.0,
                                 in1=gb[0:n, sl], op0=ALU.add, op1=ALU.mult), reads=[('pb', 3 + hf), 'gmb', 'gfb'], writes=[rk, dk])
                        else:
                            tgt = dst if dst is not None else rt
                            c.op('act', lambda hf=hf, n=n, tgt=tgt, sl=sl: nc.scalar.copy(out=tgt[0:n, sl], in_=PB[3 + hf][0:n, :]),
                                 reads=[('pb', 3 + hf)], writes=[rk, dk])
                    if kind >= 2:
                        c.dma('sp', lambda rt=rt, c0=c0, n=n, kind=kind: nc.sync.dma_start(out=modrows_t.ap()[kind - 2, c0:c0 + n, :], in_=rt[0:n, :]),
                              reads=[rk], writes=[('modrows', kind - 2, part)])
        c.barrier()
        es0 = ExitStack()
        with es0:
            rb = c.sb([32, 12], F32, es0); ohw = c.sb([32, 1530], F32, es0); vw = c.sb([4, 1530], F32, es0)
            wsb = c.sb([4, 1530], F32, es0); hall = c.sb([128, 24, 128], F32, es0); ebst = c.sb([128, 3, 1024], F32, es0)
            c.dma('sp', lambda: nc.sync.dma_start(out=rb[:], in_=relb_t.ap()), writes=['rb'])
            c.dma('sp', lambda: nc.sync.dma_start(out=ohw[:], in_=ohw_t.ap()), writes=['ohw'])
            c.dma('sp', lambda: nc.sync.dma_start(out=vw[:], in_=vw_t.ap()), writes=['vw'])
            for g in range(3):
                c.op('pe', lambda g=g: nc.tensor.matmul(PB[5][0:4, 0:510], lhsT=rb[:, 4 * g:4 * g + 4], rhs=ohw[:, g * 510:(g + 1) * 510], start=True, stop=True),
                     reads=['rb', 'ohw'], writes=[('pb', 5)])
                c.op('act', lambda g=g: nc.scalar.activation(out=wsb[:, g * 510:(g + 1) * 510], in_=PB[5][0:4, 0:510], func=AF.Exp), reads=[('pb', 5)], writes=['wsb'])
            c.op('dve', lambda: nc.vector.tensor_tensor(out=wsb[:], in0=wsb[:], in1=vw[:], op=ALU.mult), reads=['wsb', 'vw'], writes=['wsb'])
            c.dma('sp', lambda: nc.sync.dma_start(out=wd_t.ap().rearrange("g h u -> h g u"), in_=wsb[:].rearrange("h (g u) -> h g u", g=3)), reads=['wsb'], writes=['wd'])
            for g in range(3):
                for h in range(4):
                    for blk in range(2):
                        idx = (g * 4 + h) * 2 + blk
                        src = bass.AP(wd_t, (g * 4 + h) * 510 + blk * 255, [[1, 128], [1, 128]])
                        c.dma('sp', lambda idx=idx, src=src: nc.sync.dma_start(out=hall[:, idx, :], in_=src), reads=['wd'], writes=[('hall', idx)])
            for g in range(3):
                for hf in range(2):
                    c.op('pe', lambda g=g, hf=hf: nc.tensor.matmul(PB[6 + hf][:, :], lhsT=jf[:], rhs=hall[:, g * 8 + hf * 4: g * 8 + hf * 4 + 4, :].rearrange("p a q -> p (a q)"),
                         start=True, stop=True), reads=['jf'] + [('hall', g * 8 + hf * 4 + i) for i in range(4)], writes=[('pb', 6 + hf)])
                    c.op('act', lambda g=g, hf=hf: nc.scalar.copy(out=ebst[:, g, hf * 512:(hf + 1) * 512], in_=PB[6 + hf][:, :]), reads=[('pb', 6 + hf)], writes=[('ebst', g)])
                c.dma('sp', lambda g=g: nc.sync.dma_start(out=ebd_t.ap()[g], in_=ebst[:, g, :]), reads=[('ebst', g)], writes=[('ebd', g)])

        c.barrier()
        def norm_to_T(tidx, src_ap, n, arow, brow, akey, bkey, bufs):
            xt, t1, hb, stt, junk = bufs
            s = tidx % 2
            c.dma('sp', lambda: nc.sync.dma_start(out=xt[s][0:n, :], in_=src_ap), writes=[('xt', s)])
            c.op('act', lambda: nc.scalar.activation(out=junk[0:n, :], in_=xt[s][0:n, :], func=AF.Square, accum_out=stt[s][0:n, 0:1]),
                 reads=[('xt', s)], writes=[('stt', s), 'junk'])
            c.op('act', lambda: nc.scalar.activation(out=stt[s][0:n, 1:2], in_=stt[s][0:n, 0:1], func=AF.Sqrt, scale=1.0 / D, bias=epsb[0:n, :]),
                 reads=[('stt', s), 'epsb'], writes=[('stt', s)])
            c.op('dve', lambda: nc.vector.reciprocal(out=stt[s][0:n, 2:3], in_=stt[s][0:n, 1:2]), reads=[('stt', s)], writes=[('stt', s)])
            c.op('dve', lambda: nc.vector.scalar_tensor_tensor(out=t1[s][0:n, :], in0=xt[s][0:n, :], scalar=stt[s][0:n, 2:3], in1=arow[0:n, :],
                                                                op0=ALU.mult, op1=ALU.mult), reads=[('xt', s), ('stt', s), akey], writes=[('t1', s)])
            c.op('pool', lambda: nc.gpsimd.tensor_tensor(out=hb[s][0:n, :], in0=t1[s][0:n, :], in1=brow[0:n, :], op=ALU.add),
                 reads=[('t1', s), bkey], writes=[('hb', s)])
            pv = pbf(s).rearrange("p (k t) -> p k t", k=8)
            for k in range(8):
                c.op('pe', lambda k=k: nc.tensor.transpose(out=pv[:, k, 0:n], in_=hb[s][0:n, k * 128:(k + 1) * 128], identity=ident[0:n, 0:n]),
                     reads=[('hb', s), 'ident'], writes=[('pb', s)])
            return s, pv

        esA = ExitStack()
        with esA:
            xt = [c.sb([128, D], F32, esA) for _ in range(2)]; t1 = [c.sb([128, D], F32, esA) for _ in range(2)]
            hb = [c.sb([128, D], BF16, esA) for _ in range(2)]; stt = [c.sb([128, 4], F32, esA) for _ in range(2)]
            junk = c.sb([128, D], BF16, esA)
            bufsA = (xt, t1, hb, stt, junk)
            for t in range(49):
                if t < 48:
                    n = 128; src = xp[t * 128:(t + 1) * 128, :]; ar, br = a1p, b1p; col = t * 128; pk = 0
                else:
                    n = 64; src = xs; ar, br = a1s, b1s; col = NEXT; pk = 1
                s, pv = norm_to_T(t, src, n, ar, br, ('a1', pk), ('b1', pk), bufsA)
                c.op('act', lambda pv=pv, col=col, n=n: nc.scalar.copy(out=hT[:, :, col:col + n], in_=pv[:, :, 0:n]), reads=[('pb', s)], writes=['hT'])
        c.barrier()
        es_mod.close()

        winr = win.rearrange("(k p) n -> p k n", p=128)

        def load_w(dst, c0, ncol, key):
            c.dma('pool', lambda: nc.gpsimd.dma_start(out=dst, in_=winr[:, :, c0:c0 + ncol]), writes=[key])

        def proj_T(ps_ap, pkey, wt, wkey, hcols):
            for k in range(8):
                c.op('pe', lambda k=k: nc.tensor.matmul(ps_ap, lhsT=wt[:, k, :], rhs=hT[:, k, hcols], start=(k == 0), stop=(k == 7)),
                     reads=['hT', wkey], writes=[pkey])

        def psv(i, sl=slice(None), n=512):
            return PB[i][sl, 0:n]

        OT = [(HALO + 512 * m, 512 * m, 512) for m in range(8)] + [(NEXT, NOWN, NS)]

        shiftq = []
        for g, (wing, d) in enumerate(GROUPS):
            A_ = (1, 4, 28)[g]
            npart = 4 if g == 2 else 1
            for q4 in range(npart):
                shiftq.append((g, A_, q4 * (A_ // npart), A_ // npart))
        shiftq = [shiftq[2], shiftq[3], shiftq[4], shiftq[5], shiftq[0], shiftq[1]]

        def emit_shift(nmax):
            for _ in range(nmax):
                if not shiftq:
                    return
                g, A_, a0, na = shiftq.pop(0)
                wing = GROUPS[g][0]
                c.dma('act', lambda g=g, A_=A_, a0=a0, na=na, wing=wing: nc.scalar.dma_start(
                      out=kvs_t[g].ap()[:, 0:wing - 4, :].rearrange("b (a r) c -> b a (r c)", a=A_)[:, a0:a0 + na, :],
                      in_=ck_t[g].ap()[:, 4:wing, :].rearrange("b (a r) c -> b a (r c)", a=A_)[:, a0:a0 + na, :]), writes=[('kvs_shift', g, a0)])

        esG = ExitStack()
        with esG:
            wj = [c.sb([128, 8, 128], BF16, esG) for _ in range(2)]
            sgrow = [c.sb([128, NOT], BF16, esG) for _ in range(2)]
            for j in range(16):
                s = j % 2
                load_w(wj[s][:], 3328 + j * 128, 128, ('wj', s))
                for m, (hc, oi, n) in enumerate(OT):
                    pi = m % 4
                    pa = psv(pi, n=n)
                    proj_T(pa, ('pb', pi), wj[s], ('wj', s), slice(hc, hc + n))
                    c.op('act', lambda pa=pa, oi=oi, n=n, s=s: nc.scalar.activation(out=sgrow[s][:, oi:oi + n], in_=pa, func=AF.Sigmoid),
                         reads=[('pb', pi)], writes=[('sgrow', s)])
                c.dma('sp', lambda j=j, s=s: nc.sync.dma_start(out=sg_t.ap()[j], in_=sgrow[s][:]), reads=[('sgrow', s)], writes=[('sg', j)])

        c.barrier()
        if STOP_AFTER == 'A':
            c.finish()
            return nc

        esC = ExitStack()
        with esC:
            yT = c.sb([128, 4, NOT], BF16, esC)
            dwT = c.sb([128, 4, 31], F32, esC); dwb = c.sb([128, 4], F32, esC); lng = c.sb([128, 4], F32, esC); lnb = c.sb([128, 4], F32, esC)
            with nc.allow_non_contiguous_dma(reason="tiny per-channel parameter loads"):
                for cc in range(4):
                    c.dma('sp', lambda cc=cc: nc.sync.dma_start(out=dwT[:, cc, :], in_=dww_t.ap()[:, cc * 128:(cc + 1) * 128].rearrange("j p -> p j")), writes=[('dwT', cc)])
                c.dma('sp', lambda: nc.sync.dma_start(out=dwb[:], in_=dwb_t.ap().rearrange("o (c p) -> p (o c)", p=128)), writes=['dwb'])
                c.dma('sp', lambda: nc.sync.dma_start(out=lng[:], in_=lng_t.ap().rearrange("o (c p) -> p (o c)", p=128)), writes=['lng'])
                c.dma('sp', lambda: nc.sync.dma_start(out=lnb[:], in_=lnb_t.ap().rearrange("o (c p) -> p (o c)", p=128)), writes=['lnb'])
            uxT = c.sb([128, 4, NSQ, 34], BF16, esC)
            uTf = c.sb([128, 4, 94], F32, esC)
            sct = [c.sb([120, 512], F32, esC)] * 2
            for i4 in range(4):
                s = i4 % 2
                c.dma('sp', lambda i4=i4, s=s: nc.sync.dma_start(out=sct[s][:], in_=sconv_t.ap()[4 * i4:4 * i4 + 4].rearrange("b t c -> (b t) c")), writes=[('sct', 0)])
                for cc in range(4):
                    c.op('pe', lambda cc=cc, s=s: nc.tensor.transpose(out=PB[2][:, 0:120], in_=sct[s][:, cc * 128:(cc + 1) * 128], identity=identf[0:120, 0:120]),
                         reads=[('sct', 0), 'identf'], writes=[('pb', 2)])
                    c.op('act', lambda cc=cc, i4=i4: nc.scalar.copy(out=uxT[:, cc, 4 * i4:4 * i4 + 4, 0:30], in_=PB[2][:, 0:120].rearrange("p (b t) -> p b t", b=4)),
                         reads=[('pb', 2)], writes=['uxT'])
            c.dma('sp', lambda: nc.sync.dma_start(out=convs_t.ap()[:, 0:26, :], in_=sconv_t.ap()[:, 4:30, :]), writes=['convs_a'])
            esC1 = ExitStack()
            wul = [c.sb([128, 8, 128], BF16, esC1) for _ in range(2)]; wug = [c.sb([128, 8, 128], BF16, esC1) for _ in range(2)]
            diag = [c.sb([128, 31, 128], BF16, esC1)] * 2
            ucT = [c.sb([128, 30 + NOWN], BF16, esC1)] * 2
            sgt = [c.sb([128, 512], F32, esC1) for _ in range(2)]
            UT = [(HALO - 30, -30, 30)] + OT
            for cc in range(4):
                s = cc % 2
                load_w(wul[s][:], 2304 + cc * 128, 128, ('wul', s))
                load_w(wug[s][:], 2816 + cc * 128, 128, ('wug', s))
                emit_shift(1)
                for j in range(31):
                    eng = 'dve' if j % 2 == 0 else 'pool'
                    e_ = nc.vector if eng == 'dve' else nc.gpsimd
                    c.op(eng, lambda j=j, e_=e_: e_.tensor_scalar(out=diag[s][:, j, :], in0=identf[:], scalar1=dwT[:, cc, j:j + 1], scalar2=None, op0=ALU.mult),
                         reads=['identf', ('dwT', cc)], writes=[('diag', 0, j)])
                for m, (hc, oi, n) in enumerate(UT):
                    pl = psv(0, n=n); pg = psv(1, n=n)
                    proj_T(pl, ('pb', 0), wul[s], ('wul', s), slice(hc, hc + n))
                    proj_T(pg, ('pb', 1), wug[s], ('wug', s), slice(hc, hc + n))
                    b_ = m % 2
                    c.op('act', lambda pg=pg, n=n, b_=b_: nc.scalar.activation(out=sgt[b_][:, 0:n], in_=pg, func=AF.Sigmoid), reads=[('pb', 1)], writes=[('sgt', b_)])
                    if oi < 0:
                        c.op('dve', lambda pl=pl, n=n, b_=b_: nc.vector.tensor_tensor(out=sgt[b_][:, 0:n], in0=pl, in1=sgt[b_][:, 0:n], op=ALU.mult),
                             reads=[('pb', 0), ('sgt', b_)], writes=[('sgt', b_)])
                        c.op('dve', lambda n=n, b_=b_: nc.vector.tensor_scalar(out=ucT[s][:, 0:30], in0=sgt[b_][:, 0:n], scalar1=hv[:, 0:1], scalar2=None, op0=ALU.mult),
                             reads=[('sgt', b_), 'hv'], writes=[('ucT', 0, 0)])
                    elif oi < NOWN:
                        c.op('dve', lambda pl=pl, n=n, b_=b_, oi=oi: nc.vector.tensor_tensor(out=ucT[s][:, 30 + oi:30 + oi + n], in0=pl, in1=sgt[b_][:, 0:n], op=ALU.mult),
                             reads=[('pb', 0), ('sgt', b_)], writes=[('ucT', 0, 1 + oi // 512)])
                        if oi == NOWN - 512:
                            c.op('dve', lambda pl=pl, b_=b_: nc.vector.tensor_tensor(out=uTf[:, cc, 0:30], in0=pl[:, 482:512], in1=sgt[b_][:, 482:512], op=ALU.mult),
                                 reads=[('pb', 0), ('sgt', b_)], writes=[('uTf', cc)])
                    else:
                        c.op('dve', lambda pl=pl, b_=b_: nc.vector.tensor_tensor(out=uTf[:, cc, 30:94], in0=pl, in1=sgt[b_][:, 0:64], op=ALU.mult),
                             reads=[('pb', 0), ('sgt', b_)], writes=[('uTf', cc)])
                        c.op('dve', lambda: nc.vector.tensor_copy(out=uxT[:, cc, :, 30:34], in_=uTf[:, cc, 30:94].rearrange("p (b t) -> p b t", t=4)),
                             reads=[('uTf', cc)], writes=['uxT'])
                dkeys = [('diag', 0, j) for j in range(31)]
                for m in range(8):
                    py = psv(2 + m % 4)
                    for j in range(31):
                        c.op('pe', lambda j=j, m=m, py=py: nc.tensor.matmul(py, lhsT=diag[s][:, j, :], rhs=ucT[s][:, 512 * m + j: 512 * m + j + 512], start=(j == 0), stop=(j == 30)),
                             reads=[dkeys[j], ('ucT', 0, 0), ('ucT', 0, 1 + m), ('ucT', 0, m)], writes=[('pb', 2 + m % 4)])
                    c.op('act', lambda m=m, py=py: nc.scalar.activation(out=yT[:, cc, 512 * m:512 * m + 512], in_=py, func=AF.Identity, bias=dwb[:, cc:cc + 1], scale=1.0),
                         reads=[('pb', 2 + m % 4), 'dwb'], writes=[('yT', m)])
                pys = PB[2][:, 0:64]
                for j in range(31):
                    c.op('pe', lambda j=j: nc.tensor.matmul(pys.rearrange("p (b t) -> p b t", t=4), lhsT=diag[s][:, j, :], rhs=uxT[:, cc, :, j:j + 4], start=(j == 0), stop=(j == 30)),
                         reads=[dkeys[j], 'uxT'], writes=[('pb', 2)])
                c.op('act', lambda: nc.scalar.activation(out=yT[:, cc, NOWN:NOT], in_=pys, func=AF.Identity, bias=dwb[:, cc:cc + 1], scale=1.0),
                     reads=[('pb', 2), 'dwb'], writes=[('yT', 8)])
            c.barrier()
            esC1.close()
            cpo = c.sb([94, 512], F32, esC)
            for cc in range(4):
                c.op('pe', lambda cc=cc: nc.tensor.transpose(out=PB[0][0:94, 0:128], in_=uTf[:, cc, :], identity=identf[:]), reads=[('uTf', cc), 'identf'], writes=[('pb', 0)])
                c.op('act', lambda cc=cc: nc.scalar.copy(out=cpo[:, cc * 128:(cc + 1) * 128], in_=PB[0][0:94, 0:128]), reads=[('pb', 0)], writes=['cpo'])
            c.dma('sp', lambda: nc.sync.dma_start(out=convp_t.ap(), in_=cpo[0:30, :]), reads=['cpo'], writes=['convp'])
            for b in range(NSQ):
                c.dma('sp', lambda b=b: nc.sync.dma_start(out=convs_t.ap()[b, 26:30, :], in_=cpo[30 + 4 * b:34 + 4 * b, :]), reads=['cpo'], writes=[('convs_b', b)])
            emit_shift(2)
            sq = c.sb([128, 4, 512], BF16, esC); mean = c.sb([128, 512], F32, esC); msq = c.sb([128, 512], F32, esC)
            var = c.sb([128, 512], F32, esC); rstd = c.sb([128, 512], F32, esC); tt = c.sb([128, 4, 512], F32, esC)
            for m, (hc, oi, n) in enumerate(OT):
                yk = ('yT', m)
                c.op('act', lambda oi=oi, n=n: nc.scalar.activation(out=sq[:, :, 0:n], in_=yT[:, :, oi:oi + n], func=AF.Square), reads=[yk], writes=['sq'])
                p1 = psv(4, n=n); p2 = psv(5, n=n)
                for cc in range(4):
                    c.op('pe', lambda cc=cc, p1=p1, oi=oi, n=n: nc.tensor.matmul(p1, lhsT=onesb[:], rhs=yT[:, cc, oi:oi + n], start=(cc == 0), stop=(cc == 3)),
                         reads=[yk, 'onesb'], writes=[('pb', 4)])
                for cc in range(4):
                    c.op('pe', lambda cc=cc, p2=p2, n=n: nc.tensor.matmul(p2, lhsT=onesb[:], rhs=sq[:, cc, 0:n], start=(cc == 0), stop=(cc == 3)),
                         reads=['sq', 'onesb'], writes=[('pb', 5)])
                c.op('dve', lambda p1=p1, n=n: nc.vector.tensor_scalar(out=mean[:, 0:n], in0=p1, scalar1=1.0 / 512, scalar2=None, op0=ALU.mult), reads=[('pb', 4)], writes=['mean'])
                c.op('pool', lambda n=n: nc.gpsimd.tensor_tensor(out=msq[:, 0:n], in0=mean[:, 0:n], in1=mean[:, 0:n], op=ALU.mult), reads=['mean'], writes=['msq'])
                c.op('dve', lambda p2=p2, n=n: nc.vector.scalar_tensor_tensor(out=var[:, 0:n], in0=p2, scalar=1.0 / 512, in1=msq[:, 0:n], op0=ALU.mult, op1=ALU.subtract),
                     reads=[('pb', 5), 'msq'], writes=['var'])
                c.op('act', lambda n=n: nc.scalar.activation(out=var[:, 0:n], in_=var[:, 0:n], func=AF.Sqrt, scale=1.0, bias=epsb[:, :]), reads=['var', 'epsb'], writes=['var'])
                c.op('dve', lambda n=n: nc.vector.reciprocal(out=rstd[:, 0:n], in_=var[:, 0:n]), reads=['var'], writes=['rstd'])
                c.op('dve', lambda oi=oi, n=n: nc.vector.tensor_tensor(out=tt[:, :, 0:n], in0=yT[:, :, oi:oi + n], in1=mean[:, 0:n].unsqueeze(1).to_broadcast([128, 4, n]), op=ALU.subtract),
                     reads=[yk, 'mean'], writes=['tt'])
                c.op('pool', lambda n=n: nc.gpsimd.tensor_tensor(out=tt[:, :, 0:n], in0=tt[:, :, 0:n], in1=rstd[:, 0:n].unsqueeze(1).to_broadcast([128, 4, n]), op=ALU.mult),
                     reads=['tt', 'rstd'], writes=['tt'])
                for cc in range(4):
                    c.op('act', lambda cc=cc, oi=oi, n=n: nc.scalar.activation(out=yT[:, cc, oi:oi + n], in_=tt[:, cc, 0:n], func=AF.Silu, bias=lnb[:, cc:cc + 1], scale=lng[:, cc:cc + 1]),
                         reads=['tt', 'lng', 'lnb'], writes=[yk])
            for cc in range(4):
                c.dma('sp', lambda cc=cc: nc.sync.dma_start(out=yts_t.ap()[cc], in_=yT[:, cc, :]), reads=[('yT', m) for m in range(9)], writes=[('yts', cc)])

        c.barrier()
        if STOP_AFTER == 'C':
            c.finish()
            return nc

        emit_shift(99)
        esD = ExitStack()
        with esD:
            ebf = c.sb([128, 1024], F32, esD)
            smk = c.sb([128, 9, 4, 4], F32, esD)
            nmk = c.sb([64, 3, 4, 64], F32, esD)
            bdm = c.sb([64, 128], F32, esD)
            pz = [c.sb([128, 4, 64], BF16, esD) for _ in range(NSQ)]
            v1n = c.sb([64, 3, 4, 65], BF16, esD)
            wkv = c.sb([128, 8, 512], BF16, esD)
            wq = c.sb([128, 8, 128], BF16, esD); wk = c.sb([128, 8, 128], BF16, esD)
            qT = c.sb([128, NOT], BF16, esD); kT = c.sb([128, NTOK], BF16, esD)
            qs = c.sb([128, 2, 64], BF16, esD); ks = c.sb([128, 2, 64], BF16, esD)
            v1 = c.sb([128, 48, 4, 65], BF16, esD)
            kvst = [c.sb([128, 512], F32, esD) for _ in range(2)]
            ef = [c.sb([128, 512], F32, esD) for _ in range(2)]
            pt = [c.sb([128, 512], BF16, esD) for _ in range(2)]
            oev = [c.sb([128, 130], F32, esD) for _ in range(2)]
            ctile = [c.sb([128, 512], F32, esD) for _ in range(2)]
            kTs = [c.sb([128, 2, 128], BF16, esD) for _ in range(2)]
            v1s = [c.sb([128, 4, 65], BF16, esD) for _ in range(2)]
            ess2 = [c.sb([128, 16], F32, esD) for _ in range(2)]
            en = c.sb([64, 256], F32, esD); pn = c.sb([64, 4, 64], BF16, esD)
            osv = c.sb([64, 260], F32, esD)
            c.dma('sp', lambda: nc.sync.dma_start(out=bdm[:], in_=bd_t.ap()), writes=['bdm'])
            c.op('pool', lambda: nc.gpsimd.memset(smk[:], 0.0), writes=['smk'])
            for b in range(NSQ):
                c.op('pool', lambda b=b: nc.gpsimd.memset(pz[b][:], 0.0), writes=[('pz', b)])
            for s in range(2):
                c.op('pool', lambda s=s: nc.gpsimd.memset(v1s[s][:], 1.0), writes=[('v1s', s)])
            c.op('pool', lambda: nc.gpsimd.memset(v1n[:], 1.0), writes=['v1n'])
            n_os = 1
            zl = c.sb([128, 64], BF16, esD); zr = c.sb([128, 260], BF16, esD)
            c.op('pool', lambda: nc.gpsimd.memset(zl[:], 0.0), writes=['zl'])
            c.op('pool', lambda: nc.gpsimd.memset(zr[:], 0.0), writes=['zr'])
            c.op('pe', lambda: nc.tensor.matmul(PB[7][0:64, 0:260], lhsT=zl[:], rhs=zr[:], start=True, stop=False), reads=['zl', 'zr'], writes=['pb7'])
            qb_i = 0
            for g, (wing, d) in enumerate(GROUPS):
                e0 = HALO - wing
                nbl = (NEXT - e0) // (128 * d)
                ebv = ebf[:].rearrange("p (h b q) -> p h b q", h=4, b=2)
                c.dma('sp', lambda g=g: nc.sync.dma_start(out=ebf[:], in_=ebd_t.ap()[g]), reads=[('ebd', g)], writes=['ebf'])
                if g == 0:
                    c.op('dve', lambda: nc.vector.tensor_copy(out=smk[:, 0, :, :], in_=ebv[:, :, 0, 0:4]), reads=['ebf'], writes=['smk'])
                else:
                    for r in range(4):
                        c.op('dve', lambda r=r, g=g: nc.vector.tensor_copy(out=smk[:, 1 + 4 * (g - 1) + r, :, r:r + 1], in_=ebv[:, :, 0, 0:1]), reads=['ebf'], writes=['smk'])
                bsel = bdm[:, 0:64] if g == 0 else bdm[:, 64:128]
                c.op('dve', lambda g=g, bsel=bsel: nc.vector.tensor_tensor(out=nmk[:, g, :, :], in0=ebv[0:64, :, 1, 0:64], in1=bsel.unsqueeze(1).to_broadcast([64, 4, 64]), op=ALU.mult),
                     reads=['ebf', 'bdm'], writes=['nmk'])
                c.dma('pool', lambda g=g: nc.gpsimd.dma_start(out=wkv[:, :, 0:256], in_=winr[:, :, 768 + 256 * g: 1024 + 256 * g]), writes=['wkv_k'])
                c.dma('pool', lambda g=g: nc.gpsimd.dma_start(out=wkv[:, :, 256:512], in_=winr[:, :, 1536 + 256 * g: 1792 + 256 * g]), writes=['wkv_v'])
                nkv = 0
                for r in range(d):
                    for i in range(nbl):
                        bi = r * nbl + i
                        cs0 = e0 + r + 128 * d * i
                        hsl = slice(cs0, cs0 + 127 * d + 1, d)
                        pi = 4 + nkv % 3
                        for k in range(8):
                            c.op('pe', lambda k=k, hsl=hsl, pi=pi: nc.tensor.matmul(PB[pi][:, :], lhsT=hT[:, k, hsl], rhs=wkv[:, k, :], start=(k == 0), stop=(k == 7)),
                                 reads=['hT', 'wkv_k', 'wkv_v'], writes=[('pb', pi)])
                        vc = hv if i == 0 else one1
                        c.op('dve', lambda bi=bi, pi=pi, vc=vc: nc.vector.tensor_scalar(out=v1[:, bi, :, 0:64], in0=PB[pi][:, 256:512].rearrange("p (h e) -> p h e", h=4),
                             scalar1=vc[:, 0:1], scalar2=None, op0=ALU.mult), reads=[('pb', pi), 'hv', 'one1'], writes=[('v1', bi)])
                        c.op('pool', lambda bi=bi, vc=vc: nc.gpsimd.tensor_copy(out=v1[:, bi, :, 64:65], in_=vc[:, 0:1].unsqueeze(1).to_broadcast([128, 4, 1])),
                             reads=['hv', 'one1'], writes=[('v1o', bi)])
                        if i == nbl - 1:
                            s = nkv % 2
                            c.op('act', lambda s=s, pi=pi: nc.scalar.copy(out=kvst[s][:], in_=PB[pi][:, :]), reads=[('pb', pi)], writes=[('kvst', s)])
                            c.dma('sp', lambda s=s, r=r, g=g, d=d, wing=wing: nc.sync.dma_start(out=kvp_t[g].ap()[r:wing:d, :], in_=kvst[s][:]), reads=[('kvst', s)], writes=[('kvp', g, r)])
                        nkv += 1
                pi = 4 + nkv % 3
                for k in range(8):
                    c.op('pe', lambda k=k, pi=pi: nc.tensor.matmul(PB[pi][0:64, :], lhsT=hT[:, k, NEXT:NTOK], rhs=wkv[:, k, :], start=(k == 0), stop=(k == 7)),
                         reads=['hT', 'wkv_k', 'wkv_v'], writes=[('pb', pi)])
                s = nkv % 2
                c.op('act', lambda s=s, pi=pi: nc.scalar.copy(out=kvst[s][0:64, :], in_=PB[pi][0:64, :]), reads=[('pb', pi)], writes=[('kvst', s)])
                c.op('dve', lambda g=g, pi=pi: nc.vector.tensor_copy(out=v1n[:, g, :, 0:64], in_=PB[pi][0:64, 256:512].rearrange("p (h e) -> p h e", h=4)), reads=[('pb', pi)], writes=['v1n'])
                for b in range(NSQ):
                    c.dma('sp', lambda b=b, s=s, g=g, wing=wing: nc.sync.dma_start(out=kvs_t[g].ap()[b, wing - 4:wing, :], in_=kvst[s][4 * b:4 * b + 4, :]), reads=[('kvst', s)], writes=[('kvsn', g, b)])
                for pair in range(2):
                    if _os_env('MK_SKIP_ATT'):
                        continue
                    c.dma('pool', lambda g=g, pair=pair: nc.gpsimd.dma_start(out=wq[:], in_=winr[:, :, 256 * g + 128 * pair: 256 * g + 128 * pair + 128]), writes=['wq'])
                    c.dma('pool', lambda g=g, pair=pair: nc.gpsimd.dma_start(out=wk[:], in_=winr[:, :, 768 + 256 * g + 128 * pair: 768 + 256 * g + 128 * pair + 128]), writes=['wk'])
                    npj = 0
                    for (hc, oi, n) in OT:
                        pi = 4 + npj % 3
                        proj_T(PB[pi][:, 0:n], ('pb', pi), wq, 'wq', slice(hc, hc + n))
                        c.op('act', lambda pi=pi, oi=oi, n=n: nc.scalar.copy(out=qT[:, oi:oi + n], in_=PB[pi][:, 0:n]), reads=[('pb', pi)], writes=['qT'])
                        npj += 1
                    cs_ = e0
                    while cs_ < NTOK:
                        n = min(512, NTOK - cs_)
                        pi = 4 + npj % 3
                        proj_T(PB[pi][:, 0:n], ('pb', pi), wk, 'wk', slice(cs_, cs_ + n))
                        if npj % 2 == 0:
                            c.op('dve', lambda pi=pi, cs_=cs_, n=n: nc.vector.tensor_copy(out=kT[:, cs_:cs_ + n], in_=PB[pi][:, 0:n]), reads=[('pb', pi)], writes=['kT'])
                        else:
                            c.op('act', lambda pi=pi, cs_=cs_, n=n: nc.scalar.copy(out=kT[:, cs_:cs_ + n], in_=PB[pi][:, 0:n]), reads=[('pb', pi)], writes=['kT_a'])
                        npj += 1
                        cs_ += n
                    c.op('dve', lambda pair=pair: nc.vector.tensor_copy(out=qs[:, pair, :], in_=qT[:, NOWN:NOT]), reads=['qT'], writes=['qs'])
                    c.op('dve', lambda pair=pair: nc.vector.tensor_copy(out=ks[:, pair, :], in_=kT[:, NEXT:NTOK]), reads=['kT', 'kT_a'], writes=['ks'])
                    for r in range(d):
                        for i in range(1, nbl):
                            s = qb_i % 2
                            qb_i += 1
                            q0 = r + 128 * d * (i - 1)
                            qsl = slice(q0, q0 + 127 * d + 1, d)
                            SBK = ((0, 1), (4, 5))[s]
                            for hh in range(2):
                                po = hh * 64
                                for blk in range(2):
                                    k0 = e0 + r + 128 * d * (i - 1 + blk)
                                    ksl = slice(k0, k0 + 127 * d + 1, d)
                                    c.op('pe', lambda bk=SBK[hh], po=po, ksl=ksl, qsl=qsl, blk=blk: nc.tensor.matmul(PB[bk][:, blk * 128:blk * 128 + 128], lhsT=kT[po:po + 64, ksl], rhs=qT[po:po + 64, qsl], start=True, stop=True),
                                         reads=['kT', 'kT_a', 'qT'], writes=[('pb', SBK[hh])])
                            for hh in range(2):
                                c.op('act', lambda s=s, hh=hh, bk=SBK[hh]: nc.scalar.activation(out=ef[s][:, hh * 256:(hh + 1) * 256], in_=PB[bk][:, 0:256], func=AF.Exp, scale=0.125),
                                     reads=[('pb', SBK[hh])], writes=[('ef', s, hh)])
                            c.op('dve', lambda s=s, pair=pair: nc.vector.tensor_tensor(out=pt[s][:], in0=ef[s][:], in1=ebf[:, pair * 512:(pair + 1) * 512], op=ALU.mult),
                                 reads=[('ef', s, 0), ('ef', s, 1), 'ebf'], writes=[('pt', s)])
                            for hh in range(2):
                                for blk in range(2):
                                    bi = r * nbl + i - 1 + blk
                                    col = (hh * 2 + blk) * 128
                                    c.op('pe', lambda s=s, hh=hh, blk=blk, bi=bi, col=col, pair=pair: nc.tensor.matmul(PB[2 + s][:, hh * 65:hh * 65 + 65], lhsT=pt[s][:, col:col + 128],
                                         rhs=v1[:, bi, 2 * pair + hh, :], start=(blk == 0), stop=(blk == 1)), reads=[('pt', s), ('v1', bi), ('v1o', bi)], writes=[('pb', 2 + s)])
                            c.op('dve', lambda s=s: nc.vector.tensor_copy(out=oev[s][:], in_=PB[2 + s][:, 0:130]), reads=[('pb', 2 + s)], writes=[('oev', s)])
                            c.dma('sp', lambda s=s, g=g, q0=q0, d=d, pair=pair: nc.sync.dma_start(out=acc_t.ap()[g, q0:q0 + 127 * d + 1:d, pair * 130:(pair + 1) * 130], in_=oev[s][:]),
                                  reads=[('oev', s)], writes=[('acc', g, q0, pair)])
                import os as _os
                ntile = 1 if g == 0 else 4
                if _os.environ.get('MK_SKIP_SAMPLE'):
                    continue
                for b in range(NSQ):
                    for r in range(ntile):
                        s = (b * ntile + r) % 2
                        tix = 0 if g == 0 else 1 + 4 * (g - 1) + r
                        c.dma('sp', lambda s=s, b=b, r=r, g=g, d=d, wing=wing: nc.sync.dma_start(out=ctile[s][:], in_=ck_t[g].ap()[b, r:wing:d, :]), writes=[('ctile', s)])
                        TB = (0, 1)[s]
                        for pr in range(2):
                            c.op('pe', lambda s=s, pr=pr, TB=TB: nc.tensor.transpose(out=PB[TB][:, pr * 128:(pr + 1) * 128], in_=ctile[s][:, pr * 128:(pr + 1) * 128], identity=identf[:]),
                                 reads=[('ctile', s), 'identf'], writes=[('pb', TB)])
                        c.op('act', lambda s=s, TB=TB: nc.scalar.copy(out=kTs[s][:].rearrange("p a k -> p (a k)"), in_=PB[TB][:, 0:256]), reads=[('pb', TB)], writes=[('kTs', s)])
                        c.op('pool', lambda s=s: nc.gpsimd.tensor_copy(out=v1s[s][:, :, 0:64], in_=ctile[s][:, 256:512].rearrange("p (h e) -> p h e", h=4)), reads=[('ctile', s)], writes=[('v1s', s)])
                        for h in range(4):
                            po = (h % 2) * 64
                            bk = (2, 3)[s] if h % 2 == 0 else (4, 5)[s]
                            c.op('pe', lambda s=s, h=h, po=po, b=b, bk=bk: nc.tensor.matmul(PB[bk][:, 256 + 4 * h:260 + 4 * h], lhsT=kTs[s][po:po + 64, h // 2, :], rhs=qs[po:po + 64, h // 2, 4 * b:4 * b + 4], start=True, stop=True),
                                 reads=[('kTs', s), 'qs'], writes=[('pb', bk)])
                        ess = ess2[s]
                        essv = ess[:].rearrange("p (h q) -> p h q", h=4)
                        for par, bk in ((0, (2, 3)[s]), (1, (4, 5)[s])):
                            c.op('act', lambda par=par, bk=bk, essv=essv: nc.scalar.activation(out=essv[:, par:4:2, :], in_=PB[bk][:, 256:272].rearrange("p (h q) -> p h q", h=4)[:, par:4:2, :], func=AF.Exp, scale=0.125),
                                 reads=[('pb', bk)], writes=[('ess', s, par)])
                        c.op('dve', lambda b=b, tix=tix, ess=ess: nc.vector.tensor_tensor(out=pz[b][:, :, 4 * b:4 * b + 4], in0=ess[:].rearrange("p (h q) -> p h q", h=4), in1=smk[:, tix, :, :], op=ALU.mult),
                             reads=[('ess', s, 0), ('ess', s, 1), 'smk'], writes=[('pz', b)])
                        for h in range(4):
                            c.op('pe', lambda s=s, h=h, b=b, st=(n_os == 0): nc.tensor.matmul(PB[7][0:64, 65 * h:65 * h + 65], lhsT=pz[b][:, h, :], rhs=v1s[s][:, h, :], start=st, stop=False),
                                 reads=[('pz', b), ('v1s', s)], writes=['pb7'])
                        n_os += 1
                for h in range(4):
                    po = (h % 2) * 64
                    bk = 6 if h % 2 == 0 else 5
                    c.op('pe', lambda h=h, po=po, bk=bk: nc.tensor.matmul(PB[bk][0:64, 64 * h:64 * h + 64], lhsT=ks[po:po + 64, h // 2, :], rhs=qs[po:po + 64, h // 2, :], start=True, stop=True),
                         reads=['ks', 'qs'], writes=[('pb', bk)])
                for h in range(4):
                    bk = 6 if h % 2 == 0 else 5
                    c.op('act', lambda h=h, bk=bk: nc.scalar.activation(out=en[:, 64 * h:64 * h + 64], in_=PB[bk][0:64, 64 * h:64 * h + 64], func=AF.Exp, scale=0.125), reads=[('pb', bk)], writes=[('en', h)])
                c.op('dve', lambda g=g: nc.vector.tensor_tensor(out=pn[:], in0=en[:].rearrange("p (h q) -> p h q", h=4), in1=nmk[:, g, :, :], op=ALU.mult), reads=[('en', 0), ('en', 1), ('en', 2), ('en', 3), 'nmk'], writes=['pn'])
                for h in range(4):
                    c.op('pe', lambda h=h, g=g: nc.tensor.matmul(PB[7][0:64, 65 * h:65 * h + 65], lhsT=pn[:, h, :], rhs=v1n[:, g, h, :], start=False, stop=(g == 2 and h == 3)),
                         reads=['pn', 'v1n'], writes=['pb7'])
            c.op('act', lambda: nc.scalar.copy(out=osv[:], in_=PB[7][0:64, 0:260]), reads=['pb7'], writes=['osv'])
            c.dma('sp', lambda: nc.sync.dma_start(out=accs_t.ap(), in_=osv[:]), reads=['osv'], writes=['accs'])
        c.barrier()
        es_hT.close()
        if STOP_AFTER == 'D':
            c.finish()
            return nc
        wcor = wco_t.ap().rearrange("(c p) n -> p c n", p=128)
        waor = wao_t.ap().rearrange("(c p) n -> p c n", p=128)
        wor = wo_t.ap().rearrange("(c p) n -> p c n", p=128)
        wrtr = wrt_t.ap().rearrange("(c p) n -> p c n", p=128)
        h2d_t = dscr("h2scr", [NOT, D], BF16)
        esE = ExitStack()
        es.enter_context(esE)
        lgall = c.sb([128, NTILE, 36], F32, esE)
        esE1 = ExitStack()
        with esE1:
            wco = c.sb([128, 4, D], BF16, esE1); wao = c.sb([128, 2, D], BF16, esE1); wo = c.sb([128, 8, D], BF16, esE1)
            wrt = c.sb([128, 8, 36], BF16, esE1); brtb = c.sb([128, 36], F32, esE1)
            c.dma('pool', lambda: nc.gpsimd.dma_start(out=wco[:], in_=wcor), writes=['wco'])
            c.dma('pool', lambda: nc.gpsimd.dma_start(out=wao[:], in_=waor), writes=['wao'])
            c.dma('pool', lambda: nc.gpsimd.dma_start(out=wo[:], in_=wor), writes=['wo'])
            c.dma('pool', lambda: nc.gpsimd.dma_start(out=wrt[:], in_=wrtr), writes=['wrt'])
            c.dma('sp', lambda: nc.sync.dma_start(out=brtb[:], in_=brt_t.ap().partition_broadcast(128)), writes=['brtb'])
            mrow = {}
            for kind, nm in ((0, 'g1'), (1, 'b2'), (2, 'a2')):
                for part, (c0, n) in enumerate(((0, 128), (128, 64))):
                    t_ = c.sb([n, D], F32, esE1)
                    c.dma('sp', lambda t_=t_, kind=kind, c0=c0, n=n: nc.sync.dma_start(out=t_[:], in_=modrows_t.ap()[kind, c0:c0 + n, :]),
                          reads=[('modrows', kind, part)], writes=[('mrow', nm, part)])
                    mrow[(nm, part)] = t_
            c.op('pool', lambda: nc.gpsimd.memset(lgall[:], 0.0), writes=['lgall'])
            ytile = [c.sb([128, 4, 512], BF16, esE1) for _ in range(2)]
            oT = c.sb([128, 2, 512], BF16, esE1)
            acc3 = [c.sb([128, 3, 260], F32, esE1) for _ in range(2)]
            osum = c.sb([128, 260], F32, esE1); rden = c.sb([128, 4], F32, esE1); ob = c.sb([128, 256], BF16, esE1)
            sga = [c.sb([128, 512], BF16, esE1) for _ in range(2)]; sgb = [c.sb([128, 512], BF16, esE1) for _ in range(2)]
            m1 = [c.sb([128, 512], F32, esE1) for _ in range(2)]; m2 = [c.sb([128, 512], F32, esE1) for _ in range(2)]
            mixT = c.sb([128, 8, 512], BF16, esE1)
            xt2 = [c.sb([128, D], F32, esE1) for _ in range(2)]; tmp = [c.sb([128, D], F32, esE1) for _ in range(2)]
            x1t = [c.sb([128, D], F32, esE1) for _ in range(2)]; t2 = [c.sb([128, D], F32, esE1) for _ in range(2)]
            h2b = [c.sb([128, D], BF16, esE1) for _ in range(2)]
            h2T = c.sb([128, 8, 128], BF16, esE1); st2 = [c.sb([128, 4], F32, esE1) for _ in range(2)]
            junk2 = c.sb([128, D], BF16, esE1)
            sub_i = 0
            for M, (hc, oi, n) in enumerate(OT):
                part = 0 if M < 8 else 1
                rr = 128 if M < 8 else 64
                nsub = n // rr
                ys_ = M % 2
                c.dma('sp', lambda ys_=ys_, oi=oi, n=n: nc.sync.dma_start(out=ytile[ys_][:, :, 0:n], in_=yts_t.ap()[:, :, oi:oi + n].rearrange("c p t -> p c t")),
                      reads=[('yts', cc) for cc in range(4)], writes=[('ytile', ys_)])
                for t in range(nsub):
                    a_ = (M * 4 + t) % 2
                    r0 = oi + rr * t
                    if M < 8:
                        c.dma('sp', lambda a_=a_, r0=r0: nc.sync.dma_start(out=acc3[a_][:], in_=acc_t.ap()[:, r0:r0 + 128, :].rearrange("g t c -> t g c")),
                              reads=[k for k in c.state if isinstance(k, tuple) and k[0] == 'acc'], writes=[('acc3', a_)])
                        c.op('dve', lambda a_=a_: nc.vector.tensor_tensor(out=osum[:], in0=acc3[a_][:, 0, :], in1=acc3[a_][:, 1, :], op=ALU.add), reads=[('acc3', a_)], writes=['osum'])
                        c.op('dve', lambda a_=a_: nc.vector.tensor_tensor(out=osum[:], in0=osum[:], in1=acc3[a_][:, 2, :], op=ALU.add), reads=[('acc3', a_), 'osum'], writes=['osum'])
                    else:
                        c.dma('sp', lambda a_=a_: nc.sync.dma_start(out=acc3[a_][0:64, 0, :], in_=accs_t.ap()), reads=['accs'], writes=[('acc3', a_)])
                        c.op('dve', lambda a_=a_: nc.vector.tensor_copy(out=osum[0:64, :], in_=acc3[a_][0:64, 0, :]), reads=[('acc3', a_)], writes=['osum'])
                    ov = osum[0:rr, :].rearrange("p (h e) -> p h e", h=4)
                    c.op('dve', lambda ov=ov, rr=rr: nc.vector.reciprocal(out=rden[0:rr, :].unsqueeze(2), in_=ov[:, :, 64:65]), reads=['osum'], writes=['rden'])
                    c.op('dve', lambda ov=ov, rr=rr: nc.vector.tensor_tensor(out=ob[0:rr, :].rearrange("p (h e) -> p h e", h=4), in0=ov[:, :, 0:64],
                         in1=rden[0:rr, :].unsqueeze(2).to_broadcast([rr, 4, 64]), op=ALU.mult), reads=['osum', 'rden'], writes=['ob'])
                    pv4 = pbf(4).rearrange("p (k t) -> p k t", k=8)
                    for k in range(2):
                        c.op('pe', lambda k=k, rr=rr, pv4=pv4: nc.tensor.transpose(out=pv4[:, k, 0:rr], in_=ob[0:rr, k * 128:(k + 1) * 128], identity=ident[0:rr, 0:rr]),
                             reads=['ob', 'ident'], writes=[('pb', 4)])
                    c.op('act', lambda t=t, rr=rr, pv4=pv4: nc.scalar.copy(out=oT[:, :, rr * t:rr * t + rr], in_=pv4[:, 0:2, 0:rr]), reads=[('pb', 4)], writes=['oT'])
                for j in range(8):
                    gs = j % 2
                    c.dma('sp', lambda gs=gs, j=j, oi=oi, n=n: nc.sync.dma_start(out=sga[gs][:, 0:n], in_=sg_t.ap()[j, :, oi:oi + n]), reads=[('sg', j)], writes=[('sga', gs)])
                    c.dma('sp', lambda gs=gs, j=j, oi=oi, n=n: nc.sync.dma_start(out=sgb[gs][:, 0:n], in_=sg_t.ap()[8 + j, :, oi:oi + n]), reads=[('sg', 8 + j)], writes=[('sgb', gs)])
                    pa_i, pb_i = (0, 1) if gs == 0 else (6, 7)
                    for cc in range(4):
                        c.op('pe', lambda cc=cc, j=j, pa_i=pa_i, ys_=ys_, n=n: nc.tensor.matmul(PB[pa_i][:, 0:n], lhsT=wco[:, cc, j * 128:(j + 1) * 128], rhs=ytile[ys_][:, cc, 0:n], start=(cc == 0), stop=(cc == 3)),
                             reads=['wco', ('ytile', ys_)], writes=[('pb', pa_i)])
                    for cc in range(2):
                        c.op('pe', lambda cc=cc, j=j, pb_i=pb_i, n=n: nc.tensor.matmul(PB[pb_i][:, 0:n], lhsT=wao[:, cc, j * 128:(j + 1) * 128], rhs=oT[:, cc, 0:n], start=(cc == 0), stop=(cc == 1)),
                             reads=['wao', 'oT'], writes=[('pb', pb_i)])
                    c.op('dve', lambda gs=gs, pa_i=pa_i, n=n: nc.vector.tensor_tensor(out=m1[gs][:, 0:n], in0=PB[pa_i][:, 0:n], in1=sga[gs][:, 0:n], op=ALU.mult), reads=[('pb', pa_i), ('sga', gs)], writes=[('m1', gs)])
                    c.op('dve', lambda gs=gs, pb_i=pb_i, n=n: nc.vector.tensor_tensor(out=m2[gs][:, 0:n], in0=PB[pb_i][:, 0:n], in1=sgb[gs][:, 0:n], op=ALU.mult), reads=[('pb', pb_i), ('sgb', gs)], writes=[('m2', gs)])
                    c.op('pool', lambda gs=gs, j=j, n=n: nc.gpsimd.tensor_tensor(out=mixT[:, j, 0:n], in0=m1[gs][:, 0:n], in1=m2[gs][:, 0:n], op=ALU.add), reads=[('m1', gs), ('m2', gs)], writes=[('mixT', j)])
                for t in range(nsub):
                    xs_ = sub_i % 2
                    sub_i += 1
                    r0 = oi + rr * t
                    tile_i = r0 // 128 if M < 8 else 32
                    src = xp[HALO + r0:HALO + r0 + 128, :] if M < 8 else xs
                    c.dma('sp', lambda xs_=xs_, src=src, rr=rr: nc.sync.dma_start(out=xt2[xs_][0:rr, :], in_=src), writes=[('xt2', xs_)])
                    for hf in range(2):
                        for j in range(8):
                            c.op('pe', lambda hf=hf, j=j, t=t, rr=rr: nc.tensor.matmul(PB[2 + hf][0:rr, :], lhsT=mixT[:, j, rr * t:rr * t + rr], rhs=wo[:, j, hf * 512:(hf + 1) * 512], start=(j == 0), stop=(j == 7)),
                                 reads=[('mixT', j), 'wo'], writes=[('pb', 2 + hf)])
                        c.op('dve', lambda hf=hf, xs_=xs_, rr=rr, part=part: nc.vector.tensor_tensor(out=tmp[xs_][0:rr, hf * 512:(hf + 1) * 512], in0=PB[2 + hf][0:rr, :],
                             in1=mrow[('g1', part)][0:rr, hf * 512:(hf + 1) * 512], op=ALU.mult), reads=[('pb', 2 + hf), ('mrow', 'g1', part)], writes=[('tmp', xs_)])
                    c.op('pool', lambda xs_=xs_, rr=rr: nc.gpsimd.tensor_tensor(out=x1t[xs_][0:rr, :], in0=tmp[xs_][0:rr, :], in1=xt2[xs_][0:rr, :], op=ALU.add),
                         reads=[('tmp', xs_), ('xt2', xs_)], writes=[('x1t', xs_)])
                    c.dma('act', lambda xs_=xs_, r0=r0, rr=rr: nc.scalar.dma_start(out=x1_t.ap()[r0:r0 + rr, :], in_=x1t[xs_][0:rr, :]), reads=[('x1t', xs_)], writes=[('x1', tile_i)])
                    c.op('act', lambda xs_=xs_, rr=rr: nc.scalar.activation(out=junk2[0:rr, :], in_=x1t[xs_][0:rr, :], func=AF.Square, accum_out=st2[xs_][0:rr, 0:1]),
                         reads=[('x1t', xs_)], writes=[('st2', xs_), 'junk2'])
                    c.op('act', lambda xs_=xs_, rr=rr: nc.scalar.activation(out=st2[xs_][0:rr, 1:2], in_=st2[xs_][0:rr, 0:1], func=AF.Sqrt, scale=1.0 / D, bias=epsb[0:rr, :]),
                         reads=[('st2', xs_), 'epsb'], writes=[('st2', xs_)])
                    c.op('dve', lambda xs_=xs_, rr=rr: nc.vector.reciprocal(out=st2[xs_][0:rr, 2:3], in_=st2[xs_][0:rr, 1:2]), reads=[('st2', xs_)], writes=[('st2', xs_)])
                    c.op('dve', lambda xs_=xs_, rr=rr, part=part: nc.vector.scalar_tensor_tensor(out=t2[xs_][0:rr, :], in0=x1t[xs_][0:rr, :], scalar=st2[xs_][0:rr, 2:3],
                         in1=mrow[('a2', part)][0:rr, :], op0=ALU.mult, op1=ALU.mult), reads=[('x1t', xs_), ('st2', xs_), ('mrow', 'a2', part)], writes=[('t2', xs_)])
                    c.op('pool', lambda xs_=xs_, rr=rr, part=part: nc.gpsimd.tensor_tensor(out=h2b[xs_][0:rr, :], in0=t2[xs_][0:rr, :], in1=mrow[('b2', part)][0:rr, :], op=ALU.add),
                         reads=[('t2', xs_), ('mrow', 'b2', part)], writes=[('h2b', xs_)])
                    c.dma('act', lambda xs_=xs_, r0=r0, rr=rr: nc.scalar.dma_start(out=h2d_t.ap()[r0:r0 + rr, :], in_=h2b[xs_][0:rr, :]), reads=[('h2b', xs_)], writes=[('h2d', tile_i)])
                    pv5 = pbf(5).rearrange("p (k t) -> p k t", k=8)
                    for k in range(8):
                        c.op('pe', lambda k=k, xs_=xs_, rr=rr, pv5=pv5: nc.tensor.transpose(out=pv5[:, k, 0:rr], in_=h2b[xs_][0:rr, k * 128:(k + 1) * 128], identity=ident[0:rr, 0:rr]),
                             reads=[('h2b', xs_), 'ident'], writes=[('pb', 5)])
                    c.op('act', lambda rr=rr, pv5=pv5: nc.scalar.copy(out=h2T[:, :, 0:rr], in_=pv5[:, :, 0:rr]), reads=[('pb', 5)], writes=['h2T'])
                    for k in range(8):
                        c.op('pe', lambda k=k, rr=rr: nc.tensor.matmul(PB[4][0:rr, 0:36], lhsT=h2T[:, k, 0:rr], rhs=wrt[:, k, :], start=(k == 0), stop=(k == 7)),
                             reads=['h2T', 'wrt'], writes=[('pb', 4)])
                    c.op('dve', lambda rr=rr, tile_i=tile_i: nc.vector.tensor_tensor(out=lgall[0:rr, tile_i, :], in0=PB[4][0:rr, 0:36], in1=brtb[0:rr, :], op=ALU.add),
                         reads=[('pb', 4), 'brtb', 'lgall'], writes=['lgall'])
        c.barrier()
        if STOP_AFTER == 'E':
            c.finish()
            return nc
        NT = NTILE
        esF = ExitStack()
        with esF:
            def ft(shape, dt=F32):
                return c.sb(shape, dt, esF)
            gmx = ft([128, NT]); ohg = ft([128, NT, 4]); gsh = ft([128, NT, 4]); gex = ft([128, NT, 4]); gsum = ft([128, NT]); pgr = ft([128, NT])
            pen = ft([128, NT, 4]); em = ft([128, NT, 32]); m8 = ft([128, NT, 8]); i8 = ft([128, NT, 8], U32)
            e0f = ft([128, NT]); e1f = ft([128, NT]); dv = ft([128, NT]); w0 = ft([128, NT]); w1 = ft([128, NT])
            oh0 = ft([128, NT, 32]); oh1 = ft([128, NT, 32]); mm_ = ft([128, NT, 32]); cs = ft([128, NT + 1, 32])
            base = ft([128, NT, 32]); prod = ft([128, NT, 32]); d0f = ft([128, NT]); d1f = ft([128, NT])
            d0i = ft([128, NT], I32); d1i = ft([128, NT], I32)
            io32i = ft([128, 32], I32); io32 = ft([128, 32]); thri = ft([128, NBLK], I32); thr = ft([128, NBLK])
            cnt = ft([128, 32]); cni = ft([128, 32], I32); pad = ft([128, 32]); pa_ = ft([128, 32]); pb_ = ft([128, 32]); pst = ft([128, 32])
            cmpb = ft([128, NBLK, 32]); bef = ft([128, NBLK])
            c.op('pool', lambda: nc.gpsimd.iota(io32i[:], pattern=[[1, 32]], base=0, channel_multiplier=0), writes=['io32i'])
            c.op('pool', lambda: nc.gpsimd.iota(thri[:], pattern=[[BLK, NBLK]], base=0, channel_multiplier=0), writes=['thri'])
            c.op('dve', lambda: nc.vector.tensor_copy(out=io32[:], in_=io32i[:]), reads=['io32i'], writes=['io32'])
            c.op('dve', lambda: nc.vector.tensor_copy(out=thr[:], in_=thri[:]), reads=['thri'], writes=['thr'])
            R = ['lgall']
            gl = lgall[:, :, 0:4]
            V = nc.vector
            c.op('dve', lambda: V.tensor_reduce(out=gmx[:], in_=gl, axis=AX.X, op=ALU.max), reads=R, writes=['gmx'])
            c.op('dve', lambda: V.tensor_tensor(out=ohg[:], in0=gl, in1=gmx[:].unsqueeze(2).to_broadcast([128, NT, 4]), op=ALU.is_equal), reads=R + ['gmx'], writes=['ohg'])
            c.op('dve', lambda: V.tensor_tensor(out=gsh[:], in0=gl, in1=gmx[:].unsqueeze(2).to_broadcast([128, NT, 4]), op=ALU.subtract), reads=R + ['gmx'], writes=['gsh'])
            c.op('act', lambda: nc.scalar.activation(out=gex[:], in_=gsh[:], func=AF.Exp), reads=['gsh'], writes=['gex'])
            c.op('dve', lambda: V.tensor_reduce(out=gsum[:], in_=gex[:], axis=AX.X, op=ALU.add), reads=['gex'], writes=['gsum'])
            c.op('dve', lambda: V.reciprocal(out=pgr[:], in_=gsum[:]), reads=['gsum'], writes=['pgr'])
            c.op('dve', lambda: V.tensor_scalar(out=pen[:], in0=ohg[:], scalar1=-1.0, scalar2=1e30, op0=ALU.add, op1=ALU.mult), reads=['ohg'], writes=['pen'])
            c.op('dve', lambda: V.tensor_tensor(out=em[:].rearrange("p t (g e) -> p t g e", g=4), in0=lgall[:, :, 4:36].rearrange("p t (g e) -> p t g e", g=4),
                 in1=pen[:].unsqueeze(3).to_broadcast([128, NT, 4, 8]), op=ALU.add), reads=R + ['pen'], writes=['em'])
            for i in range(NT):
                c.op('dve', lambda i=i: V.max(out=m8[:, i, :], in_=em[:, i, :]), reads=['em'], writes=['m8'])
                c.op('dve', lambda i=i: V.max_index(out=i8[:, i, :], in_max=m8[:, i, :], in_values=em[:, i, :]), reads=['em', 'm8'], writes=['i8'])
            c.op('dve', lambda: V.tensor_copy(out=e0f[:], in_=i8[:, :, 0]), reads=['i8'], writes=['e0f'])
            c.op('dve', lambda: V.tensor_copy(out=e1f[:], in_=i8[:, :, 1]), reads=['i8'], writes=['e1f'])
            c.op('dve', lambda: V.tensor_tensor(out=dv[:], in0=m8[:, :, 1], in1=m8[:, :, 0], op=ALU.subtract), reads=['m8'], writes=['dv'])
            c.op('act', lambda: nc.scalar.activation(out=dv[:], in_=dv[:], func=AF.Exp), reads=['dv'], writes=['dv'])
            c.op('dve', lambda: V.tensor_scalar(out=dv[:], in0=dv[:], scalar1=1.0, scalar2=None, op0=ALU.add), reads=['dv'], writes=['dv'])
            c.op('dve', lambda: V.reciprocal(out=w0[:], in_=dv[:]), reads=['dv'], writes=['w0'])
            c.op('dve', lambda: V.tensor_tensor(out=w0[:], in0=w0[:], in1=pgr[:], op=ALU.mult), reads=['w0', 'pgr'], writes=['w0'])
            c.op('dve', lambda: V.tensor_tensor(out=w1[:], in0=pgr[:], in1=w0[:], op=ALU.subtract), reads=['w0', 'pgr'], writes=['w1'])
            iob = io32[:].unsqueeze(1).to_broadcast([128, NT, 32])
            c.op('dve', lambda: V.tensor_tensor(out=oh0[:], in0=iob, in1=e0f[:].unsqueeze(2).to_broadcast([128, NT, 32]), op=ALU.is_equal), reads=['io32', 'e0f'], writes=['oh0'])
            c.op('dve', lambda: V.tensor_tensor(out=oh1[:], in0=iob, in1=e1f[:].unsqueeze(2).to_broadcast([128, NT, 32]), op=ALU.is_equal), reads=['io32', 'e1f'], writes=['oh1'])
            c.op('dve', lambda: V.memset(oh0[64:128, NT - 1, :], 0.0), reads=['oh0'], writes=['oh0'])
            c.op('dve', lambda: V.memset(oh1[64:128, NT - 1, :], 0.0), reads=['oh1'], writes=['oh1'])
            c.op('dve', lambda: V.tensor_tensor(out=mm_[:], in0=oh0[:], in1=oh1[:], op=ALU.add), reads=['oh0', 'oh1'], writes=['mm'])
            c.op('dve', lambda: V.memset(cs[:, 0, :], 0.0), writes=['cs'])
            for i in range(NT):
                c.op('dve', lambda i=i: V.tensor_tensor(out=cs[:, i + 1, :], in0=cs[:, i, :], in1=mm_[:, i, :], op=ALU.add), reads=['cs', 'mm'], writes=['cs'])
            for i in range(NT):
                bk = i // 16
                co = (i % 16) * 32
                c.op('pe', lambda i=i, bk=bk, co=co: nc.tensor.matmul(PB[bk][:, co:co + 32], lhsT=suf[:], rhs=mm_[:, i, :], start=True, stop=False), reads=['suf', 'mm'], writes=[('pb', bk)])
                c.op('pe', lambda i=i, bk=bk, co=co: nc.tensor.matmul(PB[bk][:, co:co + 32], lhsT=onesf[:], rhs=cs[:, i, :], start=False, stop=True), reads=['onesf', 'cs'], writes=[('pb', bk)])
            c.op('pe', lambda: nc.tensor.matmul(PB[3][:, 0:32], lhsT=onesf[:], rhs=cs[:, NT, :], start=True, stop=True), reads=['onesf', 'cs'], writes=[('pb', 3)])
            c.op('dve', lambda: V.tensor_scalar(out=cni[:], in0=PB[3][:, 0:32], scalar1=float(BLK - 1), scalar2=None, op0=ALU.add), reads=[('pb', 3)], writes=['cni'])
            c.op('dve', lambda: V.tensor_scalar(out=cni[:], in0=cni[:], scalar1=int(math.log2(BLK)), scalar2=int(math.log2(BLK)), op0=ALU.arith_shift_right, op1=ALU.logical_shift_left), reads=['cni'], writes=['cni'])
            c.op('dve', lambda: V.tensor_copy(out=pad[:], in_=cni[:]), reads=['cni'], writes=['pad'])
            src_, dst_ = pad, pa_
            for sft in (1, 2, 4, 8, 16):
                c.op('dve', lambda src_=src_, dst_=dst_, sft=sft: V.tensor_copy(out=dst_[:, 0:sft], in_=src_[:, 0:sft]), reads=['pfx', 'pad'], writes=['pfx'])
                c.op('dve', lambda src_=src_, dst_=dst_, sft=sft: V.tensor_tensor(out=dst_[:, sft:32], in0=src_[:, sft:32], in1=src_[:, 0:32 - sft], op=ALU.add), reads=['pfx', 'pad'], writes=['pfx'])
                src_, dst_ = dst_, (pb_ if dst_ is pa_ else pa_)
            pend = src_
            c.op('dve', lambda: V.tensor_tensor(out=pst[:], in0=pend[:], in1=pad[:], op=ALU.subtract), reads=['pfx', 'pad'], writes=['pst'])
            for bk in range(3):
                t0 = bk * 16
                nt_ = min(16, NT - t0)
                c.op('dve', lambda bk=bk, t0=t0, nt_=nt_: V.tensor_tensor(out=base[:, t0:t0 + nt_, :], in0=PB[bk][:, 0:nt_ * 32].rearrange("p (t e) -> p t e", e=32),
                     in1=pst[:].unsqueeze(1).to_broadcast([128, nt_, 32]), op=ALU.add), reads=[('pb', bk), 'pst'], writes=['base'])
            for oh_, df_, di_, nm in ((oh0, d0f, d0i, 'd0'), (oh1, d1f, d1i, 'd1')):
                c.op('dve', lambda oh_=oh_: V.tensor_tensor(out=prod[:], in0=oh_[:], in1=base[:], op=ALU.mult), reads=['oh0', 'oh1', 'base'], writes=['prod'])
                c.op('dve', lambda df_=df_: V.tensor_reduce(out=df_[:], in_=prod[:], axis=AX.X, op=ALU.add), reads=['prod'], writes=[nm + 'f'])
                c.op('dve', lambda df_=df_, di_=di_: V.tensor_copy(out=di_[:], in_=df_[:]), reads=[nm + 'f'], writes=[nm])
            c.op('dve', lambda: V.tensor_tensor(out=cmpb[:], in0=pend[:].unsqueeze(1).to_broadcast([128, NBLK, 32]), in1=thr[:].unsqueeze(2).to_broadcast([128, NBLK, 32]), op=ALU.is_le),
                 reads=['pfx', 'thr'], writes=['cmpb'])
            c.op('dve', lambda: V.tensor_reduce(out=bef[:], in_=cmpb[:], axis=AX.X, op=ALU.add), reads=['cmpb'], writes=['bef'])
            c.op('dve', lambda: V.tensor_scalar(out=bef[:], in0=bef[:], scalar1=31.0, scalar2=None, op0=ALU.min), reads=['bef'], writes=['bef'])
            pio = ft([128, 1], I32); piof = ft([128, 1]); widxf = ft([128, NBLK]); widx = ft([128, NBLK], I32)
            c.op('pool', lambda: nc.gpsimd.iota(pio[:], pattern=[[0, 1]], base=0, channel_multiplier=1), writes=['pio'])
            c.op('dve', lambda: V.tensor_copy(out=piof[:], in_=pio[:]), reads=['pio'], writes=['piof'])
            c.op('dve', lambda: V.tensor_scalar(out=widxf[:], in0=bef[:], scalar1=128.0, scalar2=piof[:, 0:1], op0=ALU.mult, op1=ALU.add), reads=['bef', 'piof'], writes=['widxf'])
            c.op('dve', lambda: V.tensor_copy(out=widx[:], in_=widxf[:]), reads=['widxf'], writes=['widx'])
            zt = ft([128, D], BF16)
            c.op('pool', lambda: nc.gpsimd.memset(zt[:], 0.0), writes=['zt'])
            xsv = xsd_t.ap().rearrange("(b p) d -> p b d", p=128)
            zkeys = []
            NRB = CAP // 128
            for q4 in range(8):
                b0 = q4 * (NRB // 8)
                nb_ = NRB // 8
                c.dma('act', lambda b0=b0, nb_=nb_: nc.scalar.dma_start(out=xsv[:, b0:b0 + nb_, :], in_=zt[:].unsqueeze(1).to_broadcast([128, nb_, D])), reads=['zt'], writes=[('xsz', q4)])
                zkeys.append(('xsz', q4))
            h2t = [ft([128, D], BF16) for _ in range(2)]
            skeys = []
            for i in range(NT):
                s = i % 2
                rr = 128 if i < NT - 1 else 64
                c.dma('sp', lambda s=s, i=i, rr=rr: nc.sync.dma_start(out=h2t[s][0:rr, :], in_=h2d_t.ap()[i * 128:i * 128 + rr, :]), reads=[('h2d', i)], writes=[('h2t', s)])
                for di_, nm in ((d0i, 'd0'), (d1i, 'd1')):
                    c.dma('pool', lambda s=s, i=i, rr=rr, di_=di_: nc.gpsimd.indirect_dma_start(out=xsd_t.ap(), out_offset=bass.IndirectOffsetOnAxis(ap=di_[0:rr, i:i + 1], axis=0),
                          in_=h2t[s][0:rr, :], in_offset=None), reads=[('h2t', s), nm] + zkeys, writes=[('xss', i, nm)])
                    skeys.append(('xss', i, nm))
            xsb = [ft([128, D], BF16) for _ in range(2)]; xsT = [ft([128, 8, 128], BF16) for _ in range(2)]
            wg = [ft([128, 8, 512], BF16) for _ in range(2)]; wu = [ft([128, 8, 512], BF16) for _ in range(2)]; wd = [ft([128, 4, D], BF16) for _ in range(2)]
            actt = [ft([128, 512]) for _ in range(2)]; ab = [ft([128, 512], BF16) for _ in range(2)]; aT = [ft([128, 4, 128], BF16) for _ in range(2)]
            yev = [ft([128, D]) for _ in range(2)]
            wegv = weg_t.ap().rearrange("e (p k) f -> (e p) (k f)", p=128)
            weuv = weu_t.ap().rearrange("e (p k) f -> (e p) (k f)", p=128)
            wedv = wed_t.ap().rearrange("e (p k) f -> (e p) (k f)", p=128)
            items = [(b, sub) for b in range(NBLK) for sub in range(SUBB)]

            def load_xsb(n):
                b, sub = items[n]
                xs_ = n % 2
                r0 = b * BLK + sub * 128
                c.dma('sp', lambda xs_=xs_, r0=r0: nc.sync.dma_start(out=xsb[xs_][:], in_=xsd_t.ap()[r0:r0 + 128, :]), reads=skeys + zkeys, writes=[('xsb', xs_)])

            load_xsb(0)
            for n, (b, sub) in enumerate(items):
                s = b % 2
                xs_ = n % 2
                if sub == 0:
                    for wt_, wv_, nm in ((wg, wegv, 'wg'), (wu, weuv, 'wu'), (wd, wedv, 'wd')):
                        c.dma('pool', lambda s=s, b=b, wt_=wt_, wv_=wv_: nc.gpsimd.indirect_dma_start(out=wt_[s][:].rearrange("p k f -> p (k f)"), out_offset=None, in_=wv_,
                              in_offset=bass.IndirectOffsetOnAxis(ap=widx[:, b:b + 1], axis=0)), reads=['widx'], writes=[(nm, s)])
                if n + 1 < len(items):
                    load_xsb(n + 1)
                pv0 = pbf(0).rearrange("p (k t) -> p k t", k=8)
                for k in range(8):
                    c.op('pe', lambda k=k, xs_=xs_, pv0=pv0: nc.tensor.transpose(out=pv0[:, k, :], in_=xsb[xs_][:, k:k + 8 * 127 + 1:8], identity=ident[:]), reads=[('xsb', xs_), 'ident'], writes=[('pb', 0)])
                c.op('act', lambda xs_=xs_, pv0=pv0: nc.scalar.copy(out=xsT[xs_][:], in_=pv0), reads=[('pb', 0)], writes=[('xsT', xs_)])
                gi, ui = (1, 2) if xs_ == 0 else (6, 7)
                for k in range(8):
                    c.op('pe', lambda k=k, s=s, xs_=xs_, gi=gi: nc.tensor.matmul(PB[gi][:, :], lhsT=xsT[xs_][:, k, :], rhs=wg[s][:, k, :], start=(k == 0), stop=(k == 7)), reads=[('xsT', xs_), ('wg', s)], writes=[('pb', gi)])
                for k in range(8):
                    c.op('pe', lambda k=k, s=s, xs_=xs_, ui=ui: nc.tensor.matmul(PB[ui][:, :], lhsT=xsT[xs_][:, k, :], rhs=wu[s][:, k, :], start=(k == 0), stop=(k == 7)), reads=[('xsT', xs_), ('wu', s)], writes=[('pb', ui)])
                c.op('act', lambda xs_=xs_, gi=gi: nc.scalar.activation(out=actt[xs_][:], in_=PB[gi][:, :], func=AF.Silu), reads=[('pb', gi)], writes=[('actt', xs_)])
                c.op('dve', lambda xs_=xs_, ui=ui: V.tensor_tensor(out=ab[xs_][:], in0=PB[ui][:, :], in1=actt[xs_][:], op=ALU.mult), reads=[('pb', ui), ('actt', xs_)], writes=[('ab', xs_)])
                pv3 = pbf(3).rearrange("p (k t) -> p k t", k=8)
                for k in range(4):
                    c.op('pe', lambda k=k, xs_=xs_, pv3=pv3: nc.tensor.transpose(out=pv3[:, k, :], in_=ab[xs_][:, k:k + 4 * 127 + 1:4], identity=ident[:]), reads=[('ab', xs_), 'ident'], writes=[('pb', 3)])
                c.op('dve', lambda xs_=xs_, pv3=pv3: V.tensor_copy(out=aT[xs_][:], in_=pv3[:, 0:4, :]), reads=[('pb', 3)], writes=[('aT', xs_)])
                for hf in range(2):
                    for k in range(4):
                        c.op('pe', lambda k=k, s=s, xs_=xs_, hf=hf: nc.tensor.matmul(PB[4 + hf][:, :], lhsT=aT[xs_][:, k, :], rhs=wd[s][:, k, hf * 512:(hf + 1) * 512], start=(k == 0), stop=(k == 3)),
                             reads=[('aT', xs_), ('wd', s)], writes=[('pb', 4 + hf)])
                c.op('act', lambda xs_=xs_: nc.scalar.copy(out=yev[xs_][:, 0:512], in_=PB[4][:, :]), reads=[('pb', 4)], writes=[('yev', xs_, 0)])
                c.op('dve', lambda xs_=xs_: V.tensor_copy(out=yev[xs_][:, 512:1024], in_=PB[5][:, :]), reads=[('pb', 5)], writes=[('yev', xs_, 1)])
                r0 = b * BLK + sub * 128
                c.dma('act', lambda xs_=xs_, r0=r0: nc.scalar.dma_start(out=ysd_t.ap()[r0:r0 + 128, :], in_=yev[xs_][:]), reads=[('yev', xs_, 0), ('yev', xs_, 1)], writes=[('ysd', n)])
            ykeys = [('ysd', n) for n in range(len(items))]
            g2r = {}
            for part, (c0, n) in enumerate(((0, 128), (128, 64))):
                t_ = ft([n, D])
                c.dma('sp', lambda t_=t_, c0=c0, n=n: nc.sync.dma_start(out=t_[:], in_=modrows_t.ap()[3, c0:c0 + n, :]), reads=[('modrows', 3, part)], writes=[('g2r', part)])
                g2r[part] = t_
            gfin = ft([128, D])
            c.dma('sp', lambda: nc.sync.dma_start(out=gfin[:], in_=gfin_t.ap().partition_broadcast(128)), writes=['gfin'])
            NBF = 3
            y0 = [ft([128, D]) for _ in range(NBF)]; y1 = [ft([128, D]) for _ in range(NBF)]; x1r = [ft([128, D]) for _ in range(NBF)]
            fa = [ft([128, D]) for _ in range(NBF)]; st3 = [ft([128, 4]) for _ in range(NBF)]
            junk3 = ft([128, D], BF16)

            def comb_stage1(i):
                s = i % NBF
                rr = 128 if i < NT - 1 else 64
                part = 0 if i < NT - 1 else 1
                c.dma('pool', lambda: nc.gpsimd.indirect_dma_start(out=y0[s][0:rr, :], out_offset=None, in_=ysd_t.ap(),
                      in_offset=bass.IndirectOffsetOnAxis(ap=d0i[0:rr, i:i + 1], axis=0)), reads=ykeys + ['d0'], writes=[('y0', s)])
                c.dma('pool', lambda: nc.gpsimd.indirect_dma_start(out=y1[s][0:rr, :], out_offset=None, in_=ysd_t.ap(),
                      in_offset=bass.IndirectOffsetOnAxis(ap=d1i[0:rr, i:i + 1], axis=0)), reads=ykeys + ['d1'], writes=[('y1', s)])
                c.dma('sp', lambda: nc.sync.dma_start(out=x1r[s][0:rr, :], in_=x1_t.ap()[i * 128:i * 128 + rr, :]), reads=[('x1', i)], writes=[('x1r', s)])
                c.op('act', lambda: nc.scalar.activation(out=fa[s][0:rr, :], in_=y0[s][0:rr, :], func=AF.Copy, scale=w0[0:rr, i:i + 1]), reads=[('y0', s), 'w0'], writes=[('fa', s)])
                c.op('dve', lambda: V.scalar_tensor_tensor(out=fa[s][0:rr, :], in0=y1[s][0:rr, :], scalar=w1[0:rr, i:i + 1], in1=fa[s][0:rr, :], op0=ALU.mult, op1=ALU.add),
                     reads=[('y1', s), 'w1', ('fa', s)], writes=[('fa', s)])
                c.op('dve', lambda: V.tensor_tensor(out=fa[s][0:rr, :], in0=fa[s][0:rr, :], in1=g2r[part][0:rr, :], op=ALU.mult), reads=[('fa', s), ('g2r', part)], writes=[('fa', s)])
                c.op('dve', lambda: V.tensor_tensor(out=x1r[s][0:rr, :], in0=fa[s][0:rr, :], in1=x1r[s][0:rr, :], op=ALU.add), reads=[('fa', s), ('x1r', s)], writes=[('x1r', s)])

            def comb_stage2(i):
                s = i % NBF
                rr = 128 if i < NT - 1 else 64
                c.op('act', lambda: nc.scalar.activation(out=junk3[0:rr, :], in_=x1r[s][0:rr, :], func=AF.Square, accum_out=st3[s][0:rr, 0:1]), reads=[('x1r', s)], writes=[('st3', s), 'junk3'])
                c.op('act', lambda: nc.scalar.activation(out=st3[s][0:rr, 1:2], in_=st3[s][0:rr, 0:1], func=AF.Sqrt, scale=1.0 / D, bias=epsb[0:rr, :]), reads=[('st3', s), 'epsb'], writes=[('st3', s)])
                c.op('dve', lambda: V.reciprocal(out=st3[s][0:rr, 2:3], in_=st3[s][0:rr, 1:2]), reads=[('st3', s)], writes=[('st3', s)])
                c.op('dve', lambda: V.scalar_tensor_tensor(out=y0[s][0:rr, :], in0=x1r[s][0:rr, :], scalar=st3[s][0:rr, 2:3], in1=gfin[0:rr, :], op0=ALU.mult, op1=ALU.mult),
                     reads=[('x1r', s), ('st3', s), 'gfin'], writes=[('y0', s)])
                dst = yp_t.ap()[i * 128:(i + 1) * 128, :] if i < NT - 1 else ys_t.ap()
                c.dma('act', lambda: nc.scalar.dma_start(out=dst, in_=y0[s][0:rr, :]), reads=[('y0', s)], writes=[('yout', i)])

            for i in range(NT + 1):
                if i < NT:
                    comb_stage1(i)
                if i >= 1:
                    comb_stage2(i - 1)
        c.finish()
    return nc


def build_two_pass():
    nc1 = build_nc(None)
    needed = set(nc1._mk_ctx.record)
    return build_nc(needed)


def _prep_inputs(inp):
    f = lambda a: np.ascontiguousarray(a, dtype=np.float32)
    ohw, vw, sel, bd = _structure_constants()
    shared = {
        "rel_bias": f(inp["rel_bias"]), "norm_mix_g": f(inp["norm_mix_g"][0][None]), "norm_ffn_g": f(inp["norm_ffn_g"][0][None]),
        "norm_final_g": f(inp["norm_final_g"][None]), "w_mod": f(inp["w_mod"][0]), "b_mod": f(inp["b_mod"][0][None]),
        "w_in": f(inp["w_in"][0]), "dw_w": f(inp["dw_w"][0]), "dw_b": f(inp["dw_b"][0][None]), "ln_g": f(inp["ln_conv_g"][0][None]),
        "ln_b": f(inp["ln_conv_b"][0][None]), "w_conv_out": f(inp["w_conv_out"][0]), "w_attn_out": f(inp["w_attn_out"][0]),
        "w_out": f(inp["w_out"][0]),
        "w_rt": f(np.concatenate([inp["w_router_group"][0], inp["w_router_expert"][0].reshape(D, 32)], axis=1)),
        "b_rt": f(np.concatenate([inp["b_router_group"][0], inp["b_router_expert"][0].reshape(32)])[None]),
        "w_eg": f(inp["w_exp_gate"][0]), "w_eu": f(inp["w_exp_up"][0]), "w_ed": f(inp["w_exp_down"][0]),
        "ohw": ohw, "vw": vw, "sel": sel, "bd": bd,
    }
    maps = []
    for cid in range(NCORE):
        b, half = cid // 2, cid % 2
        xp = np.zeros((NEXT, D), np.float32)
        xp[HALO:] = inp["x_prompt"][b, half * NOWN:(half + 1) * NOWN]
        if half == 1:
            xp[:HALO] = inp["x_prompt"][b, NOWN - HALO:NOWN]
        sl = slice(cid * NSQ, (cid + 1) * NSQ)
        m = dict(shared)
        m["xp"] = xp
        m["xs"] = f(inp["x_sample"][sl].reshape(NS, D))
        m["cmod"] = f(np.concatenate([inp["c_prompt"][b][None], inp["c_sample"][sl]], axis=0))
        m["hv"] = np.full((128, 1), float(half), np.float32)
        m["ck128"] = f(inp["cache_kv_w128"][0, sl].reshape(NSQ, 128, 512))
        m["ck512"] = f(inp["cache_kv_w512"][0, sl].reshape(NSQ, 512, 512))
        m["ck2048"] = f(inp["cache_kv_w2048"][0, sl].reshape(NSQ, 2048, 512))
        m["sconv"] = f(inp["state_conv"][0, sl])
        maps.append(m)
    return maps


_NC_CACHE = {}


def kernel(**inp):
    import time as _t
    t0 = _t.time()
    maps = _prep_inputs(inp)
    t1 = _t.time()
    if "nc" not in _NC_CACHE:
        _NC_CACHE["nc"] = build_two_pass()
    nc = _NC_CACHE["nc"]
    t2 = _t.time()
    if STOP_AFTER is not None:
        for m in maps:
            for k in ("w_eg", "w_eu", "w_ed"):
                m.pop(k, None)
    res = run_bass_kernel_spmd(nc, maps, core_ids=list(range(NCORE)))
    print("[kernel] prep %.1fs build %.1fs run %.1fs" % (t1 - t0, t2 - t1, _t.time() - t2), flush=True)
    R = res.results
    _NC_CACHE['last'] = R
    B = 4
    yp = np.zeros((B, 8192, D), np.float32); ys = np.zeros((128, 4, D), np.float32)
    kvp = [np.zeros((1, B, w, 2, 4, 64), np.float32) for (w, _) in GROUPS]
    convp = np.zeros((1, B, 30, 512), np.float32)
    kvs = [np.zeros((1, 128, w, 2, 4, 64), np.float32) for (w, _) in GROUPS]
    convs = np.zeros((1, 128, 30, 512), np.float32)
    for cid in range(NCORE):
        b, half = cid // 2, cid % 2
        r = R[cid]
        sl = slice(cid * NSQ, (cid + 1) * NSQ)
        if "yp" in r:
            yp[b, half * NOWN:(half + 1) * NOWN] = r["yp"]
            ys[sl] = r["ys"].reshape(NSQ, 4, D)
        for gi, (w, _) in enumerate(GROUPS):
            if half == 1:
                kvp[gi][0, b] = r["kvp%d" % w].reshape(w, 2, 4, 64)
            kvs[gi][0, sl] = r["kvs%d" % w].reshape(NSQ, w, 2, 4, 64)
        if half == 1:
            convp[0, b] = r["convp"]
        convs[0, sl] = r["convs"]
    return (yp, ys, kvp[0], kvp[1], kvp[2], convp, kvs[0], kvs[1], kvs[2], convs)
```
